# Optimizing a Trainium2 kernel written in Bass

```python
import math
import jax, jax.numpy as jnp
from jax import lax
import numpy as np

D_MODEL = 1024
BATCH = 8
SEQ = 2048
DEPTH = 2

GRID_W = 64
CTX_LEN = 256
EPS = 1e-6
HA = 4
DKA = 128
DVA = 128
CONV_K = 3
CHUNK_A = 64
HB = 4
DKB = 64
DVB = 128
GATE_RANK = 16
GATE_NORM = 16.0
CHUNK_B = 16
HC = 4
DKC = 64
DVC = 128
Q_BLOCK = 128
ROPE_THETA = 10000.0
D_FF = 2816
N_EXPERTS = 8
TOP_K = 2
N_DENSE = (DEPTH + 1) // 2
N_MOE = DEPTH // 2
CONV_CH = 2 * HA * DKA + HA * DVA
IN_SIZES = (HA * DKA, HA * DKA, HA * DVA, HA * DVA, 2 * HA, 2 * HA,
            HB * DKB, HB * DKB, HB * DVB, HB * DVB, 2 * GATE_RANK,
            2 * HC * DKC, 2 * HC * DKC, HC * DVC,
            3 * D_MODEL)
IN_COLS = sum(IN_SIZES)

kernel_name = 'hybrid_deltanet_gla_diffattn_moe_dit'


def _rmsnorm(x, g):
    x32 = x.astype(jnp.float32)
    y = x32 * lax.rsqrt(jnp.mean(jnp.square(x32), axis=-1, keepdims=True) + EPS)
    return y * g.astype(jnp.float32)


def _modulate(x, g, shift, scale):
    return (_rmsnorm(x, g) * (1.0 + scale) + shift).astype(x.dtype)


def _l2norm(t):
    return t * lax.rsqrt(jnp.sum(jnp.square(t), axis=-1, keepdims=True) + EPS)


def _split(t, sizes):
    offsets, acc = [], 0
    for s in sizes[:-1]:
        acc += s
        offsets.append(acc)
    return jnp.split(t, offsets, axis=-1)


def _heads(t, n_heads):
    return t.reshape(t.shape[0], t.shape[1], n_heads, -1).transpose(0, 2, 1, 3)


def _short_conv(x, w):
    return lax.conv_general_dilated(x, w.astype(x.dtype)[:, None, :], window_strides=(1,),
                                    padding=[(CONV_K // 2, CONV_K // 2)],
                                    dimension_numbers=('NWC', 'WIO', 'NWC'),
                                    feature_group_count=x.shape[-1])


def _delta_chunks(q, k, v, g, beta, s0, with_out):
    bsz, nh, n, _ = q.shape
    nc = n // CHUNK_A
    q, k, v = (t.reshape(bsz, nh, nc, CHUNK_A, -1) for t in (q, k, v))
    gc = jnp.cumsum(g.reshape(bsz, nh, nc, CHUNK_A), axis=-1)
    beta = beta.reshape(bsz, nh, nc, CHUNK_A)[..., None]
    incl = jnp.tril(jnp.ones((CHUNK_A, CHUNK_A), bool))
    decay = jnp.exp(jnp.where(incl, gc[..., :, None] - gc[..., None, :], -jnp.inf))
    kb = k * beta
    eye = jnp.eye(CHUNK_A, dtype=jnp.float32)
    lmat = jnp.tril(jnp.einsum('bhnid,bhnjd->bhnij', kb, k) * decay, -1)
    tinv = lax.linalg.triangular_solve(eye + lmat, jnp.broadcast_to(eye, lmat.shape),
                                       left_side=True, lower=True, unit_diagonal=True)
    u = tinv @ (v * beta)
    w = tinv @ (kb * jnp.exp(gc)[..., None])
    kg = k * jnp.exp(gc[..., -1:] - gc)[..., None]
    glast = jnp.exp(gc[..., -1])[..., None, None]
    xs = (u, w, kg, glast)
    if with_out:
        qk = jnp.einsum('bhnid,bhnjd->bhnij', q, k) * decay
        xs = xs + (q * jnp.exp(gc)[..., None], qk)

    def step(s, xs_n):
        u_n, w_n, kg_n, gl_n, *rest = xs_n
        v_new = u_n - w_n @ s
        o = rest[0] @ s + rest[1] @ v_new if with_out else None
        return s * gl_n + jnp.swapaxes(kg_n, -1, -2) @ v_new, o

    s, o = lax.scan(step, s0, tuple(jnp.moveaxis(t, 2, 0) for t in xs))
    if with_out:
        o = jnp.moveaxis(o, 0, 2).reshape(bsz, nh, n, -1)
    return o, s


def _gla_chunks(q, k, v, glog, s0, with_out):
    bsz, nh, n, _ = q.shape
    nc = n // CHUNK_B
    q, k, v, glog = (t.reshape(bsz, nh, nc, CHUNK_B, -1) for t in (q, k, v, glog))
    gc = jnp.cumsum(glog, axis=3)
    kg = k * jnp.exp(gc[..., -1:, :] - gc)
    glast = jnp.exp(gc[..., -1, :])[..., None]
    xs = (kg, v, glast)
    if with_out:
        incl = jnp.tril(jnp.ones((CHUNK_B, CHUNK_B), bool))[..., None]
        rel = jnp.exp(jnp.where(incl, gc[..., :, None, :] - gc[..., None, :, :], -jnp.inf))
        o_intra = jnp.einsum('bhnid,bhnjd,bhnijd->bhnij', q, k, rel) @ v
        xs = xs + (q * jnp.exp(gc), o_intra)

    def step(s, xs_n):
        kg_n, v_n, gl_n, *rest = xs_n
        o = rest[0] @ s + rest[1] if with_out else None
        return s * gl_n + jnp.swapaxes(kg_n, -1, -2) @ v_n, o

    s, o = lax.scan(step, s0, tuple(jnp.moveaxis(t, 2, 0) for t in xs))
    if with_out:
        o = jnp.moveaxis(o, 0, 2).reshape(bsz, nh, n, -1)
    return o, s


def _bidir_scan(chunk_fn, ctx_dirs, lat_dirs, state_shape, ctx_out):
    o_ctx, o_lat = None, None
    for d in range(2):
        fl = (lambda t: jnp.flip(t, axis=2)) if d == 1 else (lambda t: t)
        oc, s_ctx = chunk_fn(*[fl(t) for t in ctx_dirs[d]], jnp.zeros(state_shape, jnp.float32), ctx_out)
        ol, _ = chunk_fn(*[fl(t) for t in lat_dirs[d]], s_ctx, True)
        o_lat = fl(ol) if o_lat is None else o_lat + fl(ol)
        if ctx_out:
            o_ctx = fl(oc) if o_ctx is None else o_ctx + fl(oc)
    return o_ctx, o_lat


def _deltanet_branch(pl, pc, conv_w, a_log, dt_bias, gn, ctx_out):
    def prep(p):
        q, k, v, z, b, a = p
        bsz, n = q.shape[:2]
        qkv = jax.nn.silu(_short_conv(jnp.concatenate([q, k, v], axis=-1), conv_w))
        q, k, v = jnp.split(qkv, [HA * DKA, 2 * HA * DKA], axis=-1)
        q = _l2norm(_heads(q, HA)) * DKA ** -0.5
        k = _l2norm(_heads(k, HA))
        v = _heads(v, HA)
        beta = jax.nn.sigmoid(b.reshape(bsz, n, 2, HA)).transpose(2, 0, 3, 1)
        g = (-jnp.exp(a_log) * jax.nn.softplus(a.reshape(bsz, n, 2, HA) + dt_bias)).transpose(2, 0, 3, 1)
        return [(q, k, v, g[d], beta[d]) for d in range(2)]

    o_c, o_l = _bidir_scan(_delta_chunks, prep(pc), prep(pl), (pl[0].shape[0], HA, DKA, DVA), ctx_out)

    def out(o, z):
        bsz, n = z.shape[:2]
        o = _rmsnorm(o.transpose(0, 2, 1, 3), gn) * jax.nn.silu(z.reshape(bsz, n, HA, DVA))
        return o.reshape(bsz, n, HA * DVA)

    return out(o_l, pl[3]), (out(o_c, pc[3]) if ctx_out else None)


def _gla_branch(pl, pc, w_gate2, b_gate, gn, ctx_out):
    def prep(p):
        q, k, v, r, glr = p
        bsz, n = q.shape[:2]
        q = _heads(q, HB) * DKB ** -0.5
        k = _heads(k, HB)
        v = _heads(v, HB)
        glr = glr.reshape(bsz, n, 2, GATE_RANK)
        return [(q, k, v, _heads(jax.nn.log_sigmoid(glr[:, :, d] @ w_gate2[d] + b_gate[d]) / GATE_NORM, HB))
                for d in range(2)]

    o_c, o_l = _bidir_scan(_gla_chunks, prep(pc), prep(pl), (pl[0].shape[0], HB, DKB, DVB), ctx_out)

    def out(o, r):
        bsz, n = r.shape[:2]
        o = _rmsnorm(o.transpose(0, 2, 1, 3), gn) * jax.nn.silu(r.reshape(bsz, n, HB, DVB))
        return o.reshape(bsz, n, HB * DVB)

    return out(o_l, pl[3]), (out(o_c, pc[3]) if ctx_out else None)


def _rope_tables(n_tokens):
    rows = n_tokens // GRID_W
    row = jnp.repeat(jnp.arange(rows, dtype=jnp.float32), GRID_W)
    col = jnp.tile(jnp.arange(GRID_W, dtype=jnp.float32), rows)
    n_freq = DKC // 4
    inv_freq = ROPE_THETA ** (-jnp.arange(n_freq, dtype=jnp.float32) / n_freq)
    ang_r = row[:, None] * inv_freq
    ang_c = col[:, None] * inv_freq
    return jnp.cos(ang_r), jnp.sin(ang_r), jnp.cos(ang_c), jnp.sin(ang_c)


def _rope_axis(x, cos, sin):
    x1, x2 = jnp.split(x, 2, axis=-1)
    return jnp.concatenate([x1 * cos - x2 * sin, x2 * cos + x1 * sin], axis=-1)


def _axial_rope(x, rope):
    cos_r, sin_r, cos_c, sin_c = rope
    xr, xc = jnp.split(x, 2, axis=-1)
    return jnp.concatenate([_rope_axis(xr, cos_r, sin_r), _rope_axis(xc, cos_c, sin_c)], axis=-1)


def _diff_branch(pl, pc, lam, gn, lam_init, ctx_out):
    def qk_heads(t):
        return t.reshape(t.shape[0], t.shape[1], HC, 2, DKC).transpose(0, 2, 3, 1, 4)

    ql, kl, vl = pl
    qc, kc, vc = pc
    bsz, n = ql.shape[:2]
    rope = _rope_tables(n)
    scale = DKC ** -0.5
    ql = _axial_rope(qk_heads(ql), rope) * scale
    kl = _axial_rope(qk_heads(kl), rope)
    qc = qk_heads(qc) * scale
    kc = qk_heads(kc)
    vl = _heads(vl, HC)
    vc = _heads(vc, HC)
    lam_f = (jnp.exp(jnp.sum(lam[0] * lam[1])) - jnp.exp(jnp.sum(lam[2] * lam[3])) + lam_init).astype(jnp.float32)

    def attend(qb, k, v):
        p = jax.nn.softmax(jnp.einsum('bhmqd,bhmkd->bhmqk', qb, k), axis=-1)
        return jnp.einsum('bhqk,bhkd->bhqd', p[:, :, 0] - lam_f * p[:, :, 1], v)

    k_all = jnp.concatenate([kc, kl], axis=3)
    v_all = jnp.concatenate([vc, vl], axis=2)
    n_blk = n // Q_BLOCK
    q_blocks = jnp.moveaxis(ql.reshape(bsz, HC, 2, n_blk, Q_BLOCK, DKC), 3, 0)
    o_l = lax.map(lambda qb: attend(qb, k_all, v_all), q_blocks)
    o_l = jnp.moveaxis(o_l, 0, 2).reshape(bsz, HC, n, DVC)

    def out(o):
        o = _rmsnorm(o, gn) * (1.0 - lam_init)
        return o.transpose(0, 2, 1, 3).reshape(o.shape[0], o.shape[2], HC * DVC)

    return out(o_l), (out(attend(qc, kc, vc)) if ctx_out else None)


def _mixer(hl, hc, w_in, conv_a, a_log, dt_bias, gn_a, w_gate2, b_gate, gn_b, lam_c, gn_c,
           w_o_a, w_o_b, w_o_c, w_out, lam_init, ctx_out):
    pl = _split((hl @ w_in).astype(jnp.float32), IN_SIZES)
    pc = _split((hc @ w_in).astype(jnp.float32), IN_SIZES)
    a_l, a_c = _deltanet_branch(pl[0:6], pc[0:6], conv_a, a_log, dt_bias, gn_a, ctx_out)
    b_l, b_c = _gla_branch(pl[6:11], pc[6:11], w_gate2, b_gate, gn_b, ctx_out)
    d_l, d_c = _diff_branch(pl[11:14], pc[11:14], lam_c, gn_c, lam_init, ctx_out)

    def merge(ya, yb, yd, gates):
        ga, gb, gd = jnp.split(jax.nn.sigmoid(gates), 3, axis=-1)
        return (ga * (ya @ w_o_a) + gb * (yb @ w_o_b) + gd * (yd @ w_o_c)) @ w_out

    return merge(a_l, b_l, d_l, pl[14]), (merge(a_c, b_c, d_c, pc[14]) if ctx_out else None)


def _swiglu(h, w1, w3, w2):
    return (jax.nn.silu(h @ w1) * (h @ w3)) @ w2


def _moe(h, router_w, w1, w3, w2):
    logits = (h @ router_w).astype(jnp.float32)
    top_val, top_idx = lax.top_k(logits, TOP_K)
    wts = jax.nn.softmax(top_val, axis=-1)
    gate = jnp.einsum('blk,blke->ble', wts, jax.nn.one_hot(top_idx, N_EXPERTS, dtype=jnp.float32))
    out = None
    for e in range(N_EXPERTS):
        contrib = gate[..., e:e + 1] * _swiglu(h, w1[e], w3[e], w2[e])
        out = contrib if out is None else out + contrib
    return out


def setup_inputs(seed: int = 0) -> dict:
    key = jax.random.key(seed)
    ks = iter(jax.random.split(key, 40))
    f32 = jnp.float32

    def nrm(shape, scale):
        return jax.random.normal(next(ks), shape, f32) * scale

    def gain(shape):
        return 1.0 + nrm(shape, 0.02)

    dt = jnp.exp(jax.random.uniform(next(ks), (DEPTH, 2, HA), f32, math.log(1e-3), math.log(1e-1)))
    return {
        'x': nrm((BATCH, SEQ, D_MODEL), 1.0),
        'c': nrm((BATCH, D_MODEL), 1.0),
        'ctx': nrm((BATCH, CTX_LEN, D_MODEL), 1.0),
        'c_ctx': nrm((D_MODEL,), 1.0),
        'w_mod': nrm((DEPTH, D_MODEL, 6 * D_MODEL), 0.5 * D_MODEL ** -0.5),
        'b_mod': nrm((DEPTH, 6 * D_MODEL), 0.01),
        'norm1_g': gain((DEPTH, D_MODEL)),
        'norm2_g': gain((DEPTH, D_MODEL)),
        'w_in': nrm((DEPTH, D_MODEL, IN_COLS), D_MODEL ** -0.5),
        'conv_a': nrm((DEPTH, CONV_K, CONV_CH), CONV_K ** -0.5),
        'a_log': jnp.log(jax.random.uniform(next(ks), (DEPTH, 2, HA), f32, 1.0, 16.0)),
        'dt_bias': dt + jnp.log(-jnp.expm1(-dt)),
        'gn_a': gain((DEPTH, DVA)),
        'w_gate2': nrm((DEPTH, 2, GATE_RANK, HB * DKB), GATE_RANK ** -0.5),
        'b_gate': nrm((DEPTH, 2, HB * DKB), 0.1),
        'gn_b': gain((DEPTH, DVB)),
        'lam_c': nrm((DEPTH, 4, DKC), 0.1),
        'gn_c': gain((DEPTH, DVC)),
        'w_o_a': nrm((DEPTH, HA * DVA, D_MODEL), (HA * DVA) ** -0.5),
        'w_o_b': nrm((DEPTH, HB * DVB, D_MODEL), (HB * DVB) ** -0.5),
        'w_o_c': nrm((DEPTH, HC * DVC, D_MODEL), (HC * DVC) ** -0.5),
        'w_out': nrm((DEPTH, D_MODEL, D_MODEL), D_MODEL ** -0.5),
        'ffn_w1': nrm((N_DENSE, D_MODEL, D_FF), D_MODEL ** -0.5),
        'ffn_w3': nrm((N_DENSE, D_MODEL, D_FF), D_MODEL ** -0.5),
        'ffn_w2': nrm((N_DENSE, D_FF, D_MODEL), D_FF ** -0.5),
        'router_w': nrm((N_MOE, D_MODEL, N_EXPERTS), D_MODEL ** -0.5),
        'moe_w1': nrm((N_MOE, N_EXPERTS, D_MODEL, D_FF), D_MODEL ** -0.5),
        'moe_w3': nrm((N_MOE, N_EXPERTS, D_MODEL, D_FF), D_MODEL ** -0.5),
        'moe_w2': nrm((N_MOE, N_EXPERTS, D_FF, D_MODEL), D_FF ** -0.5),
        'final_g': gain((D_MODEL,)),
    }


def reference(x, c, ctx, c_ctx, w_mod, b_mod, norm1_g, norm2_g, w_in, conv_a, a_log, dt_bias, gn_a,
              w_gate2, b_gate, gn_b, lam_c, gn_c, w_o_a, w_o_b, w_o_c, w_out, ffn_w1, ffn_w3, ffn_w2,
              router_w, moe_w1, moe_w3, moe_w2, final_g):
    xl, xc = x, ctx
    for l in range(DEPTH):
        ctx_out = l < DEPTH - 1
        lam_init = 0.8 - 0.6 * math.exp(-0.3 * l)
        mod_l = jnp.split((jax.nn.silu(c) @ w_mod[l] + b_mod[l])[:, None, :], 6, axis=-1)
        mod_c = jnp.split(jax.nn.silu(c_ctx) @ w_mod[l] + b_mod[l], 6, axis=-1)
        hl = _modulate(xl, norm1_g[l], mod_l[0], mod_l[1])
        hc = _modulate(xc, norm1_g[l], mod_c[0], mod_c[1])
        yl, yc = _mixer(hl, hc, w_in[l], conv_a[l], a_log[l], dt_bias[l], gn_a[l], w_gate2[l], b_gate[l],
                        gn_b[l], lam_c[l], gn_c[l], w_o_a[l], w_o_b[l], w_o_c[l], w_out[l], lam_init, ctx_out)
        xl = xl + (mod_l[2] * yl).astype(xl.dtype)
        if ctx_out:
            xc = xc + (mod_c[2] * yc).astype(xc.dtype)

        def ffn(h):
            i = l // 2
            if l % 2 == 0:
                return _swiglu(h, ffn_w1[i], ffn_w3[i], ffn_w2[i])
            return _moe(h, router_w[i], moe_w1[i], moe_w3[i], moe_w2[i])

        hl = _modulate(xl, norm2_g[l], mod_l[3], mod_l[4])
        xl = xl + (mod_l[5] * ffn(hl)).astype(xl.dtype)
        if ctx_out:
            hc = _modulate(xc, norm2_g[l], mod_c[3], mod_c[4])
            xc = xc + (mod_c[5] * ffn(hc)).astype(xc.dtype)
    return _rmsnorm(xl, final_g).astype(x.dtype)
```

```python
import contextlib
import numpy as np
import concourse.bass as bass
import concourse.mybir as mybir
from concourse.bass_utils import run_bass_kernel_spmd

F32 = mybir.dt.float32
BF16 = mybir.dt.bfloat16
AF = mybir.ActivationFunctionType
ALU = mybir.AluOpType
AX = mybir.AxisListType


class _Op:
    __slots__ = ("idx", "eng", "fn", "deps", "dma", "sem", "semval", "needs_inc", "prev_dma")


class _Unit:
    __slots__ = ("w", "r", "rd")

    def __init__(self):
        self.w = None
        self.r = {}
        self.rd = []


class V:
    __slots__ = ("ap", "key")

    def __init__(self, ap, key):
        self.ap = ap
        self.key = key


def _apk(x):
    if isinstance(x, V):
        return x.ap, x.key
    return x, None


class Prog:
    NDMA = 8

    def __init__(self):
        self.nc = bass.Bass("TRN2", target_bir_lowering=False)
        self.ops = []
        self.names = {}
        self.base_deps = []
        self.last = {}
        self.dma_since_barrier = []
        self.stack = contextlib.ExitStack()
        self.n_dma = {"sp": 0, "act": 0, "pool": 0}
        self.dma_last = {}
        self.dbg = []
        self._uid = 0
        self.psum_names = set()

    def uid(self, base):
        self._uid += 1
        return f"{base}_{self._uid}"

    def sb(self, name, shape, dtype, stack=None):
        st = stack if stack is not None else self.stack
        return st.enter_context(self.nc.sbuf_tensor(self.uid(name), list(shape), dtype))

    def ps(self, name, shape, dtype=F32, stack=None):
        st = stack if stack is not None else self.stack
        t = st.enter_context(self.nc.psum_tensor(self.uid(name), list(shape), dtype))
        self.psum_names.add(t[:].name)
        return t

    def dram(self, name, shape, dtype, kind="Internal"):
        return self.nc.dram_tensor(name, list(shape), dtype, kind=kind)

    @contextlib.contextmanager
    def phase(self):
        st = contextlib.ExitStack()
        try:
            yield st
        finally:
            self.barrier()
            st.close()

    def _conf(self, name, sub):
        d = self.names.setdefault(name, {})
        if sub is None:
            if None not in d:
                d[None] = _Unit()
            return d[None], list(d.values())
        if sub not in d:
            d[sub] = _Unit()
        res = [d[sub]]
        if None in d:
            res.append(d[None])
        return d[sub], res

    def add(self, eng, fn, reads, writes, dma=False):
        op = _Op()
        op.idx = len(self.ops)
        op.eng = eng
        op.fn = fn
        op.dma = dma
        op.sem = None
        op.semval = 0
        op.needs_inc = False
        op.prev_dma = None
        deps = {}
        for b in self.base_deps:
            deps[b.idx] = b
        for x in reads:
            if x is None or isinstance(x, (int, float)):
                continue
            ap, key = _apk(x)
            prim, conf = self._conf(ap.name, key)
            for u in conf:
                if u.w is not None:
                    deps[u.w.idx] = u.w
                if ap.name in self.psum_names:
                    for re_, r in u.r.items():
                        if re_ != eng:
                            deps[r.idx] = r
            if dma:
                prim.rd.append(op)
            else:
                prim.r[eng] = op
        for x in writes:
            ap, key = _apk(x)
            prim, conf = self._conf(ap.name, key)
            for u in conf:
                if u.w is not None:
                    deps[u.w.idx] = u.w
                for r in u.r.values():
                    deps[r.idx] = r
                for r in u.rd:
                    deps[r.idx] = r
            prim.w = op
            prim.r = {}
            prim.rd = []
        deps.pop(op.idx, None)
        if dma:
            slot = self.n_dma[eng] % self.NDMA
            self.n_dma[eng] += 1
            prev = self.dma_last.get((eng, slot))
            op.prev_dma = prev
            op.sem = (eng, slot)
            op.semval = (prev.semval if prev is not None else 0) + 16
            self.dma_last[(eng, slot)] = op
            self.dma_since_barrier.append(op)
        op.deps = list(deps.values())
        self.ops.append(op)
        self.last[eng] = op
        return op

    def barrier(self):
        b = list(self.last.values()) + list(self.dma_since_barrier)
        self.base_deps = b
        self.dma_since_barrier = []

    def mm(self, out, lhsT, rhs, start=True, stop=True, **kw):
        o, _ = _apk(out); l, _ = _apk(lhsT); r, _ = _apk(rhs)
        nc = self.nc
        return self.add("pe", lambda: nc.tensor.matmul(o, l, r, start=start, stop=stop, **kw),
                        [lhsT, rhs], [out])

    def tr(self, out, in_, ident):
        o, _ = _apk(out); i, _ = _apk(in_); d, _ = _apk(ident)
        nc = self.nc
        return self.add("pe", lambda: nc.tensor.transpose(o, i, d), [in_, ident], [out])

    def act(self, out, in_, func, bias=None, scale=None, accum_out=None):
        o, _ = _apk(out); i, _ = _apk(in_)
        kw = {}
        rd = [in_]
        wr = [out]
        if bias is not None:
            kw["bias"] = _apk(bias)[0] if not isinstance(bias, (int, float)) else bias
            rd.append(bias)
        if scale is not None:
            kw["scale"] = _apk(scale)[0] if not isinstance(scale, (int, float)) else scale
            rd.append(scale)
        if accum_out is not None:
            kw["accum_out"] = _apk(accum_out)[0]
            wr.append(accum_out)
        nc = self.nc
        return self.add("act", lambda: nc.scalar.activation(out=o, in_=i, func=func, **kw), rd, wr)

    def _ve(self, eng):
        return self.nc.vector if eng == "dve" else self.nc.gpsimd

    def tt(self, eng, out, in0, in1, op):
        o, _ = _apk(out); a, _ = _apk(in0); b, _ = _apk(in1)
        e = self._ve(eng)
        return self.add(eng, lambda: e.tensor_tensor(out=o, in0=a, in1=b, op=op), [in0, in1], [out])

    def ts(self, eng, out, in0, s1, s2=None, op0=ALU.mult, op1=None, accum_out=None):
        o, _ = _apk(out); a, _ = _apk(in0)
        e = self._ve(eng)
        s1v = s1 if isinstance(s1, (int, float)) else _apk(s1)[0]
        s2v = s2 if (s2 is None or isinstance(s2, (int, float))) else _apk(s2)[0]
        kw = {}
        wr = [out]
        if op1 is not None:
            kw["op1"] = op1
        if accum_out is not None:
            kw["accum_out"] = _apk(accum_out)[0]
            wr.append(accum_out)
        return self.add(eng, lambda: e.tensor_scalar(out=o, in0=a, scalar1=s1v, scalar2=s2v, op0=op0, **kw),
                        [in0, s1, s2], wr)

    def stt(self, eng, out, in0, scalar, in1, op0, op1):
        o, _ = _apk(out); a, _ = _apk(in0); b, _ = _apk(in1)
        e = self._ve(eng)
        sv = scalar if isinstance(scalar, (int, float)) else _apk(scalar)[0]
        return self.add(eng, lambda: e.scalar_tensor_tensor(out=o, in0=a, scalar=sv, in1=b, op0=op0, op1=op1),
                        [in0, scalar, in1], [out])

    def copy(self, eng, out, in_):
        o, _ = _apk(out); i, _ = _apk(in_)
        nc = self.nc
        if eng == "act":
            return self.add("act", lambda: nc.scalar.copy(out=o, in_=i), [in_], [out])
        e = self._ve(eng)
        return self.add(eng, lambda: e.tensor_copy(out=o, in_=i), [in_], [out])

    def memset(self, eng, out, val):
        o, _ = _apk(out)
        e = self._ve(eng)
        return self.add(eng, lambda: e.memset(o, val), [], [out])

    def reduce(self, eng, out, in_, op, axis=AX.X):
        o, _ = _apk(out); i, _ = _apk(in_)
        e = self._ve(eng)
        return self.add(eng, lambda: e.tensor_reduce(out=o, in_=i, axis=axis, op=op), [in_], [out])

    def recip(self, out, in_):
        o, _ = _apk(out); i, _ = _apk(in_)
        nc = self.nc
        return self.add("dve", lambda: nc.vector.reciprocal(out=o, in_=i), [in_], [out])

    def dma(self, q, out, in_):
        o, _ = _apk(out); i, _ = _apk(in_)
        e = {"sp": self.nc.sync, "act": self.nc.scalar, "pool": self.nc.gpsimd}[q]
        return self.add(q, lambda: e.dma_start(out=o, in_=i), [in_], [out], dma=True)

    def dump(self, name, ap, dtype=None):
        a, _ = _apk(ap)
        t = self.nc.dram_tensor("dbg_" + name, list(a.shape), dtype or a.dtype, kind="ExternalOutput")
        self.dbg.append("dbg_" + name)
        self.dma("sp", t.ap(), ap)

    def emit(self):
        nc = self.nc
        engobj = {"pe": nc.tensor, "act": nc.scalar, "dve": nc.vector, "pool": nc.gpsimd, "sp": nc.sync}
        st = self.stack
        esem = {e: st.enter_context(nc.semaphore("s_" + e)) for e in ("pe", "act", "dve", "pool")}
        dsem = {}
        for q in ("sp", "act", "pool"):
            for s in range(self.NDMA):
                if (q, s) in self.dma_last:
                    dsem[(q, s)] = st.enter_context(nc.semaphore(f"d_{q}{s}"))
        for op in self.ops:
            for p in op.deps:
                if not p.dma and not (p.eng == "pe" and op.eng == "pe"):
                    p.needs_inc = True
        finals = list(self.last.values())
        for p in finals:
            if not p.dma:
                p.needs_inc = True
        cnt = {e: 0 for e in esem}
        for op in self.ops:
            if not op.dma and op.needs_inc:
                cnt[op.eng] += 1
                op.semval = cnt[op.eng]
        waited = {e: {} for e in engobj}
        nwait = 0
        for op in self.ops:
            e = engobj[op.eng]
            need = {}
            for p in op.deps:
                if p.dma:
                    k = ("d", p.sem)
                    v = p.semval
                else:
                    if p.eng == "pe" and op.eng == "pe":
                        continue
                    k = ("e", p.eng)
                    v = p.semval
                if need.get(k, 0) < v:
                    need[k] = v
            if op.dma and op.prev_dma is not None:
                k = ("d", op.sem)
                if need.get(k, 0) < op.prev_dma.semval:
                    need[k] = op.prev_dma.semval
            w = waited[op.eng]
            for k, v in need.items():
                if w.get(k, 0) >= v:
                    continue
                w[k] = v
                sem = dsem[k[1]] if k[0] == "d" else esem[k[1]]
                e.wait_ge(sem, v)
                nwait += 1
            ins = op.fn()
            if op.dma:
                ins.then_inc(dsem[op.sem], 16)
            elif op.needs_inc:
                ins.then_inc(esem[op.eng], 1)
        sp = nc.sync
        for en, sem in esem.items():
            if cnt[en] > 0:
                sp.wait_ge(sem, cnt[en])
        for k, p in self.dma_last.items():
            sp.wait_ge(dsem[k], p.semval)
        self.stats = dict(n_ops=len(self.ops), n_wait=nwait, incs=dict(cnt))
        return nc

D = 1024
KC = 8
NL = 2048
NCX = 256
NT = 2304
NTL = 18
TB = [(0, 256), (256, 512), (768, 512), (1280, 512), (1792, 512)]
DFF = 2816
FC = 22
EPS = 1e-6
A_Q, A_K, A_V, A_Z, A_B, A_G = 0, 512, 1024, 1536, 2048, 2056
B_Q, B_K, B_V, B_R, B_GL = 2064, 2320, 2576, 3088, 3600
C_Q, C_K, C_V = 3632, 4144, 4656
G_A, G_B, G_D = 5168, 6192, 7216


class Ctx:
    pass


def build(cfg):
    P = Prog()
    nc = P.nc
    S = Ctx()
    S.P = P
    S.cfg = cfg
    L = cfg.get("layers", 2)

    def din(name, shape, dt=F32):
        return nc.dram_tensor(name, list(shape), dt, kind="ExternalInput").ap()

    S.x = din("x", [NL, D])
    S.ctx = din("ctx", [NCX, D])
    S.cvec = din("cvec", [128, KC, 2])
    S.w_mod = din("w_mod", [2, D, 6 * D])
    S.b_mod = din("b_mod", [2, 128, 48])
    S.norm_g = din("norm_g", [128, 5, KC])
    S.w_in = din("w_in", [2, D, 8240])
    S.consts = din("consts", [128, 1024])
    S.rope = din("rope", [2, 128, NL])
    S.gn = din("gn", [2, 3, 128])
    S.lam_c = din("lam_c", [2, 4, 64])
    S.wg2 = din("wg2", [2, 17, 2, 256])
    S.convw = din("convw", [2, 128, 12, 3])
    S.adt = din("adt", [2, 2, 8])
    S.w_o_a = din("w_o_a", [2, 512, D])
    S.w_o_b = din("w_o_b", [2, 512, D])
    S.w_o_c = din("w_o_c", [2, 512, D])
    S.w_out = din("w_out", [2, D, D])
    S.ffn_w1 = din("ffn_w1", [1, D, DFF])
    S.ffn_w3 = din("ffn_w3", [1, D, DFF])
    S.ffn_w2 = din("ffn_w2", [1, DFF, D])
    S.router_w = din("router_w", [1, D, 8])
    S.moe_w1 = din("moe_w1", [1, 8, D, DFF])
    S.moe_w3 = din("moe_w3", [1, 8, D, DFF])
    S.moe_w2 = din("moe_w2", [1, 8, DFF, D])
    S.out = nc.dram_tensor("out", [NL, D], F32, kind="ExternalOutput").ap()

    st = P.stack
    S.xT = P.sb("xT", [128, KC, NT], F32)
    S.hT = P.sb("hT", [128, KC, NT], BF16)
    S.cf = P.sb("cf", [128, 1024], F32)
    S.identf = S.cf[:, 0:128]
    S.identb_t = P.sb("identb", [128, 128], BF16)
    S.identb = S.identb_t[:, :]
    S.onesb_t = P.sb("onesb", [128, 128], BF16)
    S.onesb = S.onesb_t[:, :]
    S.onesf_t = P.sb("onesf", [128, 128], F32)
    S.onesf = S.onesf_t[:, :]
    S.modv = P.sb("modv", [128, 2, 48, 2], F32)
    S.ng = P.sb("ng", [128, 5, KC], F32)
    S.gs = P.sb("gs", [128, 4, KC, 2], F32)
    S.pb = [P.ps(f"pb{i}", [128, 512], F32) for i in range(8)]

    P.dma("sp", S.cf[:, :], S.consts)
    P.dma("sp", S.ng[:, :, :], S.norm_g)
    P.copy("dve", S.identb, S.identf)
    P.memset("dve", S.onesb, 1.0)
    P.memset("dve", S.onesf, 1.0)
    S.eps_t = P.sb("eps_t", [128, 1], F32)
    P.memset("dve", S.eps_t[:, :], EPS)
    S.zero_t = P.sb("zero_t", [128, 1], F32)
    P.memset("dve", S.zero_t[:, :], 0.0)

    phase_load(S)
    phase_mod(S, L)
    for l in range(L):
        if cfg.get("mixer", True):
            phase_mixer(S, l)
        if cfg.get("ffn", True):
            phase_ffn(S, l)
    phase_final(S)
    P.emit()
    return P


def phase_load(S):
    P = S.P
    with P.phase() as st:
        tin = [P.sb("ld_in", [128, D], F32, st) for _ in range(3)]
        for tt in range(NTL):
            src = S.ctx[tt * 128:(tt + 1) * 128, :] if tt < 2 else S.x[(tt - 2) * 128:(tt - 1) * 128, :]
            ti = tin[tt % 3]
            P.dma("sp", ti[:, :], src)
            for half in range(2):
                pb = S.pb[(tt * 2 + half) % 4]
                for j in range(4):
                    kc = half * 4 + j
                    P.tr(pb[:, j * 128:(j + 1) * 128], ti[:, kc * 128:(kc + 1) * 128], S.identf)
                dst = S.xT[:, half * 4:(half + 1) * 4, tt * 128:(tt + 1) * 128]
                srcp = pb[:, :].rearrange("p (j t) -> p j t", j=4)
                if half == 0:
                    P.copy("dve", dst, srcp)
                else:
                    P.copy("act", dst, srcp)


def phase_mod(S, L):
    P = S.P
    with P.phase() as st:
        cv = P.sb("cv", [128, KC, 2], F32, st)
        cvb = P.sb("cvb", [128, KC, 2], BF16, st)
        bm = P.sb("bm", [128, 2, 48], F32, st)
        wm = [P.sb("wm", [128, KC, 1024], BF16, st) for _ in range(2)]
        P.dma("sp", cv[:, :, :], S.cvec)
        P.dma("sp", bm[:, :, :], S.b_mod.rearrange("l p f -> p l f"))
        P.act(cvb[:, :, :], cv[:, :, :], AF.Silu)
        n = 0
        for l in range(L):
            for i in range(6):
                w = wm[n % 2]
                n += 1
                P.dma("pool", w[:, :, :],
                      S.w_mod[l, :, i * 1024:(i + 1) * 1024].rearrange("(kc p) f -> p kc f", p=128))
                pb = S.pb[4 + (n % 2)]
                for fc in range(8):
                    for kc in range(KC):
                        P.mm(pb[:, fc * 2:fc * 2 + 2], w[:, kc, fc * 128:(fc + 1) * 128], cvb[:, kc, :],
                             start=(kc == 0), stop=(kc == KC - 1))
                P.tt("dve", S.modv[:, l, i * 8:(i + 1) * 8, :],
                     pb[:, 0:16].rearrange("p (f w) -> p f w", w=2),
                     bm[:, l, i * 8:(i + 1) * 8].unsqueeze(2).to_broadcast([128, 8, 2]), ALU.add)
            for wn, (gi, si) in enumerate(((l, 1), (2 + l, 4))):
                dst = S.gs[:, l * 2 + wn, :, :]
                P.ts("dve", dst, S.modv[:, l, si * 8:(si + 1) * 8, :], 1.0, None, ALU.add)
                P.tt("dve", dst, dst, S.ng[:, gi, :].unsqueeze(2).to_broadcast([128, 8, 2]), ALU.mult)


def modulate(S, l, wn, blocks, st, router=None):
    P = S.P
    sq = [P.sb("sq", [128, KC, 512], BF16, st) for _ in range(2)]
    rstd = [P.sb("rstd", [128, 512], F32, st) for _ in range(2)]
    tmp = [P.sb("mtmp", [128, 512], F32, st) for _ in range(3)]
    h32 = [P.sb("mh32", [128, 512], F32, st) for _ in range(2)] if router is not None else None
    si = 0 if wn == 0 else 3
    n = 0
    for bi in blocks:
        t0, tn = TB[bi]
        which = 1 if bi == 0 else 0
        s = sq[bi % 2]
        pb = S.pb[6 + bi % 2]
        for kc in range(KC):
            P.act(s[:, kc, :tn], S.xT[:, kc, t0:t0 + tn], AF.Square)
        for kc in range(KC):
            P.mm(pb[:, :tn], S.onesb, s[:, kc, :tn], start=(kc == 0), stop=(kc == KC - 1))
        r = rstd[bi % 2]
        P.act(r[:, :tn], pb[:, :tn], AF.Sqrt, bias=S.eps_t[:, :], scale=1.0 / D)
        P.recip(r[:, :tn], r[:, :tn])
        for kc in range(KC):
            t = tmp[n % 3]
            n += 1
            P.stt("dve", t[:, :tn], S.xT[:, kc, t0:t0 + tn], S.gs[:, l * 2 + wn, kc, which:which + 1],
                  r[:, :tn], ALU.mult, ALU.mult)
            P.act(S.hT[:, kc, t0:t0 + tn], t[:, :tn], AF.Identity,
                  bias=S.modv[:, l, si * 8 + kc, which:which + 1])
            if router is not None:
                rw, logit = router
                hh = h32[kc % 2]
                P.ts("dve", hh[:, :tn], t[:, :tn], S.modv[:, l, si * 8 + kc, which:which + 1], None, ALU.add)
                pl = S.pb[5]
                for j in range(tn // 128):
                    P.mm(pl[:, j * 8:(j + 1) * 8], hh[:, j * 128:(j + 1) * 128], rw[:, kc, :],
                         start=(kc == 0 and j == 0), stop=(kc == KC - 1), skip_group_check=True)
        if router is not None:
            rw, logit = router
            tile0 = (t0 - NCX) // 128
            P.copy("dve", logit[:, tile0:tile0 + tn // 128, :],
                   S.pb[5][:, 0:(tn // 128) * 8].rearrange("p (j e) -> p j e", e=8))


def phase_final(S):
    P = S.P
    with P.phase() as st:
        sq = [P.sb("fsq", [128, KC, 512], BF16, st) for _ in range(2)]
        rstd = [P.sb("frstd", [128, 512], F32, st) for _ in range(2)]
        yT = [P.sb("fyT", [128, KC, 512], F32, st) for _ in range(2)]
        ot = [P.sb("fot", [128, D], F32, st) for _ in range(3)]
        g32 = S.ng[:, 4, :]
        n = 0
        for bi in range(1, 5):
            t0, tn = TB[bi]
            s = sq[bi % 2]
            pb = S.pb[6 + bi % 2]
            for kc in range(KC):
                P.act(s[:, kc, :], S.xT[:, kc, t0:t0 + tn], AF.Square)
            for kc in range(KC):
                P.mm(pb[:, :], S.onesb, s[:, kc, :], start=(kc == 0), stop=(kc == KC - 1))
            r = rstd[bi % 2]
            P.act(r[:, :], pb[:, :], AF.Sqrt, bias=S.eps_t[:, :], scale=1.0 / D)
            P.recip(r[:, :], r[:, :])
            y = yT[bi % 2]
            for kc in range(KC):
                P.stt("dve", y[:, kc, :], S.xT[:, kc, t0:t0 + tn], g32[:, kc:kc + 1],
                      r[:, :], ALU.mult, ALU.mult)
            for q in range(4):
                o = ot[n % 3]
                n += 1
                for half in range(2):
                    pt = S.pb[(n * 2 + half) % 4]
                    for j in range(4):
                        kc = half * 4 + j
                        P.tr(pt[:, j * 128:(j + 1) * 128], y[:, kc, q * 128:(q + 1) * 128], S.identf)
                    if half == 0:
                        P.copy("dve", o[:, 0:512], pt[:, :])
                    else:
                        P.copy("act", o[:, 512:1024], pt[:, :])
                r0 = t0 - NCX + q * 128
                P.dma("sp", S.out[r0:r0 + 128, :], o[:, :])

import math


def phase_mixer(S, l):
    P = S.P
    ctx_out = l < 1
    with P.phase() as st:
        modulate(S, l, 0, range(5), st)
    if S.cfg.get("dump_h") == l:
        P.dump("hT", S.hT[:, :, :])
    br = S.cfg.get("branches", "abc")
    if "c" in br:
        branch_c(S, l, ctx_out)
    if "b" in br:
        branch_b(S, l, ctx_out)
    if "a" in br:
        branch_a(S, l, ctx_out)
    if S.cfg.get("dump_xmix") == l:
        P.dump("xT", S.xT[:, :, :])


def merge_branch(S, l, yT, wo_dram, gcol, ctx_out, st):
    P = S.P
    wg = P.sb("wg", [128, KC, 1024], BF16, st)
    wo = P.sb("wo", [128, 4, 1024], BF16, st)
    wout = P.sb("wout", [128, KC, 1024], BF16, st)
    P.dma("pool", wg[:, :, :], S.w_in[l, :, gcol:gcol + 1024].rearrange("(kc p) f -> p kc f", p=128))
    P.dma("pool", wo[:, :, :], wo_dram[l].rearrange("(c p) f -> p c f", p=128))
    P.dma("pool", wout[:, :, :], S.w_out[l].rearrange("(kc p) f -> p kc f", p=128))
    gmb = [P.sb("gm", [128, KC, 512], BF16, st) for _ in range(2)]
    sgb = [P.sb("sg", [128, 512], F32, st) for _ in range(2)]
    for bi in (range(5) if ctx_out else range(1, 5)):
        t0, tn = TB[bi]
        which = 1 if bi == 0 else 0
        gm = gmb[bi % 2]
        for fc in range(8):
            pg = S.pb[0 + fc % 2]
            py = S.pb[2 + fc % 2]
            for kc in range(KC):
                P.mm(pg[:, :tn], wg[:, kc, fc * 128:(fc + 1) * 128], S.hT[:, kc, t0:t0 + tn],
                     start=(kc == 0), stop=(kc == KC - 1))
            for c in range(4):
                P.mm(py[:, :tn], wo[:, c, fc * 128:(fc + 1) * 128], yT[:, c, t0:t0 + tn],
                     start=(c == 0), stop=(c == 3))
            sg = sgb[fc % 2]
            P.act(sg[:, :tn], pg[:, :tn], AF.Sigmoid)
            P.tt("dve", gm[:, fc, :tn], py[:, :tn], sg[:, :tn], ALU.mult)
        for fo in range(8):
            po = S.pb[4 + fo % 2]
            for fc in range(8):
                P.mm(po[:, :tn], wout[:, fc, fo * 128:(fo + 1) * 128], gm[:, fc, :tn],
                     start=(fc == 0), stop=(fc == 7))
            P.stt("dve", S.xT[:, fo, t0:t0 + tn], po[:, :tn], S.modv[:, l, 16 + fo, which:which + 1],
                  S.xT[:, fo, t0:t0 + tn], ALU.mult, ALU.add)


def branch_c(S, l, ctx_out):
    P = S.P
    lam_init = 0.8 - 0.6 * math.exp(-0.3 * l)
    with P.phase() as sto:
        ydT = P.sb("ydT", [128, 4, NT], BF16, sto)
        if not ctx_out:
            P.memset("pool", ydT[:, :, 0:NCX], 0.0)
        with P.phase() as st:
            ropet = P.sb("ropet", [128, 2, NL], BF16, st)
            P.dma("pool", ropet[:, :, :], S.rope.rearrange("c p t -> p c t"))
            rotT = P.sb("rotT", [128, 128], BF16, st)
            P.copy("dve", rotT[:, :], S.cf[:, 128:256])
            gnbc = P.sb("gnbc", [128, 128], F32, st)
            P.dma("sp", gnbc[:, :], S.gn[l, 2].partition_broadcast(128))
            P.ts("dve", gnbc[:, :], gnbc[:, :], 1.0 - lam_init, None, ALU.mult)
            lam = P.sb("lam", [128, 4, 64], F32, st)
            P.dma("sp", lam[:, :, :], S.lam_c[l].partition_broadcast(128))
            lp = P.sb("lp", [128, 2, 64], F32, st)
            P.tt("dve", lp[:, 0, :], lam[:, 0, :], lam[:, 1, :], ALU.mult)
            P.tt("dve", lp[:, 1, :], lam[:, 2, :], lam[:, 3, :], ALU.mult)
            ls = P.sb("ls", [128, 2], F32, st)
            P.reduce("dve", ls[:, :], lp[:, :, :], ALU.add, AX.X)
            le = P.sb("le", [128, 2], F32, st)
            P.act(le[:, :], ls[:, :], AF.Exp)
            neglam = P.sb("neglam", [128, 1], F32, st)
            P.tt("dve", neglam[:, :], le[:, 1:2], le[:, 0:1], ALU.subtract)
            P.ts("dve", neglam[:, :], neglam[:, :], -lam_init, None, ALU.add)

            wC = P.sb("wC", [128, KC, 1536], BF16, st)
            P.dma("pool", wC[:, :, :], S.w_in[l, :, C_Q:C_Q + 1536].rearrange("(kc p) f -> p kc f", p=128))
            qTh = P.sb("qTh", [128, NT], BF16, st)
            kTh = P.sb("kTh", [128, NT], BF16, st)
            vaug = P.sb("vaug", [128, NTL, 130], BF16, st)
            P.memset("pool", vaug[:, :, 128:130], 1.0)
            xqb = [P.sb("xq", [128, 512], BF16, st) for _ in range(2)]
            t1b = [P.sb("t1", [128, 512], F32, st) for _ in range(2)]
            t2b = [P.sb("t2", [128, 512], F32, st) for _ in range(2)]
            NR = 6
            pring = [P.sb("pT", [128, 512], BF16, st) for _ in range(NR)]
            sm = [P.sb("sm", [128, 4], F32, st) for _ in range(4)]
            tab = [P.sb("ta", [128, 128], F32, st) for _ in range(2)]
            odb = [P.sb("od", [128, 128], F32, st) for _ in range(2)]
            junk = P.sb("junk", [128, 128], BF16, st)
            ytb = [P.sb("yt", [128, 128], BF16, st) for _ in range(2)]
            ptb = S.pb[7][:, 0:256].bitcast(BF16)
            cnt = 0
            ring = 0
            ep = 0
            for h in range(4):
                for dstT, col in ((qTh, h * 128), (kTh, 512 + h * 128)):
                    for bi in range(5):
                        t0, tn = TB[bi]
                        pb = S.pb[6]
                        for kc in range(KC):
                            P.mm(pb[:, :tn], wC[:, kc, col:col + 128], S.hT[:, kc, t0:t0 + tn],
                                 start=(kc == 0), stop=(kc == KC - 1))
                        if bi == 0:
                            P.act(dstT[:, 0:tn], pb[:, :tn], AF.Copy)
                        else:
                            xq = xqb[cnt % 2]; t1 = t1b[cnt % 2]; t2 = t2b[cnt % 2]
                            cnt += 1
                            P.act(xq[:, :], pb[:, :], AF.Copy)
                            pr = S.pb[7]
                            P.mm(pr[:, :], rotT[:, :], xq[:, :])
                            lt0 = t0 - NCX
                            P.tt("dve", t1[:, :], pr[:, :], ropet[:, 1, lt0:lt0 + 512], ALU.mult)
                            P.tt("pool", t2[:, :], xq[:, :], ropet[:, 0, lt0:lt0 + 512], ALU.mult)
                            P.tt("dve", dstT[:, t0:t0 + 512], t1[:, :], t2[:, :], ALU.add)
                vc = 1024 + h * 128
                for g0 in range(0, NTL, 4):
                    ng_ = min(4, NTL - g0)
                    pb = S.pb[6]
                    for j in range(ng_):
                        tt = g0 + j
                        for kc in range(KC):
                            P.mm(pb[:, j * 128:(j + 1) * 128], S.hT[:, kc, tt * 128:(tt + 1) * 128],
                                 wC[:, kc, vc:vc + 128], start=(kc == 0), stop=(kc == KC - 1))
                    P.act(vaug[:, g0:g0 + ng_, 0:128],
                          pb[:, 0:ng_ * 128].rearrange("p (j t) -> p j t", j=ng_), AF.Copy)
                for qb in ([0] if ctx_out else []) + [1, 2, 3, 4]:
                    q0, qn = TB[qb]
                    nq = qn // 128
                    seq = list(range(2)) if qb == 0 else list(range(NTL))

                    def acc(j, m):
                        return S.pb[2 + m * 2 + j // 2][:, (j % 2) * 256:(j % 2) * 256 + 129]

                    for m in range(2):
                        def qk(kt):
                            nonlocal ring
                            ps = S.pb[kt % 2]
                            P.mm(ps[:, :qn], kTh[m * 64:(m + 1) * 64, kt * 128:(kt + 1) * 128],
                                 qTh[m * 64:(m + 1) * 64, q0:q0 + qn])
                            pt = pring[ring % NR]
                            ring += 1
                            P.act(pt[:, :qn], ps[:, :qn], AF.Exp, scale=0.125)
                            return pt
                        LA = 3
                        pts = {}
                        for i, kt in enumerate(seq):
                            if i == 0:
                                for j in range(min(LA, len(seq))):
                                    pts[seq[j]] = qk(seq[j])
                            if i + LA < len(seq):
                                pts[seq[i + LA]] = qk(seq[i + LA])
                            pt = pts.pop(kt)
                            for j in range(nq):
                                P.mm(acc(j, m), pt[:, j * 128:(j + 1) * 128], vaug[:, kt, 0:129],
                                     start=(i == 0 and j % 2 == 0), stop=(i == len(seq) - 1),
                                     skip_group_check=True)
                    for j in range(nq):
                        o0 = acc(j, 0); o1 = acc(j, 1)
                        s = sm[ep % 4]; ta = tab[ep % 2]; od = odb[ep % 2]; yt = ytb[ep % 2]
                        ep += 1
                        P.recip(s[:, 0:1], o0[:, 128:129])
                        P.recip(s[:, 1:2], o1[:, 128:129])
                        P.tt("dve", s[:, 1:2], s[:, 1:2], neglam[:, :], ALU.mult)
                        P.ts("dve", ta[:, :], o0[:, 0:128], s[:, 0:1], None, ALU.mult)
                        P.stt("dve", od[:, :], o1[:, 0:128], s[:, 1:2], ta[:, :], ALU.mult, ALU.add)
                        P.memset("pool", s[:, 2:3], 0.0)
                        P.act(junk[:, :], od[:, :], AF.Square, accum_out=s[:, 2:3])
                        P.act(s[:, 3:4], s[:, 2:3], AF.Sqrt, bias=S.eps_t[:, :], scale=1.0 / 128)
                        P.recip(s[:, 3:4], s[:, 3:4])
                        P.stt("dve", yt[:, :], od[:, :], s[:, 3:4], gnbc[:, :], ALU.mult, ALU.mult)
                        P.tr(ptb[:, j * 128:(j + 1) * 128], yt[:, :], S.identb)
                    P.copy("act", ydT[:, h, q0:q0 + qn], ptb[:, :qn])
        if S.cfg.get("dump_yd") == l:
            P.dump("ydT", ydT[:, :, :])
        if S.cfg.get("merge", True):
            with P.phase() as st:
                merge_branch(S, l, ydT, S.w_o_c, G_D, ctx_out, st)


def branch_b(S, l, ctx_out):
    P = S.P
    U = S.cf[:, 256:384]
    Lm = S.cf[:, 384:512]
    SU = S.cf[:, 512:640]
    SL = S.cf[:, 640:768]
    with P.phase() as sto:
        ybT = P.sb("ybT", [128, 4, NT], BF16, sto)
        if not ctx_out:
            P.memset("pool", ybT[:, :, 0:NCX], 0.0)
        with P.phase() as stc:
            gnbc = P.sb("gnbcb", [128, 128], F32, stc)
            P.dma("sp", gnbc[:, :], S.gn[l, 1].partition_broadcast(128))
            wg2 = P.sb("wg2", [17, 2, 256], BF16, stc)
            if S.cfg.get("t_wg2", 1):
                P.dma("pool", wg2[:, :, :], S.wg2[l])
            one_t = P.sb("one_t", [128, 1], F32, stc)
            P.memset("dve", one_t[:, :], 1.0)
            mkb = P.sb("mkb", [128, 512], BF16, stc)
            P.copy("dve", mkb[:, :], S.cf[:, 256:768])
            Ub = mkb[:, 0:128]; Lb = mkb[:, 128:256]; SUb = mkb[:, 256:384]; SLb = mkb[:, 384:512]
            for hp in range(2):
                with P.phase() as st:
                    qT = P.sb("bqT", [128, NT], BF16, st)
                    kT = P.sb("bkT", [128, NT], BF16, st)
                    ktok = P.sb("bktok", [128, NTL, 128], BF16, st)
                    vtok = P.sb("bvtok", [128, NTL, 256], BF16, st)
                    srtok = P.sb("bsrtok", [128, NTL, 256], BF16, st)
                    glrT = P.sb("bglrT", [17, 2, NT], BF16, st)
                    Sst = P.sb("bSst", [128, 2, NTL, 128], BF16, st)
                    Sf = [P.sb("bSf", [128, 128], F32, st) for _ in range(2)]
                    if S.cfg.get("t_ms", 1):
                        P.memset("pool", glrT[:, :, :], 1.0)
                    P.memset("dve", Sf[0][:, :], 0.0)
                    P.memset("dve", Sf[1][:, :], 0.0)
                    with P.phase() as stw:
                        wB = P.sb("wBp", [128, KC, 800], BF16, stw)
                        for (dst0, n_, src0) in ((0, 128, B_Q + hp * 128), (128, 128, B_K + hp * 128),
                                                 (256, 256, B_V + hp * 256), (512, 256, B_R + hp * 256),
                                                 (768, 32, B_GL)):
                            P.dma("pool", wB[:, :, dst0:dst0 + n_],
                                  S.w_in[l, :, src0:src0 + n_].rearrange("(kc p) f -> p kc f", p=128))
                        for bi in (range(5) if S.cfg.get("b_proj", 9) >= 1 else []):
                            t0, tn = TB[bi]
                            for dstT, col in ((qT, 0), (kT, 128)):
                                pb = S.pb[4 + (col // 128)]
                                for kc in range(KC):
                                    P.mm(pb[:, :tn], wB[:, kc, col:col + 128], S.hT[:, kc, t0:t0 + tn],
                                         start=(kc == 0), stop=(kc == KC - 1))
                                P.act(dstT[:, t0:t0 + tn], pb[:, :tn], AF.Copy)
                            for d in (range(2) if S.cfg.get("t_glr", 1) else []):
                                pb = S.pb[6 + d]
                                for kc in range(KC):
                                    P.mm(pb[0:16, :tn], wB[:, kc, 768 + d * 16:768 + (d + 1) * 16],
                                         S.hT[:, kc, t0:t0 + tn], start=(kc == 0), stop=(kc == KC - 1))
                                P.copy("dve", glrT[0:16, d, t0:t0 + tn], pb[0:16, :tn])
                        for n in (range(NTL) if S.cfg.get("b_proj", 9) >= 2 else []):
                            pa = S.pb[0 + n % 2]
                            pr = S.pb[2 + n % 2]
                            for kc in range(KC):
                                P.mm(pa[:, 0:384], S.hT[:, kc, n * 128:(n + 1) * 128], wB[:, kc, 128:512],
                                     start=(kc == 0), stop=(kc == KC - 1))
                            for kc in range(KC):
                                P.mm(pr[:, 0:256], S.hT[:, kc, n * 128:(n + 1) * 128], wB[:, kc, 512:768],
                                     start=(kc == 0), stop=(kc == KC - 1))
                            P.copy("dve", ktok[:, n, :], pa[:, 0:128])
                            P.act(vtok[:, n, :], pa[:, 128:384], AF.Copy)
                            P.act(srtok[:, n, :], pr[:, 0:256], AF.Silu)

                    e1b = [P.sb("be1", [128, 128], F32, st) for _ in range(2)]
                    spb = [P.sb("bsp", [128, 128], F32, st) for _ in range(2)]
                    eremb = [P.sb("berem", [128, 128], F32, st) for _ in range(2)]
                    kgb = [P.sb("bkg", [128, 128], BF16, st) for _ in range(2)]
                    sphl = [P.sb("bsphl", [128, 2, 128], BF16, st) for _ in range(2)]
                    glb = [P.sb("bgl", [128, 1], F32, st) for _ in range(4)]
                    cnt = 0

                    def gate_sp(d, n):
                        nonlocal cnt
                        px = S.pb[0 + cnt % 2]
                        e1 = e1b[cnt % 2]; sp = spb[cnt % 2]
                        cnt += 1
                        P.mm(px[:, 0:128], glrT[0:17, d, n * 128:(n + 1) * 128],
                             wg2[0:17, d, hp * 128:(hp + 1) * 128])
                        P.act(e1[:, :], px[:, 0:128], AF.Exp, scale=-1.0)
                        P.act(sp[:, :], e1[:, :], AF.Ln, bias=one_t[:, :])
                        hl = sphl[(cnt - 1) % 2]
                        P.copy("dve", hl[:, 0, :], sp[:, :])
                        P.tt("dve", hl[:, 1, :], sp[:, :], hl[:, 0, :], ALU.subtract)
                        return hl

                    if S.cfg.get("b_stop", 9) < 1:
                        continue
                    order = [list(range(NTL)), [1, 0] + list(range(NTL - 1, 1, -1))]
                    for s in range(NTL):
                        for d in range(2):
                            n = order[d][s]
                            sp = gate_sp(d, n)
                            k_ = cnt
                            prm = S.pb[2 + d]
                            for z in range(2):
                                P.mm(prm[:, 0:128], SLb if d == 0 else SUb, sp[:, z, :], start=(z == 0), stop=(z == 1))
                            for z in range(2):
                                P.mm(prm[:, 128:129], sp[:, z, :], S.onesb[:, 0:1], start=(z == 0), stop=(z == 1))
                            erem = eremb[d]; kg = kgb[d]; gl = glb[(s * 2 + d) % 4]
                            P.act(erem[:, :], prm[:, 0:128], AF.Exp, scale=-1.0 / 16)
                            P.act(gl[:, :], prm[:, 128:129], AF.Exp, scale=-1.0 / 16)
                            P.tt("dve", kg[:, :], ktok[:, n, :], erem[:, :], ALU.mult)
                            P.copy("act", Sst[:, d, n, :], Sf[d][:, :])
                            if s == NTL - 1:
                                continue
                            pS = S.pb[4 + d]
                            P.mm(pS[:, 0:256], kg[:, :], vtok[:, n, :])
                            for hh in range(2):
                                sl = slice(hh * 64, (hh + 1) * 64)
                                P.stt("dve", Sf[d][sl, :], Sf[d][sl, :], gl[sl, :], pS[sl, hh * 128:(hh + 1) * 128],
                                      ALU.mult, ALU.add)

                    if S.cfg.get("b_stop", 9) < 2:
                        continue
                    egb = [P.sb("beg", [128, 128], F32, st) for _ in range(2)]
                    eib = [P.sb("bei", [128, 128], F32, st) for _ in range(2)]
                    qtb = [P.sb("bqt", [128, 128], BF16, st) for _ in range(4)]
                    ktb = [P.sb("bkt", [128, 128], BF16, st) for _ in range(4)]
                    atb = [P.sb("bat", [128, 2, 128], BF16, st) for _ in range(4)]
                    smb = [P.sb("bsm", [128, 2], F32, st) for _ in range(4)]
                    junk = P.sb("bjunk", [128, 128], BF16, st)
                    y1b = [P.sb("by1", [128, 128], F32, st) for _ in range(2)]
                    ytb = [P.sb("byt", [128, 128], BF16, st) for _ in range(2)]
                    ptb = S.pb[7][:, 0:256].bitcast(BF16)
                    k2 = 0
                    ep = 0
                    for n in (range(NTL) if ctx_out else range(2, NTL)):
                        dd = []
                        for d in range(2):
                            sp = gate_sp(d, n)
                            pg = S.pb[2]
                            for z in range(2):
                                P.mm(pg[:, 0:128], sp[:, z, :], Ub if d == 0 else Lb, start=(z == 0), stop=(z == 1))
                            eg = egb[d]; ei = eib[d]
                            qt = qtb[k2 % 4]; kt = ktb[k2 % 4]; at = atb[k2 % 4]
                            k2 += 1
                            P.act(eg[:, :], pg[:, 0:128], AF.Exp, scale=-1.0 / 16)
                            P.act(ei[:, :], pg[:, 0:128], AF.Exp, scale=1.0 / 16)
                            P.stt("dve", qt[:, :], qT[:, n * 128:(n + 1) * 128], 0.125, eg[:, :], ALU.mult, ALU.mult)
                            P.tt("pool", kt[:, :], kT[:, n * 128:(n + 1) * 128], ei[:, :], ALU.mult)
                            if S.cfg.get("p2", 9) < 1:
                                continue
                            for hh in range(2):
                                sl = slice(hh * 64, (hh + 1) * 64)
                                P.mm(S.pb[3 + hh][:, 0:128], kt[sl, :], qt[sl, :])
                            mask = (U if d == 0 else Lm)
                            for hh in range(2):
                                P.tt("dve", at[:, hh, :], S.pb[3 + hh][:, 0:128], mask, ALU.mult)
                            dd.append((qt, at))
                        if S.cfg.get("p2", 9) < 2:
                            continue
                        for hh in range(2):
                            sl = slice(hh * 64, (hh + 1) * 64)
                            oo = S.pb[5 + hh][:, 0:128]
                            for d in range(2):
                                qt, at = dd[d]
                                P.mm(oo, qt[sl, :], Sst[sl, d, n, :], start=(d == 0), stop=False)
                                P.mm(oo, at[:, hh, :], vtok[:, n, hh * 128:(hh + 1) * 128], start=False, stop=(d == 1))
                        if S.cfg.get("p2", 9) < 3:
                            continue
                        for hh in range(2):
                            oo = S.pb[5 + hh][:, 0:128]
                            s_ = smb[ep % 4]; y1 = y1b[ep % 2]; yt = ytb[ep % 2]
                            ep += 1
                            P.memset("pool", s_[:, 0:1], 0.0)
                            P.act(junk[:, :], oo, AF.Square, accum_out=s_[:, 0:1])
                            P.act(s_[:, 1:2], s_[:, 0:1], AF.Sqrt, bias=S.eps_t[:, :], scale=1.0 / 128)
                            P.recip(s_[:, 1:2], s_[:, 1:2])
                            P.stt("dve", y1[:, :], oo, s_[:, 1:2], gnbc[:, :], ALU.mult, ALU.mult)
                            P.tt("pool", yt[:, :], y1[:, :], srtok[:, n, hh * 128:(hh + 1) * 128], ALU.mult)
                            P.tr(ptb[:, hh * 128:(hh + 1) * 128], yt[:, :], S.identb)
                        P.copy("act", ybT[:, hp * 2:hp * 2 + 2, n * 128:(n + 1) * 128],
                               ptb[:, 0:256].rearrange("p (h t) -> p h t", h=2))
        if S.cfg.get("dump_yb") == l:
            P.dump("ybT", ybT[:, :, :])
        if S.cfg.get("merge", True):
            with P.phase() as st:
                merge_branch(S, l, ybT, S.w_o_b, G_B, ctx_out, st)


def branch_a(S, l, ctx_out):
    P = S.P
    U = S.cf[:, 256:384]
    Lm = S.cf[:, 384:512]
    SU = S.cf[:, 512:640]
    SL = S.cf[:, 640:768]
    with P.phase() as sto:
        yaT = P.sb("yaT", [128, 4, NT], BF16, sto)
        if not ctx_out:
            P.memset("pool", yaT[:, :, 0:NCX], 0.0)
        with P.phase() as stc:
            gnbc = P.sb("gnbca", [128, 128], F32, stc)
            P.dma("sp", gnbc[:, :], S.gn[l, 0].partition_broadcast(128))
            one_t = P.sb("one_ta", [128, 1], F32, stc)
            P.memset("dve", one_t[:, :], 1.0)
            mkb = P.sb("mkba", [128, 4, 128], BF16, stc)
            P.copy("dve", mkb[:, :, :], S.cf[:, 256:768].rearrange("p (m c) -> p m c", m=4))
            ones_col = P.sb("onescol", [128, 1], BF16, stc)
            P.memset("dve", ones_col[:, :], 1.0)
            cum1 = P.sb("cum1", [128, 2, 129], BF16, stc)
            P.memset("dve", cum1[:, :, :], 1.0)
            P.copy("dve", cum1[:, 0, 0:128], U)
            P.copy("dve", cum1[:, 1, 0:128], Lm)
            cumb = [mkb[:, 0, :], mkb[:, 1, :]]
            gmf = [SL, SU]
            strict = [SL, SU]
            maskT = [U, Lm]
            cw = P.sb("convw", [128, 12, 3], F32, stc)
            P.dma("sp", cw[:, :, :], S.convw[l])
            adt = P.sb("adt", [128, 2, 8], F32, stc)
            P.dma("sp", adt[:, :, :], S.adt[l].partition_broadcast(128))
            nA = P.sb("nA", [128, 8], F32, stc)
            P.act(nA[:, :], adt[:, 0, :], AF.Exp)
            P.ts("dve", nA[:, :], nA[:, :], -1.0, None, ALU.mult)
            betat = P.sb("betat", [128, NTL, 8], F32, stc)
            nbetat = P.sb("nbetat", [128, NTL, 8], F32, stc)
            gt = P.sb("gt", [128, NTL, 8], F32, stc)
            with P.phase() as stw:
                wbg = P.sb("wbg", [128, KC, 16], BF16, stw)
                P.dma("pool", wbg[:, :, :], S.w_in[l, :, A_B:A_B + 16].rearrange("(kc p) f -> p kc f", p=128))
                tmpe = P.sb("tmpe", [128, NTL, 8], F32, stw)
                for n in range(NTL):
                    pb = S.pb[n % 2]
                    for kc in range(KC):
                        P.mm(pb[:, 0:16], S.hT[:, kc, n * 128:(n + 1) * 128], wbg[:, kc, :],
                             start=(kc == 0), stop=(kc == KC - 1))
                    P.act(betat[:, n, :], pb[:, 0:8], AF.Sigmoid)
                    P.tt("dve", gt[:, n, :], pb[:, 8:16], adt[:, 1, :], ALU.add)
                P.act(tmpe[:, :, :], gt[:, :, :], AF.Exp)
                P.act(gt[:, :, :], tmpe[:, :, :], AF.Ln, bias=one_t[:, :])
                P.tt("dve", gt[:, :, :], gt[:, :, :], nA[:, :].unsqueeze(1).to_broadcast([128, NTL, 8]), ALU.mult)
                P.ts("dve", nbetat[:, :, :], betat[:, :, :], -1.0, None, ALU.mult)

            for h in range(4):
                with P.phase() as st:
                    qT = P.sb("aqT", [128, NT], BF16, st)
                    kT = P.sb("akT", [128, NT], BF16, st)
                    ktok = P.sb("aktok", [128, NTL, 128], BF16, st)
                    vtok = P.sb("avtok", [128, NTL, 128], BF16, st)
                    sztok = P.sb("asztok", [128, NTL, 128], BF16, st)
                    oacc = P.sb("aoacc", [128, NTL, 128], F32, st)
                    P.memset("pool", oacc[:, :, :], 0.0)
                    ptb = S.pb[7][:, 0:256].bitcast(BF16)
                    with P.phase() as stw:
                        wA = P.sb("wAh", [128, KC, 512], BF16, stw)
                        for i, c0 in enumerate((A_Q, A_K, A_V, A_Z)):
                            P.dma("pool", wA[:, :, i * 128:(i + 1) * 128],
                                  S.w_in[l, :, c0 + h * 128:c0 + (h + 1) * 128].rearrange("(kc p) f -> p kc f", p=128))
                        pre = [P.sb("apre", [128, NT], F32, stw) for _ in range(1)]
                        cv = [P.sb("acv", [128, NT], F32, stw) for _ in range(1)]
                        sqb = P.sb("asq", [128, 512], BF16, stw)
                        rs = P.sb("ars", [128, 512], F32, stw)
                        vT = P.sb("avT", [128, NT], BF16, stw)
                        for i in range(3):
                            pr = pre[0]; c = cv[0]
                            for bi in range(5):
                                t0, tn = TB[bi]
                                pb = S.pb[bi % 2]
                                for kc in range(KC):
                                    P.mm(pb[:, :tn], wA[:, kc, i * 128:(i + 1) * 128], S.hT[:, kc, t0:t0 + tn],
                                         start=(kc == 0), stop=(kc == KC - 1))
                                P.act(pr[:, t0:t0 + tn], pb[:, :tn], AF.Copy)
                            ch = i * 4 + h
                            P.ts("dve", c[:, :], pr[:, :], cw[:, ch, 1:2], None, ALU.mult)
                            for (a, b) in ((0, NCX), (NCX, NT)):
                                P.stt("dve", c[:, a + 1:b], pr[:, a:b - 1], cw[:, ch, 0:1], c[:, a + 1:b], ALU.mult, ALU.add)
                                P.stt("dve", c[:, a:b - 1], pr[:, a + 1:b], cw[:, ch, 2:3], c[:, a:b - 1], ALU.mult, ALU.add)
                            if i == 2:
                                P.act(vT[:, :], c[:, :], AF.Silu)
                            else:
                                dst = qT if i == 0 else kT
                                P.act(c[:, :], c[:, :], AF.Silu)
                                for bi in range(5):
                                    t0, tn = TB[bi]
                                    pb = S.pb[2 + bi % 2]
                                    P.act(sqb[:, :tn], c[:, t0:t0 + tn], AF.Square)
                                    P.mm(pb[:, :tn], S.onesb, sqb[:, :tn])
                                    P.act(rs[:, :tn], pb[:, :tn], AF.Sqrt, bias=S.eps_t[:, :])
                                    P.recip(rs[:, :tn], rs[:, :tn])
                                    if i == 0:
                                        P.stt("dve", dst[:, t0:t0 + tn], c[:, t0:t0 + tn], 128.0 ** -0.5, rs[:, :tn],
                                              ALU.mult, ALU.mult)
                                    else:
                                        P.tt("dve", dst[:, t0:t0 + tn], c[:, t0:t0 + tn], rs[:, :tn], ALU.mult)
                        for n in range(NTL):
                            P.tr(ptb[:, 0:128], kT[:, n * 128:(n + 1) * 128], S.identb)
                            P.tr(ptb[:, 128:256], vT[:, n * 128:(n + 1) * 128], S.identb)
                            P.copy("dve", ktok[:, n, :], ptb[:, 0:128])
                            P.copy("dve", vtok[:, n, :], ptb[:, 128:256])
                            pz = S.pb[n % 2]
                            for kc in range(KC):
                                P.mm(pz[:, 0:128], S.hT[:, kc, n * 128:(n + 1) * 128], wA[:, kc, 384:512],
                                     start=(kc == 0), stop=(kc == KC - 1))
                            P.act(sztok[:, n, :], pz[:, 0:128], AF.Silu)

                    NRG = 2
                    def ring(nm, shape, dt):
                        return [[P.sb(nm, shape, dt, st) for _ in range(NRG)] for _ in range(2)]
                    u_r = ring("au", [128, 128], F32)
                    wT_r = ring("awT", [128, 128], BF16)
                    kg_r = ring("akg", [128, 128], BF16)
                    AT_r = ring("aAT", [128, 128], BF16)
                    sc_r = ring("asc", [128, 8], F32)
                    gmf32 = [P.sb("agmf", [128, 129], F32, st) for _ in range(2)]
                    gmhl = [P.sb("agmhl", [128, 2, 129], BF16, st) for _ in range(2)]
                    E1 = [P.sb("aE1", [128, 129], F32, st) for _ in range(2)]
                    E2 = [P.sb("aE2", [128, 129], F32, st) for _ in range(2)]
                    Dms = [P.sb("aDms", [128, 128], F32, st) for _ in range(2)]
                    DmT = [P.sb("aDmT", [128, 128], F32, st) for _ in range(2)]
                    kkf = P.sb("akk", [128, 128], F32, st)
                    kqf = P.sb("akq", [128, 128], F32, st)
                    Xb = [[P.sb("aX", [128, 128], F32, st) for _ in range(2)] for _ in range(2)]
                    XTb = [[P.sb("aXT", [128, 128], F32, st) for _ in range(2)] for _ in range(2)]
                    PTb = [[P.sb("aPT", [128, 128], F32, st) for _ in range(2)] for _ in range(2)]
                    vbb = [P.sb("avb", [128, 128], BF16, st) for _ in range(2)]
                    TTb = [P.sb("aTTb", [128, 128], BF16, st) for _ in range(2)]
                    kbgb = [P.sb("akbg", [128, 128], BF16, st) for _ in range(2)]
                    vnew = [P.sb("avnew", [128, 128], BF16, st) for _ in range(2)]
                    o1s = [P.sb("ao1s", [128, 128], F32, st) for _ in range(2)]
                    ot = [P.sb("aot", [128, 128], F32, st) for _ in range(2)]
                    Sf = [P.sb("aSf", [128, 128], F32, st) for _ in range(2)]
                    Sb = [P.sb("aSb", [128, 128], BF16, st) for _ in range(2)]
                    identb = S.identb
                    for d in range(2):
                        P.memset("dve", Sf[d][:, :], 0.0)
                        P.memset("dve", Sb[d][:, :], 0.0)
                    order = [list(range(NTL)), [1, 0] + list(range(NTL - 1, 1, -1))]
                    kk_cache = {}

                    def pre_tile(n):
                        tl = slice(n * 128, (n + 1) * 128)
                        pk = S.pb[3]
                        P.mm(pk[:, 0:128], kT[:, tl], kT[:, tl])
                        P.mm(pk[:, 128:256], kT[:, tl], qT[:, tl])
                        kk = P.sb("akkc", [128, 2, 128], F32, st) if False else None
                        return pk

                    def precompute(d, n, slot):
                        col = d * 4 + h
                        g = gt[:, n, col:col + 1]
                        beta = betat[:, n, col:col + 1]
                        nbeta = nbetat[:, n, col:col + 1]
                        tl = slice(n * 128, (n + 1) * 128)
                        sc = sc_r[d][slot]
                        gf = gmf32[d]; ghl = gmhl[d]
                        P.ts("dve", gf[:, 0:128], gmf[d], g, None, ALU.mult)
                        P.copy("dve", gf[:, 128:129], g)
                        P.copy("dve", ghl[:, 0, :], gf[:, :])
                        P.tt("dve", ghl[:, 1, :], gf[:, :], ghl[:, 0, :], ALU.subtract)
                        pD = S.pb[2]
                        for z in range(2):
                            P.mm(pD[:, 0:129], cumb[d], ghl[:, z, :], start=(z == 0), stop=(z == 1))
                        e1 = E1[d]; e2 = E2[d]
                        P.act(e1[:, :], pD[:, 0:129], AF.Exp)
                        for z in range(2):
                            P.mm(pD[:, 256:385], ghl[:, z, 0:128], cum1[:, d, :], start=(z == 0), stop=(z == 1))
                        P.act(e2[:, :], pD[:, 256:385], AF.Exp)
                        P.tt("pool", Dms[d][:, :], e1[:, 0:128], strict[d], ALU.mult)
                        P.tt("pool", DmT[d][:, :], e2[:, 0:128], maskT[d], ALU.mult)
                        P.tt("dve", sc[:, 2:3], e1[:, 128:129], e2[:, 128:129], ALU.mult)
                        P.tt("dve", sc[:, 3:4], e1[:, 128:129], beta, ALU.mult)
                        P.copy("dve", sc[:, 0:1], e1[:, 128:129])
                        pk = S.pb[3]
                        P.mm(pk[:, 0:128], kT[:, tl], kT[:, tl])
                        P.mm(pk[:, 128:256], kT[:, tl], qT[:, tl])
                        X = Xb[d]; XT = XTb[d]; PT = PTb[d]
                        P.stt("dve", X[0][:, :], pk[:, 0:128], nbeta, Dms[d][:, :], ALU.mult, ALU.mult)
                        P.tt("dve", AT_r[d][slot][:, :], pk[:, 128:256], DmT[d][:, :], ALU.mult)
                        pc = S.pb[4 + d]
                        P.tr(pc[:, 384:512], X[0][:, :], S.identf)
                        P.copy("act", XT[0][:, :], pc[:, 384:512])
                        P.tt("dve", PT[0][:, :], pc[:, 384:512], S.identf, ALU.add)
                        cur = 0
                        for lev in range(1, 7):
                            nxt = 1 - cur
                            P.mm(pc[:, 0:128], XT[cur][:, :], X[cur][:, :])
                            if lev < 6:
                                P.mm(pc[:, 128:256], X[cur][:, :], XT[cur][:, :])
                            P.copy("act", X[nxt][:, :], pc[:, 0:128])
                            if lev < 6:
                                P.copy("dve", XT[nxt][:, :], pc[:, 128:256])
                            P.mm(pc[:, 256:384], S.identf, PT[cur][:, :], start=True, stop=False)
                            P.mm(pc[:, 256:384], X[nxt][:, :], PT[cur][:, :], start=False, stop=True)
                            P.copy("dve", PT[nxt][:, :], pc[:, 256:384])
                            cur = nxt
                        TT = TTb[d]
                        P.copy("act", TT[:, :], PT[cur][:, :])
                        vb = vbb[d]; kbg = kbgb[d]
                        P.ts("dve", vb[:, :], vtok[:, n, :], beta, None, ALU.mult)
                        P.ts("dve", kbg[:, :], ktok[:, n, :], sc[:, 3:4], None, ALU.mult)
                        P.ts("dve", kg_r[d][slot][:, :], ktok[:, n, :], e2[:, 128:129], None, ALU.mult)
                        pu = S.pb[6]
                        P.mm(pu[:, 0:128], TT[:, :], vb[:, :])
                        P.mm(pu[:, 128:256], kbg[:, :], TT[:, :])
                        P.copy("act", u_r[d][slot][:, :], pu[:, 0:128])
                        P.copy("act", wT_r[d][slot][:, :], pu[:, 128:256])

                    def recur(d, n, slot, last):
                        tl = slice(n * 128, (n + 1) * 128)
                        pr = S.pb[d]
                        sc = sc_r[d][slot]
                        P.mm(pr[:, 0:128], wT_r[d][slot][:, :], Sb[d][:, :])
                        P.mm(pr[:, 128:256], qT[:, tl], Sb[d][:, :])
                        P.tt("dve", vnew[d][:, :], u_r[d][slot][:, :], pr[:, 0:128], ALU.subtract)
                        want_out = ctx_out or n >= 2
                        if want_out:
                            P.mm(pr[:, 256:384], AT_r[d][slot][:, :], vnew[d][:, :])
                            P.act(o1s[d][:, :], pr[:, 128:256], AF.Copy, scale=sc[:, 0:1])
                            P.tt("dve", ot[d][:, :], pr[:, 256:384], o1s[d][:, :], ALU.add)
                            P.tt("pool", oacc[:, n, :], oacc[:, n, :], ot[d][:, :], ALU.add)
                        if not last:
                            P.mm(pr[:, 384:512], kg_r[d][slot][:, :], vnew[d][:, :])
                            P.stt("dve", Sf[d][:, :], Sf[d][:, :], sc[:, 2:3], pr[:, 384:512], ALU.mult, ALU.add)
                            P.copy("act", Sb[d][:, :], Sf[d][:, :])

                    for s in range(NTL):
                        for d in range(2):
                            precompute(d, order[d][s], s % NRG)
                        for d in range(2):
                            recur(d, order[d][s], s % NRG, s == NTL - 1)

                    smb = [P.sb("asm", [128, 2], F32, st) for _ in range(4)]
                    junk = P.sb("ajunk", [128, 128], BF16, st)
                    y1b = [P.sb("ay1", [128, 128], F32, st) for _ in range(2)]
                    ytb = [P.sb("ayt", [128, 128], BF16, st) for _ in range(2)]
                    ep = 0
                    for n in (range(NTL) if ctx_out else range(2, NTL)):
                        s_ = smb[ep % 4]; y1 = y1b[ep % 2]; yt = ytb[ep % 2]
                        ep += 1
                        P.memset("pool", s_[:, 0:1], 0.0)
                        P.act(junk[:, :], oacc[:, n, :], AF.Square, accum_out=s_[:, 0:1])
                        P.act(s_[:, 1:2], s_[:, 0:1], AF.Sqrt, bias=S.eps_t[:, :], scale=1.0 / 128)
                        P.recip(s_[:, 1:2], s_[:, 1:2])
                        P.stt("dve", y1[:, :], oacc[:, n, :], s_[:, 1:2], gnbc[:, :], ALU.mult, ALU.mult)
                        P.tt("pool", yt[:, :], y1[:, :], sztok[:, n, :], ALU.mult)
                        P.tr(ptb[:, 0:128], yt[:, :], S.identb)
                        P.copy("act", yaT[:, h, n * 128:(n + 1) * 128], ptb[:, 0:128])
        if S.cfg.get("dump_ya") == l:
            P.dump("yaT", yaT[:, :, :])
        if S.cfg.get("merge", True):
            with P.phase() as st:
                merge_branch(S, l, yaT, S.w_o_a, G_A, ctx_out, st)


def phase_ffn(S, l):
    P = S.P
    moe = (l % 2 == 1)
    ctx_out = l < 1
    blocks = list(range(5)) if ctx_out else list(range(1, 5))
    i_ = l // 2
    with P.phase() as sto:
        gate = None
        if moe:
            logit = P.sb("logit", [128, 16, 8], F32, sto)
            gate = P.sb("gate", [128, 16, 8], F32, sto)
        with P.phase() as st:
            router = None
            if moe:
                rw = P.sb("rw", [128, KC, 8], F32, st)
                P.dma("sp", rw[:, :, :], S.router_w[i_].rearrange("(kc p) e -> p kc e", p=128))
                router = (rw, logit)
            modulate(S, l, 1, blocks, st, router=router)
        if moe:
            with P.phase() as st:
                m1 = P.sb("m1", [128, 16], F32, st)
                m2 = P.sb("m2", [128, 16], F32, st)
                eq1 = P.sb("eq1", [128, 16, 8], F32, st)
                eq2 = P.sb("eq2", [128, 16, 8], F32, st)
                l2 = P.sb("l2", [128, 16, 8], F32, st)
                ww = P.sb("ww", [128, 3, 16], F32, st)
                bc = lambda a: a.unsqueeze(2).to_broadcast([128, 16, 8])
                P.reduce("dve", m1[:, :], logit[:, :, :], ALU.max, AX.X)
                P.tt("dve", eq1[:, :, :], logit[:, :, :], bc(m1[:, :]), ALU.is_equal)
                P.stt("dve", l2[:, :, :], eq1[:, :, :], -1e30, logit[:, :, :], ALU.mult, ALU.add)
                P.reduce("dve", m2[:, :], l2[:, :, :], ALU.max, AX.X)
                P.tt("dve", eq2[:, :, :], l2[:, :, :], bc(m2[:, :]), ALU.is_equal)
                P.tt("dve", ww[:, 0, :], m2[:, :], m1[:, :], ALU.subtract)
                P.act(ww[:, 0, :], ww[:, 0, :], AF.Exp)
                P.ts("dve", ww[:, 1, :], ww[:, 0, :], 1.0, None, ALU.add)
                P.recip(ww[:, 1, :], ww[:, 1, :])
                P.tt("dve", ww[:, 2, :], ww[:, 0, :], ww[:, 1, :], ALU.mult)
                P.tt("dve", eq1[:, :, :], eq1[:, :, :], bc(ww[:, 1, :]), ALU.mult)
                P.tt("dve", eq2[:, :, :], eq2[:, :, :], bc(ww[:, 2, :]), ALU.mult)
                P.tt("dve", gate[:, :, :], eq1[:, :, :], eq2[:, :, :], ALU.add)
            if S.cfg.get("dump_gate"):
                P.dump("gate", gate[:, :, :])
        with P.phase() as st:
            GR = 6
            groups = [(0, 6), (6, 6), (12, 6), (18, 4)]
            gbuf = P.sb("gbuf", [128, GR, NL if moe else NT], BF16, st)
            w13 = [P.sb("w13", [128, 2, KC, 384], BF16, st) for _ in range(2)]
            w2b = [P.sb("w2b", [128, GR, 1024], BF16, st) for _ in range(2)]
            sil = [P.sb("sil", [128, 512], F32, st) for _ in range(2)]
            tmb = [P.sb("tmb", [128, 512], F32 if moe else BF16, st) for _ in range(2)]
            gbce = P.sb("gbce", [128, NL], BF16, st) if moe else None
            gdg = [P.sb("gdg", [128, 128], F32, st) for _ in range(2)] if moe else None
            nw13 = 0; nw2 = 0; nt = 0; ng_ = 0
            experts = range(8) if moe else range(1)
            if moe:
                blks = [(bi, TB[bi][0], TB[bi][0] - NCX, TB[bi][1]) for bi in blocks]
            else:
                blks = [(bi, TB[bi][0], TB[bi][0], TB[bi][1]) for bi in blocks]
            for e in experts:
                W1 = S.moe_w1[i_, e] if moe else S.ffn_w1[i_]
                W3 = S.moe_w3[i_, e] if moe else S.ffn_w3[i_]
                W2 = S.moe_w2[i_, e] if moe else S.ffn_w2[i_]
                if moe:
                    for b4 in range(4):
                        pg = S.pb[7]
                        for j in range(4):
                            tile = b4 * 4 + j
                            gd = gdg[tile % 2]
                            P.ts("dve", gd[:, :], S.identf, gate[:, tile, e:e + 1], None, ALU.mult)
                            P.mm(pg[:, j * 128:(j + 1) * 128], S.onesf, gd[:, :])
                        P.copy("act", gbce[:, b4 * 512:(b4 + 1) * 512], pg[:, :])
                for (f0, nf) in groups:
                    w2t = w2b[nw2 % 2]; nw2 += 1
                    P.dma("pool", w2t[:, 0:nf, :],
                          W2[f0 * 128:(f0 + nf) * 128, :].rearrange("(c p) f -> p c f", p=128))
                    for p0 in range(0, nf, 3):
                        npf = min(3, nf - p0)
                        wt = w13[nw13 % 2]; nw13 += 1
                        c0 = (f0 + p0) * 128
                        P.dma("pool", wt[:, 0, :, 0:npf * 128],
                              W1[:, c0:c0 + npf * 128].rearrange("(kc p) f -> p kc f", p=128))
                        P.dma("pool", wt[:, 1, :, 0:npf * 128],
                              W3[:, c0:c0 + npf * 128].rearrange("(kc p) f -> p kc f", p=128))
                        for (bi, x0, g0, tn) in blks:
                            for fi in range(npf):
                                pa = S.pb[0 + nt % 2]; pbb = S.pb[2 + nt % 2]
                                sl_ = sil[nt % 2]; tm = tmb[nt % 2]
                                nt += 1
                                for kc in range(KC):
                                    P.mm(pa[:, :tn], wt[:, 0, kc, fi * 128:(fi + 1) * 128], S.hT[:, kc, x0:x0 + tn],
                                         start=(kc == 0), stop=(kc == KC - 1))
                                for kc in range(KC):
                                    P.mm(pbb[:, :tn], wt[:, 1, kc, fi * 128:(fi + 1) * 128], S.hT[:, kc, x0:x0 + tn],
                                         start=(kc == 0), stop=(kc == KC - 1))
                                P.act(sl_[:, :tn], pa[:, :tn], AF.Silu)
                                gdst = gbuf[:, p0 + fi, g0:g0 + tn]
                                if moe:
                                    P.tt("dve", tm[:, :tn], pbb[:, :tn], sl_[:, :tn], ALU.mult)
                                    P.tt("pool", gdst, tm[:, :tn], gbce[:, g0:g0 + tn], ALU.mult)
                                else:
                                    P.tt("dve", gdst, pbb[:, :tn], sl_[:, :tn], ALU.mult)
                    for fo in range(8):
                        for (bi, x0, g0, tn) in blks:
                            which = 1 if bi == 0 else 0
                            po = S.pb[4 + ng_ % 3]; ng_ += 1
                            for fc in range(nf):
                                P.mm(po[:, :tn], w2t[:, fc, fo * 128:(fo + 1) * 128], gbuf[:, fc, g0:g0 + tn],
                                     start=(fc == 0), stop=(fc == nf - 1))
                            P.stt("dve", S.xT[:, fo, x0:x0 + tn], po[:, :tn], S.modv[:, l, 40 + fo, which:which + 1],
                                  S.xT[:, fo, x0:x0 + tn], ALU.mult, ALU.add)
    if S.cfg.get("dump_xffn") == l:
        P.dump("xT", S.xT[:, :, :])

def extra_shared(shared, inputs, f):
    shared["rope"] = _rope_tables()
    shared["gn"] = f(np.stack([inputs["gn_a"], inputs["gn_b"], inputs["gn_c"]], axis=1))
    shared["lam_c"] = f(inputs["lam_c"])
    cw = np.asarray(inputs["conv_a"], np.float32)
    shared["convw"] = f(cw.reshape(2, 3, 12, 128).transpose(0, 3, 2, 1))
    shared["adt"] = f(np.stack([np.asarray(inputs["a_log"], np.float32).reshape(2, 8),
                                np.asarray(inputs["dt_bias"], np.float32).reshape(2, 8)], axis=1))
    wg2 = np.concatenate([np.asarray(inputs["w_gate2"], np.float32),
                          np.asarray(inputs["b_gate"], np.float32)[:, :, None, :]], axis=2)
    shared["wg2"] = f(wg2.transpose(0, 2, 1, 3))
    for k in ("w_o_a", "w_o_b", "w_o_c", "w_out", "ffn_w1", "ffn_w3", "ffn_w2", "router_w", "moe_w1", "moe_w3", "moe_w2"):
        shared[k] = f(inputs[k])

def _consts():
    c = np.zeros((128, 1024), np.float32)
    c[:, 0:128] = np.eye(128, dtype=np.float32)
    for p in range(128):
        if (p % 32) < 16:
            c[p + 16, 128 + p] = -1.0
        else:
            c[p - 16, 128 + p] = 1.0
    k = np.arange(128)[:, None]
    i = np.arange(128)[None, :]
    c[:, 256:384] = (k <= i)
    c[:, 384:512] = (k >= i)
    c[:, 512:640] = (k < i)
    c[:, 640:768] = (k > i)
    return c


def _rope_tables():
    t = np.arange(2048)
    row = (t // 64).astype(np.float32)
    col = (t % 64).astype(np.float32)
    inv_freq = (np.float32(10000.0) ** (-np.arange(16, dtype=np.float32) / np.float32(16))).astype(np.float32)
    tab = np.zeros((2, 128, 2048), np.float32)
    for p in range(128):
        d = p % 64
        pos = row if d < 32 else col
        ang = (pos * inv_freq[d % 16]).astype(np.float32)
        tab[0, p] = np.cos(ang)
        tab[1, p] = np.sin(ang)
    return tab


def _fm(v):
    v = np.asarray(v)
    lead = v.shape[:-1]
    n = v.shape[-1] // 128
    w = v.reshape(lead + (n, 128))
    return np.ascontiguousarray(np.moveaxis(w, -1, 0))


_CACHE = {}


def _get_prog(cfg_key, cfg):
    if cfg_key not in _CACHE:
        _CACHE[cfg_key] = build(cfg)
    return _CACHE[cfg_key]


def make_in_maps(inputs, cfg):
    f = lambda a: np.ascontiguousarray(np.asarray(a, dtype=np.float32))
    x = f(inputs["x"]); c = f(inputs["c"]); ctx = f(inputs["ctx"]); c_ctx = f(inputs["c_ctx"])
    shared = {}
    shared["w_mod"] = f(inputs["w_mod"])
    shared["b_mod"] = np.ascontiguousarray(f(inputs["b_mod"]).reshape(2, 48, 128).transpose(0, 2, 1))
    ng = np.stack([_fm(inputs["norm1_g"][0]), _fm(inputs["norm1_g"][1]), _fm(inputs["norm2_g"][0]),
                   _fm(inputs["norm2_g"][1]), _fm(inputs["final_g"])], axis=1)
    shared["norm_g"] = f(ng)
    shared["w_in"] = f(inputs["w_in"])
    shared["consts"] = _consts()
    extra_shared(shared, inputs, f)
    maps = []
    for b in range(8):
        m = dict(shared)
        m["x"] = x[b]
        m["ctx"] = ctx[b]
        m["cvec"] = f(np.stack([_fm(c[b]), _fm(c_ctx)], axis=-1))
        maps.append(m)
    return maps


def run(inputs, cfg, trace=False):
    P = _get_prog(repr(sorted(cfg.items())), cfg)
    maps = make_in_maps(inputs, cfg)
    names = set()
    for ins in P.nc.main_func.allocations if False else []:
        pass
    n = cfg.get("ncores", 8)
    res = run_bass_kernel_spmd(P.nc, maps[:n], core_ids=list(range(n)), trace=trace)
    return P, res


def kernel(**inputs):
    cfg = dict(layers=2)
    P, res = run(inputs, cfg)
    out = np.stack([np.asarray(res.results[b]["out"], dtype=np.float32) for b in range(8)], axis=0)
    return out
```

```python
import contextlib
import numpy as np
import concourse.bass as bass
import concourse.mybir as mybir
from concourse.bass_utils import run_bass_kernel_spmd

F32 = mybir.dt.float32
BF16 = mybir.dt.bfloat16
AF = mybir.ActivationFunctionType
ALU = mybir.AluOpType
AX = mybir.AxisListType


class _Op:
    __slots__ = ("idx", "eng", "fn", "deps", "dma", "sem", "semval", "needs_inc", "prev_dma")


class _Unit:
    __slots__ = ("w", "r", "rd")

    def __init__(self):
        self.w = None
        self.r = {}
        self.rd = []


class V:
    __slots__ = ("ap", "key")

    def __init__(self, ap, key):
        self.ap = ap
        self.key = key


def _apk(x):
    if isinstance(x, V):
        return x.ap, x.key
    return x, None


class Prog:
    NDMA = 8

    def __init__(self):
        self.nc = bass.Bass("TRN2", target_bir_lowering=False)
        self.ops = []
        self.names = {}
        self.base_deps = []
        self.last = {}
        self.dma_since_barrier = []
        self.stack = contextlib.ExitStack()
        self.n_dma = {"sp": 0, "act": 0, "pool": 0}
        self.dma_last = {}
        self.dbg = []
        self._uid = 0
        self.psum_names = set()

    def uid(self, base):
        self._uid += 1
        return f"{base}_{self._uid}"

    def sb(self, name, shape, dtype, stack=None):
        st = stack if stack is not None else self.stack
        return st.enter_context(self.nc.sbuf_tensor(self.uid(name), list(shape), dtype))

    def ps(self, name, shape, dtype=F32, stack=None):
        st = stack if stack is not None else self.stack
        t = st.enter_context(self.nc.psum_tensor(self.uid(name), list(shape), dtype))
        self.psum_names.add(t[:].name)
        return t

    def dram(self, name, shape, dtype, kind="Internal"):
        return self.nc.dram_tensor(name, list(shape), dtype, kind=kind)

    @contextlib.contextmanager
    def phase(self):
        st = contextlib.ExitStack()
        try:
            yield st
        finally:
            self.barrier()
            st.close()

    def _conf(self, name, sub):
        d = self.names.setdefault(name, {})
        if sub is None:
            if None not in d:
                d[None] = _Unit()
            return d[None], list(d.values())
        if sub not in d:
            d[sub] = _Unit()
        res = [d[sub]]
        if None in d:
            res.append(d[None])
        return d[sub], res

    def add(self, eng, fn, reads, writes, dma=False):
        op = _Op()
        op.idx = len(self.ops)
        op.eng = eng
        op.fn = fn
        op.dma = dma
        op.sem = None
        op.semval = 0
        op.needs_inc = False
        op.prev_dma = None
        deps = {}
        for b in self.base_deps:
            deps[b.idx] = b
        for x in reads:
            if x is None or isinstance(x, (int, float)):
                continue
            ap, key = _apk(x)
            prim, conf = self._conf(ap.name, key)
            for u in conf:
                if u.w is not None:
                    deps[u.w.idx] = u.w
                if ap.name in self.psum_names:
                    for re_, r in u.r.items():
                        if re_ != eng:
                            deps[r.idx] = r
            if dma:
                prim.rd.append(op)
            else:
                prim.r[eng] = op
        for x in writes:
            ap, key = _apk(x)
            prim, conf = self._conf(ap.name, key)
            for u in conf:
                if u.w is not None:
                    deps[u.w.idx] = u.w
                for r in u.r.values():
                    deps[r.idx] = r
                for r in u.rd:
                    deps[r.idx] = r
            prim.w = op
            prim.r = {}
            prim.rd = []
        deps.pop(op.idx, None)
        if dma:
            slot = self.n_dma[eng] % self.NDMA
            self.n_dma[eng] += 1
            prev = self.dma_last.get((eng, slot))
            op.prev_dma = prev
            op.sem = (eng, slot)
            op.semval = (prev.semval if prev is not None else 0) + 16
            self.dma_last[(eng, slot)] = op
            self.dma_since_barrier.append(op)
        op.deps = list(deps.values())
        self.ops.append(op)
        self.last[eng] = op
        return op

    def barrier(self):
        b = list(self.last.values()) + list(self.dma_since_barrier)
        self.base_deps = b
        self.dma_since_barrier = []

    def mm(self, out, lhsT, rhs, start=True, stop=True, **kw):
        o, _ = _apk(out); l, _ = _apk(lhsT); r, _ = _apk(rhs)
        nc = self.nc
        return self.add("pe", lambda: nc.tensor.matmul(o, l, r, start=start, stop=stop, **kw),
                        [lhsT, rhs], [out])

    def tr(self, out, in_, ident):
        o, _ = _apk(out); i, _ = _apk(in_); d, _ = _apk(ident)
        nc = self.nc
        return self.add("pe", lambda: nc.tensor.transpose(o, i, d), [in_, ident], [out])

    def act(self, out, in_, func, bias=None, scale=None, accum_out=None):
        o, _ = _apk(out); i, _ = _apk(in_)
        kw = {}
        rd = [in_]
        wr = [out]
        if bias is not None:
            kw["bias"] = _apk(bias)[0] if not isinstance(bias, (int, float)) else bias
            rd.append(bias)
        if scale is not None:
            kw["scale"] = _apk(scale)[0] if not isinstance(scale, (int, float)) else scale
            rd.append(scale)
        if accum_out is not None:
            kw["accum_out"] = _apk(accum_out)[0]
            wr.append(accum_out)
        nc = self.nc
        return self.add("act", lambda: nc.scalar.activation(out=o, in_=i, func=func, **kw), rd, wr)

    def _ve(self, eng):
        return self.nc.vector if eng == "dve" else self.nc.gpsimd

    def tt(self, eng, out, in0, in1, op):
        o, _ = _apk(out); a, _ = _apk(in0); b, _ = _apk(in1)
        e = self._ve(eng)
        return self.add(eng, lambda: e.tensor_tensor(out=o, in0=a, in1=b, op=op), [in0, in1], [out])

    def ts(self, eng, out, in0, s1, s2=None, op0=ALU.mult, op1=None, accum_out=None):
        o, _ = _apk(out); a, _ = _apk(in0)
        e = self._ve(eng)
        s1v = s1 if isinstance(s1, (int, float)) else _apk(s1)[0]
        s2v = s2 if (s2 is None or isinstance(s2, (int, float))) else _apk(s2)[0]
        kw = {}
        wr = [out]
        if op1 is not None:
            kw["op1"] = op1
        if accum_out is not None:
            kw["accum_out"] = _apk(accum_out)[0]
            wr.append(accum_out)
        return self.add(eng, lambda: e.tensor_scalar(out=o, in0=a, scalar1=s1v, scalar2=s2v, op0=op0, **kw),
                        [in0, s1, s2], wr)

    def stt(self, eng, out, in0, scalar, in1, op0, op1):
        o, _ = _apk(out); a, _ = _apk(in0); b, _ = _apk(in1)
        e = self._ve(eng)
        sv = scalar if isinstance(scalar, (int, float)) else _apk(scalar)[0]
        return self.add(eng, lambda: e.scalar_tensor_tensor(out=o, in0=a, scalar=sv, in1=b, op0=op0, op1=op1),
                        [in0, scalar, in1], [out])

    def copy(self, eng, out, in_):
        o, _ = _apk(out); i, _ = _apk(in_)
        nc = self.nc
        if eng == "act":
            return self.add("act", lambda: nc.scalar.copy(out=o, in_=i), [in_], [out])
        e = self._ve(eng)
        return self.add(eng, lambda: e.tensor_copy(out=o, in_=i), [in_], [out])

    def memset(self, eng, out, val):
        o, _ = _apk(out)
        e = self._ve(eng)
        return self.add(eng, lambda: e.memset(o, val), [], [out])

    def reduce(self, eng, out, in_, op, axis=AX.X):
        o, _ = _apk(out); i, _ = _apk(in_)
        e = self._ve(eng)
        return self.add(eng, lambda: e.tensor_reduce(out=o, in_=i, axis=axis, op=op), [in_], [out])

    def recip(self, out, in_):
        o, _ = _apk(out); i, _ = _apk(in_)
        nc = self.nc
        return self.add("dve", lambda: nc.vector.reciprocal(out=o, in_=i), [in_], [out])

    def dma(self, q, out, in_):
        o, _ = _apk(out); i, _ = _apk(in_)
        e = {"sp": self.nc.sync, "act": self.nc.scalar, "pool": self.nc.gpsimd}[q]
        return self.add(q, lambda: e.dma_start(out=o, in_=i), [in_], [out], dma=True)

    def dump(self, name, ap, dtype=None):
        a, _ = _apk(ap)
        t = self.nc.dram_tensor("dbg_" + name, list(a.shape), dtype or a.dtype, kind="ExternalOutput")
        self.dbg.append("dbg_" + name)
        self.dma("sp", t.ap(), ap)

    def emit(self):
        nc = self.nc
        engobj = {"pe": nc.tensor, "act": nc.scalar, "dve": nc.vector, "pool": nc.gpsimd, "sp": nc.sync}
        st = self.stack
        esem = {e: st.enter_context(nc.semaphore("s_" + e)) for e in ("pe", "act", "dve", "pool")}
        dsem = {}
        for q in ("sp", "act", "pool"):
            for s in range(self.NDMA):
                if (q, s) in self.dma_last:
                    dsem[(q, s)] = st.enter_context(nc.semaphore(f"d_{q}{s}"))
        for op in self.ops:
            for p in op.deps:
                if not p.dma and not (p.eng == "pe" and op.eng == "pe"):
                    p.needs_inc = True
        finals = list(self.last.values())
        for p in finals:
            if not p.dma:
                p.needs_inc = True
        cnt = {e: 0 for e in esem}
        for op in self.ops:
            if not op.dma and op.needs_inc:
                cnt[op.eng] += 1
                op.semval = cnt[op.eng]
        waited = {e: {} for e in engobj}
        nwait = 0
        for op in self.ops:
            e = engobj[op.eng]
            need = {}
            for p in op.deps:
                if p.dma:
                    k = ("d", p.sem)
                    v = p.semval
                else:
                    if p.eng == "pe" and op.eng == "pe":
                        continue
                    k = ("e", p.eng)
                    v = p.semval
                if need.get(k, 0) < v:
                    need[k] = v
            if op.dma and op.prev_dma is not None:
                k = ("d", op.sem)
                if need.get(k, 0) < op.prev_dma.semval:
                    need[k] = op.prev_dma.semval
            w = waited[op.eng]
            for k, v in need.items():
                if w.get(k, 0) >= v:
                    continue
                w[k] = v
                sem = dsem[k[1]] if k[0] == "d" else esem[k[1]]
                e.wait_ge(sem, v)
                nwait += 1
            ins = op.fn()
            if op.dma:
                ins.then_inc(dsem[op.sem], 16)
            elif op.needs_inc:
                ins.then_inc(esem[op.eng], 1)
        sp = nc.sync
        for en, sem in esem.items():
            if cnt[en] > 0:
                sp.wait_ge(sem, cnt[en])
        for k, p in self.dma_last.items():
            sp.wait_ge(dsem[k], p.semval)
        self.stats = dict(n_ops=len(self.ops), n_wait=nwait, incs=dict(cnt))
        return nc

D = 1024
KC = 8
NL = 2048
NCX = 256
NT = 2304
NTL = 18
TB = [(0, 256), (256, 512), (768, 512), (1280, 512), (1792, 512)]
DFF = 2816
FC = 22
EPS = 1e-6
A_Q, A_K, A_V, A_Z, A_B, A_G = 0, 512, 1024, 1536, 2048, 2056
B_Q, B_K, B_V, B_R, B_GL = 2064, 2320, 2576, 3088, 3600
C_Q, C_K, C_V = 3632, 4144, 4656
G_A, G_B, G_D = 5168, 6192, 7216


class Ctx:
    pass


def build(cfg):
    P = Prog()
    nc = P.nc
    S = Ctx()
    S.P = P
    S.cfg = cfg
    L = cfg.get("layers", 2)

    def din(name, shape, dt=F32):
        return nc.dram_tensor(name, list(shape), dt, kind="ExternalInput").ap()

    S.x = din("x", [NL, D])
    S.ctx = din("ctx", [NCX, D])
    S.cvec = din("cvec", [128, KC, 2])
    S.w_mod = din("w_mod", [2, D, 6 * D])
    S.b_mod = din("b_mod", [2, 128, 48])
    S.norm_g = din("norm_g", [128, 5, KC])
    S.w_in = din("w_in", [2, D, 8240])
    S.consts = din("consts", [128, 1024])
    S.rope = din("rope", [2, 128, NL])
    S.gn = din("gn", [2, 3, 128])
    S.lam_c = din("lam_c", [2, 4, 64])
    S.wg2 = din("wg2", [2, 17, 2, 256])
    S.convw = din("convw", [2, 128, 12, 3])
    S.adt = din("adt", [2, 2, 8])
    S.w_o_a = din("w_o_a", [2, 512, D])
    S.w_o_b = din("w_o_b", [2, 512, D])
    S.w_o_c = din("w_o_c", [2, 512, D])
    S.w_out = din("w_out", [2, D, D])
    S.ffn_w1 = din("ffn_w1", [1, D, DFF])
    S.ffn_w3 = din("ffn_w3", [1, D, DFF])
    S.ffn_w2 = din("ffn_w2", [1, DFF, D])
    S.router_w = din("router_w", [1, D, 8])
    S.moe_w1 = din("moe_w1", [1, 8, D, DFF])
    S.moe_w3 = din("moe_w3", [1, 8, D, DFF])
    S.moe_w2 = din("moe_w2", [1, 8, DFF, D])
    S.out = nc.dram_tensor("out", [NL, D], F32, kind="ExternalOutput").ap()

    st = P.stack
    S.xT = P.sb("xT", [128, KC, NT], F32)
    S.hT = P.sb("hT", [128, KC, NT], BF16)
    S.cf = P.sb("cf", [128, 1024], F32)
    S.identf = S.cf[:, 0:128]
    S.identb_t = P.sb("identb", [128, 128], BF16)
    S.identb = S.identb_t[:, :]
    S.onesb_t = P.sb("onesb", [128, 128], BF16)
    S.onesb = S.onesb_t[:, :]
    S.onesf_t = P.sb("onesf", [128, 128], F32)
    S.onesf = S.onesf_t[:, :]
    S.modv = P.sb("modv", [128, 2, 48, 2], F32)
    S.ng = P.sb("ng", [128, 5, KC], F32)
    S.gs = P.sb("gs", [128, 4, KC, 2], F32)
    S.pb = [P.ps(f"pb{i}", [128, 512], F32) for i in range(8)]

    P.dma("sp", S.cf[:, :], S.consts)
    P.dma("sp", S.ng[:, :, :], S.norm_g)
    P.copy("dve", S.identb, S.identf)
    P.memset("dve", S.onesb, 1.0)
    P.memset("dve", S.onesf, 1.0)
    S.eps_t = P.sb("eps_t", [128, 1], F32)
    P.memset("dve", S.eps_t[:, :], EPS)
    S.zero_t = P.sb("zero_t", [128, 1], F32)
    P.memset("dve", S.zero_t[:, :], 0.0)

    phase_load(S)
    phase_mod(S, L)
    for l in range(L):
        if cfg.get("mixer", True):
            phase_mixer(S, l)
        if cfg.get("ffn", True):
            phase_ffn(S, l)
    phase_final(S)
    P.emit()
    return P


def phase_load(S):
    P = S.P
    with P.phase() as st:
        tin = [P.sb("ld_in", [128, D], F32, st) for _ in range(3)]
        for tt in range(NTL):
            src = S.ctx[tt * 128:(tt + 1) * 128, :] if tt < 2 else S.x[(tt - 2) * 128:(tt - 1) * 128, :]
            ti = tin[tt % 3]
            P.dma("sp", ti[:, :], src)
            for half in range(2):
                pb = S.pb[(tt * 2 + half) % 4]
                for j in range(4):
                    kc = half * 4 + j
                    P.tr(pb[:, j * 128:(j + 1) * 128], ti[:, kc * 128:(kc + 1) * 128], S.identf)
                dst = S.xT[:, half * 4:(half + 1) * 4, tt * 128:(tt + 1) * 128]
                srcp = pb[:, :].rearrange("p (j t) -> p j t", j=4)
                if half == 0:
                    P.copy("dve", dst, srcp)
                else:
                    P.copy("act", dst, srcp)


def phase_mod(S, L):
    P = S.P
    with P.phase() as st:
        cv = P.sb("cv", [128, KC, 2], F32, st)
        cvb = P.sb("cvb", [128, KC, 2], BF16, st)
        bm = P.sb("bm", [128, 2, 48], F32, st)
        wm = [P.sb("wm", [128, KC, 1024], BF16, st) for _ in range(2)]
        P.dma("sp", cv[:, :, :], S.cvec)
        P.dma("sp", bm[:, :, :], S.b_mod.rearrange("l p f -> p l f"))
        P.act(cvb[:, :, :], cv[:, :, :], AF.Silu)
        n = 0
        for l in range(L):
            for i in range(6):
                w = wm[n % 2]
                n += 1
                P.dma("pool", w[:, :, :],
                      S.w_mod[l, :, i * 1024:(i + 1) * 1024].rearrange("(kc p) f -> p kc f", p=128))
                pb = S.pb[4 + (n % 2)]
                for fc in range(8):
                    for kc in range(KC):
                        P.mm(pb[:, fc * 2:fc * 2 + 2], w[:, kc, fc * 128:(fc + 1) * 128], cvb[:, kc, :],
                             start=(kc == 0), stop=(kc == KC - 1))
                P.tt("dve", S.modv[:, l, i * 8:(i + 1) * 8, :],
                     pb[:, 0:16].rearrange("p (f w) -> p f w", w=2),
                     bm[:, l, i * 8:(i + 1) * 8].unsqueeze(2).to_broadcast([128, 8, 2]), ALU.add)
            for wn, (gi, si) in enumerate(((l, 1), (2 + l, 4))):
                dst = S.gs[:, l * 2 + wn, :, :]
                P.ts("dve", dst, S.modv[:, l, si * 8:(si + 1) * 8, :], 1.0, None, ALU.add)
                P.tt("dve", dst, dst, S.ng[:, gi, :].unsqueeze(2).to_broadcast([128, 8, 2]), ALU.mult)


def modulate(S, l, wn, blocks, st, router=None):
    P = S.P
    sq = [P.sb("sq", [128, KC, 512], BF16, st) for _ in range(2)]
    rstd = [P.sb("rstd", [128, 512], F32, st) for _ in range(2)]
    tmp = [P.sb("mtmp", [128, 512], F32, st) for _ in range(3)]
    h32 = [P.sb("mh32", [128, 512], F32, st) for _ in range(2)] if router is not None else None
    si = 0 if wn == 0 else 3
    n = 0
    for bi in blocks:
        t0, tn = TB[bi]
        which = 1 if bi == 0 else 0
        s = sq[bi % 2]
        pb = S.pb[6 + bi % 2]
        for kc in range(KC):
            P.act(s[:, kc, :tn], S.xT[:, kc, t0:t0 + tn], AF.Square)
        for kc in range(KC):
            P.mm(pb[:, :tn], S.onesb, s[:, kc, :tn], start=(kc == 0), stop=(kc == KC - 1))
        r = rstd[bi % 2]
        P.act(r[:, :tn], pb[:, :tn], AF.Sqrt, bias=S.eps_t[:, :], scale=1.0 / D)
        P.recip(r[:, :tn], r[:, :tn])
        for kc in range(KC):
            t = tmp[n % 3]
            n += 1
            P.stt("dve", t[:, :tn], S.xT[:, kc, t0:t0 + tn], S.gs[:, l * 2 + wn, kc, which:which + 1],
                  r[:, :tn], ALU.mult, ALU.mult)
            P.act(S.hT[:, kc, t0:t0 + tn], t[:, :tn], AF.Identity,
                  bias=S.modv[:, l, si * 8 + kc, which:which + 1])
            if router is not None:
                rw, logit = router
                hh = h32[kc % 2]
                P.ts("dve", hh[:, :tn], t[:, :tn], S.modv[:, l, si * 8 + kc, which:which + 1], None, ALU.add)
                pl = S.pb[5]
                for j in range(tn // 128):
                    P.mm(pl[:, j * 8:(j + 1) * 8], hh[:, j * 128:(j + 1) * 128], rw[:, kc, :],
                         start=(kc == 0 and j == 0), stop=(kc == KC - 1), skip_group_check=True)
        if router is not None:
            rw, logit = router
            tile0 = (t0 - NCX) // 128
            P.copy("dve", logit[:, tile0:tile0 + tn // 128, :],
                   S.pb[5][:, 0:(tn // 128) * 8].rearrange("p (j e) -> p j e", e=8))


def phase_final(S):
    P = S.P
    with P.phase() as st:
        sq = [P.sb("fsq", [128, KC, 512], BF16, st) for _ in range(2)]
        rstd = [P.sb("frstd", [128, 512], F32, st) for _ in range(2)]
        yT = [P.sb("fyT", [128, KC, 512], F32, st) for _ in range(2)]
        ot = [P.sb("fot", [128, D], F32, st) for _ in range(3)]
        g32 = S.ng[:, 4, :]
        n = 0
        for bi in range(1, 5):
            t0, tn = TB[bi]
            s = sq[bi % 2]
            pb = S.pb[6 + bi % 2]
            for kc in range(KC):
                P.act(s[:, kc, :], S.xT[:, kc, t0:t0 + tn], AF.Square)
            for kc in range(KC):
                P.mm(pb[:, :], S.onesb, s[:, kc, :], start=(kc == 0), stop=(kc == KC - 1))
            r = rstd[bi % 2]
            P.act(r[:, :], pb[:, :], AF.Sqrt, bias=S.eps_t[:, :], scale=1.0 / D)
            P.recip(r[:, :], r[:, :])
            y = yT[bi % 2]
            for kc in range(KC):
                P.stt("dve", y[:, kc, :], S.xT[:, kc, t0:t0 + tn], g32[:, kc:kc + 1],
                      r[:, :], ALU.mult, ALU.mult)
            for q in range(4):
                o = ot[n % 3]
                n += 1
                for half in range(2):
                    pt = S.pb[(n * 2 + half) % 4]
                    for j in range(4):
                        kc = half * 4 + j
                        P.tr(pt[:, j * 128:(j + 1) * 128], y[:, kc, q * 128:(q + 1) * 128], S.identf)
                    if half == 0:
                        P.copy("dve", o[:, 0:512], pt[:, :])
                    else:
                        P.copy("act", o[:, 512:1024], pt[:, :])
                r0 = t0 - NCX + q * 128
                P.dma("sp", S.out[r0:r0 + 128, :], o[:, :])

import math


def phase_mixer(S, l):
    P = S.P
    ctx_out = l < 1
    with P.phase() as st:
        modulate(S, l, 0, range(5), st)
    if S.cfg.get("dump_h") == l:
        P.dump("hT", S.hT[:, :, :])
    br = S.cfg.get("branches", "abc")
    if "c" in br:
        branch_c(S, l, ctx_out)
    if "b" in br:
        branch_b(S, l, ctx_out)
    if "a" in br:
        branch_a(S, l, ctx_out)
    if S.cfg.get("dump_xmix") == l:
        P.dump("xT", S.xT[:, :, :])


def merge_branch(S, l, yT, wo_dram, gcol, ctx_out, st):
    P = S.P
    wg = P.sb("wg", [128, KC, 1024], BF16, st)
    wo = P.sb("wo", [128, 4, 1024], BF16, st)
    wout = P.sb("wout", [128, KC, 1024], BF16, st)
    P.dma("pool", wg[:, :, :], S.w_in[l, :, gcol:gcol + 1024].rearrange("(kc p) f -> p kc f", p=128))
    P.dma("pool", wo[:, :, :], wo_dram[l].rearrange("(c p) f -> p c f", p=128))
    P.dma("pool", wout[:, :, :], S.w_out[l].rearrange("(kc p) f -> p kc f", p=128))
    gmb = [P.sb("gm", [128, KC, 512], BF16, st) for _ in range(2)]
    sgb = [P.sb("sg", [128, 512], F32, st) for _ in range(2)]
    for bi in (range(5) if ctx_out else range(1, 5)):
        t0, tn = TB[bi]
        which = 1 if bi == 0 else 0
        gm = gmb[bi % 2]
        for fc in range(8):
            pg = S.pb[0 + fc % 2]
            py = S.pb[2 + fc % 2]
            for kc in range(KC):
                P.mm(pg[:, :tn], wg[:, kc, fc * 128:(fc + 1) * 128], S.hT[:, kc, t0:t0 + tn],
                     start=(kc == 0), stop=(kc == KC - 1))
            for c in range(4):
                P.mm(py[:, :tn], wo[:, c, fc * 128:(fc + 1) * 128], yT[:, c, t0:t0 + tn],
                     start=(c == 0), stop=(c == 3))
            sg = sgb[fc % 2]
            P.act(sg[:, :tn], pg[:, :tn], AF.Sigmoid)
            P.tt("dve", gm[:, fc, :tn], py[:, :tn], sg[:, :tn], ALU.mult)
        for fo in range(8):
            po = S.pb[4 + fo % 2]
            for fc in range(8):
                P.mm(po[:, :tn], wout[:, fc, fo * 128:(fo + 1) * 128], gm[:, fc, :tn],
                     start=(fc == 0), stop=(fc == 7))
            P.stt("dve", S.xT[:, fo, t0:t0 + tn], po[:, :tn], S.modv[:, l, 16 + fo, which:which + 1],
                  S.xT[:, fo, t0:t0 + tn], ALU.mult, ALU.add)


def branch_c(S, l, ctx_out):
    P = S.P
    lam_init = 0.8 - 0.6 * math.exp(-0.3 * l)
    with P.phase() as sto:
        ydT = P.sb("ydT", [128, 4, NT], BF16, sto)
        if not ctx_out:
            P.memset("pool", ydT[:, :, 0:NCX], 0.0)
        with P.phase() as st:
            ropet = P.sb("ropet", [128, 2, NL], BF16, st)
            P.dma("pool", ropet[:, :, :], S.rope.rearrange("c p t -> p c t"))
            rotT = P.sb("rotT", [128, 128], BF16, st)
            P.copy("dve", rotT[:, :], S.cf[:, 128:256])
            gnbc = P.sb("gnbc", [128, 128], F32, st)
            P.dma("sp", gnbc[:, :], S.gn[l, 2].partition_broadcast(128))
            P.ts("dve", gnbc[:, :], gnbc[:, :], 1.0 - lam_init, None, ALU.mult)
            lam = P.sb("lam", [128, 4, 64], F32, st)
            P.dma("sp", lam[:, :, :], S.lam_c[l].partition_broadcast(128))
            lp = P.sb("lp", [128, 2, 64], F32, st)
            P.tt("dve", lp[:, 0, :], lam[:, 0, :], lam[:, 1, :], ALU.mult)
            P.tt("dve", lp[:, 1, :], lam[:, 2, :], lam[:, 3, :], ALU.mult)
            ls = P.sb("ls", [128, 2], F32, st)
            P.reduce("dve", ls[:, :], lp[:, :, :], ALU.add, AX.X)
            le = P.sb("le", [128, 2], F32, st)
            P.act(le[:, :], ls[:, :], AF.Exp)
            neglam = P.sb("neglam", [128, 1], F32, st)
            P.tt("dve", neglam[:, :], le[:, 1:2], le[:, 0:1], ALU.subtract)
            P.ts("dve", neglam[:, :], neglam[:, :], -lam_init, None, ALU.add)

            wC = P.sb("wC", [128, KC, 1536], BF16, st)
            P.dma("pool", wC[:, :, :], S.w_in[l, :, C_Q:C_Q + 1536].rearrange("(kc p) f -> p kc f", p=128))
            qTh = P.sb("qTh", [128, NT], BF16, st)
            kTh = P.sb("kTh", [128, NT], BF16, st)
            vaug = P.sb("vaug", [128, NTL, 130], BF16, st)
            P.memset("pool", vaug[:, :, 128:130], 1.0)
            xqb = [P.sb("xq", [128, 512], BF16, st) for _ in range(2)]
            t1b = [P.sb("t1", [128, 512], F32, st) for _ in range(2)]
            t2b = [P.sb("t2", [128, 512], F32, st) for _ in range(2)]
            NR = 6
            pring = [P.sb("pT", [128, 512], BF16, st) for _ in range(NR)]
            sm = [P.sb("sm", [128, 4], F32, st) for _ in range(4)]
            tab = [P.sb("ta", [128, 128], F32, st) for _ in range(2)]
            odb = [P.sb("od", [128, 128], F32, st) for _ in range(2)]
            junk = P.sb("junk", [128, 128], BF16, st)
            ytb = [P.sb("yt", [128, 128], BF16, st) for _ in range(2)]
            ptb = S.pb[7][:, 0:256].bitcast(BF16)
            cnt = 0
            ring = 0
            ep = 0
            for h in range(4):
                for dstT, col in ((qTh, h * 128), (kTh, 512 + h * 128)):
                    for bi in range(5):
                        t0, tn = TB[bi]
                        pb = S.pb[6]
                        for kc in range(KC):
                            P.mm(pb[:, :tn], wC[:, kc, col:col + 128], S.hT[:, kc, t0:t0 + tn],
                                 start=(kc == 0), stop=(kc == KC - 1))
                        if bi == 0:
                            P.act(dstT[:, 0:tn], pb[:, :tn], AF.Copy)
                        else:
                            xq = xqb[cnt % 2]; t1 = t1b[cnt % 2]; t2 = t2b[cnt % 2]
                            cnt += 1
                            P.act(xq[:, :], pb[:, :], AF.Copy)
                            pr = S.pb[7]
                            P.mm(pr[:, :], rotT[:, :], xq[:, :])
                            lt0 = t0 - NCX
                            P.tt("dve", t1[:, :], pr[:, :], ropet[:, 1, lt0:lt0 + 512], ALU.mult)
                            P.tt("pool", t2[:, :], xq[:, :], ropet[:, 0, lt0:lt0 + 512], ALU.mult)
                            P.tt("dve", dstT[:, t0:t0 + 512], t1[:, :], t2[:, :], ALU.add)
                vc = 1024 + h * 128
                for g0 in range(0, NTL, 4):
                    ng_ = min(4, NTL - g0)
                    pb = S.pb[6]
                    for j in range(ng_):
                        tt = g0 + j
                        for kc in range(KC):
                            P.mm(pb[:, j * 128:(j + 1) * 128], S.hT[:, kc, tt * 128:(tt + 1) * 128],
                                 wC[:, kc, vc:vc + 128], start=(kc == 0), stop=(kc == KC - 1))
                    P.act(vaug[:, g0:g0 + ng_, 0:128],
                          pb[:, 0:ng_ * 128].rearrange("p (j t) -> p j t", j=ng_), AF.Copy)
                for qb in ([0] if ctx_out else []) + [1, 2, 3, 4]:
                    q0, qn = TB[qb]
                    nq = qn // 128
                    seq = list(range(2)) if qb == 0 else list(range(NTL))

                    def acc(j, m):
                        return S.pb[2 + m * 2 + j // 2][:, (j % 2) * 256:(j % 2) * 256 + 129]

                    for m in range(2):
                        def qk(kt):
                            nonlocal ring
                            ps = S.pb[kt % 2]
                            P.mm(ps[:, :qn], kTh[m * 64:(m + 1) * 64, kt * 128:(kt + 1) * 128],
                                 qTh[m * 64:(m + 1) * 64, q0:q0 + qn])
                            pt = pring[ring % NR]
                            ring += 1
                            P.act(pt[:, :qn], ps[:, :qn], AF.Exp, scale=0.125)
                            return pt
                        LA = 3
                        pts = {}
                        for i, kt in enumerate(seq):
                            if i == 0:
                                for j in range(min(LA, len(seq))):
                                    pts[seq[j]] = qk(seq[j])
                            if i + LA < len(seq):
                                pts[seq[i + LA]] = qk(seq[i + LA])
                            pt = pts.pop(kt)
                            for j in range(nq):
                                P.mm(acc(j, m), pt[:, j * 128:(j + 1) * 128], vaug[:, kt, 0:129],
                                     start=(i == 0 and j % 2 == 0), stop=(i == len(seq) - 1),
                                     skip_group_check=True)
                    for j in range(nq):
                        o0 = acc(j, 0); o1 = acc(j, 1)
                        s = sm[ep % 4]; ta = tab[ep % 2]; od = odb[ep % 2]; yt = ytb[ep % 2]
                        ep += 1
                        P.recip(s[:, 0:1], o0[:, 128:129])
                        P.recip(s[:, 1:2], o1[:, 128:129])
                        P.tt("dve", s[:, 1:2], s[:, 1:2], neglam[:, :], ALU.mult)
                        P.ts("dve", ta[:, :], o0[:, 0:128], s[:, 0:1], None, ALU.mult)
                        P.stt("dve", od[:, :], o1[:, 0:128], s[:, 1:2], ta[:, :], ALU.mult, ALU.add)
                        P.memset("pool", s[:, 2:3], 0.0)
                        P.act(junk[:, :], od[:, :], AF.Square, accum_out=s[:, 2:3])
                        P.act(s[:, 3:4], s[:, 2:3], AF.Sqrt, bias=S.eps_t[:, :], scale=1.0 / 128)
                        P.recip(s[:, 3:4], s[:, 3:4])
                        P.stt("dve", yt[:, :], od[:, :], s[:, 3:4], gnbc[:, :], ALU.mult, ALU.mult)
                        P.tr(ptb[:, j * 128:(j + 1) * 128], yt[:, :], S.identb)
                    P.copy("act", ydT[:, h, q0:q0 + qn], ptb[:, :qn])
        if S.cfg.get("dump_yd") == l:
            P.dump("ydT", ydT[:, :, :])
        if S.cfg.get("merge", True):
            with P.phase() as st:
                merge_branch(S, l, ydT, S.w_o_c, G_D, ctx_out, st)


def branch_b(S, l, ctx_out):
    P = S.P
    U = S.cf[:, 256:384]
    Lm = S.cf[:, 384:512]
    SU = S.cf[:, 512:640]
    SL = S.cf[:, 640:768]
    with P.phase() as sto:
        ybT = P.sb("ybT", [128, 4, NT], BF16, sto)
        if not ctx_out:
            P.memset("pool", ybT[:, :, 0:NCX], 0.0)
        with P.phase() as stc:
            gnbc = P.sb("gnbcb", [128, 128], F32, stc)
            P.dma("sp", gnbc[:, :], S.gn[l, 1].partition_broadcast(128))
            wg2 = P.sb("wg2", [17, 2, 256], BF16, stc)
            if S.cfg.get("t_wg2", 1):
                P.dma("pool", wg2[:, :, :], S.wg2[l])
            one_t = P.sb("one_t", [128, 1], F32, stc)
            P.memset("dve", one_t[:, :], 1.0)
            mkb = P.sb("mkb", [128, 512], BF16, stc)
            P.copy("dve", mkb[:, :], S.cf[:, 256:768])
            Ub = mkb[:, 0:128]; Lb = mkb[:, 128:256]; SUb = mkb[:, 256:384]; SLb = mkb[:, 384:512]
            for hp in range(2):
                with P.phase() as st:
                    qT = P.sb("bqT", [128, NT], BF16, st)
                    kT = P.sb("bkT", [128, NT], BF16, st)
                    ktok = P.sb("bktok", [128, NTL, 128], BF16, st)
                    vtok = P.sb("bvtok", [128, NTL, 256], BF16, st)
                    srtok = P.sb("bsrtok", [128, NTL, 256], BF16, st)
                    glrT = P.sb("bglrT", [17, 2, NT], BF16, st)
                    Sst = P.sb("bSst", [128, 2, NTL, 128], BF16, st)
                    Sf = [P.sb("bSf", [128, 128], F32, st) for _ in range(2)]
                    if S.cfg.get("t_ms", 1):
                        P.memset("pool", glrT[:, :, :], 1.0)
                    P.memset("dve", Sf[0][:, :], 0.0)
                    P.memset("dve", Sf[1][:, :], 0.0)
                    with P.phase() as stw:
                        wB = P.sb("wBp", [128, KC, 800], BF16, stw)
                        for (dst0, n_, src0) in ((0, 128, B_Q + hp * 128), (128, 128, B_K + hp * 128),
                                                 (256, 256, B_V + hp * 256), (512, 256, B_R + hp * 256),
                                                 (768, 32, B_GL)):
                            P.dma("pool", wB[:, :, dst0:dst0 + n_],
                                  S.w_in[l, :, src0:src0 + n_].rearrange("(kc p) f -> p kc f", p=128))
                        for bi in (range(5) if S.cfg.get("b_proj", 9) >= 1 else []):
                            t0, tn = TB[bi]
                            for dstT, col in ((qT, 0), (kT, 128)):
                                pb = S.pb[4 + (col // 128)]
                                for kc in range(KC):
                                    P.mm(pb[:, :tn], wB[:, kc, col:col + 128], S.hT[:, kc, t0:t0 + tn],
                                         start=(kc == 0), stop=(kc == KC - 1))
                                P.act(dstT[:, t0:t0 + tn], pb[:, :tn], AF.Copy)
                            for d in (range(2) if S.cfg.get("t_glr", 1) else []):
                                pb = S.pb[6 + d]
                                for kc in range(KC):
                                    P.mm(pb[0:16, :tn], wB[:, kc, 768 + d * 16:768 + (d + 1) * 16],
                                         S.hT[:, kc, t0:t0 + tn], start=(kc == 0), stop=(kc == KC - 1))
                                P.copy("dve", glrT[0:16, d, t0:t0 + tn], pb[0:16, :tn])
                        for n in (range(NTL) if S.cfg.get("b_proj", 9) >= 2 else []):
                            pa = S.pb[0 + n % 2]
                            pr = S.pb[2 + n % 2]
                            for kc in range(KC):
                                P.mm(pa[:, 0:384], S.hT[:, kc, n * 128:(n + 1) * 128], wB[:, kc, 128:512],
                                     start=(kc == 0), stop=(kc == KC - 1))
                            for kc in range(KC):
                                P.mm(pr[:, 0:256], S.hT[:, kc, n * 128:(n + 1) * 128], wB[:, kc, 512:768],
                                     start=(kc == 0), stop=(kc == KC - 1))
                            P.copy("dve", ktok[:, n, :], pa[:, 0:128])
                            P.act(vtok[:, n, :], pa[:, 128:384], AF.Copy)
                            P.act(srtok[:, n, :], pr[:, 0:256], AF.Silu)

                    e1b = [P.sb("be1", [128, 128], F32, st) for _ in range(2)]
                    spb = [P.sb("bsp", [128, 128], F32, st) for _ in range(2)]
                    eremb = [P.sb("berem", [128, 128], F32, st) for _ in range(2)]
                    kgb = [P.sb("bkg", [128, 128], BF16, st) for _ in range(2)]
                    sphl = [P.sb("bsphl", [128, 2, 128], BF16, st) for _ in range(2)]
                    glb = [P.sb("bgl", [128, 1], F32, st) for _ in range(4)]
                    cnt = 0

                    def gate_sp(d, n):
                        nonlocal cnt
                        px = S.pb[0 + cnt % 2]
                        e1 = e1b[cnt % 2]; sp = spb[cnt % 2]
                        cnt += 1
                        P.mm(px[:, 0:128], glrT[0:17, d, n * 128:(n + 1) * 128],
                             wg2[0:17, d, hp * 128:(hp + 1) * 128])
                        P.act(e1[:, :], px[:, 0:128], AF.Exp, scale=-1.0)
                        P.act(sp[:, :], e1[:, :], AF.Ln, bias=one_t[:, :])
                        hl = sphl[(cnt - 1) % 2]
                        P.copy("dve", hl[:, 0, :], sp[:, :])
                        P.tt("dve", hl[:, 1, :], sp[:, :], hl[:, 0, :], ALU.subtract)
                        return hl

                    if S.cfg.get("b_stop", 9) < 1:
                        continue
                    order = [list(range(NTL)), [1, 0] + list(range(NTL - 1, 1, -1))]
                    for s in range(NTL):
                        for d in range(2):
                            n = order[d][s]
                            sp = gate_sp(d, n)
                            k_ = cnt
                            prm = S.pb[2 + d]
                            for z in range(2):
                                P.mm(prm[:, 0:128], SLb if d == 0 else SUb, sp[:, z, :], start=(z == 0), stop=(z == 1))
                            for z in range(2):
                                P.mm(prm[:, 128:129], sp[:, z, :], S.onesb[:, 0:1], start=(z == 0), stop=(z == 1))
                            erem = eremb[d]; kg = kgb[d]; gl = glb[(s * 2 + d) % 4]
                            P.act(erem[:, :], prm[:, 0:128], AF.Exp, scale=-1.0 / 16)
                            P.act(gl[:, :], prm[:, 128:129], AF.Exp, scale=-1.0 / 16)
                            P.tt("dve", kg[:, :], ktok[:, n, :], erem[:, :], ALU.mult)
                            P.copy("act", Sst[:, d, n, :], Sf[d][:, :])
                            if s == NTL - 1:
                                continue
                            pS = S.pb[4 + d]
                            P.mm(pS[:, 0:256], kg[:, :], vtok[:, n, :])
                            for hh in range(2):
                                sl = slice(hh * 64, (hh + 1) * 64)
                                P.stt("dve", Sf[d][sl, :], Sf[d][sl, :], gl[sl, :], pS[sl, hh * 128:(hh + 1) * 128],
                                      ALU.mult, ALU.add)

                    if S.cfg.get("b_stop", 9) < 2:
                        continue
                    egb = [P.sb("beg", [128, 128], F32, st) for _ in range(2)]
                    eib = [P.sb("bei", [128, 128], F32, st) for _ in range(2)]
                    qtb = [P.sb("bqt", [128, 128], BF16, st) for _ in range(4)]
                    ktb = [P.sb("bkt", [128, 128], BF16, st) for _ in range(4)]
                    atb = [P.sb("bat", [128, 2, 128], BF16, st) for _ in range(4)]
                    smb = [P.sb("bsm", [128, 2], F32, st) for _ in range(4)]
                    junk = P.sb("bjunk", [128, 128], BF16, st)
                    y1b = [P.sb("by1", [128, 128], F32, st) for _ in range(2)]
                    ytb = [P.sb("byt", [128, 128], BF16, st) for _ in range(2)]
                    ptb = S.pb[7][:, 0:256].bitcast(BF16)
                    k2 = 0
                    ep = 0
                    for n in (range(NTL) if ctx_out else range(2, NTL)):
                        dd = []
                        for d in range(2):
                            sp = gate_sp(d, n)
                            pg = S.pb[2]
                            for z in range(2):
                                P.mm(pg[:, 0:128], sp[:, z, :], Ub if d == 0 else Lb, start=(z == 0), stop=(z == 1))
                            eg = egb[d]; ei = eib[d]
                            qt = qtb[k2 % 4]; kt = ktb[k2 % 4]; at = atb[k2 % 4]
                            k2 += 1
                            P.act(eg[:, :], pg[:, 0:128], AF.Exp, scale=-1.0 / 16)
                            P.act(ei[:, :], pg[:, 0:128], AF.Exp, scale=1.0 / 16)
                            P.stt("dve", qt[:, :], qT[:, n * 128:(n + 1) * 128], 0.125, eg[:, :], ALU.mult, ALU.mult)
                            P.tt("pool", kt[:, :], kT[:, n * 128:(n + 1) * 128], ei[:, :], ALU.mult)
                            if S.cfg.get("p2", 9) < 1:
                                continue
                            for hh in range(2):
                                sl = slice(hh * 64, (hh + 1) * 64)
                                P.mm(S.pb[3 + hh][:, 0:128], kt[sl, :], qt[sl, :])
                            mask = (U if d == 0 else Lm)
                            for hh in range(2):
                                P.tt("dve", at[:, hh, :], S.pb[3 + hh][:, 0:128], mask, ALU.mult)
                            dd.append((qt, at))
                        if S.cfg.get("p2", 9) < 2:
                            continue
                        for hh in range(2):
                            sl = slice(hh * 64, (hh + 1) * 64)
                            oo = S.pb[5 + hh][:, 0:128]
                            for d in range(2):
                                qt, at = dd[d]
                                P.mm(oo, qt[sl, :], Sst[sl, d, n, :], start=(d == 0), stop=False)
                                P.mm(oo, at[:, hh, :], vtok[:, n, hh * 128:(hh + 1) * 128], start=False, stop=(d == 1))
                        if S.cfg.get("p2", 9) < 3:
                            continue
                        for hh in range(2):
                            oo = S.pb[5 + hh][:, 0:128]
                            s_ = smb[ep % 4]; y1 = y1b[ep % 2]; yt = ytb[ep % 2]
                            ep += 1
                            P.memset("pool", s_[:, 0:1], 0.0)
                            P.act(junk[:, :], oo, AF.Square, accum_out=s_[:, 0:1])
                            P.act(s_[:, 1:2], s_[:, 0:1], AF.Sqrt, bias=S.eps_t[:, :], scale=1.0 / 128)
                            P.recip(s_[:, 1:2], s_[:, 1:2])
                            P.stt("dve", y1[:, :], oo, s_[:, 1:2], gnbc[:, :], ALU.mult, ALU.mult)
                            P.tt("pool", yt[:, :], y1[:, :], srtok[:, n, hh * 128:(hh + 1) * 128], ALU.mult)
                            P.tr(ptb[:, hh * 128:(hh + 1) * 128], yt[:, :], S.identb)
                        P.copy("act", ybT[:, hp * 2:hp * 2 + 2, n * 128:(n + 1) * 128],
                               ptb[:, 0:256].rearrange("p (h t) -> p h t", h=2))
        if S.cfg.get("dump_yb") == l:
            P.dump("ybT", ybT[:, :, :])
        if S.cfg.get("merge", True):
            with P.phase() as st:
                merge_branch(S, l, ybT, S.w_o_b, G_B, ctx_out, st)


def branch_a(S, l, ctx_out):
    P = S.P
    U = S.cf[:, 256:384]
    Lm = S.cf[:, 384:512]
    SU = S.cf[:, 512:640]
    SL = S.cf[:, 640:768]
    with P.phase() as sto:
        yaT = P.sb("yaT", [128, 4, NT], BF16, sto)
        if not ctx_out:
            P.memset("pool", yaT[:, :, 0:NCX], 0.0)
        with P.phase() as stc:
            gnbc = P.sb("gnbca", [128, 128], F32, stc)
            P.dma("sp", gnbc[:, :], S.gn[l, 0].partition_broadcast(128))
            one_t = P.sb("one_ta", [128, 1], F32, stc)
            P.memset("dve", one_t[:, :], 1.0)
            mkb = P.sb("mkba", [128, 4, 128], BF16, stc)
            P.copy("dve", mkb[:, :, :], S.cf[:, 256:768].rearrange("p (m c) -> p m c", m=4))
            ones_col = P.sb("onescol", [128, 1], BF16, stc)
            P.memset("dve", ones_col[:, :], 1.0)
            cum1 = P.sb("cum1", [128, 2, 129], BF16, stc)
            P.memset("dve", cum1[:, :, :], 1.0)
            P.copy("dve", cum1[:, 0, 0:128], U)
            P.copy("dve", cum1[:, 1, 0:128], Lm)
            cumb = [mkb[:, 0, :], mkb[:, 1, :]]
            gmf = [SL, SU]
            strict = [SL, SU]
            maskT = [U, Lm]
            cw = P.sb("convw", [128, 12, 3], F32, stc)
            P.dma("sp", cw[:, :, :], S.convw[l])
            adt = P.sb("adt", [128, 2, 8], F32, stc)
            P.dma("sp", adt[:, :, :], S.adt[l].partition_broadcast(128))
            nA = P.sb("nA", [128, 8], F32, stc)
            P.act(nA[:, :], adt[:, 0, :], AF.Exp)
            P.ts("dve", nA[:, :], nA[:, :], -1.0, None, ALU.mult)
            betat = P.sb("betat", [128, NTL, 8], F32, stc)
            nbetat = P.sb("nbetat", [128, NTL, 8], F32, stc)
            gt = P.sb("gt", [128, NTL, 8], F32, stc)
            with P.phase() as stw:
                wbg = P.sb("wbg", [128, KC, 16], BF16, stw)
                P.dma("pool", wbg[:, :, :], S.w_in[l, :, A_B:A_B + 16].rearrange("(kc p) f -> p kc f", p=128))
                tmpe = P.sb("tmpe", [128, NTL, 8], F32, stw)
                for n in range(NTL):
                    pb = S.pb[n % 2]
                    for kc in range(KC):
                        P.mm(pb[:, 0:16], S.hT[:, kc, n * 128:(n + 1) * 128], wbg[:, kc, :],
                             start=(kc == 0), stop=(kc == KC - 1))
                    P.act(betat[:, n, :], pb[:, 0:8], AF.Sigmoid)
                    P.tt("dve", gt[:, n, :], pb[:, 8:16], adt[:, 1, :], ALU.add)
                P.act(tmpe[:, :, :], gt[:, :, :], AF.Exp)
                P.act(gt[:, :, :], tmpe[:, :, :], AF.Ln, bias=one_t[:, :])
                P.tt("dve", gt[:, :, :], gt[:, :, :], nA[:, :].unsqueeze(1).to_broadcast([128, NTL, 8]), ALU.mult)
                P.ts("dve", nbetat[:, :, :], betat[:, :, :], -1.0, None, ALU.mult)

            for h in range(4):
                with P.phase() as st:
                    qT = P.sb("aqT", [128, NT], BF16, st)
                    kT = P.sb("akT", [128, NT], BF16, st)
                    ktok = P.sb("aktok", [128, NTL, 128], BF16, st)
                    vtok = P.sb("avtok", [128, NTL, 128], BF16, st)
                    sztok = P.sb("asztok", [128, NTL, 128], BF16, st)
                    oacc = P.sb("aoacc", [128, NTL, 128], F32, st)
                    P.memset("pool", oacc[:, :, :], 0.0)
                    ptb = S.pb[7][:, 0:256].bitcast(BF16)
                    with P.phase() as stw:
                        wA = P.sb("wAh", [128, KC, 512], BF16, stw)
                        for i, c0 in enumerate((A_Q, A_K, A_V, A_Z)):
                            P.dma("pool", wA[:, :, i * 128:(i + 1) * 128],
                                  S.w_in[l, :, c0 + h * 128:c0 + (h + 1) * 128].rearrange("(kc p) f -> p kc f", p=128))
                        pre = [P.sb("apre", [128, NT], F32, stw) for _ in range(1)]
                        cv = [P.sb("acv", [128, NT], F32, stw) for _ in range(1)]
                        sqb = P.sb("asq", [128, 512], BF16, stw)
                        rs = P.sb("ars", [128, 512], F32, stw)
                        vT = P.sb("avT", [128, NT], BF16, stw)
                        for i in range(3):
                            pr = pre[0]; c = cv[0]
                            for bi in range(5):
                                t0, tn = TB[bi]
                                pb = S.pb[bi % 2]
                                for kc in range(KC):
                                    P.mm(pb[:, :tn], wA[:, kc, i * 128:(i + 1) * 128], S.hT[:, kc, t0:t0 + tn],
                                         start=(kc == 0), stop=(kc == KC - 1))
                                P.act(pr[:, t0:t0 + tn], pb[:, :tn], AF.Copy)
                            ch = i * 4 + h
                            P.ts("dve", c[:, :], pr[:, :], cw[:, ch, 1:2], None, ALU.mult)
                            for (a, b) in ((0, NCX), (NCX, NT)):
                                P.stt("dve", c[:, a + 1:b], pr[:, a:b - 1], cw[:, ch, 0:1], c[:, a + 1:b], ALU.mult, ALU.add)
                                P.stt("dve", c[:, a:b - 1], pr[:, a + 1:b], cw[:, ch, 2:3], c[:, a:b - 1], ALU.mult, ALU.add)
                            if i == 2:
                                P.act(vT[:, :], c[:, :], AF.Silu)
                            else:
                                dst = qT if i == 0 else kT
                                P.act(c[:, :], c[:, :], AF.Silu)
                                for bi in range(5):
                                    t0, tn = TB[bi]
                                    pb = S.pb[2 + bi % 2]
                                    P.act(sqb[:, :tn], c[:, t0:t0 + tn], AF.Square)
                                    P.mm(pb[:, :tn], S.onesb, sqb[:, :tn])
                                    P.act(rs[:, :tn], pb[:, :tn], AF.Sqrt, bias=S.eps_t[:, :])
                                    P.recip(rs[:, :tn], rs[:, :tn])
                                    if i == 0:
                                        P.stt("dve", dst[:, t0:t0 + tn], c[:, t0:t0 + tn], 128.0 ** -0.5, rs[:, :tn],
                                              ALU.mult, ALU.mult)
                                    else:
                                        P.tt("dve", dst[:, t0:t0 + tn], c[:, t0:t0 + tn], rs[:, :tn], ALU.mult)
                        for n in range(NTL):
                            P.tr(ptb[:, 0:128], kT[:, n * 128:(n + 1) * 128], S.identb)
                            P.tr(ptb[:, 128:256], vT[:, n * 128:(n + 1) * 128], S.identb)
                            P.copy("dve", ktok[:, n, :], ptb[:, 0:128])
                            P.copy("dve", vtok[:, n, :], ptb[:, 128:256])
                            pz = S.pb[n % 2]
                            for kc in range(KC):
                                P.mm(pz[:, 0:128], S.hT[:, kc, n * 128:(n + 1) * 128], wA[:, kc, 384:512],
                                     start=(kc == 0), stop=(kc == KC - 1))
                            P.act(sztok[:, n, :], pz[:, 0:128], AF.Silu)

                    G = S.cfg.get("a_G", 4)
                    NRG = 4
                    def ring(nm, shape, dt):
                        return [[P.sb(nm, shape, dt, st) for _ in range(NRG)] for _ in range(2)]
                    u_r = ring("au", [128, 128], F32)
                    wT_r = ring("awT", [128, 128], BF16)
                    kg_r = ring("akg", [128, 128], BF16)
                    AT_r = ring("aAT", [128, 128], BF16)
                    sc_r = ring("asc", [128, 4], F32)

                    class WS:
                        pass
                    wss = []
                    for gi in range(G):
                        w_ = WS()
                        w_.gf = P.sb("agmf", [128, 129], F32, st)
                        w_.ghl = P.sb("agmhl", [128, 2, 129], BF16, st)
                        w_.e1 = P.sb("aE1", [128, 129], F32, st)
                        w_.e2 = P.sb("aE2", [128, 129], F32, st)
                        w_.XX = [P.sb("aXX", [128, 2, 128], F32, st) for _ in range(2)]
                        w_.PT = [P.sb("aPT", [128, 128], F32, st) for _ in range(2)]
                        w_.vb = P.sb("avb", [128, 128], BF16, st)
                        w_.kbg = P.sb("akbg", [128, 128], BF16, st)
                        w_.TT = P.sb("aTTb", [128, 128], BF16, st)
                        w_.pc = S.pb[2 + gi]
                        w_.ev = "act" if gi % 2 == 0 else "dve"
                        wss.append(w_)
                    vnew = [P.sb("avnew", [128, 128], BF16, st) for _ in range(2)]
                    o1s = [P.sb("ao1s", [128, 128], F32, st) for _ in range(2)]
                    ot = [P.sb("aot", [128, 128], F32, st) for _ in range(2)]
                    Sf = [P.sb("aSf", [128, 128], F32, st) for _ in range(2)]
                    Sb = [P.sb("aSb", [128, 128], BF16, st) for _ in range(2)]
                    for d in range(2):
                        P.memset("dve", Sf[d][:, :], 0.0)
                        P.memset("dve", Sb[d][:, :], 0.0)
                    order = [list(range(NTL)), [1, 0] + list(range(NTL - 1, 1, -1))]

                    def precompute(d, n, slot, w_):
                        col = d * 4 + h
                        g = gt[:, n, col:col + 1]
                        beta = betat[:, n, col:col + 1]
                        nbeta = nbetat[:, n, col:col + 1]
                        tl = slice(n * 128, (n + 1) * 128)
                        sc = sc_r[d][slot]
                        pc = w_.pc
                        gf = w_.gf; ghl = w_.ghl; e1 = w_.e1; e2 = w_.e2
                        P.ts("dve", gf[:, 0:128], gmf[d], g, None, ALU.mult)
                        P.copy("dve", gf[:, 128:129], g)
                        P.copy("dve", ghl[:, 0, :], gf[:, :])
                        P.tt("dve", ghl[:, 1, :], gf[:, :], ghl[:, 0, :], ALU.subtract)
                        for z in range(2):
                            P.mm(pc[:, 0:129], cumb[d], ghl[:, z, :], start=(z == 0), stop=(z == 1))
                        for z in range(2):
                            P.mm(pc[:, 256:385], ghl[:, z, 0:128], cum1[:, d, :], start=(z == 0), stop=(z == 1))
                        P.act(e1[:, :], pc[:, 0:129], AF.Exp)
                        P.act(e2[:, :], pc[:, 256:385], AF.Exp)
                        yield
                        P.tt("pool", e1[:, 0:128], e1[:, 0:128], strict[d], ALU.mult)
                        P.tt("pool", e2[:, 0:128], e2[:, 0:128], maskT[d], ALU.mult)
                        P.tt("dve", sc[:, 2:3], e1[:, 128:129], e2[:, 128:129], ALU.mult)
                        P.tt("dve", sc[:, 3:4], e1[:, 128:129], beta, ALU.mult)
                        P.copy("dve", sc[:, 0:1], e1[:, 128:129])
                        P.mm(pc[:, 0:128], kT[:, tl], kT[:, tl])
                        P.mm(pc[:, 128:256], kT[:, tl], qT[:, tl])
                        XX = w_.XX; PT = w_.PT
                        P.stt("dve", XX[0][:, 0, :], pc[:, 0:128], nbeta, e1[:, 0:128], ALU.mult, ALU.mult)
                        P.tt("dve", AT_r[d][slot][:, :], pc[:, 128:256], e2[:, 0:128], ALU.mult)
                        P.ts("dve", kg_r[d][slot][:, :], ktok[:, n, :], e2[:, 128:129], None, ALU.mult)
                        P.ts("dve", w_.vb[:, :], vtok[:, n, :], beta, None, ALU.mult)
                        P.ts("dve", w_.kbg[:, :], ktok[:, n, :], sc[:, 3:4], None, ALU.mult)
                        yield
                        P.tr(pc[:, 384:512], XX[0][:, 0, :], S.identf)
                        P.copy(w_.ev, XX[0][:, 1, :], pc[:, 384:512])
                        P.tt("dve", PT[0][:, :], pc[:, 384:512], S.identf, ALU.add)
                        yield
                        cur = 0
                        for lev in range(1, 7):
                            nxt = 1 - cur
                            if lev > 1:
                                P.mm(pc[:, 256:384], XX[cur][:, 0, :], PT[lev % 2][:, :])
                                P.tt("dve", PT[1 - (lev % 2)][:, :], pc[:, 256:384], PT[lev % 2][:, :], ALU.add)
                            P.mm(pc[:, 0:128], XX[cur][:, 1, :], XX[cur][:, 0, :])
                            if lev < 6:
                                P.mm(pc[:, 128:256], XX[cur][:, 0, :], XX[cur][:, 1, :])
                                P.copy(w_.ev, XX[nxt][:, :, :], pc[:, 0:256].rearrange("p (a b) -> p a b", a=2))
                            else:
                                P.copy(w_.ev, XX[nxt][:, 0, :], pc[:, 0:128])
                            cur = nxt
                            yield
                        P.mm(pc[:, 256:384], XX[cur][:, 0, :], PT[1][:, :])
                        P.tt("dve", PT[0][:, :], pc[:, 256:384], PT[1][:, :], ALU.add)
                        P.copy("act", w_.TT[:, :], PT[0][:, :])
                        yield
                        P.mm(pc[:, 0:128], w_.TT[:, :], w_.vb[:, :])
                        P.mm(pc[:, 128:256], w_.kbg[:, :], w_.TT[:, :])
                        P.copy("act", u_r[d][slot][:, :], pc[:, 0:128])
                        P.copy("act", wT_r[d][slot][:, :], pc[:, 128:256])

                    def recur(d, n, slot, last):
                        tl = slice(n * 128, (n + 1) * 128)
                        pr = S.pb[d]
                        sc = sc_r[d][slot]
                        P.mm(pr[:, 0:128], wT_r[d][slot][:, :], Sb[d][:, :])
                        P.mm(pr[:, 128:256], qT[:, tl], Sb[d][:, :])
                        P.tt("dve", vnew[d][:, :], u_r[d][slot][:, :], pr[:, 0:128], ALU.subtract)
                        want_out = ctx_out or n >= 2
                        yield
                        if want_out:
                            P.mm(pr[:, 256:384], AT_r[d][slot][:, :], vnew[d][:, :])
                        if not last:
                            P.mm(pr[:, 384:512], kg_r[d][slot][:, :], vnew[d][:, :])
                        if want_out:
                            P.act(o1s[d][:, :], pr[:, 128:256], AF.Copy, scale=sc[:, 0:1])
                        if not last:
                            P.stt("dve", Sf[d][:, :], Sf[d][:, :], sc[:, 2:3], pr[:, 384:512], ALU.mult, ALU.add)
                            P.copy("act", Sb[d][:, :], Sf[d][:, :])
                        if want_out:
                            P.tt("dve", ot[d][:, :], pr[:, 256:384], o1s[d][:, :], ALU.add)
                            P.tt("pool", oacc[:, n, :], oacc[:, n, :], ot[d][:, :], ALU.add)
                        yield

                    units = [(d, s) for s in range(NTL) for d in range(2)]
                    pre_done = set()
                    active = []
                    next_unit = 0
                    rec_step = [0, 0]
                    rec_gen = [None, None]
                    free_ws = list(range(G))
                    while True:
                        while free_ws and next_unit < len(units):
                            d_, s_ = units[next_unit]
                            if s_ - rec_step[d_] >= NRG - 1:
                                break
                            wi = free_ws.pop(0)
                            active.append(("pre", (d_, s_, wi), precompute(d_, order[d_][s_], s_ % NRG, wss[wi])))
                            next_unit += 1
                        for d_ in range(2):
                            if rec_gen[d_] is None and rec_step[d_] < NTL and (d_, rec_step[d_]) in pre_done:
                                s_ = rec_step[d_]
                                rec_gen[d_] = recur(d_, order[d_][s_], s_ % NRG, s_ == NTL - 1)
                        if not active and rec_gen[0] is None and rec_gen[1] is None:
                            if next_unit >= len(units) and rec_step[0] >= NTL and rec_step[1] >= NTL:
                                break
                        for item in list(active):
                            kind, key, gen = item
                            try:
                                next(gen)
                            except StopIteration:
                                active.remove(item)
                                pre_done.add((key[0], key[1]))
                                free_ws.append(key[2])
                        for d_ in range(2):
                            if rec_gen[d_] is not None:
                                try:
                                    next(rec_gen[d_])
                                except StopIteration:
                                    rec_gen[d_] = None
                                    rec_step[d_] += 1

                    smb = [P.sb("asm", [128, 2], F32, st) for _ in range(4)]
                    junk = P.sb("ajunk", [128, 128], BF16, st)
                    y1b = [P.sb("ay1", [128, 128], F32, st) for _ in range(2)]
                    ytb = [P.sb("ayt", [128, 128], BF16, st) for _ in range(2)]
                    ep = 0
                    for n in (range(NTL) if ctx_out else range(2, NTL)):
                        s_ = smb[ep % 4]; y1 = y1b[ep % 2]; yt = ytb[ep % 2]
                        ep += 1
                        P.memset("pool", s_[:, 0:1], 0.0)
                        P.act(junk[:, :], oacc[:, n, :], AF.Square, accum_out=s_[:, 0:1])
                        P.act(s_[:, 1:2], s_[:, 0:1], AF.Sqrt, bias=S.eps_t[:, :], scale=1.0 / 128)
                        P.recip(s_[:, 1:2], s_[:, 1:2])
                        P.stt("dve", y1[:, :], oacc[:, n, :], s_[:, 1:2], gnbc[:, :], ALU.mult, ALU.mult)
                        P.tt("pool", yt[:, :], y1[:, :], sztok[:, n, :], ALU.mult)
                        P.tr(ptb[:, 0:128], yt[:, :], S.identb)
                        P.copy("act", yaT[:, h, n * 128:(n + 1) * 128], ptb[:, 0:128])
        if S.cfg.get("dump_ya") == l:
            P.dump("yaT", yaT[:, :, :])
        if S.cfg.get("merge", True):
            with P.phase() as st:
                merge_branch(S, l, yaT, S.w_o_a, G_A, ctx_out, st)


def phase_ffn(S, l):
    P = S.P
    moe = (l % 2 == 1)
    ctx_out = l < 1
    blocks = list(range(5)) if ctx_out else list(range(1, 5))
    i_ = l // 2
    with P.phase() as sto:
        gate = None
        if moe:
            logit = P.sb("logit", [128, 16, 8], F32, sto)
            gate = P.sb("gate", [128, 16, 8], F32, sto)
        with P.phase() as st:
            router = None
            if moe:
                rw = P.sb("rw", [128, KC, 8], F32, st)
                P.dma("sp", rw[:, :, :], S.router_w[i_].rearrange("(kc p) e -> p kc e", p=128))
                router = (rw, logit)
            modulate(S, l, 1, blocks, st, router=router)
        if moe:
            with P.phase() as st:
                m1 = P.sb("m1", [128, 16], F32, st)
                m2 = P.sb("m2", [128, 16], F32, st)
                eq1 = P.sb("eq1", [128, 16, 8], F32, st)
                eq2 = P.sb("eq2", [128, 16, 8], F32, st)
                l2 = P.sb("l2", [128, 16, 8], F32, st)
                ww = P.sb("ww", [128, 3, 16], F32, st)
                bc = lambda a: a.unsqueeze(2).to_broadcast([128, 16, 8])
                P.reduce("dve", m1[:, :], logit[:, :, :], ALU.max, AX.X)
                P.tt("dve", eq1[:, :, :], logit[:, :, :], bc(m1[:, :]), ALU.is_equal)
                P.stt("dve", l2[:, :, :], eq1[:, :, :], -1e30, logit[:, :, :], ALU.mult, ALU.add)
                P.reduce("dve", m2[:, :], l2[:, :, :], ALU.max, AX.X)
                P.tt("dve", eq2[:, :, :], l2[:, :, :], bc(m2[:, :]), ALU.is_equal)
                P.tt("dve", ww[:, 0, :], m2[:, :], m1[:, :], ALU.subtract)
                P.act(ww[:, 0, :], ww[:, 0, :], AF.Exp)
                P.ts("dve", ww[:, 1, :], ww[:, 0, :], 1.0, None, ALU.add)
                P.recip(ww[:, 1, :], ww[:, 1, :])
                P.tt("dve", ww[:, 2, :], ww[:, 0, :], ww[:, 1, :], ALU.mult)
                P.tt("dve", eq1[:, :, :], eq1[:, :, :], bc(ww[:, 1, :]), ALU.mult)
                P.tt("dve", eq2[:, :, :], eq2[:, :, :], bc(ww[:, 2, :]), ALU.mult)
                P.tt("dve", gate[:, :, :], eq1[:, :, :], eq2[:, :, :], ALU.add)
            if S.cfg.get("dump_gate"):
                P.dump("gate", gate[:, :, :])
        with P.phase() as st:
            GR = 6
            groups = [(0, 6), (6, 6), (12, 6), (18, 4)]
            gbuf = P.sb("gbuf", [128, GR, NL if moe else NT], BF16, st)
            w13 = [P.sb("w13", [128, 2, KC, 384], BF16, st) for _ in range(2)]
            w2b = [P.sb("w2b", [128, GR, 1024], BF16, st) for _ in range(2)]
            sil = [P.sb("sil", [128, 512], F32, st) for _ in range(2)]
            tmb = [P.sb("tmb", [128, 512], F32 if moe else BF16, st) for _ in range(2)]
            gbce = P.sb("gbce", [128, NL], BF16, st) if moe else None
            gdg = [P.sb("gdg", [128, 128], F32, st) for _ in range(2)] if moe else None
            nw13 = 0; nw2 = 0; nt = 0; ng_ = 0
            experts = range(8) if moe else range(1)
            if moe:
                blks = [(bi, TB[bi][0], TB[bi][0] - NCX, TB[bi][1]) for bi in blocks]
            else:
                blks = [(bi, TB[bi][0], TB[bi][0], TB[bi][1]) for bi in blocks]
            for e in experts:
                W1 = S.moe_w1[i_, e] if moe else S.ffn_w1[i_]
                W3 = S.moe_w3[i_, e] if moe else S.ffn_w3[i_]
                W2 = S.moe_w2[i_, e] if moe else S.ffn_w2[i_]
                if moe:
                    for b4 in range(4):
                        pg = S.pb[7]
                        for j in range(4):
                            tile = b4 * 4 + j
                            gd = gdg[tile % 2]
                            P.ts("dve", gd[:, :], S.identf, gate[:, tile, e:e + 1], None, ALU.mult)
                            P.mm(pg[:, j * 128:(j + 1) * 128], S.onesf, gd[:, :])
                        P.copy("act", gbce[:, b4 * 512:(b4 + 1) * 512], pg[:, :])
                for (f0, nf) in groups:
                    w2t = w2b[nw2 % 2]; nw2 += 1
                    P.dma("pool", w2t[:, 0:nf, :],
                          W2[f0 * 128:(f0 + nf) * 128, :].rearrange("(c p) f -> p c f", p=128))
                    for p0 in range(0, nf, 3):
                        npf = min(3, nf - p0)
                        wt = w13[nw13 % 2]; nw13 += 1
                        c0 = (f0 + p0) * 128
                        P.dma("pool", wt[:, 0, :, 0:npf * 128],
                              W1[:, c0:c0 + npf * 128].rearrange("(kc p) f -> p kc f", p=128))
                        P.dma("pool", wt[:, 1, :, 0:npf * 128],
                              W3[:, c0:c0 + npf * 128].rearrange("(kc p) f -> p kc f", p=128))
                        for (bi, x0, g0, tn) in blks:
                            for fi in range(npf):
                                pa = S.pb[0 + nt % 2]; pbb = S.pb[2 + nt % 2]
                                sl_ = sil[nt % 2]; tm = tmb[nt % 2]
                                nt += 1
                                for kc in range(KC):
                                    P.mm(pa[:, :tn], wt[:, 0, kc, fi * 128:(fi + 1) * 128], S.hT[:, kc, x0:x0 + tn],
                                         start=(kc == 0), stop=(kc == KC - 1))
                                for kc in range(KC):
                                    P.mm(pbb[:, :tn], wt[:, 1, kc, fi * 128:(fi + 1) * 128], S.hT[:, kc, x0:x0 + tn],
                                         start=(kc == 0), stop=(kc == KC - 1))
                                P.act(sl_[:, :tn], pa[:, :tn], AF.Silu)
                                gdst = gbuf[:, p0 + fi, g0:g0 + tn]
                                if moe:
                                    P.tt("dve", tm[:, :tn], pbb[:, :tn], sl_[:, :tn], ALU.mult)
                                    P.tt("pool", gdst, tm[:, :tn], gbce[:, g0:g0 + tn], ALU.mult)
                                else:
                                    P.tt("dve", gdst, pbb[:, :tn], sl_[:, :tn], ALU.mult)
                    for fo in range(8):
                        for (bi, x0, g0, tn) in blks:
                            which = 1 if bi == 0 else 0
                            po = S.pb[4 + ng_ % 3]; ng_ += 1
                            for fc in range(nf):
                                P.mm(po[:, :tn], w2t[:, fc, fo * 128:(fo + 1) * 128], gbuf[:, fc, g0:g0 + tn],
                                     start=(fc == 0), stop=(fc == nf - 1))
                            P.stt("dve", S.xT[:, fo, x0:x0 + tn], po[:, :tn], S.modv[:, l, 40 + fo, which:which + 1],
                                  S.xT[:, fo, x0:x0 + tn], ALU.mult, ALU.add)
    if S.cfg.get("dump_xffn") == l:
        P.dump("xT", S.xT[:, :, :])

def extra_shared(shared, inputs, f):
    shared["rope"] = _rope_tables()
    shared["gn"] = f(np.stack([inputs["gn_a"], inputs["gn_b"], inputs["gn_c"]], axis=1))
    shared["lam_c"] = f(inputs["lam_c"])
    cw = np.asarray(inputs["conv_a"], np.float32)
    shared["convw"] = f(cw.reshape(2, 3, 12, 128).transpose(0, 3, 2, 1))
    shared["adt"] = f(np.stack([np.asarray(inputs["a_log"], np.float32).reshape(2, 8),
                                np.asarray(inputs["dt_bias"], np.float32).reshape(2, 8)], axis=1))
    wg2 = np.concatenate([np.asarray(inputs["w_gate2"], np.float32),
                          np.asarray(inputs["b_gate"], np.float32)[:, :, None, :]], axis=2)
    shared["wg2"] = f(wg2.transpose(0, 2, 1, 3))
    for k in ("w_o_a", "w_o_b", "w_o_c", "w_out", "ffn_w1", "ffn_w3", "ffn_w2", "router_w", "moe_w1", "moe_w3", "moe_w2"):
        shared[k] = f(inputs[k])

def _consts():
    c = np.zeros((128, 1024), np.float32)
    c[:, 0:128] = np.eye(128, dtype=np.float32)
    for p in range(128):
        if (p % 32) < 16:
            c[p + 16, 128 + p] = -1.0
        else:
            c[p - 16, 128 + p] = 1.0
    k = np.arange(128)[:, None]
    i = np.arange(128)[None, :]
    c[:, 256:384] = (k <= i)
    c[:, 384:512] = (k >= i)
    c[:, 512:640] = (k < i)
    c[:, 640:768] = (k > i)
    return c


def _rope_tables():
    t = np.arange(2048)
    row = (t // 64).astype(np.float32)
    col = (t % 64).astype(np.float32)
    inv_freq = (np.float32(10000.0) ** (-np.arange(16, dtype=np.float32) / np.float32(16))).astype(np.float32)
    tab = np.zeros((2, 128, 2048), np.float32)
    for p in range(128):
        d = p % 64
        pos = row if d < 32 else col
        ang = (pos * inv_freq[d % 16]).astype(np.float32)
        tab[0, p] = np.cos(ang)
        tab[1, p] = np.sin(ang)
    return tab


def _fm(v):
    v = np.asarray(v)
    lead = v.shape[:-1]
    n = v.shape[-1] // 128
    w = v.reshape(lead + (n, 128))
    return np.ascontiguousarray(np.moveaxis(w, -1, 0))


_CACHE = {}


def _get_prog(cfg_key, cfg):
    if cfg_key not in _CACHE:
        _CACHE[cfg_key] = build(cfg)
    return _CACHE[cfg_key]


def make_in_maps(inputs, cfg):
    f = lambda a: np.ascontiguousarray(np.asarray(a, dtype=np.float32))
    x = f(inputs["x"]); c = f(inputs["c"]); ctx = f(inputs["ctx"]); c_ctx = f(inputs["c_ctx"])
    shared = {}
    shared["w_mod"] = f(inputs["w_mod"])
    shared["b_mod"] = np.ascontiguousarray(f(inputs["b_mod"]).reshape(2, 48, 128).transpose(0, 2, 1))
    ng = np.stack([_fm(inputs["norm1_g"][0]), _fm(inputs["norm1_g"][1]), _fm(inputs["norm2_g"][0]),
                   _fm(inputs["norm2_g"][1]), _fm(inputs["final_g"])], axis=1)
    shared["norm_g"] = f(ng)
    shared["w_in"] = f(inputs["w_in"])
    shared["consts"] = _consts()
    extra_shared(shared, inputs, f)
    maps = []
    for b in range(8):
        m = dict(shared)
        m["x"] = x[b]
        m["ctx"] = ctx[b]
        m["cvec"] = f(np.stack([_fm(c[b]), _fm(c_ctx)], axis=-1))
        maps.append(m)
    return maps


def run(inputs, cfg, trace=False):
    P = _get_prog(repr(sorted(cfg.items())), cfg)
    maps = make_in_maps(inputs, cfg)
    names = set()
    for ins in P.nc.main_func.allocations if False else []:
        pass
    n = cfg.get("ncores", 8)
    res = run_bass_kernel_spmd(P.nc, maps[:n], core_ids=list(range(n)), trace=trace)
    return P, res


def kernel(**inputs):
    cfg = dict(layers=2)
    P, res = run(inputs, cfg)
    out = np.stack([np.asarray(res.results[b]["out"], dtype=np.float32) for b in range(8)], axis=0)
    return out
```

```python
import contextlib
import numpy as np
import concourse.bass as bass
import concourse.mybir as mybir
from concourse.bass_utils import run_bass_kernel_spmd

F32 = mybir.dt.float32
BF16 = mybir.dt.bfloat16
AF = mybir.ActivationFunctionType
ALU = mybir.AluOpType
AX = mybir.AxisListType


class _Op:
    __slots__ = ("idx", "eng", "fn", "deps", "dma", "sem", "semval", "needs_inc", "prev_dma")


class _Unit:
    __slots__ = ("w", "r", "rd")

    def __init__(self):
        self.w = None
        self.r = {}
        self.rd = []


class V:
    __slots__ = ("ap", "key")

    def __init__(self, ap, key):
        self.ap = ap
        self.key = key


def _apk(x):
    if isinstance(x, V):
        return x.ap, x.key
    return x, None


class Prog:
    NDMA = 8

    def __init__(self):
        self.nc = bass.Bass("TRN2", target_bir_lowering=False)
        self.ops = []
        self.names = {}
        self.base_deps = []
        self.last = {}
        self.dma_since_barrier = []
        self.stack = contextlib.ExitStack()
        self.n_dma = {"sp": 0, "act": 0, "pool": 0}
        self.dma_last = {}
        self.dbg = []
        self._uid = 0
        self.psum_names = set()

    def uid(self, base):
        self._uid += 1
        return f"{base}_{self._uid}"

    def sb(self, name, shape, dtype, stack=None):
        st = stack if stack is not None else self.stack
        return st.enter_context(self.nc.sbuf_tensor(self.uid(name), list(shape), dtype))

    def ps(self, name, shape, dtype=F32, stack=None):
        st = stack if stack is not None else self.stack
        t = st.enter_context(self.nc.psum_tensor(self.uid(name), list(shape), dtype))
        self.psum_names.add(t[:].name)
        return t

    def dram(self, name, shape, dtype, kind="Internal"):
        return self.nc.dram_tensor(name, list(shape), dtype, kind=kind)

    @contextlib.contextmanager
    def phase(self):
        st = contextlib.ExitStack()
        try:
            yield st
        finally:
            self.barrier()
            st.close()

    def _conf(self, name, sub):
        d = self.names.setdefault(name, {})
        if sub is None:
            if None not in d:
                d[None] = _Unit()
            return d[None], list(d.values())
        if sub not in d:
            d[sub] = _Unit()
        res = [d[sub]]
        if None in d:
            res.append(d[None])
        return d[sub], res

    def add(self, eng, fn, reads, writes, dma=False):
        op = _Op()
        op.idx = len(self.ops)
        op.eng = eng
        op.fn = fn
        op.dma = dma
        op.sem = None
        op.semval = 0
        op.needs_inc = False
        op.prev_dma = None
        deps = {}
        for b in self.base_deps:
            deps[b.idx] = b
        for x in reads:
            if x is None or isinstance(x, (int, float)):
                continue
            ap, key = _apk(x)
            prim, conf = self._conf(ap.name, key)
            for u in conf:
                if u.w is not None:
                    deps[u.w.idx] = u.w
                if ap.name in self.psum_names:
                    for re_, r in u.r.items():
                        if re_ != eng:
                            deps[r.idx] = r
            if dma:
                prim.rd.append(op)
            else:
                prim.r[eng] = op
        for x in writes:
            ap, key = _apk(x)
            prim, conf = self._conf(ap.name, key)
            for u in conf:
                if u.w is not None:
                    deps[u.w.idx] = u.w
                for r in u.r.values():
                    deps[r.idx] = r
                for r in u.rd:
                    deps[r.idx] = r
            prim.w = op
            prim.r = {}
            prim.rd = []
        deps.pop(op.idx, None)
        if dma:
            slot = self.n_dma[eng] % self.NDMA
            self.n_dma[eng] += 1
            prev = self.dma_last.get((eng, slot))
            op.prev_dma = prev
            op.sem = (eng, slot)
            op.semval = (prev.semval if prev is not None else 0) + 16
            self.dma_last[(eng, slot)] = op
            self.dma_since_barrier.append(op)
        op.deps = list(deps.values())
        self.ops.append(op)
        self.last[eng] = op
        return op

    def barrier(self):
        b = list(self.last.values()) + list(self.dma_since_barrier)
        self.base_deps = b
        self.dma_since_barrier = []

    def mm(self, out, lhsT, rhs, start=True, stop=True, **kw):
        o, _ = _apk(out); l, _ = _apk(lhsT); r, _ = _apk(rhs)
        nc = self.nc
        return self.add("pe", lambda: nc.tensor.matmul(o, l, r, start=start, stop=stop, **kw),
                        [lhsT, rhs], [out])

    def tr(self, out, in_, ident):
        o, _ = _apk(out); i, _ = _apk(in_); d, _ = _apk(ident)
        nc = self.nc
        return self.add("pe", lambda: nc.tensor.transpose(o, i, d), [in_, ident], [out])

    def act(self, out, in_, func, bias=None, scale=None, accum_out=None):
        o, _ = _apk(out); i, _ = _apk(in_)
        kw = {}
        rd = [in_]
        wr = [out]
        if bias is not None:
            kw["bias"] = _apk(bias)[0] if not isinstance(bias, (int, float)) else bias
            rd.append(bias)
        if scale is not None:
            kw["scale"] = _apk(scale)[0] if not isinstance(scale, (int, float)) else scale
            rd.append(scale)
        if accum_out is not None:
            kw["accum_out"] = _apk(accum_out)[0]
            wr.append(accum_out)
        nc = self.nc
        return self.add("act", lambda: nc.scalar.activation(out=o, in_=i, func=func, **kw), rd, wr)

    def _ve(self, eng):
        return self.nc.vector if eng == "dve" else self.nc.gpsimd

    def tt(self, eng, out, in0, in1, op):
        o, _ = _apk(out); a, _ = _apk(in0); b, _ = _apk(in1)
        e = self._ve(eng)
        return self.add(eng, lambda: e.tensor_tensor(out=o, in0=a, in1=b, op=op), [in0, in1], [out])

    def ts(self, eng, out, in0, s1, s2=None, op0=ALU.mult, op1=None, accum_out=None):
        o, _ = _apk(out); a, _ = _apk(in0)
        e = self._ve(eng)
        s1v = s1 if isinstance(s1, (int, float)) else _apk(s1)[0]
        s2v = s2 if (s2 is None or isinstance(s2, (int, float))) else _apk(s2)[0]
        kw = {}
        wr = [out]
        if op1 is not None:
            kw["op1"] = op1
        if accum_out is not None:
            kw["accum_out"] = _apk(accum_out)[0]
            wr.append(accum_out)
        return self.add(eng, lambda: e.tensor_scalar(out=o, in0=a, scalar1=s1v, scalar2=s2v, op0=op0, **kw),
                        [in0, s1, s2], wr)

    def stt(self, eng, out, in0, scalar, in1, op0, op1):
        o, _ = _apk(out); a, _ = _apk(in0); b, _ = _apk(in1)
        e = self._ve(eng)
        sv = scalar if isinstance(scalar, (int, float)) else _apk(scalar)[0]
        return self.add(eng, lambda: e.scalar_tensor_tensor(out=o, in0=a, scalar=sv, in1=b, op0=op0, op1=op1),
                        [in0, scalar, in1], [out])

    def copy(self, eng, out, in_):
        o, _ = _apk(out); i, _ = _apk(in_)
        nc = self.nc
        if eng == "act":
            return self.add("act", lambda: nc.scalar.copy(out=o, in_=i), [in_], [out])
        e = self._ve(eng)
        return self.add(eng, lambda: e.tensor_copy(out=o, in_=i), [in_], [out])

    def memset(self, eng, out, val):
        o, _ = _apk(out)
        e = self._ve(eng)
        return self.add(eng, lambda: e.memset(o, val), [], [out])

    def reduce(self, eng, out, in_, op, axis=AX.X):
        o, _ = _apk(out); i, _ = _apk(in_)
        e = self._ve(eng)
        return self.add(eng, lambda: e.tensor_reduce(out=o, in_=i, axis=axis, op=op), [in_], [out])

    def recip(self, out, in_):
        o, _ = _apk(out); i, _ = _apk(in_)
        nc = self.nc
        return self.add("dve", lambda: nc.vector.reciprocal(out=o, in_=i), [in_], [out])

    def dma(self, q, out, in_):
        o, _ = _apk(out); i, _ = _apk(in_)
        e = {"sp": self.nc.sync, "act": self.nc.scalar, "pool": self.nc.gpsimd}[q]
        return self.add(q, lambda: e.dma_start(out=o, in_=i), [in_], [out], dma=True)

    def dump(self, name, ap, dtype=None):
        a, _ = _apk(ap)
        t = self.nc.dram_tensor("dbg_" + name, list(a.shape), dtype or a.dtype, kind="ExternalOutput")
        self.dbg.append("dbg_" + name)
        self.dma("sp", t.ap(), ap)

    def emit(self):
        nc = self.nc
        engobj = {"pe": nc.tensor, "act": nc.scalar, "dve": nc.vector, "pool": nc.gpsimd, "sp": nc.sync}
        st = self.stack
        esem = {e: st.enter_context(nc.semaphore("s_" + e)) for e in ("pe", "act", "dve", "pool")}
        dsem = {}
        for q in ("sp", "act", "pool"):
            for s in range(self.NDMA):
                if (q, s) in self.dma_last:
                    dsem[(q, s)] = st.enter_context(nc.semaphore(f"d_{q}{s}"))
        for op in self.ops:
            for p in op.deps:
                if not p.dma and not (p.eng == "pe" and op.eng == "pe"):
                    p.needs_inc = True
        finals = list(self.last.values())
        for p in finals:
            if not p.dma:
                p.needs_inc = True
        cnt = {e: 0 for e in esem}
        for op in self.ops:
            if not op.dma and op.needs_inc:
                cnt[op.eng] += 1
                op.semval = cnt[op.eng]
        waited = {e: {} for e in engobj}
        nwait = 0
        for op in self.ops:
            e = engobj[op.eng]
            need = {}
            for p in op.deps:
                if p.dma:
                    k = ("d", p.sem)
                    v = p.semval
                else:
                    if p.eng == "pe" and op.eng == "pe":
                        continue
                    k = ("e", p.eng)
                    v = p.semval
                if need.get(k, 0) < v:
                    need[k] = v
            if op.dma and op.prev_dma is not None:
                k = ("d", op.sem)
                if need.get(k, 0) < op.prev_dma.semval:
                    need[k] = op.prev_dma.semval
            w = waited[op.eng]
            for k, v in need.items():
                if w.get(k, 0) >= v:
                    continue
                w[k] = v
                sem = dsem[k[1]] if k[0] == "d" else esem[k[1]]
                e.wait_ge(sem, v)
                nwait += 1
            ins = op.fn()
            if op.dma:
                ins.then_inc(dsem[op.sem], 16)
            elif op.needs_inc:
                ins.then_inc(esem[op.eng], 1)
        sp = nc.sync
        for en, sem in esem.items():
            if cnt[en] > 0:
                sp.wait_ge(sem, cnt[en])
        for k, p in self.dma_last.items():
            sp.wait_ge(dsem[k], p.semval)
        self.stats = dict(n_ops=len(self.ops), n_wait=nwait, incs=dict(cnt))
        return nc

D = 1024
KC = 8
NL = 2048
NCX = 256
NT = 2304
NTL = 18
TB = [(0, 256), (256, 512), (768, 512), (1280, 512), (1792, 512)]
DFF = 2816
FC = 22
EPS = 1e-6
A_Q, A_K, A_V, A_Z, A_B, A_G = 0, 512, 1024, 1536, 2048, 2056
B_Q, B_K, B_V, B_R, B_GL = 2064, 2320, 2576, 3088, 3600
C_Q, C_K, C_V = 3632, 4144, 4656
G_A, G_B, G_D = 5168, 6192, 7216


class Ctx:
    pass


def build(cfg):
    P = Prog()
    nc = P.nc
    S = Ctx()
    S.P = P
    S.cfg = cfg
    L = cfg.get("layers", 2)

    def din(name, shape, dt=F32):
        return nc.dram_tensor(name, list(shape), dt, kind="ExternalInput").ap()

    S.x = din("x", [NL, D])
    S.ctx = din("ctx", [NCX, D])
    S.cvec = din("cvec", [128, KC, 2])
    S.w_mod = din("w_mod", [2, D, 6 * D])
    S.b_mod = din("b_mod", [2, 128, 48])
    S.norm_g = din("norm_g", [128, 5, KC])
    S.w_in = din("w_in", [2, D, 8240])
    S.consts = din("consts", [128, 1024])
    S.rope = din("rope", [2, 128, NL])
    S.gn = din("gn", [2, 3, 128])
    S.lam_c = din("lam_c", [2, 4, 64])
    S.wg2 = din("wg2", [2, 17, 2, 256])
    S.convw = din("convw", [2, 128, 12, 3])
    S.adt = din("adt", [2, 2, 8])
    S.w_o_a = din("w_o_a", [2, 512, D])
    S.w_o_b = din("w_o_b", [2, 512, D])
    S.w_o_c = din("w_o_c", [2, 512, D])
    S.w_out = din("w_out", [2, D, D])
    S.ffn_w1 = din("ffn_w1", [1, D, DFF])
    S.ffn_w3 = din("ffn_w3", [1, D, DFF])
    S.ffn_w2 = din("ffn_w2", [1, DFF, D])
    S.router_w = din("router_w", [1, D, 8])
    S.moe_w1 = din("moe_w1", [1, 8, D, DFF])
    S.moe_w3 = din("moe_w3", [1, 8, D, DFF])
    S.moe_w2 = din("moe_w2", [1, 8, DFF, D])
    S.out = nc.dram_tensor("out", [NL, D], F32, kind="ExternalOutput").ap()

    st = P.stack
    S.xT = P.sb("xT", [128, KC, NT], F32)
    S.hT = P.sb("hT", [128, KC, NT], BF16)
    S.cf = P.sb("cf", [128, 1024], F32)
    S.identf = S.cf[:, 0:128]
    S.identb_t = P.sb("identb", [128, 128], BF16)
    S.identb = S.identb_t[:, :]
    S.onesb_t = P.sb("onesb", [128, 128], BF16)
    S.onesb = S.onesb_t[:, :]
    S.onesf_t = P.sb("onesf", [128, 128], F32)
    S.onesf = S.onesf_t[:, :]
    S.modv = P.sb("modv", [128, 2, 48, 2], F32)
    S.ng = P.sb("ng", [128, 5, KC], F32)
    S.gs = P.sb("gs", [128, 4, KC, 2], F32)
    S.pb = [P.ps(f"pb{i}", [128, 512], F32) for i in range(8)]

    P.dma("sp", S.cf[:, :], S.consts)
    P.dma("sp", S.ng[:, :, :], S.norm_g)
    P.copy("dve", S.identb, S.identf)
    P.memset("dve", S.onesb, 1.0)
    P.memset("dve", S.onesf, 1.0)
    S.eps_t = P.sb("eps_t", [128, 1], F32)
    P.memset("dve", S.eps_t[:, :], EPS)
    S.zero_t = P.sb("zero_t", [128, 1], F32)
    P.memset("dve", S.zero_t[:, :], 0.0)

    phase_load(S)
    phase_mod(S, L)
    for l in range(L):
        if cfg.get("mixer", True):
            phase_mixer(S, l)
        if cfg.get("ffn", True):
            phase_ffn(S, l)
    phase_final(S)
    P.emit()
    return P


def phase_load(S):
    P = S.P
    with P.phase() as st:
        tin = [P.sb("ld_in", [128, D], F32, st) for _ in range(3)]
        for tt in range(NTL):
            src = S.ctx[tt * 128:(tt + 1) * 128, :] if tt < 2 else S.x[(tt - 2) * 128:(tt - 1) * 128, :]
            ti = tin[tt % 3]
            P.dma("sp", ti[:, :], src)
            for half in range(2):
                pb = S.pb[(tt * 2 + half) % 4]
                for j in range(4):
                    kc = half * 4 + j
                    P.tr(pb[:, j * 128:(j + 1) * 128], ti[:, kc * 128:(kc + 1) * 128], S.identf)
                dst = S.xT[:, half * 4:(half + 1) * 4, tt * 128:(tt + 1) * 128]
                srcp = pb[:, :].rearrange("p (j t) -> p j t", j=4)
                if half == 0:
                    P.copy("dve", dst, srcp)
                else:
                    P.copy("act", dst, srcp)


def phase_mod(S, L):
    P = S.P
    with P.phase() as st:
        cv = P.sb("cv", [128, KC, 2], F32, st)
        cvb = P.sb("cvb", [128, KC, 2], BF16, st)
        bm = P.sb("bm", [128, 2, 48], F32, st)
        wm = [P.sb("wm", [128, KC, 1024], BF16, st) for _ in range(2)]
        P.dma("sp", cv[:, :, :], S.cvec)
        P.dma("sp", bm[:, :, :], S.b_mod.rearrange("l p f -> p l f"))
        P.act(cvb[:, :, :], cv[:, :, :], AF.Silu)
        n = 0
        for l in range(L):
            for i in range(6):
                w = wm[n % 2]
                n += 1
                P.dma("pool", w[:, :, :],
                      S.w_mod[l, :, i * 1024:(i + 1) * 1024].rearrange("(kc p) f -> p kc f", p=128))
                pb = S.pb[4 + (n % 2)]
                for fc in range(8):
                    for kc in range(KC):
                        P.mm(pb[:, fc * 2:fc * 2 + 2], w[:, kc, fc * 128:(fc + 1) * 128], cvb[:, kc, :],
                             start=(kc == 0), stop=(kc == KC - 1))
                P.tt("dve", S.modv[:, l, i * 8:(i + 1) * 8, :],
                     pb[:, 0:16].rearrange("p (f w) -> p f w", w=2),
                     bm[:, l, i * 8:(i + 1) * 8].unsqueeze(2).to_broadcast([128, 8, 2]), ALU.add)
            for wn, (gi, si) in enumerate(((l, 1), (2 + l, 4))):
                dst = S.gs[:, l * 2 + wn, :, :]
                P.ts("dve", dst, S.modv[:, l, si * 8:(si + 1) * 8, :], 1.0, None, ALU.add)
                P.tt("dve", dst, dst, S.ng[:, gi, :].unsqueeze(2).to_broadcast([128, 8, 2]), ALU.mult)


def modulate(S, l, wn, blocks, st, router=None):
    P = S.P
    sq = [P.sb("sq", [128, KC, 512], BF16, st) for _ in range(2)]
    rstd = [P.sb("rstd", [128, 512], F32, st) for _ in range(2)]
    tmp = [P.sb("mtmp", [128, 512], F32, st) for _ in range(3)]
    h32 = [P.sb("mh32", [128, 512], F32, st) for _ in range(2)] if router is not None else None
    si = 0 if wn == 0 else 3
    n = 0
    for bi in blocks:
        t0, tn = TB[bi]
        which = 1 if bi == 0 else 0
        s = sq[bi % 2]
        pb = S.pb[6 + bi % 2]
        for kc in range(KC):
            P.act(s[:, kc, :tn], S.xT[:, kc, t0:t0 + tn], AF.Square)
        for kc in range(KC):
            P.mm(pb[:, :tn], S.onesb, s[:, kc, :tn], start=(kc == 0), stop=(kc == KC - 1))
        r = rstd[bi % 2]
        P.act(r[:, :tn], pb[:, :tn], AF.Sqrt, bias=S.eps_t[:, :], scale=1.0 / D)
        P.recip(r[:, :tn], r[:, :tn])
        for kc in range(KC):
            t = tmp[n % 3]
            n += 1
            P.stt("dve", t[:, :tn], S.xT[:, kc, t0:t0 + tn], S.gs[:, l * 2 + wn, kc, which:which + 1],
                  r[:, :tn], ALU.mult, ALU.mult)
            P.act(S.hT[:, kc, t0:t0 + tn], t[:, :tn], AF.Identity,
                  bias=S.modv[:, l, si * 8 + kc, which:which + 1])
            if router is not None:
                rw, logit = router
                hh = h32[kc % 2]
                P.ts("dve", hh[:, :tn], t[:, :tn], S.modv[:, l, si * 8 + kc, which:which + 1], None, ALU.add)
                pl = S.pb[5]
                for j in range(tn // 128):
                    P.mm(pl[:, j * 8:(j + 1) * 8], hh[:, j * 128:(j + 1) * 128], rw[:, kc, :],
                         start=(kc == 0 and j == 0), stop=(kc == KC - 1), skip_group_check=True)
        if router is not None:
            rw, logit = router
            tile0 = (t0 - NCX) // 128
            P.copy("dve", logit[:, tile0:tile0 + tn // 128, :],
                   S.pb[5][:, 0:(tn // 128) * 8].rearrange("p (j e) -> p j e", e=8))


def phase_final(S):
    P = S.P
    with P.phase() as st:
        sq = [P.sb("fsq", [128, KC, 512], BF16, st) for _ in range(2)]
        rstd = [P.sb("frstd", [128, 512], F32, st) for _ in range(2)]
        yT = [P.sb("fyT", [128, KC, 512], F32, st) for _ in range(2)]
        ot = [P.sb("fot", [128, D], F32, st) for _ in range(3)]
        g32 = S.ng[:, 4, :]
        n = 0
        for bi in range(1, 5):
            t0, tn = TB[bi]
            s = sq[bi % 2]
            pb = S.pb[6 + bi % 2]
            for kc in range(KC):
                P.act(s[:, kc, :], S.xT[:, kc, t0:t0 + tn], AF.Square)
            for kc in range(KC):
                P.mm(pb[:, :], S.onesb, s[:, kc, :], start=(kc == 0), stop=(kc == KC - 1))
            r = rstd[bi % 2]
            P.act(r[:, :], pb[:, :], AF.Sqrt, bias=S.eps_t[:, :], scale=1.0 / D)
            P.recip(r[:, :], r[:, :])
            y = yT[bi % 2]
            for kc in range(KC):
                P.stt("dve", y[:, kc, :], S.xT[:, kc, t0:t0 + tn], g32[:, kc:kc + 1],
                      r[:, :], ALU.mult, ALU.mult)
            for q in range(4):
                o = ot[n % 3]
                n += 1
                for half in range(2):
                    pt = S.pb[(n * 2 + half) % 4]
                    for j in range(4):
                        kc = half * 4 + j
                        P.tr(pt[:, j * 128:(j + 1) * 128], y[:, kc, q * 128:(q + 1) * 128], S.identf)
                    if half == 0:
                        P.copy("dve", o[:, 0:512], pt[:, :])
                    else:
                        P.copy("act", o[:, 512:1024], pt[:, :])
                r0 = t0 - NCX + q * 128
                P.dma("sp", S.out[r0:r0 + 128, :], o[:, :])

import math


def phase_mixer(S, l):
    P = S.P
    ctx_out = l < 1
    with P.phase() as st:
        modulate(S, l, 0, range(5), st)
    if S.cfg.get("dump_h") == l:
        P.dump("hT", S.hT[:, :, :])
    br = S.cfg.get("branches", "abc")
    if "c" in br:
        branch_c(S, l, ctx_out)
    if "b" in br:
        branch_b(S, l, ctx_out)
    if "a" in br:
        branch_a(S, l, ctx_out)
    if S.cfg.get("dump_xmix") == l:
        P.dump("xT", S.xT[:, :, :])


def merge_branch(S, l, yT, wo_dram, gcol, ctx_out, st):
    P = S.P
    wg = P.sb("wg", [128, KC, 1024], BF16, st)
    wo = P.sb("wo", [128, 4, 1024], BF16, st)
    wout = P.sb("wout", [128, KC, 1024], BF16, st)
    P.dma("pool", wg[:, :, :], S.w_in[l, :, gcol:gcol + 1024].rearrange("(kc p) f -> p kc f", p=128))
    P.dma("pool", wo[:, :, :], wo_dram[l].rearrange("(c p) f -> p c f", p=128))
    P.dma("pool", wout[:, :, :], S.w_out[l].rearrange("(kc p) f -> p kc f", p=128))
    gmb = [P.sb("gm", [128, KC, 512], BF16, st) for _ in range(2)]
    sgb = [P.sb("sg", [128, 512], F32, st) for _ in range(2)]
    for bi in (range(5) if ctx_out else range(1, 5)):
        t0, tn = TB[bi]
        which = 1 if bi == 0 else 0
        gm = gmb[bi % 2]
        for fc in range(8):
            pg = S.pb[0 + fc % 2]
            py = S.pb[2 + fc % 2]
            for kc in range(KC):
                P.mm(pg[:, :tn], wg[:, kc, fc * 128:(fc + 1) * 128], S.hT[:, kc, t0:t0 + tn],
                     start=(kc == 0), stop=(kc == KC - 1))
            for c in range(4):
                P.mm(py[:, :tn], wo[:, c, fc * 128:(fc + 1) * 128], yT[:, c, t0:t0 + tn],
                     start=(c == 0), stop=(c == 3))
            sg = sgb[fc % 2]
            P.act(sg[:, :tn], pg[:, :tn], AF.Sigmoid)
            P.tt("dve", gm[:, fc, :tn], py[:, :tn], sg[:, :tn], ALU.mult)
        for fo in range(8):
            po = S.pb[4 + fo % 2]
            for fc in range(8):
                P.mm(po[:, :tn], wout[:, fc, fo * 128:(fo + 1) * 128], gm[:, fc, :tn],
                     start=(fc == 0), stop=(fc == 7))
            P.stt("dve", S.xT[:, fo, t0:t0 + tn], po[:, :tn], S.modv[:, l, 16 + fo, which:which + 1],
                  S.xT[:, fo, t0:t0 + tn], ALU.mult, ALU.add)


def branch_c(S, l, ctx_out):
    P = S.P
    lam_init = 0.8 - 0.6 * math.exp(-0.3 * l)
    with P.phase() as sto:
        ydT = P.sb("ydT", [128, 4, NT], BF16, sto)
        if not ctx_out:
            P.memset("pool", ydT[:, :, 0:NCX], 0.0)
        with P.phase() as st:
            ropet = P.sb("ropet", [128, 2, NL], BF16, st)
            P.dma("pool", ropet[:, :, :], S.rope.rearrange("c p t -> p c t"))
            rotT = P.sb("rotT", [128, 128], BF16, st)
            P.copy("dve", rotT[:, :], S.cf[:, 128:256])
            gnbc = P.sb("gnbc", [128, 128], F32, st)
            P.dma("sp", gnbc[:, :], S.gn[l, 2].partition_broadcast(128))
            P.ts("dve", gnbc[:, :], gnbc[:, :], 1.0 - lam_init, None, ALU.mult)
            lam = P.sb("lam", [128, 4, 64], F32, st)
            P.dma("sp", lam[:, :, :], S.lam_c[l].partition_broadcast(128))
            lp = P.sb("lp", [128, 2, 64], F32, st)
            P.tt("dve", lp[:, 0, :], lam[:, 0, :], lam[:, 1, :], ALU.mult)
            P.tt("dve", lp[:, 1, :], lam[:, 2, :], lam[:, 3, :], ALU.mult)
            ls = P.sb("ls", [128, 2], F32, st)
            P.reduce("dve", ls[:, :], lp[:, :, :], ALU.add, AX.X)
            le = P.sb("le", [128, 2], F32, st)
            P.act(le[:, :], ls[:, :], AF.Exp)
            neglam = P.sb("neglam", [128, 1], F32, st)
            P.tt("dve", neglam[:, :], le[:, 1:2], le[:, 0:1], ALU.subtract)
            P.ts("dve", neglam[:, :], neglam[:, :], -lam_init, None, ALU.add)

            wC = P.sb("wC", [128, KC, 1536], BF16, st)
            P.dma("pool", wC[:, :, :], S.w_in[l, :, C_Q:C_Q + 1536].rearrange("(kc p) f -> p kc f", p=128))
            qTh = P.sb("qTh", [128, NT], BF16, st)
            kTh = P.sb("kTh", [128, NT], BF16, st)
            vaug = P.sb("vaug", [128, NTL, 130], BF16, st)
            P.memset("pool", vaug[:, :, 128:130], 1.0)
            xqb = [P.sb("xq", [128, 512], BF16, st) for _ in range(2)]
            t1b = [P.sb("t1", [128, 512], F32, st) for _ in range(2)]
            t2b = [P.sb("t2", [128, 512], F32, st) for _ in range(2)]
            NR = 6
            pring = [P.sb("pT", [128, 512], BF16, st) for _ in range(NR)]
            sm = [P.sb("sm", [128, 4], F32, st) for _ in range(4)]
            tab = [P.sb("ta", [128, 128], F32, st) for _ in range(2)]
            odb = [P.sb("od", [128, 128], F32, st) for _ in range(2)]
            junk = P.sb("junk", [128, 128], BF16, st)
            ytb = [P.sb("yt", [128, 128], BF16, st) for _ in range(2)]
            ptb = S.pb[7][:, 0:256].bitcast(BF16)
            cnt = 0
            ring = 0
            ep = 0
            for h in range(4):
                for dstT, col in ((qTh, h * 128), (kTh, 512 + h * 128)):
                    for bi in range(5):
                        t0, tn = TB[bi]
                        pb = S.pb[6]
                        for kc in range(KC):
                            P.mm(pb[:, :tn], wC[:, kc, col:col + 128], S.hT[:, kc, t0:t0 + tn],
                                 start=(kc == 0), stop=(kc == KC - 1))
                        if bi == 0:
                            P.act(dstT[:, 0:tn], pb[:, :tn], AF.Copy)
                        else:
                            xq = xqb[cnt % 2]; t1 = t1b[cnt % 2]; t2 = t2b[cnt % 2]
                            cnt += 1
                            P.act(xq[:, :], pb[:, :], AF.Copy)
                            pr = S.pb[7]
                            P.mm(pr[:, :], rotT[:, :], xq[:, :])
                            lt0 = t0 - NCX
                            P.tt("dve", t1[:, :], pr[:, :], ropet[:, 1, lt0:lt0 + 512], ALU.mult)
                            P.tt("pool", t2[:, :], xq[:, :], ropet[:, 0, lt0:lt0 + 512], ALU.mult)
                            P.tt("dve", dstT[:, t0:t0 + 512], t1[:, :], t2[:, :], ALU.add)
                vc = 1024 + h * 128
                for g0 in range(0, NTL, 4):
                    ng_ = min(4, NTL - g0)
                    pb = S.pb[6]
                    for j in range(ng_):
                        tt = g0 + j
                        for kc in range(KC):
                            P.mm(pb[:, j * 128:(j + 1) * 128], S.hT[:, kc, tt * 128:(tt + 1) * 128],
                                 wC[:, kc, vc:vc + 128], start=(kc == 0), stop=(kc == KC - 1))
                    P.act(vaug[:, g0:g0 + ng_, 0:128],
                          pb[:, 0:ng_ * 128].rearrange("p (j t) -> p j t", j=ng_), AF.Copy)
                for qb in ([0] if ctx_out else []) + [1, 2, 3, 4]:
                    q0, qn = TB[qb]
                    nq = qn // 128
                    seq = list(range(2)) if qb == 0 else list(range(NTL))

                    def acc(j, m):
                        return S.pb[2 + m * 2 + j // 2][:, (j % 2) * 256:(j % 2) * 256 + 129]

                    for m in range(2):
                        def qk(kt):
                            nonlocal ring
                            ps = S.pb[kt % 2]
                            P.mm(ps[:, :qn], kTh[m * 64:(m + 1) * 64, kt * 128:(kt + 1) * 128],
                                 qTh[m * 64:(m + 1) * 64, q0:q0 + qn])
                            pt = pring[ring % NR]
                            ring += 1
                            P.act(pt[:, :qn], ps[:, :qn], AF.Exp, scale=0.125)
                            return pt
                        LA = 3
                        pts = {}
                        for i, kt in enumerate(seq):
                            if i == 0:
                                for j in range(min(LA, len(seq))):
                                    pts[seq[j]] = qk(seq[j])
                            if i + LA < len(seq):
                                pts[seq[i + LA]] = qk(seq[i + LA])
                            pt = pts.pop(kt)
                            for j in range(nq):
                                P.mm(acc(j, m), pt[:, j * 128:(j + 1) * 128], vaug[:, kt, 0:129],
                                     start=(i == 0 and j % 2 == 0), stop=(i == len(seq) - 1),
                                     skip_group_check=True)
                    for j in range(nq):
                        o0 = acc(j, 0); o1 = acc(j, 1)
                        s = sm[ep % 4]; ta = tab[ep % 2]; od = odb[ep % 2]; yt = ytb[ep % 2]
                        ep += 1
                        P.recip(s[:, 0:1], o0[:, 128:129])
                        P.recip(s[:, 1:2], o1[:, 128:129])
                        P.tt("dve", s[:, 1:2], s[:, 1:2], neglam[:, :], ALU.mult)
                        P.ts("dve", ta[:, :], o0[:, 0:128], s[:, 0:1], None, ALU.mult)
                        P.stt("dve", od[:, :], o1[:, 0:128], s[:, 1:2], ta[:, :], ALU.mult, ALU.add)
                        P.memset("pool", s[:, 2:3], 0.0)
                        P.act(junk[:, :], od[:, :], AF.Square, accum_out=s[:, 2:3])
                        P.act(s[:, 3:4], s[:, 2:3], AF.Sqrt, bias=S.eps_t[:, :], scale=1.0 / 128)
                        P.recip(s[:, 3:4], s[:, 3:4])
                        P.stt("dve", yt[:, :], od[:, :], s[:, 3:4], gnbc[:, :], ALU.mult, ALU.mult)
                        P.tr(ptb[:, j * 128:(j + 1) * 128], yt[:, :], S.identb)
                    P.copy("act", ydT[:, h, q0:q0 + qn], ptb[:, :qn])
        if S.cfg.get("dump_yd") == l:
            P.dump("ydT", ydT[:, :, :])
        if S.cfg.get("merge", True):
            with P.phase() as st:
                merge_branch(S, l, ydT, S.w_o_c, G_D, ctx_out, st)


def branch_b(S, l, ctx_out):
    P = S.P
    U = S.cf[:, 256:384]
    Lm = S.cf[:, 384:512]
    SU = S.cf[:, 512:640]
    SL = S.cf[:, 640:768]
    with P.phase() as sto:
        ybT = P.sb("ybT", [128, 4, NT], BF16, sto)
        if not ctx_out:
            P.memset("pool", ybT[:, :, 0:NCX], 0.0)
        with P.phase() as stc:
            gnbc = P.sb("gnbcb", [128, 128], F32, stc)
            P.dma("sp", gnbc[:, :], S.gn[l, 1].partition_broadcast(128))
            wg2 = P.sb("wg2", [17, 2, 256], BF16, stc)
            if S.cfg.get("t_wg2", 1):
                P.dma("pool", wg2[:, :, :], S.wg2[l])
            one_t = P.sb("one_t", [128, 1], F32, stc)
            P.memset("dve", one_t[:, :], 1.0)
            mkb = P.sb("mkb", [128, 512], BF16, stc)
            P.copy("dve", mkb[:, :], S.cf[:, 256:768])
            Ub = mkb[:, 0:128]; Lb = mkb[:, 128:256]; SUb = mkb[:, 256:384]; SLb = mkb[:, 384:512]
            for hp in range(2):
                with P.phase() as st:
                    qT = P.sb("bqT", [128, NT], BF16, st)
                    kT = P.sb("bkT", [128, NT], BF16, st)
                    ktok = P.sb("bktok", [128, NTL, 128], BF16, st)
                    vtok = P.sb("bvtok", [128, NTL, 256], BF16, st)
                    srtok = P.sb("bsrtok", [128, NTL, 256], BF16, st)
                    glrT = P.sb("bglrT", [17, 2, NT], BF16, st)
                    Sst = P.sb("bSst", [128, 2, NTL, 128], BF16, st)
                    Sf = [P.sb("bSf", [128, 128], F32, st) for _ in range(2)]
                    if S.cfg.get("t_ms", 1):
                        P.memset("pool", glrT[:, :, :], 1.0)
                    P.memset("dve", Sf[0][:, :], 0.0)
                    P.memset("dve", Sf[1][:, :], 0.0)
                    with P.phase() as stw:
                        wB = P.sb("wBp", [128, KC, 800], BF16, stw)
                        for (dst0, n_, src0) in ((0, 128, B_Q + hp * 128), (128, 128, B_K + hp * 128),
                                                 (256, 256, B_V + hp * 256), (512, 256, B_R + hp * 256),
                                                 (768, 32, B_GL)):
                            P.dma("pool", wB[:, :, dst0:dst0 + n_],
                                  S.w_in[l, :, src0:src0 + n_].rearrange("(kc p) f -> p kc f", p=128))
                        for bi in (range(5) if S.cfg.get("b_proj", 9) >= 1 else []):
                            t0, tn = TB[bi]
                            for dstT, col in ((qT, 0), (kT, 128)):
                                pb = S.pb[4 + (col // 128)]
                                for kc in range(KC):
                                    P.mm(pb[:, :tn], wB[:, kc, col:col + 128], S.hT[:, kc, t0:t0 + tn],
                                         start=(kc == 0), stop=(kc == KC - 1))
                                P.act(dstT[:, t0:t0 + tn], pb[:, :tn], AF.Copy)
                            for d in (range(2) if S.cfg.get("t_glr", 1) else []):
                                pb = S.pb[6 + d]
                                for kc in range(KC):
                                    P.mm(pb[0:16, :tn], wB[:, kc, 768 + d * 16:768 + (d + 1) * 16],
                                         S.hT[:, kc, t0:t0 + tn], start=(kc == 0), stop=(kc == KC - 1))
                                P.copy("dve", glrT[0:16, d, t0:t0 + tn], pb[0:16, :tn])
                        for n in (range(NTL) if S.cfg.get("b_proj", 9) >= 2 else []):
                            pa = S.pb[0 + n % 2]
                            pr = S.pb[2 + n % 2]
                            for kc in range(KC):
                                P.mm(pa[:, 0:384], S.hT[:, kc, n * 128:(n + 1) * 128], wB[:, kc, 128:512],
                                     start=(kc == 0), stop=(kc == KC - 1))
                            for kc in range(KC):
                                P.mm(pr[:, 0:256], S.hT[:, kc, n * 128:(n + 1) * 128], wB[:, kc, 512:768],
                                     start=(kc == 0), stop=(kc == KC - 1))
                            P.copy("dve", ktok[:, n, :], pa[:, 0:128])
                            P.act(vtok[:, n, :], pa[:, 128:384], AF.Copy)
                            P.act(srtok[:, n, :], pr[:, 0:256], AF.Silu)

                    e1b = [P.sb("be1", [128, 128], F32, st) for _ in range(2)]
                    spb = [P.sb("bsp", [128, 128], F32, st) for _ in range(2)]
                    eremb = [P.sb("berem", [128, 128], F32, st) for _ in range(2)]
                    kgb = [P.sb("bkg", [128, 128], BF16, st) for _ in range(2)]
                    sphl = [P.sb("bsphl", [128, 2, 128], BF16, st) for _ in range(2)]
                    glb = [P.sb("bgl", [128, 1], F32, st) for _ in range(4)]
                    cnt = 0

                    def gate_sp(d, n):
                        nonlocal cnt
                        px = S.pb[0 + cnt % 2]
                        e1 = e1b[cnt % 2]; sp = spb[cnt % 2]
                        cnt += 1
                        P.mm(px[:, 0:128], glrT[0:17, d, n * 128:(n + 1) * 128],
                             wg2[0:17, d, hp * 128:(hp + 1) * 128])
                        P.act(e1[:, :], px[:, 0:128], AF.Exp, scale=-1.0)
                        P.act(sp[:, :], e1[:, :], AF.Ln, bias=one_t[:, :])
                        hl = sphl[(cnt - 1) % 2]
                        P.copy("dve", hl[:, 0, :], sp[:, :])
                        P.tt("dve", hl[:, 1, :], sp[:, :], hl[:, 0, :], ALU.subtract)
                        return hl

                    if S.cfg.get("b_stop", 9) < 1:
                        continue
                    order = [list(range(NTL)), [1, 0] + list(range(NTL - 1, 1, -1))]
                    for s in range(NTL):
                        for d in range(2):
                            n = order[d][s]
                            sp = gate_sp(d, n)
                            k_ = cnt
                            prm = S.pb[2 + d]
                            for z in range(2):
                                P.mm(prm[:, 0:128], SLb if d == 0 else SUb, sp[:, z, :], start=(z == 0), stop=(z == 1))
                            for z in range(2):
                                P.mm(prm[:, 128:129], sp[:, z, :], S.onesb[:, 0:1], start=(z == 0), stop=(z == 1))
                            erem = eremb[d]; kg = kgb[d]; gl = glb[(s * 2 + d) % 4]
                            P.act(erem[:, :], prm[:, 0:128], AF.Exp, scale=-1.0 / 16)
                            P.act(gl[:, :], prm[:, 128:129], AF.Exp, scale=-1.0 / 16)
                            P.tt("dve", kg[:, :], ktok[:, n, :], erem[:, :], ALU.mult)
                            P.copy("act", Sst[:, d, n, :], Sf[d][:, :])
                            if s == NTL - 1:
                                continue
                            pS = S.pb[4 + d]
                            P.mm(pS[:, 0:256], kg[:, :], vtok[:, n, :])
                            for hh in range(2):
                                sl = slice(hh * 64, (hh + 1) * 64)
                                P.stt("dve", Sf[d][sl, :], Sf[d][sl, :], gl[sl, :], pS[sl, hh * 128:(hh + 1) * 128],
                                      ALU.mult, ALU.add)

                    if S.cfg.get("b_stop", 9) < 2:
                        continue
                    egb = [P.sb("beg", [128, 128], F32, st) for _ in range(2)]
                    eib = [P.sb("bei", [128, 128], F32, st) for _ in range(2)]
                    qtb = [P.sb("bqt", [128, 128], BF16, st) for _ in range(4)]
                    ktb = [P.sb("bkt", [128, 128], BF16, st) for _ in range(4)]
                    atb = [P.sb("bat", [128, 2, 128], BF16, st) for _ in range(4)]
                    smb = [P.sb("bsm", [128, 2], F32, st) for _ in range(4)]
                    junk = P.sb("bjunk", [128, 128], BF16, st)
                    y1b = [P.sb("by1", [128, 128], F32, st) for _ in range(2)]
                    ytb = [P.sb("byt", [128, 128], BF16, st) for _ in range(2)]
                    ptb = S.pb[7][:, 0:256].bitcast(BF16)
                    k2 = 0
                    ep = 0
                    for n in (range(NTL) if ctx_out else range(2, NTL)):
                        dd = []
                        for d in range(2):
                            sp = gate_sp(d, n)
                            pg = S.pb[2]
                            for z in range(2):
                                P.mm(pg[:, 0:128], sp[:, z, :], Ub if d == 0 else Lb, start=(z == 0), stop=(z == 1))
                            eg = egb[d]; ei = eib[d]
                            qt = qtb[k2 % 4]; kt = ktb[k2 % 4]; at = atb[k2 % 4]
                            k2 += 1
                            P.act(eg[:, :], pg[:, 0:128], AF.Exp, scale=-1.0 / 16)
                            P.act(ei[:, :], pg[:, 0:128], AF.Exp, scale=1.0 / 16)
                            P.stt("dve", qt[:, :], qT[:, n * 128:(n + 1) * 128], 0.125, eg[:, :], ALU.mult, ALU.mult)
                            P.tt("pool", kt[:, :], kT[:, n * 128:(n + 1) * 128], ei[:, :], ALU.mult)
                            if S.cfg.get("p2", 9) < 1:
                                continue
                            for hh in range(2):
                                sl = slice(hh * 64, (hh + 1) * 64)
                                P.mm(S.pb[3 + hh][:, 0:128], kt[sl, :], qt[sl, :])
                            mask = (U if d == 0 else Lm)
                            for hh in range(2):
                                P.tt("dve", at[:, hh, :], S.pb[3 + hh][:, 0:128], mask, ALU.mult)
                            dd.append((qt, at))
                        if S.cfg.get("p2", 9) < 2:
                            continue
                        for hh in range(2):
                            sl = slice(hh * 64, (hh + 1) * 64)
                            oo = S.pb[5 + hh][:, 0:128]
                            for d in range(2):
                                qt, at = dd[d]
                                P.mm(oo, qt[sl, :], Sst[sl, d, n, :], start=(d == 0), stop=False)
                                P.mm(oo, at[:, hh, :], vtok[:, n, hh * 128:(hh + 1) * 128], start=False, stop=(d == 1))
                        if S.cfg.get("p2", 9) < 3:
                            continue
                        for hh in range(2):
                            oo = S.pb[5 + hh][:, 0:128]
                            s_ = smb[ep % 4]; y1 = y1b[ep % 2]; yt = ytb[ep % 2]
                            ep += 1
                            P.memset("pool", s_[:, 0:1], 0.0)
                            P.act(junk[:, :], oo, AF.Square, accum_out=s_[:, 0:1])
                            P.act(s_[:, 1:2], s_[:, 0:1], AF.Sqrt, bias=S.eps_t[:, :], scale=1.0 / 128)
                            P.recip(s_[:, 1:2], s_[:, 1:2])
                            P.stt("dve", y1[:, :], oo, s_[:, 1:2], gnbc[:, :], ALU.mult, ALU.mult)
                            P.tt("pool", yt[:, :], y1[:, :], srtok[:, n, hh * 128:(hh + 1) * 128], ALU.mult)
                            P.tr(ptb[:, hh * 128:(hh + 1) * 128], yt[:, :], S.identb)
                        P.copy("act", ybT[:, hp * 2:hp * 2 + 2, n * 128:(n + 1) * 128],
                               ptb[:, 0:256].rearrange("p (h t) -> p h t", h=2))
        if S.cfg.get("dump_yb") == l:
            P.dump("ybT", ybT[:, :, :])
        if S.cfg.get("merge", True):
            with P.phase() as st:
                merge_branch(S, l, ybT, S.w_o_b, G_B, ctx_out, st)


def branch_a(S, l, ctx_out):
    P = S.P
    U = S.cf[:, 256:384]
    Lm = S.cf[:, 384:512]
    SU = S.cf[:, 512:640]
    SL = S.cf[:, 640:768]
    with P.phase() as sto:
        yaT = P.sb("yaT", [128, 4, NT], BF16, sto)
        if not ctx_out:
            P.memset("pool", yaT[:, :, 0:NCX], 0.0)
        with P.phase() as stc:
            gnbc = P.sb("gnbca", [128, 128], F32, stc)
            P.dma("sp", gnbc[:, :], S.gn[l, 0].partition_broadcast(128))
            one_t = P.sb("one_ta", [128, 1], F32, stc)
            P.memset("dve", one_t[:, :], 1.0)
            mkb = P.sb("mkba", [128, 4, 128], BF16, stc)
            P.copy("dve", mkb[:, :, :], S.cf[:, 256:768].rearrange("p (m c) -> p m c", m=4))
            ones_col = P.sb("onescol", [128, 1], BF16, stc)
            P.memset("dve", ones_col[:, :], 1.0)
            cum1 = P.sb("cum1", [128, 2, 129], BF16, stc)
            P.memset("dve", cum1[:, :, :], 1.0)
            P.copy("dve", cum1[:, 0, 0:128], U)
            P.copy("dve", cum1[:, 1, 0:128], Lm)
            cumb = [mkb[:, 0, :], mkb[:, 1, :]]
            gmf = [SL, SU]
            strict = [SL, SU]
            maskT = [U, Lm]
            cw = P.sb("convw", [128, 12, 3], F32, stc)
            P.dma("sp", cw[:, :, :], S.convw[l])
            adt = P.sb("adt", [128, 2, 8], F32, stc)
            P.dma("sp", adt[:, :, :], S.adt[l].partition_broadcast(128))
            nA = P.sb("nA", [128, 8], F32, stc)
            P.act(nA[:, :], adt[:, 0, :], AF.Exp)
            P.ts("dve", nA[:, :], nA[:, :], -1.0, None, ALU.mult)
            betat = P.sb("betat", [128, NTL, 8], F32, stc)
            nbetat = P.sb("nbetat", [128, NTL, 8], F32, stc)
            gt = P.sb("gt", [128, NTL, 8], F32, stc)
            with P.phase() as stw:
                wbg = P.sb("wbg", [128, KC, 16], BF16, stw)
                P.dma("pool", wbg[:, :, :], S.w_in[l, :, A_B:A_B + 16].rearrange("(kc p) f -> p kc f", p=128))
                tmpe = P.sb("tmpe", [128, NTL, 8], F32, stw)
                for n in range(NTL):
                    pb = S.pb[n % 2]
                    for kc in range(KC):
                        P.mm(pb[:, 0:16], S.hT[:, kc, n * 128:(n + 1) * 128], wbg[:, kc, :],
                             start=(kc == 0), stop=(kc == KC - 1))
                    P.act(betat[:, n, :], pb[:, 0:8], AF.Sigmoid)
                    P.tt("dve", gt[:, n, :], pb[:, 8:16], adt[:, 1, :], ALU.add)
                P.act(tmpe[:, :, :], gt[:, :, :], AF.Exp)
                P.act(gt[:, :, :], tmpe[:, :, :], AF.Ln, bias=one_t[:, :])
                P.tt("dve", gt[:, :, :], gt[:, :, :], nA[:, :].unsqueeze(1).to_broadcast([128, NTL, 8]), ALU.mult)
                P.ts("dve", nbetat[:, :, :], betat[:, :, :], -1.0, None, ALU.mult)

            for h in range(4):
                with P.phase() as st:
                    qT = P.sb("aqT", [128, NT], BF16, st)
                    kT = P.sb("akT", [128, NT], BF16, st)
                    ktok = P.sb("aktok", [128, NTL, 128], BF16, st)
                    vtok = P.sb("avtok", [128, NTL, 128], BF16, st)
                    sztok = P.sb("asztok", [128, NTL, 128], BF16, st)
                    oacc = P.sb("aoacc", [128, NTL, 128], F32, st)
                    P.memset("pool", oacc[:, :, :], 0.0)
                    ptb = S.pb[7][:, 0:256].bitcast(BF16)
                    with P.phase() as stw:
                        wA = P.sb("wAh", [128, KC, 512], BF16, stw)
                        for i, c0 in enumerate((A_Q, A_K, A_V, A_Z)):
                            P.dma("pool", wA[:, :, i * 128:(i + 1) * 128],
                                  S.w_in[l, :, c0 + h * 128:c0 + (h + 1) * 128].rearrange("(kc p) f -> p kc f", p=128))
                        pre = [P.sb("apre", [128, NT], F32, stw) for _ in range(1)]
                        cv = [P.sb("acv", [128, NT], F32, stw) for _ in range(1)]
                        sqb = P.sb("asq", [128, 512], BF16, stw)
                        rs = P.sb("ars", [128, 512], F32, stw)
                        vT = P.sb("avT", [128, NT], BF16, stw)
                        for i in range(3):
                            pr = pre[0]; c = cv[0]
                            for bi in range(5):
                                t0, tn = TB[bi]
                                pb = S.pb[bi % 2]
                                for kc in range(KC):
                                    P.mm(pb[:, :tn], wA[:, kc, i * 128:(i + 1) * 128], S.hT[:, kc, t0:t0 + tn],
                                         start=(kc == 0), stop=(kc == KC - 1))
                                P.act(pr[:, t0:t0 + tn], pb[:, :tn], AF.Copy)
                            ch = i * 4 + h
                            P.ts("dve", c[:, :], pr[:, :], cw[:, ch, 1:2], None, ALU.mult)
                            for (a, b) in ((0, NCX), (NCX, NT)):
                                P.stt("dve", c[:, a + 1:b], pr[:, a:b - 1], cw[:, ch, 0:1], c[:, a + 1:b], ALU.mult, ALU.add)
                                P.stt("dve", c[:, a:b - 1], pr[:, a + 1:b], cw[:, ch, 2:3], c[:, a:b - 1], ALU.mult, ALU.add)
                            if i == 2:
                                P.act(vT[:, :], c[:, :], AF.Silu)
                            else:
                                dst = qT if i == 0 else kT
                                P.act(c[:, :], c[:, :], AF.Silu)
                                for bi in range(5):
                                    t0, tn = TB[bi]
                                    pb = S.pb[2 + bi % 2]
                                    P.act(sqb[:, :tn], c[:, t0:t0 + tn], AF.Square)
                                    P.mm(pb[:, :tn], S.onesb, sqb[:, :tn])
                                    P.act(rs[:, :tn], pb[:, :tn], AF.Sqrt, bias=S.eps_t[:, :])
                                    P.recip(rs[:, :tn], rs[:, :tn])
                                    if i == 0:
                                        P.stt("dve", dst[:, t0:t0 + tn], c[:, t0:t0 + tn], 128.0 ** -0.5, rs[:, :tn],
                                              ALU.mult, ALU.mult)
                                    else:
                                        P.tt("dve", dst[:, t0:t0 + tn], c[:, t0:t0 + tn], rs[:, :tn], ALU.mult)
                        for n in range(NTL):
                            P.tr(ptb[:, 0:128], kT[:, n * 128:(n + 1) * 128], S.identb)
                            P.tr(ptb[:, 128:256], vT[:, n * 128:(n + 1) * 128], S.identb)
                            P.copy("dve", ktok[:, n, :], ptb[:, 0:128])
                            P.copy("dve", vtok[:, n, :], ptb[:, 128:256])
                            pz = S.pb[n % 2]
                            for kc in range(KC):
                                P.mm(pz[:, 0:128], S.hT[:, kc, n * 128:(n + 1) * 128], wA[:, kc, 384:512],
                                     start=(kc == 0), stop=(kc == KC - 1))
                            P.act(sztok[:, n, :], pz[:, 0:128], AF.Silu)

                    G = S.cfg.get("a_G", 4)
                    NRG = 4
                    def ring(nm, shape, dt):
                        return [[P.sb(nm, shape, dt, st) for _ in range(NRG)] for _ in range(2)]
                    u_r = ring("au", [128, 128], F32)
                    wT_r = ring("awT", [128, 128], BF16)
                    kg_r = ring("akg", [128, 128], BF16)
                    AT_r = ring("aAT", [128, 128], BF16)
                    sc_r = ring("asc", [128, 4], F32)

                    class WS:
                        pass
                    wss = []
                    for gi in range(G):
                        w_ = WS()
                        w_.gf = P.sb("agmf", [128, 129], F32, st)
                        w_.ghl = P.sb("agmhl", [128, 2, 129], BF16, st)
                        w_.e1 = P.sb("aE1", [128, 129], F32, st)
                        w_.e2 = P.sb("aE2", [128, 129], F32, st)
                        w_.XX = [P.sb("aXX", [128, 2, 128], F32, st) for _ in range(2)]
                        w_.PT = [P.sb("aPT", [128, 128], F32, st) for _ in range(2)]
                        w_.vb = P.sb("avb", [128, 128], BF16, st)
                        w_.kbg = P.sb("akbg", [128, 128], BF16, st)
                        w_.TT = P.sb("aTTb", [128, 128], BF16, st)
                        w_.pc = S.pb[2 + gi]
                        w_.pP = S.pb[6 + gi % 2]
                        w_.ev = "act" if gi % 2 == 0 else "dve"
                        wss.append(w_)
                    vnew = [P.sb("avnew", [128, 128], BF16, st) for _ in range(2)]
                    o1s = [P.sb("ao1s", [128, 128], F32, st) for _ in range(2)]
                    ot = [P.sb("aot", [128, 128], F32, st) for _ in range(2)]
                    Sf = [P.sb("aSf", [128, 128], F32, st) for _ in range(2)]
                    Sb = [P.sb("aSb", [128, 128], BF16, st) for _ in range(2)]
                    for d in range(2):
                        P.memset("dve", Sf[d][:, :], 0.0)
                        P.memset("dve", Sb[d][:, :], 0.0)
                    order = [list(range(NTL)), [1, 0] + list(range(NTL - 1, 1, -1))]

                    def precompute(d, n, slot, w_):
                        col = d * 4 + h
                        g = gt[:, n, col:col + 1]
                        beta = betat[:, n, col:col + 1]
                        nbeta = nbetat[:, n, col:col + 1]
                        tl = slice(n * 128, (n + 1) * 128)
                        sc = sc_r[d][slot]
                        pc = w_.pc
                        gf = w_.gf; ghl = w_.ghl; e1 = w_.e1; e2 = w_.e2
                        P.ts("dve", gf[:, 0:128], gmf[d], g, None, ALU.mult)
                        P.copy("dve", gf[:, 128:129], g)
                        P.copy("dve", ghl[:, 0, :], gf[:, :])
                        P.tt("dve", ghl[:, 1, :], gf[:, :], ghl[:, 0, :], ALU.subtract)
                        for z in range(2):
                            P.mm(pc[:, 0:129], cumb[d], ghl[:, z, :], start=(z == 0), stop=(z == 1))
                        for z in range(2):
                            P.mm(pc[:, 256:385], ghl[:, z, 0:128], cum1[:, d, :], start=(z == 0), stop=(z == 1))
                        P.act(e1[:, :], pc[:, 0:129], AF.Exp)
                        P.act(e2[:, :], pc[:, 256:385], AF.Exp)
                        yield
                        P.tt("pool", e1[:, 0:128], e1[:, 0:128], strict[d], ALU.mult)
                        P.tt("pool", e2[:, 0:128], e2[:, 0:128], maskT[d], ALU.mult)
                        P.tt("dve", sc[:, 2:3], e1[:, 128:129], e2[:, 128:129], ALU.mult)
                        P.tt("dve", sc[:, 3:4], e1[:, 128:129], beta, ALU.mult)
                        P.copy("dve", sc[:, 0:1], e1[:, 128:129])
                        P.mm(pc[:, 0:128], kT[:, tl], kT[:, tl])
                        P.mm(pc[:, 128:256], kT[:, tl], qT[:, tl])
                        XX = w_.XX; PT = w_.PT
                        P.stt("dve", XX[0][:, 0, :], pc[:, 0:128], nbeta, e1[:, 0:128], ALU.mult, ALU.mult)
                        P.tt("dve", AT_r[d][slot][:, :], pc[:, 128:256], e2[:, 0:128], ALU.mult)
                        P.ts("dve", kg_r[d][slot][:, :], ktok[:, n, :], e2[:, 128:129], None, ALU.mult)
                        P.ts("dve", w_.vb[:, :], vtok[:, n, :], beta, None, ALU.mult)
                        P.ts("dve", w_.kbg[:, :], ktok[:, n, :], sc[:, 3:4], None, ALU.mult)
                        yield
                        P.tr(pc[:, 384:512], XX[0][:, 0, :], S.identf)
                        P.copy(w_.ev, XX[0][:, 1, :], pc[:, 384:512])
                        P.tt("dve", PT[0][:, :], pc[:, 384:512], S.identf, ALU.add)
                        yield
                        cur = 0
                        for lev in range(1, 7):
                            nxt = 1 - cur
                            if lev > 1:
                                P.mm(w_.pP[:, 0:128], XX[cur][:, 0, :], PT[lev % 2][:, :])
                                P.tt("dve", PT[1 - (lev % 2)][:, :], w_.pP[:, 0:128], PT[lev % 2][:, :], ALU.add)
                            P.mm(pc[:, 0:128], XX[cur][:, 1, :], XX[cur][:, 0, :])
                            if lev < 6:
                                P.mm(pc[:, 128:256], XX[cur][:, 0, :], XX[cur][:, 1, :])
                                P.copy(w_.ev, XX[nxt][:, :, :], pc[:, 0:256].rearrange("p (a b) -> p a b", a=2))
                            else:
                                P.copy(w_.ev, XX[nxt][:, 0, :], pc[:, 0:128])
                            cur = nxt
                            yield
                        P.mm(w_.pP[:, 0:128], XX[cur][:, 0, :], PT[1][:, :])
                        P.tt("dve", PT[0][:, :], w_.pP[:, 0:128], PT[1][:, :], ALU.add)
                        P.copy("act", w_.TT[:, :], PT[0][:, :])
                        yield
                        P.mm(pc[:, 0:128], w_.TT[:, :], w_.vb[:, :])
                        P.mm(pc[:, 128:256], w_.kbg[:, :], w_.TT[:, :])
                        P.copy("act", u_r[d][slot][:, :], pc[:, 0:128])
                        P.copy("act", wT_r[d][slot][:, :], pc[:, 128:256])

                    def recur(d, n, slot, last):
                        tl = slice(n * 128, (n + 1) * 128)
                        pr = S.pb[d]
                        sc = sc_r[d][slot]
                        P.mm(pr[:, 0:128], wT_r[d][slot][:, :], Sb[d][:, :])
                        P.mm(pr[:, 128:256], qT[:, tl], Sb[d][:, :])
                        P.tt("dve", vnew[d][:, :], u_r[d][slot][:, :], pr[:, 0:128], ALU.subtract)
                        want_out = ctx_out or n >= 2
                        yield
                        if want_out:
                            P.mm(pr[:, 256:384], AT_r[d][slot][:, :], vnew[d][:, :])
                        if not last:
                            P.mm(pr[:, 384:512], kg_r[d][slot][:, :], vnew[d][:, :])
                        if want_out:
                            P.act(o1s[d][:, :], pr[:, 128:256], AF.Copy, scale=sc[:, 0:1])
                        if not last:
                            P.stt("dve", Sf[d][:, :], Sf[d][:, :], sc[:, 2:3], pr[:, 384:512], ALU.mult, ALU.add)
                            P.copy("act", Sb[d][:, :], Sf[d][:, :])
                        if want_out:
                            P.tt("dve", ot[d][:, :], pr[:, 256:384], o1s[d][:, :], ALU.add)
                            P.tt("pool", oacc[:, n, :], oacc[:, n, :], ot[d][:, :], ALU.add)
                        yield

                    units = [(d, s) for s in range(NTL) for d in range(2)]
                    pre_done = set()
                    active = []
                    next_unit = 0
                    rec_step = [0, 0]
                    rec_gen = [None, None]
                    free_ws = list(range(G))
                    while True:
                        while free_ws and next_unit < len(units):
                            d_, s_ = units[next_unit]
                            if s_ - rec_step[d_] >= NRG - 1:
                                break
                            wi = free_ws.pop(0)
                            active.append(("pre", (d_, s_, wi), precompute(d_, order[d_][s_], s_ % NRG, wss[wi])))
                            next_unit += 1
                        for d_ in range(2):
                            if rec_gen[d_] is None and rec_step[d_] < NTL and (d_, rec_step[d_]) in pre_done:
                                s_ = rec_step[d_]
                                rec_gen[d_] = recur(d_, order[d_][s_], s_ % NRG, s_ == NTL - 1)
                        if not active and rec_gen[0] is None and rec_gen[1] is None:
                            if next_unit >= len(units) and rec_step[0] >= NTL and rec_step[1] >= NTL:
                                break
                        S.a_hist = getattr(S, "a_hist", [])
                        S.a_hist.append((len(active), rec_gen[0] is not None, rec_gen[1] is not None))
                        for item in list(active):
                            kind, key, gen = item
                            try:
                                next(gen)
                            except StopIteration:
                                active.remove(item)
                                pre_done.add((key[0], key[1]))
                                free_ws.append(key[2])
                        for d_ in range(2):
                            if rec_gen[d_] is not None:
                                try:
                                    next(rec_gen[d_])
                                except StopIteration:
                                    rec_gen[d_] = None
                                    rec_step[d_] += 1

                    smb = [P.sb("asm", [128, 2], F32, st) for _ in range(4)]
                    junk = P.sb("ajunk", [128, 128], BF16, st)
                    y1b = [P.sb("ay1", [128, 128], F32, st) for _ in range(2)]
                    ytb = [P.sb("ayt", [128, 128], BF16, st) for _ in range(2)]
                    ep = 0
                    for n in (range(NTL) if ctx_out else range(2, NTL)):
                        s_ = smb[ep % 4]; y1 = y1b[ep % 2]; yt = ytb[ep % 2]
                        ep += 1
                        P.memset("pool", s_[:, 0:1], 0.0)
                        P.act(junk[:, :], oacc[:, n, :], AF.Square, accum_out=s_[:, 0:1])
                        P.act(s_[:, 1:2], s_[:, 0:1], AF.Sqrt, bias=S.eps_t[:, :], scale=1.0 / 128)
                        P.recip(s_[:, 1:2], s_[:, 1:2])
                        P.stt("dve", y1[:, :], oacc[:, n, :], s_[:, 1:2], gnbc[:, :], ALU.mult, ALU.mult)
                        P.tt("pool", yt[:, :], y1[:, :], sztok[:, n, :], ALU.mult)
                        P.tr(ptb[:, 0:128], yt[:, :], S.identb)
                        P.copy("act", yaT[:, h, n * 128:(n + 1) * 128], ptb[:, 0:128])
        if S.cfg.get("dump_ya") == l:
            P.dump("yaT", yaT[:, :, :])
        if S.cfg.get("merge", True):
            with P.phase() as st:
                merge_branch(S, l, yaT, S.w_o_a, G_A, ctx_out, st)


def phase_ffn(S, l):
    P = S.P
    moe = (l % 2 == 1)
    ctx_out = l < 1
    blocks = list(range(5)) if ctx_out else list(range(1, 5))
    i_ = l // 2
    with P.phase() as sto:
        gate = None
        if moe:
            logit = P.sb("logit", [128, 16, 8], F32, sto)
            gate = P.sb("gate", [128, 16, 8], F32, sto)
        with P.phase() as st:
            router = None
            if moe:
                rw = P.sb("rw", [128, KC, 8], F32, st)
                P.dma("sp", rw[:, :, :], S.router_w[i_].rearrange("(kc p) e -> p kc e", p=128))
                router = (rw, logit)
            modulate(S, l, 1, blocks, st, router=router)
        if moe:
            with P.phase() as st:
                m1 = P.sb("m1", [128, 16], F32, st)
                m2 = P.sb("m2", [128, 16], F32, st)
                eq1 = P.sb("eq1", [128, 16, 8], F32, st)
                eq2 = P.sb("eq2", [128, 16, 8], F32, st)
                l2 = P.sb("l2", [128, 16, 8], F32, st)
                ww = P.sb("ww", [128, 3, 16], F32, st)
                bc = lambda a: a.unsqueeze(2).to_broadcast([128, 16, 8])
                P.reduce("dve", m1[:, :], logit[:, :, :], ALU.max, AX.X)
                P.tt("dve", eq1[:, :, :], logit[:, :, :], bc(m1[:, :]), ALU.is_equal)
                P.stt("dve", l2[:, :, :], eq1[:, :, :], -1e30, logit[:, :, :], ALU.mult, ALU.add)
                P.reduce("dve", m2[:, :], l2[:, :, :], ALU.max, AX.X)
                P.tt("dve", eq2[:, :, :], l2[:, :, :], bc(m2[:, :]), ALU.is_equal)
                P.tt("dve", ww[:, 0, :], m2[:, :], m1[:, :], ALU.subtract)
                P.act(ww[:, 0, :], ww[:, 0, :], AF.Exp)
                P.ts("dve", ww[:, 1, :], ww[:, 0, :], 1.0, None, ALU.add)
                P.recip(ww[:, 1, :], ww[:, 1, :])
                P.tt("dve", ww[:, 2, :], ww[:, 0, :], ww[:, 1, :], ALU.mult)
                P.tt("dve", eq1[:, :, :], eq1[:, :, :], bc(ww[:, 1, :]), ALU.mult)
                P.tt("dve", eq2[:, :, :], eq2[:, :, :], bc(ww[:, 2, :]), ALU.mult)
                P.tt("dve", gate[:, :, :], eq1[:, :, :], eq2[:, :, :], ALU.add)
            if S.cfg.get("dump_gate"):
                P.dump("gate", gate[:, :, :])
        with P.phase() as st:
            GR = 6
            groups = [(0, 6), (6, 6), (12, 6), (18, 4)]
            gbuf = P.sb("gbuf", [128, GR, NL if moe else NT], BF16, st)
            w13 = [P.sb("w13", [128, 2, KC, 384], BF16, st) for _ in range(2)]
            w2b = [P.sb("w2b", [128, GR, 1024], BF16, st) for _ in range(2)]
            sil = [P.sb("sil", [128, 512], F32, st) for _ in range(2)]
            tmb = [P.sb("tmb", [128, 512], F32 if moe else BF16, st) for _ in range(2)]
            gbce = P.sb("gbce", [128, NL], BF16, st) if moe else None
            gdg = [P.sb("gdg", [128, 128], F32, st) for _ in range(2)] if moe else None
            nw13 = 0; nw2 = 0; nt = 0; ng_ = 0
            experts = range(8) if moe else range(1)
            if moe:
                blks = [(bi, TB[bi][0], TB[bi][0] - NCX, TB[bi][1]) for bi in blocks]
            else:
                blks = [(bi, TB[bi][0], TB[bi][0], TB[bi][1]) for bi in blocks]
            for e in experts:
                W1 = S.moe_w1[i_, e] if moe else S.ffn_w1[i_]
                W3 = S.moe_w3[i_, e] if moe else S.ffn_w3[i_]
                W2 = S.moe_w2[i_, e] if moe else S.ffn_w2[i_]
                if moe:
                    for b4 in range(4):
                        pg = S.pb[7]
                        for j in range(4):
                            tile = b4 * 4 + j
                            gd = gdg[tile % 2]
                            P.ts("dve", gd[:, :], S.identf, gate[:, tile, e:e + 1], None, ALU.mult)
                            P.mm(pg[:, j * 128:(j + 1) * 128], S.onesf, gd[:, :])
                        P.copy("act", gbce[:, b4 * 512:(b4 + 1) * 512], pg[:, :])
                for (f0, nf) in groups:
                    w2t = w2b[nw2 % 2]; nw2 += 1
                    P.dma("pool", w2t[:, 0:nf, :],
                          W2[f0 * 128:(f0 + nf) * 128, :].rearrange("(c p) f -> p c f", p=128))
                    for p0 in range(0, nf, 3):
                        npf = min(3, nf - p0)
                        wt = w13[nw13 % 2]; nw13 += 1
                        c0 = (f0 + p0) * 128
                        P.dma("pool", wt[:, 0, :, 0:npf * 128],
                              W1[:, c0:c0 + npf * 128].rearrange("(kc p) f -> p kc f", p=128))
                        P.dma("pool", wt[:, 1, :, 0:npf * 128],
                              W3[:, c0:c0 + npf * 128].rearrange("(kc p) f -> p kc f", p=128))
                        for (bi, x0, g0, tn) in blks:
                            for fi in range(npf):
                                pa = S.pb[0 + nt % 2]; pbb = S.pb[2 + nt % 2]
                                sl_ = sil[nt % 2]; tm = tmb[nt % 2]
                                nt += 1
                                for kc in range(KC):
                                    P.mm(pa[:, :tn], wt[:, 0, kc, fi * 128:(fi + 1) * 128], S.hT[:, kc, x0:x0 + tn],
                                         start=(kc == 0), stop=(kc == KC - 1))
                                for kc in range(KC):
                                    P.mm(pbb[:, :tn], wt[:, 1, kc, fi * 128:(fi + 1) * 128], S.hT[:, kc, x0:x0 + tn],
                                         start=(kc == 0), stop=(kc == KC - 1))
                                P.act(sl_[:, :tn], pa[:, :tn], AF.Silu)
                                gdst = gbuf[:, p0 + fi, g0:g0 + tn]
                                if moe:
                                    P.tt("dve", tm[:, :tn], pbb[:, :tn], sl_[:, :tn], ALU.mult)
                                    P.tt("pool", gdst, tm[:, :tn], gbce[:, g0:g0 + tn], ALU.mult)
                                else:
                                    P.tt("dve", gdst, pbb[:, :tn], sl_[:, :tn], ALU.mult)
                    for fo in range(8):
                        for (bi, x0, g0, tn) in blks:
                            which = 1 if bi == 0 else 0
                            po = S.pb[4 + ng_ % 3]; ng_ += 1
                            for fc in range(nf):
                                P.mm(po[:, :tn], w2t[:, fc, fo * 128:(fo + 1) * 128], gbuf[:, fc, g0:g0 + tn],
                                     start=(fc == 0), stop=(fc == nf - 1))
                            P.stt("dve", S.xT[:, fo, x0:x0 + tn], po[:, :tn], S.modv[:, l, 40 + fo, which:which + 1],
                                  S.xT[:, fo, x0:x0 + tn], ALU.mult, ALU.add)
    if S.cfg.get("dump_xffn") == l:
        P.dump("xT", S.xT[:, :, :])

def extra_shared(shared, inputs, f):
    shared["rope"] = _rope_tables()
    shared["gn"] = f(np.stack([inputs["gn_a"], inputs["gn_b"], inputs["gn_c"]], axis=1))
    shared["lam_c"] = f(inputs["lam_c"])
    cw = np.asarray(inputs["conv_a"], np.float32)
    shared["convw"] = f(cw.reshape(2, 3, 12, 128).transpose(0, 3, 2, 1))
    shared["adt"] = f(np.stack([np.asarray(inputs["a_log"], np.float32).reshape(2, 8),
                                np.asarray(inputs["dt_bias"], np.float32).reshape(2, 8)], axis=1))
    wg2 = np.concatenate([np.asarray(inputs["w_gate2"], np.float32),
                          np.asarray(inputs["b_gate"], np.float32)[:, :, None, :]], axis=2)
    shared["wg2"] = f(wg2.transpose(0, 2, 1, 3))
    for k in ("w_o_a", "w_o_b", "w_o_c", "w_out", "ffn_w1", "ffn_w3", "ffn_w2", "router_w", "moe_w1", "moe_w3", "moe_w2"):
        shared[k] = f(inputs[k])

def _consts():
    c = np.zeros((128, 1024), np.float32)
    c[:, 0:128] = np.eye(128, dtype=np.float32)
    for p in range(128):
        if (p % 32) < 16:
            c[p + 16, 128 + p] = -1.0
        else:
            c[p - 16, 128 + p] = 1.0
    k = np.arange(128)[:, None]
    i = np.arange(128)[None, :]
    c[:, 256:384] = (k <= i)
    c[:, 384:512] = (k >= i)
    c[:, 512:640] = (k < i)
    c[:, 640:768] = (k > i)
    return c


def _rope_tables():
    t = np.arange(2048)
    row = (t // 64).astype(np.float32)
    col = (t % 64).astype(np.float32)
    inv_freq = (np.float32(10000.0) ** (-np.arange(16, dtype=np.float32) / np.float32(16))).astype(np.float32)
    tab = np.zeros((2, 128, 2048), np.float32)
    for p in range(128):
        d = p % 64
        pos = row if d < 32 else col
        ang = (pos * inv_freq[d % 16]).astype(np.float32)
        tab[0, p] = np.cos(ang)
        tab[1, p] = np.sin(ang)
    return tab


def _fm(v):
    v = np.asarray(v)
    lead = v.shape[:-1]
    n = v.shape[-1] // 128
    w = v.reshape(lead + (n, 128))
    return np.ascontiguousarray(np.moveaxis(w, -1, 0))


_CACHE = {}


def _get_prog(cfg_key, cfg):
    if cfg_key not in _CACHE:
        _CACHE[cfg_key] = build(cfg)
    return _CACHE[cfg_key]


def make_in_maps(inputs, cfg):
    f = lambda a: np.ascontiguousarray(np.asarray(a, dtype=np.float32))
    x = f(inputs["x"]); c = f(inputs["c"]); ctx = f(inputs["ctx"]); c_ctx = f(inputs["c_ctx"])
    shared = {}
    shared["w_mod"] = f(inputs["w_mod"])
    shared["b_mod"] = np.ascontiguousarray(f(inputs["b_mod"]).reshape(2, 48, 128).transpose(0, 2, 1))
    ng = np.stack([_fm(inputs["norm1_g"][0]), _fm(inputs["norm1_g"][1]), _fm(inputs["norm2_g"][0]),
                   _fm(inputs["norm2_g"][1]), _fm(inputs["final_g"])], axis=1)
    shared["norm_g"] = f(ng)
    shared["w_in"] = f(inputs["w_in"])
    shared["consts"] = _consts()
    extra_shared(shared, inputs, f)
    maps = []
    for b in range(8):
        m = dict(shared)
        m["x"] = x[b]
        m["ctx"] = ctx[b]
        m["cvec"] = f(np.stack([_fm(c[b]), _fm(c_ctx)], axis=-1))
        maps.append(m)
    return maps


def run(inputs, cfg, trace=False):
    P = _get_prog(repr(sorted(cfg.items())), cfg)
    maps = make_in_maps(inputs, cfg)
    names = set()
    for ins in P.nc.main_func.allocations if False else []:
        pass
    n = cfg.get("ncores", 8)
    res = run_bass_kernel_spmd(P.nc, maps[:n], core_ids=list(range(n)), trace=trace)
    return P, res


def kernel(**inputs):
    cfg = dict(layers=2)
    P, res = run(inputs, cfg)
    out = np.stack([np.asarray(res.results[b]["out"], dtype=np.float32) for b in range(8)], axis=0)
    return out
```

```python
import contextlib
import numpy as np
import concourse.bass as bass
import concourse.mybir as mybir
from concourse.bass_utils import run_bass_kernel_spmd

F32 = mybir.dt.float32
BF16 = mybir.dt.bfloat16
AF = mybir.ActivationFunctionType
ALU = mybir.AluOpType
AX = mybir.AxisListType


class _Op:
    __slots__ = ("idx", "eng", "fn", "deps", "dma", "sem", "semval", "needs_inc", "prev_dma")


class _Unit:
    __slots__ = ("w", "r", "rd")

    def __init__(self):
        self.w = None
        self.r = {}
        self.rd = []


class V:
    __slots__ = ("ap", "key")

    def __init__(self, ap, key):
        self.ap = ap
        self.key = key


def _apk(x):
    if isinstance(x, V):
        return x.ap, x.key
    return x, None


class Prog:
    NDMA = 8

    def __init__(self):
        self.nc = bass.Bass("TRN2", target_bir_lowering=False)
        self.ops = []
        self.names = {}
        self.base_deps = []
        self.last = {}
        self.dma_since_barrier = []
        self.stack = contextlib.ExitStack()
        self.n_dma = {"sp": 0, "act": 0, "pool": 0}
        self.dma_last = {}
        self.dbg = []
        self._uid = 0
        self.psum_names = set()

    def uid(self, base):
        self._uid += 1
        return f"{base}_{self._uid}"

    def sb(self, name, shape, dtype, stack=None):
        st = stack if stack is not None else self.stack
        return st.enter_context(self.nc.sbuf_tensor(self.uid(name), list(shape), dtype))

    def ps(self, name, shape, dtype=F32, stack=None):
        st = stack if stack is not None else self.stack
        t = st.enter_context(self.nc.psum_tensor(self.uid(name), list(shape), dtype))
        self.psum_names.add(t[:].name)
        return t

    def dram(self, name, shape, dtype, kind="Internal"):
        return self.nc.dram_tensor(name, list(shape), dtype, kind=kind)

    @contextlib.contextmanager
    def phase(self):
        st = contextlib.ExitStack()
        try:
            yield st
        finally:
            self.barrier()
            st.close()

    def _conf(self, name, sub):
        d = self.names.setdefault(name, {})
        if sub is None:
            if None not in d:
                d[None] = _Unit()
            return d[None], list(d.values())
        if sub not in d:
            d[sub] = _Unit()
        res = [d[sub]]
        if None in d:
            res.append(d[None])
        return d[sub], res

    def add(self, eng, fn, reads, writes, dma=False):
        op = _Op()
        op.idx = len(self.ops)
        op.eng = eng
        op.fn = fn
        op.dma = dma
        op.sem = None
        op.semval = 0
        op.needs_inc = False
        op.prev_dma = None
        deps = {}
        for b in self.base_deps:
            deps[b.idx] = b
        for x in reads:
            if x is None or isinstance(x, (int, float)):
                continue
            ap, key = _apk(x)
            prim, conf = self._conf(ap.name, key)
            for u in conf:
                if u.w is not None:
                    deps[u.w.idx] = u.w
                if ap.name in self.psum_names:
                    for re_, r in u.r.items():
                        if re_ != eng:
                            deps[r.idx] = r
            if dma:
                prim.rd.append(op)
            else:
                prim.r[eng] = op
        for x in writes:
            ap, key = _apk(x)
            prim, conf = self._conf(ap.name, key)
            for u in conf:
                if u.w is not None:
                    deps[u.w.idx] = u.w
                for r in u.r.values():
                    deps[r.idx] = r
                for r in u.rd:
                    deps[r.idx] = r
            prim.w = op
            prim.r = {}
            prim.rd = []
        deps.pop(op.idx, None)
        if dma:
            slot = self.n_dma[eng] % self.NDMA
            self.n_dma[eng] += 1
            prev = self.dma_last.get((eng, slot))
            op.prev_dma = prev
            op.sem = (eng, slot)
            op.semval = (prev.semval if prev is not None else 0) + 16
            self.dma_last[(eng, slot)] = op
            self.dma_since_barrier.append(op)
        op.deps = list(deps.values())
        self.ops.append(op)
        self.last[eng] = op
        return op

    def barrier(self):
        b = list(self.last.values()) + list(self.dma_since_barrier)
        self.base_deps = b
        self.dma_since_barrier = []

    def mm(self, out, lhsT, rhs, start=True, stop=True, **kw):
        o, _ = _apk(out); l, _ = _apk(lhsT); r, _ = _apk(rhs)
        nc = self.nc
        return self.add("pe", lambda: nc.tensor.matmul(o, l, r, start=start, stop=stop, **kw),
                        [lhsT, rhs], [out])

    def tr(self, out, in_, ident):
        o, _ = _apk(out); i, _ = _apk(in_); d, _ = _apk(ident)
        nc = self.nc
        return self.add("pe", lambda: nc.tensor.transpose(o, i, d), [in_, ident], [out])

    def act(self, out, in_, func, bias=None, scale=None, accum_out=None):
        o, _ = _apk(out); i, _ = _apk(in_)
        kw = {}
        rd = [in_]
        wr = [out]
        if bias is not None:
            kw["bias"] = _apk(bias)[0] if not isinstance(bias, (int, float)) else bias
            rd.append(bias)
        if scale is not None:
            kw["scale"] = _apk(scale)[0] if not isinstance(scale, (int, float)) else scale
            rd.append(scale)
        if accum_out is not None:
            kw["accum_out"] = _apk(accum_out)[0]
            wr.append(accum_out)
        nc = self.nc
        return self.add("act", lambda: nc.scalar.activation(out=o, in_=i, func=func, **kw), rd, wr)

    def _ve(self, eng):
        return self.nc.vector if eng == "dve" else self.nc.gpsimd

    def tt(self, eng, out, in0, in1, op):
        o, _ = _apk(out); a, _ = _apk(in0); b, _ = _apk(in1)
        e = self._ve(eng)
        return self.add(eng, lambda: e.tensor_tensor(out=o, in0=a, in1=b, op=op), [in0, in1], [out])

    def ts(self, eng, out, in0, s1, s2=None, op0=ALU.mult, op1=None, accum_out=None):
        o, _ = _apk(out); a, _ = _apk(in0)
        e = self._ve(eng)
        s1v = s1 if isinstance(s1, (int, float)) else _apk(s1)[0]
        s2v = s2 if (s2 is None or isinstance(s2, (int, float))) else _apk(s2)[0]
        kw = {}
        wr = [out]
        if op1 is not None:
            kw["op1"] = op1
        if accum_out is not None:
            kw["accum_out"] = _apk(accum_out)[0]
            wr.append(accum_out)
        return self.add(eng, lambda: e.tensor_scalar(out=o, in0=a, scalar1=s1v, scalar2=s2v, op0=op0, **kw),
                        [in0, s1, s2], wr)

    def stt(self, eng, out, in0, scalar, in1, op0, op1):
        o, _ = _apk(out); a, _ = _apk(in0); b, _ = _apk(in1)
        e = self._ve(eng)
        sv = scalar if isinstance(scalar, (int, float)) else _apk(scalar)[0]
        return self.add(eng, lambda: e.scalar_tensor_tensor(out=o, in0=a, scalar=sv, in1=b, op0=op0, op1=op1),
                        [in0, scalar, in1], [out])

    def copy(self, eng, out, in_):
        o, _ = _apk(out); i, _ = _apk(in_)
        nc = self.nc
        if eng == "act":
            return self.add("act", lambda: nc.scalar.copy(out=o, in_=i), [in_], [out])
        e = self._ve(eng)
        return self.add(eng, lambda: e.tensor_copy(out=o, in_=i), [in_], [out])

    def memset(self, eng, out, val):
        o, _ = _apk(out)
        e = self._ve(eng)
        return self.add(eng, lambda: e.memset(o, val), [], [out])

    def reduce(self, eng, out, in_, op, axis=AX.X):
        o, _ = _apk(out); i, _ = _apk(in_)
        e = self._ve(eng)
        return self.add(eng, lambda: e.tensor_reduce(out=o, in_=i, axis=axis, op=op), [in_], [out])

    def recip(self, out, in_):
        o, _ = _apk(out); i, _ = _apk(in_)
        nc = self.nc
        return self.add("dve", lambda: nc.vector.reciprocal(out=o, in_=i), [in_], [out])

    def dma(self, q, out, in_):
        o, _ = _apk(out); i, _ = _apk(in_)
        e = {"sp": self.nc.sync, "act": self.nc.scalar, "pool": self.nc.gpsimd}[q]
        return self.add(q, lambda: e.dma_start(out=o, in_=i), [in_], [out], dma=True)

    def dump(self, name, ap, dtype=None):
        a, _ = _apk(ap)
        t = self.nc.dram_tensor("dbg_" + name, list(a.shape), dtype or a.dtype, kind="ExternalOutput")
        self.dbg.append("dbg_" + name)
        self.dma("sp", t.ap(), ap)

    def emit(self):
        nc = self.nc
        engobj = {"pe": nc.tensor, "act": nc.scalar, "dve": nc.vector, "pool": nc.gpsimd, "sp": nc.sync}
        st = self.stack
        esem = {e: st.enter_context(nc.semaphore("s_" + e)) for e in ("pe", "act", "dve", "pool")}
        dsem = {}
        for q in ("sp", "act", "pool"):
            for s in range(self.NDMA):
                if (q, s) in self.dma_last:
                    dsem[(q, s)] = st.enter_context(nc.semaphore(f"d_{q}{s}"))
        for op in self.ops:
            for p in op.deps:
                if not p.dma and not (p.eng == "pe" and op.eng == "pe"):
                    p.needs_inc = True
        finals = list(self.last.values())
        for p in finals:
            if not p.dma:
                p.needs_inc = True
        cnt = {e: 0 for e in esem}
        for op in self.ops:
            if not op.dma and op.needs_inc:
                cnt[op.eng] += 1
                op.semval = cnt[op.eng]
        waited = {e: {} for e in engobj}
        nwait = 0
        for op in self.ops:
            e = engobj[op.eng]
            need = {}
            for p in op.deps:
                if p.dma:
                    k = ("d", p.sem)
                    v = p.semval
                else:
                    if p.eng == "pe" and op.eng == "pe":
                        continue
                    k = ("e", p.eng)
                    v = p.semval
                if need.get(k, 0) < v:
                    need[k] = v
            if op.dma and op.prev_dma is not None:
                k = ("d", op.sem)
                if need.get(k, 0) < op.prev_dma.semval:
                    need[k] = op.prev_dma.semval
            w = waited[op.eng]
            for k, v in need.items():
                if w.get(k, 0) >= v:
                    continue
                w[k] = v
                sem = dsem[k[1]] if k[0] == "d" else esem[k[1]]
                e.wait_ge(sem, v)
                nwait += 1
            ins = op.fn()
            if op.dma:
                ins.then_inc(dsem[op.sem], 16)
            elif op.needs_inc:
                ins.then_inc(esem[op.eng], 1)
        sp = nc.sync
        for en, sem in esem.items():
            if cnt[en] > 0:
                sp.wait_ge(sem, cnt[en])
        for k, p in self.dma_last.items():
            sp.wait_ge(dsem[k], p.semval)
        self.stats = dict(n_ops=len(self.ops), n_wait=nwait, incs=dict(cnt))
        return nc

D = 1024
KC = 8
NL = 2048
NCX = 256
NT = 2304
NTL = 18
TB = [(0, 256), (256, 512), (768, 512), (1280, 512), (1792, 512)]
DFF = 2816
FC = 22
EPS = 1e-6
A_Q, A_K, A_V, A_Z, A_B, A_G = 0, 512, 1024, 1536, 2048, 2056
B_Q, B_K, B_V, B_R, B_GL = 2064, 2320, 2576, 3088, 3600
C_Q, C_K, C_V = 3632, 4144, 4656
G_A, G_B, G_D = 5168, 6192, 7216


class Ctx:
    pass


def build(cfg):
    P = Prog()
    nc = P.nc
    S = Ctx()
    S.P = P
    S.cfg = cfg
    L = cfg.get("layers", 2)

    def din(name, shape, dt=F32):
        return nc.dram_tensor(name, list(shape), dt, kind="ExternalInput").ap()

    S.x = din("x", [NL, D])
    S.ctx = din("ctx", [NCX, D])
    S.cvec = din("cvec", [128, KC, 2])
    S.w_mod = din("w_mod", [2, D, 6 * D])
    S.b_mod = din("b_mod", [2, 128, 48])
    S.norm_g = din("norm_g", [128, 5, KC])
    S.w_in = din("w_in", [2, D, 8240])
    S.consts = din("consts", [128, 1024])
    S.rope = din("rope", [2, 128, NL])
    S.gn = din("gn", [2, 3, 128])
    S.lam_c = din("lam_c", [2, 4, 64])
    S.wg2 = din("wg2", [2, 17, 2, 256])
    S.convw = din("convw", [2, 128, 12, 3])
    S.adt = din("adt", [2, 2, 8])
    S.w_o_a = din("w_o_a", [2, 512, D])
    S.w_o_b = din("w_o_b", [2, 512, D])
    S.w_o_c = din("w_o_c", [2, 512, D])
    S.w_out = din("w_out", [2, D, D])
    S.ffn_w1 = din("ffn_w1", [1, D, DFF])
    S.ffn_w3 = din("ffn_w3", [1, D, DFF])
    S.ffn_w2 = din("ffn_w2", [1, DFF, D])
    S.router_w = din("router_w", [1, D, 8])
    S.moe_w1 = din("moe_w1", [1, 8, D, DFF])
    S.moe_w3 = din("moe_w3", [1, 8, D, DFF])
    S.moe_w2 = din("moe_w2", [1, 8, DFF, D])
    S.out = nc.dram_tensor("out", [NL, D], F32, kind="ExternalOutput").ap()

    st = P.stack
    S.xT = P.sb("xT", [128, KC, NT], F32)
    S.hT = P.sb("hT", [128, KC, NT], BF16)
    S.cf = P.sb("cf", [128, 1024], F32)
    S.identf = S.cf[:, 0:128]
    S.identb_t = P.sb("identb", [128, 128], BF16)
    S.identb = S.identb_t[:, :]
    S.onesb_t = P.sb("onesb", [128, 128], BF16)
    S.onesb = S.onesb_t[:, :]
    S.onesf_t = P.sb("onesf", [128, 128], F32)
    S.onesf = S.onesf_t[:, :]
    S.modv = P.sb("modv", [128, 2, 48, 2], F32)
    S.ng = P.sb("ng", [128, 5, KC], F32)
    S.gs = P.sb("gs", [128, 4, KC, 2], F32)
    S.pb = [P.ps(f"pb{i}", [128, 512], F32) for i in range(8)]

    P.dma("sp", S.cf[:, :], S.consts)
    P.dma("sp", S.ng[:, :, :], S.norm_g)
    P.copy("dve", S.identb, S.identf)
    P.memset("dve", S.onesb, 1.0)
    P.memset("dve", S.onesf, 1.0)
    S.eps_t = P.sb("eps_t", [128, 1], F32)
    P.memset("dve", S.eps_t[:, :], EPS)
    S.zero_t = P.sb("zero_t", [128, 1], F32)
    P.memset("dve", S.zero_t[:, :], 0.0)

    phase_load(S)
    phase_mod(S, L)
    for l in range(L):
        if cfg.get("mixer", True):
            phase_mixer(S, l)
        if cfg.get("ffn", True):
            phase_ffn(S, l)
    phase_final(S)
    P.emit()
    return P


def phase_load(S):
    P = S.P
    with P.phase() as st:
        tin = [P.sb("ld_in", [128, D], F32, st) for _ in range(3)]
        for tt in range(NTL):
            src = S.ctx[tt * 128:(tt + 1) * 128, :] if tt < 2 else S.x[(tt - 2) * 128:(tt - 1) * 128, :]
            ti = tin[tt % 3]
            P.dma("sp", ti[:, :], src)
            for half in range(2):
                pb = S.pb[(tt * 2 + half) % 4]
                for j in range(4):
                    kc = half * 4 + j
                    P.tr(pb[:, j * 128:(j + 1) * 128], ti[:, kc * 128:(kc + 1) * 128], S.identf)
                dst = S.xT[:, half * 4:(half + 1) * 4, tt * 128:(tt + 1) * 128]
                srcp = pb[:, :].rearrange("p (j t) -> p j t", j=4)
                if half == 0:
                    P.copy("dve", dst, srcp)
                else:
                    P.copy("act", dst, srcp)


def phase_mod(S, L):
    P = S.P
    with P.phase() as st:
        cv = P.sb("cv", [128, KC, 2], F32, st)
        cvb = P.sb("cvb", [128, KC, 2], BF16, st)
        bm = P.sb("bm", [128, 2, 48], F32, st)
        wm = [P.sb("wm", [128, KC, 1024], BF16, st) for _ in range(2)]
        P.dma("sp", cv[:, :, :], S.cvec)
        P.dma("sp", bm[:, :, :], S.b_mod.rearrange("l p f -> p l f"))
        P.act(cvb[:, :, :], cv[:, :, :], AF.Silu)
        n = 0
        for l in range(L):
            for i in range(6):
                w = wm[n % 2]
                n += 1
                P.dma("pool", w[:, :, :],
                      S.w_mod[l, :, i * 1024:(i + 1) * 1024].rearrange("(kc p) f -> p kc f", p=128))
                pb = S.pb[4 + (n % 2)]
                for fc in range(8):
                    for kc in range(KC):
                        P.mm(pb[:, fc * 2:fc * 2 + 2], w[:, kc, fc * 128:(fc + 1) * 128], cvb[:, kc, :],
                             start=(kc == 0), stop=(kc == KC - 1))
                P.tt("dve", S.modv[:, l, i * 8:(i + 1) * 8, :],
                     pb[:, 0:16].rearrange("p (f w) -> p f w", w=2),
                     bm[:, l, i * 8:(i + 1) * 8].unsqueeze(2).to_broadcast([128, 8, 2]), ALU.add)
            for wn, (gi, si) in enumerate(((l, 1), (2 + l, 4))):
                dst = S.gs[:, l * 2 + wn, :, :]
                P.ts("dve", dst, S.modv[:, l, si * 8:(si + 1) * 8, :], 1.0, None, ALU.add)
                P.tt("dve", dst, dst, S.ng[:, gi, :].unsqueeze(2).to_broadcast([128, 8, 2]), ALU.mult)


def modulate(S, l, wn, blocks, st, router=None):
    P = S.P
    sq = [P.sb("sq", [128, KC, 512], BF16, st) for _ in range(2)]
    rstd = [P.sb("rstd", [128, 512], F32, st) for _ in range(2)]
    tmp = [P.sb("mtmp", [128, 512], F32, st) for _ in range(3)]
    h32 = [P.sb("mh32", [128, 512], F32, st) for _ in range(2)] if router is not None else None
    si = 0 if wn == 0 else 3
    n = 0
    for bi in blocks:
        t0, tn = TB[bi]
        which = 1 if bi == 0 else 0
        s = sq[bi % 2]
        pb = S.pb[6 + bi % 2]
        for kc in range(KC):
            P.act(s[:, kc, :tn], S.xT[:, kc, t0:t0 + tn], AF.Square)
        for kc in range(KC):
            P.mm(pb[:, :tn], S.onesb, s[:, kc, :tn], start=(kc == 0), stop=(kc == KC - 1))
        r = rstd[bi % 2]
        P.act(r[:, :tn], pb[:, :tn], AF.Sqrt, bias=S.eps_t[:, :], scale=1.0 / D)
        P.recip(r[:, :tn], r[:, :tn])
        for kc in range(KC):
            t = tmp[n % 3]
            n += 1
            P.stt("dve", t[:, :tn], S.xT[:, kc, t0:t0 + tn], S.gs[:, l * 2 + wn, kc, which:which + 1],
                  r[:, :tn], ALU.mult, ALU.mult)
            P.act(S.hT[:, kc, t0:t0 + tn], t[:, :tn], AF.Identity,
                  bias=S.modv[:, l, si * 8 + kc, which:which + 1])
            if router is not None:
                rw, logit = router
                hh = h32[kc % 2]
                P.ts("dve", hh[:, :tn], t[:, :tn], S.modv[:, l, si * 8 + kc, which:which + 1], None, ALU.add)
                pl = S.pb[5]
                for j in range(tn // 128):
                    P.mm(pl[:, j * 8:(j + 1) * 8], hh[:, j * 128:(j + 1) * 128], rw[:, kc, :],
                         start=(kc == 0 and j == 0), stop=(kc == KC - 1), skip_group_check=True)
        if router is not None:
            rw, logit = router
            tile0 = (t0 - NCX) // 128
            P.copy("dve", logit[:, tile0:tile0 + tn // 128, :],
                   S.pb[5][:, 0:(tn // 128) * 8].rearrange("p (j e) -> p j e", e=8))


def phase_final(S):
    P = S.P
    with P.phase() as st:
        sq = [P.sb("fsq", [128, KC, 512], BF16, st) for _ in range(2)]
        rstd = [P.sb("frstd", [128, 512], F32, st) for _ in range(2)]
        yT = [P.sb("fyT", [128, KC, 512], F32, st) for _ in range(2)]
        ot = [P.sb("fot", [128, D], F32, st) for _ in range(3)]
        g32 = S.ng[:, 4, :]
        n = 0
        for bi in range(1, 5):
            t0, tn = TB[bi]
            s = sq[bi % 2]
            pb = S.pb[6 + bi % 2]
            for kc in range(KC):
                P.act(s[:, kc, :], S.xT[:, kc, t0:t0 + tn], AF.Square)
            for kc in range(KC):
                P.mm(pb[:, :], S.onesb, s[:, kc, :], start=(kc == 0), stop=(kc == KC - 1))
            r = rstd[bi % 2]
            P.act(r[:, :], pb[:, :], AF.Sqrt, bias=S.eps_t[:, :], scale=1.0 / D)
            P.recip(r[:, :], r[:, :])
            y = yT[bi % 2]
            for kc in range(KC):
                P.stt("dve", y[:, kc, :], S.xT[:, kc, t0:t0 + tn], g32[:, kc:kc + 1],
                      r[:, :], ALU.mult, ALU.mult)
            for q in range(4):
                o = ot[n % 3]
                n += 1
                for half in range(2):
                    pt = S.pb[(n * 2 + half) % 4]
                    for j in range(4):
                        kc = half * 4 + j
                        P.tr(pt[:, j * 128:(j + 1) * 128], y[:, kc, q * 128:(q + 1) * 128], S.identf)
                    if half == 0:
                        P.copy("dve", o[:, 0:512], pt[:, :])
                    else:
                        P.copy("act", o[:, 512:1024], pt[:, :])
                r0 = t0 - NCX + q * 128
                P.dma("sp", S.out[r0:r0 + 128, :], o[:, :])

import math


def phase_mixer(S, l):
    P = S.P
    ctx_out = l < 1
    with P.phase() as st:
        modulate(S, l, 0, range(5), st)
    if S.cfg.get("dump_h") == l:
        P.dump("hT", S.hT[:, :, :])
    br = S.cfg.get("branches", "abc")
    if "c" in br:
        branch_c(S, l, ctx_out)
    if "b" in br:
        branch_b(S, l, ctx_out)
    if "a" in br:
        branch_a(S, l, ctx_out)
    if S.cfg.get("dump_xmix") == l:
        P.dump("xT", S.xT[:, :, :])


def merge_branch(S, l, yT, wo_dram, gcol, ctx_out, st):
    P = S.P
    wg = P.sb("wg", [128, KC, 1024], BF16, st)
    wo = P.sb("wo", [128, 4, 1024], BF16, st)
    wout = P.sb("wout", [128, KC, 1024], BF16, st)
    P.dma("pool", wg[:, :, :], S.w_in[l, :, gcol:gcol + 1024].rearrange("(kc p) f -> p kc f", p=128))
    P.dma("pool", wo[:, :, :], wo_dram[l].rearrange("(c p) f -> p c f", p=128))
    P.dma("pool", wout[:, :, :], S.w_out[l].rearrange("(kc p) f -> p kc f", p=128))
    gmb = [P.sb("gm", [128, KC, 512], BF16, st) for _ in range(2)]
    sgb = [P.sb("sg", [128, 512], F32, st) for _ in range(2)]
    for bi in (range(5) if ctx_out else range(1, 5)):
        t0, tn = TB[bi]
        which = 1 if bi == 0 else 0
        gm = gmb[bi % 2]
        for fc in range(8):
            pg = S.pb[0 + fc % 2]
            py = S.pb[2 + fc % 2]
            for kc in range(KC):
                P.mm(pg[:, :tn], wg[:, kc, fc * 128:(fc + 1) * 128], S.hT[:, kc, t0:t0 + tn],
                     start=(kc == 0), stop=(kc == KC - 1))
            for c in range(4):
                P.mm(py[:, :tn], wo[:, c, fc * 128:(fc + 1) * 128], yT[:, c, t0:t0 + tn],
                     start=(c == 0), stop=(c == 3))
            sg = sgb[fc % 2]
            P.act(sg[:, :tn], pg[:, :tn], AF.Sigmoid)
            P.tt("dve", gm[:, fc, :tn], py[:, :tn], sg[:, :tn], ALU.mult)
        for fo in range(8):
            po = S.pb[4 + fo % 2]
            for fc in range(8):
                P.mm(po[:, :tn], wout[:, fc, fo * 128:(fo + 1) * 128], gm[:, fc, :tn],
                     start=(fc == 0), stop=(fc == 7))
            P.stt("dve", S.xT[:, fo, t0:t0 + tn], po[:, :tn], S.modv[:, l, 16 + fo, which:which + 1],
                  S.xT[:, fo, t0:t0 + tn], ALU.mult, ALU.add)


def branch_c(S, l, ctx_out):
    P = S.P
    lam_init = 0.8 - 0.6 * math.exp(-0.3 * l)
    with P.phase() as sto:
        ydT = P.sb("ydT", [128, 4, NT], BF16, sto)
        if not ctx_out:
            P.memset("pool", ydT[:, :, 0:NCX], 0.0)
        with P.phase() as st:
            ropet = P.sb("ropet", [128, 2, NL], BF16, st)
            P.dma("pool", ropet[:, :, :], S.rope.rearrange("c p t -> p c t"))
            rotT = P.sb("rotT", [128, 128], BF16, st)
            P.copy("dve", rotT[:, :], S.cf[:, 128:256])
            gnbc = P.sb("gnbc", [128, 128], F32, st)
            P.dma("sp", gnbc[:, :], S.gn[l, 2].partition_broadcast(128))
            P.ts("dve", gnbc[:, :], gnbc[:, :], 1.0 - lam_init, None, ALU.mult)
            lam = P.sb("lam", [128, 4, 64], F32, st)
            P.dma("sp", lam[:, :, :], S.lam_c[l].partition_broadcast(128))
            lp = P.sb("lp", [128, 2, 64], F32, st)
            P.tt("dve", lp[:, 0, :], lam[:, 0, :], lam[:, 1, :], ALU.mult)
            P.tt("dve", lp[:, 1, :], lam[:, 2, :], lam[:, 3, :], ALU.mult)
            ls = P.sb("ls", [128, 2], F32, st)
            P.reduce("dve", ls[:, :], lp[:, :, :], ALU.add, AX.X)
            le = P.sb("le", [128, 2], F32, st)
            P.act(le[:, :], ls[:, :], AF.Exp)
            neglam = P.sb("neglam", [128, 1], F32, st)
            P.tt("dve", neglam[:, :], le[:, 1:2], le[:, 0:1], ALU.subtract)
            P.ts("dve", neglam[:, :], neglam[:, :], -lam_init, None, ALU.add)

            wC = P.sb("wC", [128, KC, 1536], BF16, st)
            P.dma("pool", wC[:, :, :], S.w_in[l, :, C_Q:C_Q + 1536].rearrange("(kc p) f -> p kc f", p=128))
            gncol = P.sb("gncol", [128, 1], F32, st)
            P.dma("sp", gncol[:, :], S.gn[l, 2, :].rearrange("(p o) -> p o", o=1))
            P.ts("dve", gncol[:, :], gncol[:, :], 1.0 - lam_init, None, ALU.mult)
            qTh = P.sb("qTh", [128, NT], BF16, st)
            kTh = P.sb("kTh", [128, NT], BF16, st)
            vtok = P.sb("vtokc", [128, NTL, 128], BF16, st)
            xqb = [P.sb("xq", [128, 512], BF16, st) for _ in range(2)]
            t1b = [P.sb("t1", [128, 512], F32, st) for _ in range(2)]
            t2b = [P.sb("t2", [128, 512], F32, st) for _ in range(2)]
            NR = 6
            pring = [P.sb("pT", [128, 512], BF16, st) for _ in range(NR)]
            r0b = [P.sb("cr0", [128, 512], F32, st) for _ in range(1)]
            r1b = [P.sb("cr1", [128, 512], F32, st) for _ in range(1)]
            t0b = [P.sb("ct0", [128, 512], F32, st) for _ in range(1)]
            odb = [P.sb("cod", [128, 512], F32, st) for _ in range(1)]
            sqb = [P.sb("csq", [128, 512], BF16, st) for _ in range(1)]
            cnt = 0
            ring = 0
            ep = 0
            for h in range(4):
                for dstT, col in ((qTh, h * 128), (kTh, 512 + h * 128)):
                    for bi in range(5):
                        t0, tn = TB[bi]
                        pb = S.pb[6]
                        for kc in range(KC):
                            P.mm(pb[:, :tn], wC[:, kc, col:col + 128], S.hT[:, kc, t0:t0 + tn],
                                 start=(kc == 0), stop=(kc == KC - 1))
                        if bi == 0:
                            P.act(dstT[:, 0:tn], pb[:, :tn], AF.Copy)
                        else:
                            xq = xqb[cnt % 2]; t1 = t1b[cnt % 2]; t2 = t2b[cnt % 2]
                            cnt += 1
                            P.act(xq[:, :], pb[:, :], AF.Copy)
                            pr = S.pb[7]
                            P.mm(pr[:, :], rotT[:, :], xq[:, :])
                            lt0 = t0 - NCX
                            P.tt("dve", t1[:, :], pr[:, :], ropet[:, 1, lt0:lt0 + 512], ALU.mult)
                            P.tt("pool", t2[:, :], xq[:, :], ropet[:, 0, lt0:lt0 + 512], ALU.mult)
                            P.tt("dve", dstT[:, t0:t0 + 512], t1[:, :], t2[:, :], ALU.add)
                vc = 1024 + h * 128
                for g0 in range(0, NTL, 4):
                    ng_ = min(4, NTL - g0)
                    pb = S.pb[6]
                    for j in range(ng_):
                        tt = g0 + j
                        for kc in range(KC):
                            P.mm(pb[:, j * 128:(j + 1) * 128], S.hT[:, kc, tt * 128:(tt + 1) * 128],
                                 wC[:, kc, vc:vc + 128], start=(kc == 0), stop=(kc == KC - 1))
                    P.act(vtok[:, g0:g0 + ng_, :],
                          pb[:, 0:ng_ * 128].rearrange("p (j t) -> p j t", j=ng_), AF.Copy)
                for qb in ([0] if ctx_out else []) + [1, 2, 3, 4]:
                    q0, qn = TB[qb]
                    seq = list(range(2)) if qb == 0 else list(range(NTL))
                    for m in range(2):
                        oT = S.pb[2 + m]
                        sT = S.pb[4 + m]

                        def qk(kt):
                            nonlocal ring
                            ps = S.pb[kt % 2]
                            P.mm(ps[:, :qn], kTh[m * 64:(m + 1) * 64, kt * 128:(kt + 1) * 128],
                                 qTh[m * 64:(m + 1) * 64, q0:q0 + qn])
                            pt = pring[ring % NR]
                            ring += 1
                            P.act(pt[:, :qn], ps[:, :qn], AF.Exp, scale=0.125)
                            return pt
                        LA = 3
                        pts = {}
                        for i, kt in enumerate(seq):
                            if i == 0:
                                for j in range(min(LA, len(seq))):
                                    pts[seq[j]] = qk(seq[j])
                            if i + LA < len(seq):
                                pts[seq[i + LA]] = qk(seq[i + LA])
                            pt = pts.pop(kt)
                            P.mm(oT[:, :qn], vtok[:, kt, :], pt[:, :qn], start=(i == 0), stop=(i == len(seq) - 1))
                            P.mm(sT[:, :qn], S.onesb, pt[:, :qn], start=(i == 0), stop=(i == len(seq) - 1))
                    r0 = r0b[0]; r1 = r1b[0]; t0_ = t0b[0]; od = odb[0]; sq = sqb[0]
                    ep += 1
                    P.recip(r0[:, :qn], S.pb[4][:, :qn])
                    P.recip(r1[:, :qn], S.pb[5][:, :qn])
                    P.ts("dve", r1[:, :qn], r1[:, :qn], neglam[:, :], None, ALU.mult)
                    P.tt("dve", t0_[:, :qn], S.pb[2][:, :qn], r0[:, :qn], ALU.mult)
                    P.tt("dve", r1[:, :qn], S.pb[3][:, :qn], r1[:, :qn], ALU.mult)
                    P.tt("pool", od[:, :qn], t0_[:, :qn], r1[:, :qn], ALU.add)
                    P.act(sq[:, :qn], od[:, :qn], AF.Square)
                    pn = S.pb[7]
                    P.mm(pn[:, :qn], S.onesb, sq[:, :qn])
                    P.act(r0[:, :qn], pn[:, :qn], AF.Sqrt, bias=S.eps_t[:, :], scale=1.0 / 128)
                    P.recip(r0[:, :qn], r0[:, :qn])
                    P.stt("dve", ydT[:, h, q0:q0 + qn], od[:, :qn], gncol[:, :], r0[:, :qn], ALU.mult, ALU.mult)
        if S.cfg.get("dump_yd") == l:
            P.dump("ydT", ydT[:, :, :])
        if S.cfg.get("merge", True):
            with P.phase() as st:
                merge_branch(S, l, ydT, S.w_o_c, G_D, ctx_out, st)


def branch_b(S, l, ctx_out):
    P = S.P
    U = S.cf[:, 256:384]
    Lm = S.cf[:, 384:512]
    SU = S.cf[:, 512:640]
    SL = S.cf[:, 640:768]
    with P.phase() as sto:
        ybT = P.sb("ybT", [128, 4, NT], BF16, sto)
        if not ctx_out:
            P.memset("pool", ybT[:, :, 0:NCX], 0.0)
        with P.phase() as stc:
            gnbc = P.sb("gnbcb", [128, 128], F32, stc)
            P.dma("sp", gnbc[:, :], S.gn[l, 1].partition_broadcast(128))
            wg2 = P.sb("wg2", [17, 2, 256], BF16, stc)
            if S.cfg.get("t_wg2", 1):
                P.dma("pool", wg2[:, :, :], S.wg2[l])
            one_t = P.sb("one_t", [128, 1], F32, stc)
            P.memset("dve", one_t[:, :], 1.0)
            mkb = P.sb("mkb", [128, 512], BF16, stc)
            P.copy("dve", mkb[:, :], S.cf[:, 256:768])
            Ub = mkb[:, 0:128]; Lb = mkb[:, 128:256]; SUb = mkb[:, 256:384]; SLb = mkb[:, 384:512]
            for hp in range(2):
                with P.phase() as st:
                    qT = P.sb("bqT", [128, NT], BF16, st)
                    kT = P.sb("bkT", [128, NT], BF16, st)
                    ktok = P.sb("bktok", [128, NTL, 128], BF16, st)
                    vtok = P.sb("bvtok", [128, NTL, 256], BF16, st)
                    srtok = P.sb("bsrtok", [128, NTL, 256], BF16, st)
                    glrT = P.sb("bglrT", [17, 2, NT], BF16, st)
                    Sst = P.sb("bSst", [128, 2, NTL, 128], BF16, st)
                    Sf = [P.sb("bSf", [128, 128], F32, st) for _ in range(2)]
                    if S.cfg.get("t_ms", 1):
                        P.memset("pool", glrT[:, :, :], 1.0)
                    P.memset("dve", Sf[0][:, :], 0.0)
                    P.memset("dve", Sf[1][:, :], 0.0)
                    with P.phase() as stw:
                        wB = P.sb("wBp", [128, KC, 800], BF16, stw)
                        for (dst0, n_, src0) in ((0, 128, B_Q + hp * 128), (128, 128, B_K + hp * 128),
                                                 (256, 256, B_V + hp * 256), (512, 256, B_R + hp * 256),
                                                 (768, 32, B_GL)):
                            P.dma("pool", wB[:, :, dst0:dst0 + n_],
                                  S.w_in[l, :, src0:src0 + n_].rearrange("(kc p) f -> p kc f", p=128))
                        for bi in (range(5) if S.cfg.get("b_proj", 9) >= 1 else []):
                            t0, tn = TB[bi]
                            for dstT, col in ((qT, 0), (kT, 128)):
                                pb = S.pb[4 + (col // 128)]
                                for kc in range(KC):
                                    P.mm(pb[:, :tn], wB[:, kc, col:col + 128], S.hT[:, kc, t0:t0 + tn],
                                         start=(kc == 0), stop=(kc == KC - 1))
                                P.act(dstT[:, t0:t0 + tn], pb[:, :tn], AF.Copy)
                            for d in (range(2) if S.cfg.get("t_glr", 1) else []):
                                pb = S.pb[6 + d]
                                for kc in range(KC):
                                    P.mm(pb[0:16, :tn], wB[:, kc, 768 + d * 16:768 + (d + 1) * 16],
                                         S.hT[:, kc, t0:t0 + tn], start=(kc == 0), stop=(kc == KC - 1))
                                P.copy("dve", glrT[0:16, d, t0:t0 + tn], pb[0:16, :tn])
                        for n in (range(NTL) if S.cfg.get("b_proj", 9) >= 2 else []):
                            pa = S.pb[0 + n % 2]
                            pr = S.pb[2 + n % 2]
                            for kc in range(KC):
                                P.mm(pa[:, 0:384], S.hT[:, kc, n * 128:(n + 1) * 128], wB[:, kc, 128:512],
                                     start=(kc == 0), stop=(kc == KC - 1))
                            for kc in range(KC):
                                P.mm(pr[:, 0:256], S.hT[:, kc, n * 128:(n + 1) * 128], wB[:, kc, 512:768],
                                     start=(kc == 0), stop=(kc == KC - 1))
                            P.copy("dve", ktok[:, n, :], pa[:, 0:128])
                            P.act(vtok[:, n, :], pa[:, 128:384], AF.Copy)
                            P.act(srtok[:, n, :], pr[:, 0:256], AF.Silu)

                    e1b = [P.sb("be1", [128, 128], F32, st) for _ in range(2)]
                    spb = [P.sb("bsp", [128, 128], F32, st) for _ in range(2)]
                    eremb = [P.sb("berem", [128, 128], F32, st) for _ in range(2)]
                    kgb = [P.sb("bkg", [128, 128], BF16, st) for _ in range(2)]
                    sphl = [P.sb("bsphl", [128, 2, 128], BF16, st) for _ in range(2)]
                    glb = [P.sb("bgl", [128, 1], F32, st) for _ in range(4)]
                    cnt = 0

                    def gate_sp(d, n):
                        nonlocal cnt
                        px = S.pb[0 + cnt % 2]
                        e1 = e1b[cnt % 2]; sp = spb[cnt % 2]
                        cnt += 1
                        P.mm(px[:, 0:128], glrT[0:17, d, n * 128:(n + 1) * 128],
                             wg2[0:17, d, hp * 128:(hp + 1) * 128])
                        P.act(e1[:, :], px[:, 0:128], AF.Exp, scale=-1.0)
                        P.act(sp[:, :], e1[:, :], AF.Ln, bias=one_t[:, :])
                        hl = sphl[(cnt - 1) % 2]
                        P.copy("dve", hl[:, 0, :], sp[:, :])
                        P.tt("dve", hl[:, 1, :], sp[:, :], hl[:, 0, :], ALU.subtract)
                        return hl

                    if S.cfg.get("b_stop", 9) < 1:
                        continue
                    order = [list(range(NTL)), [1, 0] + list(range(NTL - 1, 1, -1))]
                    for s in range(NTL):
                        for d in range(2):
                            n = order[d][s]
                            sp = gate_sp(d, n)
                            k_ = cnt
                            prm = S.pb[2 + d]
                            for z in range(2):
                                P.mm(prm[:, 0:128], SLb if d == 0 else SUb, sp[:, z, :], start=(z == 0), stop=(z == 1))
                            for z in range(2):
                                P.mm(prm[:, 128:129], sp[:, z, :], S.onesb[:, 0:1], start=(z == 0), stop=(z == 1))
                            erem = eremb[d]; kg = kgb[d]; gl = glb[(s * 2 + d) % 4]
                            P.act(erem[:, :], prm[:, 0:128], AF.Exp, scale=-1.0 / 16)
                            P.act(gl[:, :], prm[:, 128:129], AF.Exp, scale=-1.0 / 16)
                            P.tt("dve", kg[:, :], ktok[:, n, :], erem[:, :], ALU.mult)
                            P.copy("act", Sst[:, d, n, :], Sf[d][:, :])
                            if s == NTL - 1:
                                continue
                            pS = S.pb[4 + d]
                            P.mm(pS[:, 0:256], kg[:, :], vtok[:, n, :])
                            for hh in range(2):
                                sl = slice(hh * 64, (hh + 1) * 64)
                                P.stt("dve", Sf[d][sl, :], Sf[d][sl, :], gl[sl, :], pS[sl, hh * 128:(hh + 1) * 128],
                                      ALU.mult, ALU.add)

                    if S.cfg.get("b_stop", 9) < 2:
                        continue
                    egb = [P.sb("beg", [128, 128], F32, st) for _ in range(2)]
                    eib = [P.sb("bei", [128, 128], F32, st) for _ in range(2)]
                    qtb = [P.sb("bqt", [128, 128], BF16, st) for _ in range(4)]
                    ktb = [P.sb("bkt", [128, 128], BF16, st) for _ in range(4)]
                    atb = [P.sb("bat", [128, 2, 128], BF16, st) for _ in range(4)]
                    smb = [P.sb("bsm", [128, 2], F32, st) for _ in range(4)]
                    junk = P.sb("bjunk", [128, 128], BF16, st)
                    y1b = [P.sb("by1", [128, 128], F32, st) for _ in range(2)]
                    ytb = [P.sb("byt", [128, 128], BF16, st) for _ in range(2)]
                    ptb = S.pb[7][:, 0:256].bitcast(BF16)
                    k2 = 0
                    ep = 0
                    for n in (range(NTL) if ctx_out else range(2, NTL)):
                        dd = []
                        for d in range(2):
                            sp = gate_sp(d, n)
                            pg = S.pb[2]
                            for z in range(2):
                                P.mm(pg[:, 0:128], sp[:, z, :], Ub if d == 0 else Lb, start=(z == 0), stop=(z == 1))
                            eg = egb[d]; ei = eib[d]
                            qt = qtb[k2 % 4]; kt = ktb[k2 % 4]; at = atb[k2 % 4]
                            k2 += 1
                            P.act(eg[:, :], pg[:, 0:128], AF.Exp, scale=-1.0 / 16)
                            P.act(ei[:, :], pg[:, 0:128], AF.Exp, scale=1.0 / 16)
                            P.stt("dve", qt[:, :], qT[:, n * 128:(n + 1) * 128], 0.125, eg[:, :], ALU.mult, ALU.mult)
                            P.tt("pool", kt[:, :], kT[:, n * 128:(n + 1) * 128], ei[:, :], ALU.mult)
                            if S.cfg.get("p2", 9) < 1:
                                continue
                            for hh in range(2):
                                sl = slice(hh * 64, (hh + 1) * 64)
                                P.mm(S.pb[3 + hh][:, 0:128], kt[sl, :], qt[sl, :])
                            mask = (U if d == 0 else Lm)
                            for hh in range(2):
                                P.tt("dve", at[:, hh, :], S.pb[3 + hh][:, 0:128], mask, ALU.mult)
                            dd.append((qt, at))
                        if S.cfg.get("p2", 9) < 2:
                            continue
                        for hh in range(2):
                            sl = slice(hh * 64, (hh + 1) * 64)
                            oo = S.pb[5 + hh][:, 0:128]
                            for d in range(2):
                                qt, at = dd[d]
                                P.mm(oo, qt[sl, :], Sst[sl, d, n, :], start=(d == 0), stop=False)
                                P.mm(oo, at[:, hh, :], vtok[:, n, hh * 128:(hh + 1) * 128], start=False, stop=(d == 1))
                        if S.cfg.get("p2", 9) < 3:
                            continue
                        for hh in range(2):
                            oo = S.pb[5 + hh][:, 0:128]
                            s_ = smb[ep % 4]; y1 = y1b[ep % 2]; yt = ytb[ep % 2]
                            ep += 1
                            P.memset("pool", s_[:, 0:1], 0.0)
                            P.act(junk[:, :], oo, AF.Square, accum_out=s_[:, 0:1])
                            P.act(s_[:, 1:2], s_[:, 0:1], AF.Sqrt, bias=S.eps_t[:, :], scale=1.0 / 128)
                            P.recip(s_[:, 1:2], s_[:, 1:2])
                            P.stt("dve", y1[:, :], oo, s_[:, 1:2], gnbc[:, :], ALU.mult, ALU.mult)
                            P.tt("pool", yt[:, :], y1[:, :], srtok[:, n, hh * 128:(hh + 1) * 128], ALU.mult)
                            P.tr(ptb[:, hh * 128:(hh + 1) * 128], yt[:, :], S.identb)
                        P.copy("act", ybT[:, hp * 2:hp * 2 + 2, n * 128:(n + 1) * 128],
                               ptb[:, 0:256].rearrange("p (h t) -> p h t", h=2))
        if S.cfg.get("dump_yb") == l:
            P.dump("ybT", ybT[:, :, :])
        if S.cfg.get("merge", True):
            with P.phase() as st:
                merge_branch(S, l, ybT, S.w_o_b, G_B, ctx_out, st)


def branch_a(S, l, ctx_out):
    P = S.P
    U = S.cf[:, 256:384]
    Lm = S.cf[:, 384:512]
    SU = S.cf[:, 512:640]
    SL = S.cf[:, 640:768]
    with P.phase() as sto:
        yaT = P.sb("yaT", [128, 4, NT], BF16, sto)
        if not ctx_out:
            P.memset("pool", yaT[:, :, 0:NCX], 0.0)
        with P.phase() as stc:
            gnbc = P.sb("gnbca", [128, 128], F32, stc)
            P.dma("sp", gnbc[:, :], S.gn[l, 0].partition_broadcast(128))
            one_t = P.sb("one_ta", [128, 1], F32, stc)
            P.memset("dve", one_t[:, :], 1.0)
            mkb = P.sb("mkba", [128, 4, 128], BF16, stc)
            P.copy("dve", mkb[:, :, :], S.cf[:, 256:768].rearrange("p (m c) -> p m c", m=4))
            ones_col = P.sb("onescol", [128, 1], BF16, stc)
            P.memset("dve", ones_col[:, :], 1.0)
            cum1 = P.sb("cum1", [128, 2, 129], BF16, stc)
            P.memset("dve", cum1[:, :, :], 1.0)
            P.copy("dve", cum1[:, 0, 0:128], U)
            P.copy("dve", cum1[:, 1, 0:128], Lm)
            cumb = [mkb[:, 0, :], mkb[:, 1, :]]
            gmf = [SL, SU]
            strict = [SL, SU]
            maskT = [U, Lm]
            cw = P.sb("convw", [128, 12, 3], F32, stc)
            P.dma("sp", cw[:, :, :], S.convw[l])
            adt = P.sb("adt", [128, 2, 8], F32, stc)
            P.dma("sp", adt[:, :, :], S.adt[l].partition_broadcast(128))
            nA = P.sb("nA", [128, 8], F32, stc)
            P.act(nA[:, :], adt[:, 0, :], AF.Exp)
            P.ts("dve", nA[:, :], nA[:, :], -1.0, None, ALU.mult)
            betat = P.sb("betat", [128, NTL, 8], F32, stc)
            nbetat = P.sb("nbetat", [128, NTL, 8], F32, stc)
            gt = P.sb("gt", [128, NTL, 8], F32, stc)
            with P.phase() as stw:
                wbg = P.sb("wbg", [128, KC, 16], BF16, stw)
                P.dma("pool", wbg[:, :, :], S.w_in[l, :, A_B:A_B + 16].rearrange("(kc p) f -> p kc f", p=128))
                tmpe = P.sb("tmpe", [128, NTL, 8], F32, stw)
                for n in range(NTL):
                    pb = S.pb[n % 2]
                    for kc in range(KC):
                        P.mm(pb[:, 0:16], S.hT[:, kc, n * 128:(n + 1) * 128], wbg[:, kc, :],
                             start=(kc == 0), stop=(kc == KC - 1))
                    P.act(betat[:, n, :], pb[:, 0:8], AF.Sigmoid)
                    P.tt("dve", gt[:, n, :], pb[:, 8:16], adt[:, 1, :], ALU.add)
                P.act(tmpe[:, :, :], gt[:, :, :], AF.Exp)
                P.act(gt[:, :, :], tmpe[:, :, :], AF.Ln, bias=one_t[:, :])
                P.tt("dve", gt[:, :, :], gt[:, :, :], nA[:, :].unsqueeze(1).to_broadcast([128, NTL, 8]), ALU.mult)
                P.ts("dve", nbetat[:, :, :], betat[:, :, :], -1.0, None, ALU.mult)

            for h in range(4):
                with P.phase() as st:
                    qT = P.sb("aqT", [128, NT], BF16, st)
                    kT = P.sb("akT", [128, NT], BF16, st)
                    ktok = P.sb("aktok", [128, NTL, 128], BF16, st)
                    vtok = P.sb("avtok", [128, NTL, 128], BF16, st)
                    sztok = P.sb("asztok", [128, NTL, 128], BF16, st)
                    oacc = P.sb("aoacc", [128, NTL, 128], F32, st)
                    P.memset("pool", oacc[:, :, :], 0.0)
                    ptb = S.pb[7][:, 0:256].bitcast(BF16)
                    with P.phase() as stw:
                        wA = P.sb("wAh", [128, KC, 512], BF16, stw)
                        for i, c0 in enumerate((A_Q, A_K, A_V, A_Z)):
                            P.dma("pool", wA[:, :, i * 128:(i + 1) * 128],
                                  S.w_in[l, :, c0 + h * 128:c0 + (h + 1) * 128].rearrange("(kc p) f -> p kc f", p=128))
                        pre = [P.sb("apre", [128, NT], BF16, stw) for _ in range(2)]
                        cv = [P.sb("acv", [128, NT], BF16, stw) for _ in range(2)]
                        sqb = P.sb("asq", [128, 512], BF16, stw)
                        rs = P.sb("ars", [128, 512], F32, stw)
                        vT = P.sb("avT", [128, NT], BF16, stw)
                        for i in range(3):
                            pr = pre[i % 2]; c = cv[i % 2]
                            for bi in range(5):
                                t0, tn = TB[bi]
                                pb = S.pb[bi % 2]
                                for kc in range(KC):
                                    P.mm(pb[:, :tn], wA[:, kc, i * 128:(i + 1) * 128], S.hT[:, kc, t0:t0 + tn],
                                         start=(kc == 0), stop=(kc == KC - 1))
                                P.act(pr[:, t0:t0 + tn], pb[:, :tn], AF.Copy)
                            ch = i * 4 + h
                            P.ts("dve", c[:, :], pr[:, :], cw[:, ch, 1:2], None, ALU.mult)
                            for (a, b) in ((0, NCX), (NCX, NT)):
                                P.stt("dve", c[:, a + 1:b], pr[:, a:b - 1], cw[:, ch, 0:1], c[:, a + 1:b], ALU.mult, ALU.add)
                                P.stt("dve", c[:, a:b - 1], pr[:, a + 1:b], cw[:, ch, 2:3], c[:, a:b - 1], ALU.mult, ALU.add)
                            if i == 2:
                                P.act(vT[:, :], c[:, :], AF.Silu)
                            else:
                                dst = qT if i == 0 else kT
                                P.act(c[:, :], c[:, :], AF.Silu)
                                for bi in range(5):
                                    t0, tn = TB[bi]
                                    pb = S.pb[2 + bi % 2]
                                    P.act(sqb[:, :tn], c[:, t0:t0 + tn], AF.Square)
                                    P.mm(pb[:, :tn], S.onesb, sqb[:, :tn])
                                    P.act(rs[:, :tn], pb[:, :tn], AF.Sqrt, bias=S.eps_t[:, :])
                                    P.recip(rs[:, :tn], rs[:, :tn])
                                    if i == 0:
                                        P.stt("dve", dst[:, t0:t0 + tn], c[:, t0:t0 + tn], 128.0 ** -0.5, rs[:, :tn],
                                              ALU.mult, ALU.mult)
                                    else:
                                        P.tt("dve", dst[:, t0:t0 + tn], c[:, t0:t0 + tn], rs[:, :tn], ALU.mult)
                        for n in range(NTL):
                            P.tr(ptb[:, 0:128], kT[:, n * 128:(n + 1) * 128], S.identb)
                            P.tr(ptb[:, 128:256], vT[:, n * 128:(n + 1) * 128], S.identb)
                            P.copy("dve", ktok[:, n, :], ptb[:, 0:128])
                            P.copy("dve", vtok[:, n, :], ptb[:, 128:256])
                            pz = S.pb[n % 2]
                            for kc in range(KC):
                                P.mm(pz[:, 0:128], S.hT[:, kc, n * 128:(n + 1) * 128], wA[:, kc, 384:512],
                                     start=(kc == 0), stop=(kc == KC - 1))
                            P.act(sztok[:, n, :], pz[:, 0:128], AF.Silu)

                    G = S.cfg.get("a_G", 4)
                    NRG = 4
                    def ring(nm, shape, dt):
                        return [[P.sb(nm, shape, dt, st) for _ in range(NRG)] for _ in range(2)]
                    u_r = ring("au", [128, 128], F32)
                    wT_r = ring("awT", [128, 128], BF16)
                    kg_r = ring("akg", [128, 128], BF16)
                    AT_r = ring("aAT", [128, 128], BF16)
                    sc_r = ring("asc", [128, 4], F32)

                    class WS:
                        pass
                    wss = []
                    for gi in range(G):
                        w_ = WS()
                        w_.gf = P.sb("agmf", [128, 129], F32, st)
                        w_.ghl = P.sb("agmhl", [128, 2, 129], BF16, st)
                        w_.e1 = P.sb("aE1", [128, 129], F32, st)
                        w_.e2 = P.sb("aE2", [128, 129], F32, st)
                        w_.XX = [P.sb("aXX", [128, 2, 128], F32, st) for _ in range(2)]
                        w_.PT = [P.sb("aPT", [128, 128], F32, st) for _ in range(2)]
                        w_.vb = P.sb("avb", [128, 128], BF16, st)
                        w_.kbg = P.sb("akbg", [128, 128], BF16, st)
                        w_.TT = P.sb("aTTb", [128, 128], BF16, st)
                        w_.pc = S.pb[2 + gi]
                        w_.pP = S.pb[6 + gi % 2]
                        w_.ev = "act"
                        wss.append(w_)
                    vnew = [P.sb("avnew", [128, 128], BF16, st) for _ in range(2)]
                    o1s = [P.sb("ao1s", [128, 128], F32, st) for _ in range(2)]
                    ot = [P.sb("aot", [128, 128], F32, st) for _ in range(2)]
                    Sf = [P.sb("aSf", [128, 128], F32, st) for _ in range(2)]
                    Sb = [P.sb("aSb", [128, 128], BF16, st) for _ in range(2)]
                    for d in range(2):
                        P.memset("dve", Sf[d][:, :], 0.0)
                        P.memset("dve", Sb[d][:, :], 0.0)
                    order = [list(range(NTL)), [1, 0] + list(range(NTL - 1, 1, -1))]

                    def precompute(d, n, slot, w_):
                        col = d * 4 + h
                        g = gt[:, n, col:col + 1]
                        beta = betat[:, n, col:col + 1]
                        nbeta = nbetat[:, n, col:col + 1]
                        tl = slice(n * 128, (n + 1) * 128)
                        sc = sc_r[d][slot]
                        pc = w_.pc
                        gf = w_.gf; ghl = w_.ghl; e1 = w_.e1; e2 = w_.e2
                        P.ts("dve", gf[:, 0:128], gmf[d], g, None, ALU.mult)
                        P.copy("dve", gf[:, 128:129], g)
                        P.copy("dve", ghl[:, 0, :], gf[:, :])
                        P.tt("dve", ghl[:, 1, :], gf[:, :], ghl[:, 0, :], ALU.subtract)
                        for z in range(2):
                            P.mm(pc[:, 0:129], cumb[d], ghl[:, z, :], start=(z == 0), stop=(z == 1))
                        for z in range(2):
                            P.mm(pc[:, 256:385], ghl[:, z, 0:128], cum1[:, d, :], start=(z == 0), stop=(z == 1))
                        P.act(e1[:, :], pc[:, 0:129], AF.Exp)
                        P.act(e2[:, :], pc[:, 256:385], AF.Exp)
                        yield
                        P.tt("pool", e1[:, 0:128], e1[:, 0:128], strict[d], ALU.mult)
                        P.tt("pool", e2[:, 0:128], e2[:, 0:128], maskT[d], ALU.mult)
                        P.tt("dve", sc[:, 2:3], e1[:, 128:129], e2[:, 128:129], ALU.mult)
                        P.tt("dve", sc[:, 3:4], e1[:, 128:129], beta, ALU.mult)
                        P.copy("dve", sc[:, 0:1], e1[:, 128:129])
                        P.mm(pc[:, 0:128], kT[:, tl], kT[:, tl])
                        P.mm(pc[:, 128:256], kT[:, tl], qT[:, tl])
                        XX = w_.XX; PT = w_.PT
                        P.stt("dve", XX[0][:, 0, :], pc[:, 0:128], nbeta, e1[:, 0:128], ALU.mult, ALU.mult)
                        P.tt("dve", AT_r[d][slot][:, :], pc[:, 128:256], e2[:, 0:128], ALU.mult)
                        P.ts("dve", kg_r[d][slot][:, :], ktok[:, n, :], e2[:, 128:129], None, ALU.mult)
                        P.ts("dve", w_.vb[:, :], vtok[:, n, :], beta, None, ALU.mult)
                        P.ts("dve", w_.kbg[:, :], ktok[:, n, :], sc[:, 3:4], None, ALU.mult)
                        yield
                        P.tr(pc[:, 384:512], XX[0][:, 0, :], S.identf)
                        P.copy(w_.ev, XX[0][:, 1, :], pc[:, 384:512])
                        P.tt("dve", PT[0][:, :], pc[:, 384:512], S.identf, ALU.add)
                        yield
                        cur = 0
                        for lev in range(1, 7):
                            nxt = 1 - cur
                            if lev > 1:
                                P.mm(w_.pP[:, 0:128], XX[cur][:, 0, :], PT[lev % 2][:, :])
                                P.tt("dve", PT[1 - (lev % 2)][:, :], w_.pP[:, 0:128], PT[lev % 2][:, :], ALU.add)
                            P.mm(pc[:, 0:128], XX[cur][:, 1, :], XX[cur][:, 0, :])
                            if lev < 6:
                                P.mm(pc[:, 128:256], XX[cur][:, 0, :], XX[cur][:, 1, :])
                                P.copy(w_.ev, XX[nxt][:, :, :], pc[:, 0:256].rearrange("p (a b) -> p a b", a=2))
                            else:
                                P.copy(w_.ev, XX[nxt][:, 0, :], pc[:, 0:128])
                            cur = nxt
                            yield
                        P.mm(w_.pP[:, 0:128], XX[cur][:, 0, :], PT[1][:, :])
                        P.tt("dve", PT[0][:, :], w_.pP[:, 0:128], PT[1][:, :], ALU.add)
                        P.copy("act", w_.TT[:, :], PT[0][:, :])
                        yield
                        P.mm(pc[:, 0:128], w_.TT[:, :], w_.vb[:, :])
                        P.mm(pc[:, 128:256], w_.kbg[:, :], w_.TT[:, :])
                        P.copy("act", u_r[d][slot][:, :], pc[:, 0:128])
                        P.copy("act", wT_r[d][slot][:, :], pc[:, 128:256])

                    def recur(d, n, slot, last):
                        tl = slice(n * 128, (n + 1) * 128)
                        pr = S.pb[d]
                        sc = sc_r[d][slot]
                        P.mm(pr[:, 0:128], wT_r[d][slot][:, :], Sb[d][:, :])
                        P.mm(pr[:, 128:256], qT[:, tl], Sb[d][:, :])
                        P.tt("dve", vnew[d][:, :], u_r[d][slot][:, :], pr[:, 0:128], ALU.subtract)
                        want_out = ctx_out or n >= 2
                        yield
                        if want_out:
                            P.mm(pr[:, 256:384], AT_r[d][slot][:, :], vnew[d][:, :])
                        if not last:
                            P.mm(pr[:, 384:512], kg_r[d][slot][:, :], vnew[d][:, :])
                        if want_out:
                            P.act(o1s[d][:, :], pr[:, 128:256], AF.Copy, scale=sc[:, 0:1])
                        if not last:
                            P.stt("dve", Sf[d][:, :], Sf[d][:, :], sc[:, 2:3], pr[:, 384:512], ALU.mult, ALU.add)
                            P.copy("act", Sb[d][:, :], Sf[d][:, :])
                        if want_out:
                            P.tt("dve", ot[d][:, :], pr[:, 256:384], o1s[d][:, :], ALU.add)
                            P.tt("pool", oacc[:, n, :], oacc[:, n, :], ot[d][:, :], ALU.add)
                        yield

                    units = [(d, s) for s in range(NTL) for d in range(2)]
                    pre_done = set()
                    active = []
                    next_unit = 0
                    rec_step = [0, 0]
                    rec_gen = [None, None]
                    free_ws = list(range(G))
                    while True:
                        while free_ws and next_unit < len(units):
                            d_, s_ = units[next_unit]
                            if s_ - rec_step[d_] >= NRG - 1:
                                break
                            wi = free_ws.pop(0)
                            active.append(("pre", (d_, s_, wi), precompute(d_, order[d_][s_], s_ % NRG, wss[wi])))
                            next_unit += 1
                        for d_ in range(2):
                            if rec_gen[d_] is None and rec_step[d_] < NTL and (d_, rec_step[d_]) in pre_done:
                                s_ = rec_step[d_]
                                rec_gen[d_] = recur(d_, order[d_][s_], s_ % NRG, s_ == NTL - 1)
                        if not active and rec_gen[0] is None and rec_gen[1] is None:
                            if next_unit >= len(units) and rec_step[0] >= NTL and rec_step[1] >= NTL:
                                break
                        S.a_hist = getattr(S, "a_hist", [])
                        S.a_hist.append((len(active), rec_gen[0] is not None, rec_gen[1] is not None))
                        for item in list(active):
                            kind, key, gen = item
                            try:
                                next(gen)
                            except StopIteration:
                                active.remove(item)
                                pre_done.add((key[0], key[1]))
                                free_ws.append(key[2])
                        for d_ in range(2):
                            if rec_gen[d_] is not None:
                                try:
                                    next(rec_gen[d_])
                                except StopIteration:
                                    rec_gen[d_] = None
                                    rec_step[d_] += 1

                    smb = [P.sb("asm", [128, 2], F32, st) for _ in range(4)]
                    junk = P.sb("ajunk", [128, 128], BF16, st)
                    y1b = [P.sb("ay1", [128, 128], F32, st) for _ in range(2)]
                    ytb = [P.sb("ayt", [128, 128], BF16, st) for _ in range(2)]
                    ep = 0
                    for n in (range(NTL) if ctx_out else range(2, NTL)):
                        s_ = smb[ep % 4]; y1 = y1b[ep % 2]; yt = ytb[ep % 2]
                        ep += 1
                        P.memset("pool", s_[:, 0:1], 0.0)
                        P.act(junk[:, :], oacc[:, n, :], AF.Square, accum_out=s_[:, 0:1])
                        P.act(s_[:, 1:2], s_[:, 0:1], AF.Sqrt, bias=S.eps_t[:, :], scale=1.0 / 128)
                        P.recip(s_[:, 1:2], s_[:, 1:2])
                        P.stt("dve", y1[:, :], oacc[:, n, :], s_[:, 1:2], gnbc[:, :], ALU.mult, ALU.mult)
                        P.tt("pool", yt[:, :], y1[:, :], sztok[:, n, :], ALU.mult)
                        P.tr(ptb[:, 0:128], yt[:, :], S.identb)
                        P.copy("act", yaT[:, h, n * 128:(n + 1) * 128], ptb[:, 0:128])
        if S.cfg.get("dump_ya") == l:
            P.dump("yaT", yaT[:, :, :])
        if S.cfg.get("merge", True):
            with P.phase() as st:
                merge_branch(S, l, yaT, S.w_o_a, G_A, ctx_out, st)


def phase_ffn(S, l):
    P = S.P
    moe = (l % 2 == 1)
    ctx_out = l < 1
    blocks = list(range(5)) if ctx_out else list(range(1, 5))
    i_ = l // 2
    with P.phase() as sto:
        gate = None
        if moe:
            logit = P.sb("logit", [128, 16, 8], F32, sto)
            gate = P.sb("gate", [128, 16, 8], F32, sto)
        with P.phase() as st:
            router = None
            if moe:
                rw = P.sb("rw", [128, KC, 8], F32, st)
                P.dma("sp", rw[:, :, :], S.router_w[i_].rearrange("(kc p) e -> p kc e", p=128))
                router = (rw, logit)
            modulate(S, l, 1, blocks, st, router=router)
        if moe:
            with P.phase() as st:
                m1 = P.sb("m1", [128, 16], F32, st)
                m2 = P.sb("m2", [128, 16], F32, st)
                eq1 = P.sb("eq1", [128, 16, 8], F32, st)
                eq2 = P.sb("eq2", [128, 16, 8], F32, st)
                l2 = P.sb("l2", [128, 16, 8], F32, st)
                ww = P.sb("ww", [128, 3, 16], F32, st)
                bc = lambda a: a.unsqueeze(2).to_broadcast([128, 16, 8])
                P.reduce("dve", m1[:, :], logit[:, :, :], ALU.max, AX.X)
                P.tt("dve", eq1[:, :, :], logit[:, :, :], bc(m1[:, :]), ALU.is_equal)
                P.stt("dve", l2[:, :, :], eq1[:, :, :], -1e30, logit[:, :, :], ALU.mult, ALU.add)
                P.reduce("dve", m2[:, :], l2[:, :, :], ALU.max, AX.X)
                P.tt("dve", eq2[:, :, :], l2[:, :, :], bc(m2[:, :]), ALU.is_equal)
                P.tt("dve", ww[:, 0, :], m2[:, :], m1[:, :], ALU.subtract)
                P.act(ww[:, 0, :], ww[:, 0, :], AF.Exp)
                P.ts("dve", ww[:, 1, :], ww[:, 0, :], 1.0, None, ALU.add)
                P.recip(ww[:, 1, :], ww[:, 1, :])
                P.tt("dve", ww[:, 2, :], ww[:, 0, :], ww[:, 1, :], ALU.mult)
                P.tt("dve", eq1[:, :, :], eq1[:, :, :], bc(ww[:, 1, :]), ALU.mult)
                P.tt("dve", eq2[:, :, :], eq2[:, :, :], bc(ww[:, 2, :]), ALU.mult)
                P.tt("dve", gate[:, :, :], eq1[:, :, :], eq2[:, :, :], ALU.add)
            if S.cfg.get("dump_gate"):
                P.dump("gate", gate[:, :, :])
        with P.phase() as st:
            GR = 6
            groups = [(0, 6), (6, 6), (12, 6), (18, 4)]
            gbuf = P.sb("gbuf", [128, GR, NL if moe else NT], BF16, st)
            w13 = [P.sb("w13", [128, 2, KC, 384], BF16, st) for _ in range(2)]
            w2b = [P.sb("w2b", [128, GR, 1024], BF16, st) for _ in range(2)]
            sil = [P.sb("sil", [128, 512], F32, st) for _ in range(2)]
            tmb = [P.sb("tmb", [128, 512], F32 if moe else BF16, st) for _ in range(2)]
            gbce = P.sb("gbce", [128, NL], BF16, st) if moe else None
            gdg = [P.sb("gdg", [128, 128], F32, st) for _ in range(2)] if moe else None
            nw13 = 0; nw2 = 0; nt = 0; ng_ = 0
            experts = range(8) if moe else range(1)
            if moe:
                blks = [(bi, TB[bi][0], TB[bi][0] - NCX, TB[bi][1]) for bi in blocks]
            else:
                blks = [(bi, TB[bi][0], TB[bi][0], TB[bi][1]) for bi in blocks]
            for e in experts:
                W1 = S.moe_w1[i_, e] if moe else S.ffn_w1[i_]
                W3 = S.moe_w3[i_, e] if moe else S.ffn_w3[i_]
                W2 = S.moe_w2[i_, e] if moe else S.ffn_w2[i_]
                if moe:
                    for b4 in range(4):
                        pg = S.pb[7]
                        for j in range(4):
                            tile = b4 * 4 + j
                            gd = gdg[tile % 2]
                            P.ts("dve", gd[:, :], S.identf, gate[:, tile, e:e + 1], None, ALU.mult)
                            P.mm(pg[:, j * 128:(j + 1) * 128], S.onesf, gd[:, :])
                        P.copy("act", gbce[:, b4 * 512:(b4 + 1) * 512], pg[:, :])
                for (f0, nf) in groups:
                    w2t = w2b[nw2 % 2]; nw2 += 1
                    P.dma("pool", w2t[:, 0:nf, :],
                          W2[f0 * 128:(f0 + nf) * 128, :].rearrange("(c p) f -> p c f", p=128))
                    for p0 in range(0, nf, 3):
                        npf = min(3, nf - p0)
                        wt = w13[nw13 % 2]; nw13 += 1
                        c0 = (f0 + p0) * 128
                        P.dma("pool", wt[:, 0, :, 0:npf * 128],
                              W1[:, c0:c0 + npf * 128].rearrange("(kc p) f -> p kc f", p=128))
                        P.dma("pool", wt[:, 1, :, 0:npf * 128],
                              W3[:, c0:c0 + npf * 128].rearrange("(kc p) f -> p kc f", p=128))
                        for (bi, x0, g0, tn) in blks:
                            for fi in range(npf):
                                pa = S.pb[0 + nt % 2]; pbb = S.pb[2 + nt % 2]
                                sl_ = sil[nt % 2]; tm = tmb[nt % 2]
                                nt += 1
                                for kc in range(KC):
                                    P.mm(pa[:, :tn], wt[:, 0, kc, fi * 128:(fi + 1) * 128], S.hT[:, kc, x0:x0 + tn],
                                         start=(kc == 0), stop=(kc == KC - 1))
                                for kc in range(KC):
                                    P.mm(pbb[:, :tn], wt[:, 1, kc, fi * 128:(fi + 1) * 128], S.hT[:, kc, x0:x0 + tn],
                                         start=(kc == 0), stop=(kc == KC - 1))
                                P.act(sl_[:, :tn], pa[:, :tn], AF.Silu)
                                gdst = gbuf[:, p0 + fi, g0:g0 + tn]
                                if moe:
                                    P.tt("dve", tm[:, :tn], pbb[:, :tn], sl_[:, :tn], ALU.mult)
                                    P.tt("pool", gdst, tm[:, :tn], gbce[:, g0:g0 + tn], ALU.mult)
                                else:
                                    P.tt("dve", gdst, pbb[:, :tn], sl_[:, :tn], ALU.mult)
                    for fo in range(8):
                        for (bi, x0, g0, tn) in blks:
                            which = 1 if bi == 0 else 0
                            po = S.pb[4 + ng_ % 3]; ng_ += 1
                            for fc in range(nf):
                                P.mm(po[:, :tn], w2t[:, fc, fo * 128:(fo + 1) * 128], gbuf[:, fc, g0:g0 + tn],
                                     start=(fc == 0), stop=(fc == nf - 1))
                            P.stt("dve", S.xT[:, fo, x0:x0 + tn], po[:, :tn], S.modv[:, l, 40 + fo, which:which + 1],
                                  S.xT[:, fo, x0:x0 + tn], ALU.mult, ALU.add)
    if S.cfg.get("dump_xffn") == l:
        P.dump("xT", S.xT[:, :, :])

def extra_shared(shared, inputs, f):
    shared["rope"] = _rope_tables()
    shared["gn"] = f(np.stack([inputs["gn_a"], inputs["gn_b"], inputs["gn_c"]], axis=1))
    shared["lam_c"] = f(inputs["lam_c"])
    cw = np.asarray(inputs["conv_a"], np.float32)
    shared["convw"] = f(cw.reshape(2, 3, 12, 128).transpose(0, 3, 2, 1))
    shared["adt"] = f(np.stack([np.asarray(inputs["a_log"], np.float32).reshape(2, 8),
                                np.asarray(inputs["dt_bias"], np.float32).reshape(2, 8)], axis=1))
    wg2 = np.concatenate([np.asarray(inputs["w_gate2"], np.float32),
                          np.asarray(inputs["b_gate"], np.float32)[:, :, None, :]], axis=2)
    shared["wg2"] = f(wg2.transpose(0, 2, 1, 3))
    for k in ("w_o_a", "w_o_b", "w_o_c", "w_out", "ffn_w1", "ffn_w3", "ffn_w2", "router_w", "moe_w1", "moe_w3", "moe_w2"):
        shared[k] = f(inputs[k])

def _consts():
    c = np.zeros((128, 1024), np.float32)
    c[:, 0:128] = np.eye(128, dtype=np.float32)
    for p in range(128):
        if (p % 32) < 16:
            c[p + 16, 128 + p] = -1.0
        else:
            c[p - 16, 128 + p] = 1.0
    k = np.arange(128)[:, None]
    i = np.arange(128)[None, :]
    c[:, 256:384] = (k <= i)
    c[:, 384:512] = (k >= i)
    c[:, 512:640] = (k < i)
    c[:, 640:768] = (k > i)
    return c


def _rope_tables():
    t = np.arange(2048)
    row = (t // 64).astype(np.float32)
    col = (t % 64).astype(np.float32)
    inv_freq = (np.float32(10000.0) ** (-np.arange(16, dtype=np.float32) / np.float32(16))).astype(np.float32)
    tab = np.zeros((2, 128, 2048), np.float32)
    for p in range(128):
        d = p % 64
        pos = row if d < 32 else col
        ang = (pos * inv_freq[d % 16]).astype(np.float32)
        tab[0, p] = np.cos(ang)
        tab[1, p] = np.sin(ang)
    return tab


def _fm(v):
    v = np.asarray(v)
    lead = v.shape[:-1]
    n = v.shape[-1] // 128
    w = v.reshape(lead + (n, 128))
    return np.ascontiguousarray(np.moveaxis(w, -1, 0))


_CACHE = {}


def _get_prog(cfg_key, cfg):
    if cfg_key not in _CACHE:
        _CACHE[cfg_key] = build(cfg)
    return _CACHE[cfg_key]


def make_in_maps(inputs, cfg):
    f = lambda a: np.ascontiguousarray(np.asarray(a, dtype=np.float32))
    x = f(inputs["x"]); c = f(inputs["c"]); ctx = f(inputs["ctx"]); c_ctx = f(inputs["c_ctx"])
    shared = {}
    shared["w_mod"] = f(inputs["w_mod"])
    shared["b_mod"] = np.ascontiguousarray(f(inputs["b_mod"]).reshape(2, 48, 128).transpose(0, 2, 1))
    ng = np.stack([_fm(inputs["norm1_g"][0]), _fm(inputs["norm1_g"][1]), _fm(inputs["norm2_g"][0]),
                   _fm(inputs["norm2_g"][1]), _fm(inputs["final_g"])], axis=1)
    shared["norm_g"] = f(ng)
    shared["w_in"] = f(inputs["w_in"])
    shared["consts"] = _consts()
    extra_shared(shared, inputs, f)
    maps = []
    for b in range(8):
        m = dict(shared)
        m["x"] = x[b]
        m["ctx"] = ctx[b]
        m["cvec"] = f(np.stack([_fm(c[b]), _fm(c_ctx)], axis=-1))
        maps.append(m)
    return maps


def run(inputs, cfg, trace=False):
    P = _get_prog(repr(sorted(cfg.items())), cfg)
    maps = make_in_maps(inputs, cfg)
    names = set()
    for ins in P.nc.main_func.allocations if False else []:
        pass
    n = cfg.get("ncores", 8)
    res = run_bass_kernel_spmd(P.nc, maps[:n], core_ids=list(range(n)), trace=trace)
    return P, res


def kernel(**inputs):
    cfg = dict(layers=2)
    P, res = run(inputs, cfg)
    out = np.stack([np.asarray(res.results[b]["out"], dtype=np.float32) for b in range(8)], axis=0)
    return out
```

```python
import contextlib
import numpy as np
import concourse.bass as bass
import concourse.mybir as mybir
from concourse.bass_utils import run_bass_kernel_spmd

F32 = mybir.dt.float32
BF16 = mybir.dt.bfloat16
AF = mybir.ActivationFunctionType
ALU = mybir.AluOpType
AX = mybir.AxisListType


class _Op:
    __slots__ = ("idx", "eng", "fn", "deps", "dma", "sem", "semval", "needs_inc", "prev_dma")


class _Unit:
    __slots__ = ("w", "r", "rd")

    def __init__(self):
        self.w = None
        self.r = {}
        self.rd = []


class V:
    __slots__ = ("ap", "key")

    def __init__(self, ap, key):
        self.ap = ap
        self.key = key


def _apk(x):
    if isinstance(x, V):
        return x.ap, x.key
    return x, None


class Prog:
    NDMA = 8

    def __init__(self):
        self.nc = bass.Bass("TRN2", target_bir_lowering=False)
        self.ops = []
        self.names = {}
        self.base_deps = []
        self.last = {}
        self.dma_since_barrier = []
        self.stack = contextlib.ExitStack()
        self.n_dma = {"sp": 0, "act": 0, "pool": 0}
        self.dma_last = {}
        self.dbg = []
        self._uid = 0
        self.psum_names = set()

    def uid(self, base):
        self._uid += 1
        return f"{base}_{self._uid}"

    def sb(self, name, shape, dtype, stack=None):
        st = stack if stack is not None else self.stack
        return st.enter_context(self.nc.sbuf_tensor(self.uid(name), list(shape), dtype))

    def ps(self, name, shape, dtype=F32, stack=None):
        st = stack if stack is not None else self.stack
        t = st.enter_context(self.nc.psum_tensor(self.uid(name), list(shape), dtype))
        self.psum_names.add(t[:].name)
        return t

    def dram(self, name, shape, dtype, kind="Internal"):
        return self.nc.dram_tensor(name, list(shape), dtype, kind=kind)

    @contextlib.contextmanager
    def phase(self):
        st = contextlib.ExitStack()
        try:
            yield st
        finally:
            self.barrier()
            st.close()

    def _conf(self, name, sub):
        d = self.names.setdefault(name, {})
        if sub is None:
            if None not in d:
                d[None] = _Unit()
            return d[None], list(d.values())
        if sub not in d:
            d[sub] = _Unit()
        res = [d[sub]]
        if None in d:
            res.append(d[None])
        return d[sub], res

    def add(self, eng, fn, reads, writes, dma=False):
        op = _Op()
        op.idx = len(self.ops)
        op.eng = eng
        op.fn = fn
        op.dma = dma
        op.sem = None
        op.semval = 0
        op.needs_inc = False
        op.prev_dma = None
        deps = {}
        for b in self.base_deps:
            deps[b.idx] = b
        for x in reads:
            if x is None or isinstance(x, (int, float)):
                continue
            ap, key = _apk(x)
            prim, conf = self._conf(ap.name, key)
            for u in conf:
                if u.w is not None:
                    deps[u.w.idx] = u.w
                if ap.name in self.psum_names:
                    for re_, r in u.r.items():
                        if re_ != eng:
                            deps[r.idx] = r
            if dma:
                prim.rd.append(op)
            else:
                prim.r[eng] = op
        for x in writes:
            ap, key = _apk(x)
            prim, conf = self._conf(ap.name, key)
            for u in conf:
                if u.w is not None:
                    deps[u.w.idx] = u.w
                for r in u.r.values():
                    deps[r.idx] = r
                for r in u.rd:
                    deps[r.idx] = r
            prim.w = op
            prim.r = {}
            prim.rd = []
        deps.pop(op.idx, None)
        if dma:
            slot = self.n_dma[eng] % self.NDMA
            self.n_dma[eng] += 1
            prev = self.dma_last.get((eng, slot))
            op.prev_dma = prev
            op.sem = (eng, slot)
            op.semval = (prev.semval if prev is not None else 0) + 16
            self.dma_last[(eng, slot)] = op
            self.dma_since_barrier.append(op)
        op.deps = list(deps.values())
        self.ops.append(op)
        self.last[eng] = op
        return op

    def barrier(self):
        b = list(self.last.values()) + list(self.dma_since_barrier)
        self.base_deps = b
        self.dma_since_barrier = []

    def mm(self, out, lhsT, rhs, start=True, stop=True, **kw):
        o, _ = _apk(out); l, _ = _apk(lhsT); r, _ = _apk(rhs)
        nc = self.nc
        return self.add("pe", lambda: nc.tensor.matmul(o, l, r, start=start, stop=stop, **kw),
                        [lhsT, rhs], [out])

    def tr(self, out, in_, ident):
        o, _ = _apk(out); i, _ = _apk(in_); d, _ = _apk(ident)
        nc = self.nc
        return self.add("pe", lambda: nc.tensor.transpose(o, i, d), [in_, ident], [out])

    def act(self, out, in_, func, bias=None, scale=None, accum_out=None):
        o, _ = _apk(out); i, _ = _apk(in_)
        kw = {}
        rd = [in_]
        wr = [out]
        if bias is not None:
            kw["bias"] = _apk(bias)[0] if not isinstance(bias, (int, float)) else bias
            rd.append(bias)
        if scale is not None:
            kw["scale"] = _apk(scale)[0] if not isinstance(scale, (int, float)) else scale
            rd.append(scale)
        if accum_out is not None:
            kw["accum_out"] = _apk(accum_out)[0]
            wr.append(accum_out)
        nc = self.nc
        return self.add("act", lambda: nc.scalar.activation(out=o, in_=i, func=func, **kw), rd, wr)

    def _ve(self, eng):
        return self.nc.vector if eng == "dve" else self.nc.gpsimd

    def tt(self, eng, out, in0, in1, op):
        o, _ = _apk(out); a, _ = _apk(in0); b, _ = _apk(in1)
        e = self._ve(eng)
        return self.add(eng, lambda: e.tensor_tensor(out=o, in0=a, in1=b, op=op), [in0, in1], [out])

    def ts(self, eng, out, in0, s1, s2=None, op0=ALU.mult, op1=None, accum_out=None):
        o, _ = _apk(out); a, _ = _apk(in0)
        e = self._ve(eng)
        s1v = s1 if isinstance(s1, (int, float)) else _apk(s1)[0]
        s2v = s2 if (s2 is None or isinstance(s2, (int, float))) else _apk(s2)[0]
        kw = {}
        wr = [out]
        if op1 is not None:
            kw["op1"] = op1
        if accum_out is not None:
            kw["accum_out"] = _apk(accum_out)[0]
            wr.append(accum_out)
        return self.add(eng, lambda: e.tensor_scalar(out=o, in0=a, scalar1=s1v, scalar2=s2v, op0=op0, **kw),
                        [in0, s1, s2], wr)

    def stt(self, eng, out, in0, scalar, in1, op0, op1):
        o, _ = _apk(out); a, _ = _apk(in0); b, _ = _apk(in1)
        e = self._ve(eng)
        sv = scalar if isinstance(scalar, (int, float)) else _apk(scalar)[0]
        return self.add(eng, lambda: e.scalar_tensor_tensor(out=o, in0=a, scalar=sv, in1=b, op0=op0, op1=op1),
                        [in0, scalar, in1], [out])

    def copy(self, eng, out, in_):
        o, _ = _apk(out); i, _ = _apk(in_)
        nc = self.nc
        if eng == "act":
            return self.add("act", lambda: nc.scalar.copy(out=o, in_=i), [in_], [out])
        e = self._ve(eng)
        return self.add(eng, lambda: e.tensor_copy(out=o, in_=i), [in_], [out])

    def memset(self, eng, out, val):
        o, _ = _apk(out)
        e = self._ve(eng)
        return self.add(eng, lambda: e.memset(o, val), [], [out])

    def reduce(self, eng, out, in_, op, axis=AX.X):
        o, _ = _apk(out); i, _ = _apk(in_)
        e = self._ve(eng)
        return self.add(eng, lambda: e.tensor_reduce(out=o, in_=i, axis=axis, op=op), [in_], [out])

    def recip(self, out, in_):
        o, _ = _apk(out); i, _ = _apk(in_)
        nc = self.nc
        return self.add("dve", lambda: nc.vector.reciprocal(out=o, in_=i), [in_], [out])

    def dma(self, q, out, in_):
        o, _ = _apk(out); i, _ = _apk(in_)
        e = {"sp": self.nc.sync, "act": self.nc.scalar, "pool": self.nc.gpsimd}[q]
        return self.add(q, lambda: e.dma_start(out=o, in_=i), [in_], [out], dma=True)

    def dump(self, name, ap, dtype=None):
        a, _ = _apk(ap)
        t = self.nc.dram_tensor("dbg_" + name, list(a.shape), dtype or a.dtype, kind="ExternalOutput")
        self.dbg.append("dbg_" + name)
        self.dma("sp", t.ap(), ap)

    def emit(self):
        nc = self.nc
        engobj = {"pe": nc.tensor, "act": nc.scalar, "dve": nc.vector, "pool": nc.gpsimd, "sp": nc.sync}
        st = self.stack
        esem = {e: st.enter_context(nc.semaphore("s_" + e)) for e in ("pe", "act", "dve", "pool")}
        dsem = {}
        for q in ("sp", "act", "pool"):
            for s in range(self.NDMA):
                if (q, s) in self.dma_last:
                    dsem[(q, s)] = st.enter_context(nc.semaphore(f"d_{q}{s}"))
        for op in self.ops:
            for p in op.deps:
                if not p.dma and not (p.eng == "pe" and op.eng == "pe"):
                    p.needs_inc = True
        finals = list(self.last.values())
        for p in finals:
            if not p.dma:
                p.needs_inc = True
        cnt = {e: 0 for e in esem}
        for op in self.ops:
            if not op.dma and op.needs_inc:
                cnt[op.eng] += 1
                op.semval = cnt[op.eng]
        waited = {e: {} for e in engobj}
        nwait = 0
        for op in self.ops:
            e = engobj[op.eng]
            need = {}
            for p in op.deps:
                if p.dma:
                    k = ("d", p.sem)
                    v = p.semval
                else:
                    if p.eng == "pe" and op.eng == "pe":
                        continue
                    k = ("e", p.eng)
                    v = p.semval
                if need.get(k, 0) < v:
                    need[k] = v
            if op.dma and op.prev_dma is not None:
                k = ("d", op.sem)
                if need.get(k, 0) < op.prev_dma.semval:
                    need[k] = op.prev_dma.semval
            w = waited[op.eng]
            for k, v in need.items():
                if w.get(k, 0) >= v:
                    continue
                w[k] = v
                sem = dsem[k[1]] if k[0] == "d" else esem[k[1]]
                e.wait_ge(sem, v)
                nwait += 1
            ins = op.fn()
            if op.dma:
                ins.then_inc(dsem[op.sem], 16)
            elif op.needs_inc:
                ins.then_inc(esem[op.eng], 1)
        sp = nc.sync
        for en, sem in esem.items():
            if cnt[en] > 0:
                sp.wait_ge(sem, cnt[en])
        for k, p in self.dma_last.items():
            sp.wait_ge(dsem[k], p.semval)
        self.stats = dict(n_ops=len(self.ops), n_wait=nwait, incs=dict(cnt))
        return nc

D = 1024
KC = 8
NL = 2048
NCX = 256
NT = 2304
NTL = 18
TB = [(0, 256), (256, 512), (768, 512), (1280, 512), (1792, 512)]
DFF = 2816
FC = 22
EPS = 1e-6
A_Q, A_K, A_V, A_Z, A_B, A_G = 0, 512, 1024, 1536, 2048, 2056
B_Q, B_K, B_V, B_R, B_GL = 2064, 2320, 2576, 3088, 3600
C_Q, C_K, C_V = 3632, 4144, 4656
G_A, G_B, G_D = 5168, 6192, 7216


class Ctx:
    pass


def build(cfg):
    P = Prog()
    nc = P.nc
    S = Ctx()
    S.P = P
    S.cfg = cfg
    L = cfg.get("layers", 2)

    def din(name, shape, dt=F32):
        return nc.dram_tensor(name, list(shape), dt, kind="ExternalInput").ap()

    S.x = din("x", [NL, D])
    S.ctx = din("ctx", [NCX, D])
    S.cvec = din("cvec", [128, KC, 2])
    S.w_mod = din("w_mod", [2, D, 6 * D])
    S.b_mod = din("b_mod", [2, 128, 48])
    S.norm_g = din("norm_g", [128, 5, KC])
    S.w_in = din("w_in", [2, D, 8240])
    S.consts = din("consts", [128, 1024])
    S.rope = din("rope", [2, 128, NL])
    S.gn = din("gn", [2, 3, 128])
    S.lam_c = din("lam_c", [2, 4, 64])
    S.wg2 = din("wg2", [2, 17, 2, 256])
    S.convw = din("convw", [2, 128, 12, 3])
    S.adt = din("adt", [2, 2, 8])
    S.w_o_a = din("w_o_a", [2, 512, D])
    S.w_o_b = din("w_o_b", [2, 512, D])
    S.w_o_c = din("w_o_c", [2, 512, D])
    S.w_out = din("w_out", [2, D, D])
    S.ffn_w1 = din("ffn_w1", [1, D, DFF])
    S.ffn_w3 = din("ffn_w3", [1, D, DFF])
    S.ffn_w2 = din("ffn_w2", [1, DFF, D])
    S.router_w = din("router_w", [1, D, 8])
    S.moe_w1 = din("moe_w1", [1, 8, D, DFF])
    S.moe_w3 = din("moe_w3", [1, 8, D, DFF])
    S.moe_w2 = din("moe_w2", [1, 8, DFF, D])
    S.out = nc.dram_tensor("out", [NL, D], F32, kind="ExternalOutput").ap()

    st = P.stack
    S.xT = P.sb("xT", [128, KC, NT], F32)
    S.hT = P.sb("hT", [128, KC, NT], BF16)
    S.cf = P.sb("cf", [128, 1024], F32)
    S.identf = S.cf[:, 0:128]
    S.identb_t = P.sb("identb", [128, 128], BF16)
    S.identb = S.identb_t[:, :]
    S.onesb_t = P.sb("onesb", [128, 128], BF16)
    S.onesb = S.onesb_t[:, :]
    S.onesf_t = P.sb("onesf", [128, 128], F32)
    S.onesf = S.onesf_t[:, :]
    S.modv = P.sb("modv", [128, 2, 48, 2], F32)
    S.ng = P.sb("ng", [128, 5, KC], F32)
    S.gs = P.sb("gs", [128, 4, KC, 2], F32)
    S.pb = [P.ps(f"pb{i}", [128, 512], F32) for i in range(8)]

    P.dma("sp", S.cf[:, :], S.consts)
    P.dma("sp", S.ng[:, :, :], S.norm_g)
    P.copy("dve", S.identb, S.identf)
    P.memset("dve", S.onesb, 1.0)
    P.memset("dve", S.onesf, 1.0)
    S.eps_t = P.sb("eps_t", [128, 1], F32)
    P.memset("dve", S.eps_t[:, :], EPS)
    S.zero_t = P.sb("zero_t", [128, 1], F32)
    P.memset("dve", S.zero_t[:, :], 0.0)

    phase_load(S)
    phase_mod(S, L)
    for l in range(L):
        if cfg.get("mixer", True):
            phase_mixer(S, l)
        if cfg.get("ffn", True):
            phase_ffn(S, l)
    phase_final(S)
    P.emit()
    return P


def phase_load(S):
    P = S.P
    with P.phase() as st:
        tin = [P.sb("ld_in", [128, D], F32, st) for _ in range(3)]
        for tt in range(NTL):
            src = S.ctx[tt * 128:(tt + 1) * 128, :] if tt < 2 else S.x[(tt - 2) * 128:(tt - 1) * 128, :]
            ti = tin[tt % 3]
            P.dma("sp", ti[:, :], src)
            for half in range(2):
                pb = S.pb[(tt * 2 + half) % 4]
                for j in range(4):
                    kc = half * 4 + j
                    P.tr(pb[:, j * 128:(j + 1) * 128], ti[:, kc * 128:(kc + 1) * 128], S.identf)
                dst = S.xT[:, half * 4:(half + 1) * 4, tt * 128:(tt + 1) * 128]
                srcp = pb[:, :].rearrange("p (j t) -> p j t", j=4)
                if half == 0:
                    P.copy("dve", dst, srcp)
                else:
                    P.copy("act", dst, srcp)


def phase_mod(S, L):
    P = S.P
    with P.phase() as st:
        cv = P.sb("cv", [128, KC, 2], F32, st)
        cvb = P.sb("cvb", [128, KC, 2], BF16, st)
        bm = P.sb("bm", [128, 2, 48], F32, st)
        wm = [P.sb("wm", [128, KC, 1024], BF16, st) for _ in range(2)]
        P.dma("sp", cv[:, :, :], S.cvec)
        P.dma("sp", bm[:, :, :], S.b_mod.rearrange("l p f -> p l f"))
        P.act(cvb[:, :, :], cv[:, :, :], AF.Silu)
        n = 0
        for l in range(L):
            for i in range(6):
                w = wm[n % 2]
                n += 1
                P.dma("pool", w[:, :, :],
                      S.w_mod[l, :, i * 1024:(i + 1) * 1024].rearrange("(kc p) f -> p kc f", p=128))
                pb = S.pb[4 + (n % 2)]
                for fc in range(8):
                    for kc in range(KC):
                        P.mm(pb[:, fc * 2:fc * 2 + 2], w[:, kc, fc * 128:(fc + 1) * 128], cvb[:, kc, :],
                             start=(kc == 0), stop=(kc == KC - 1))
                P.tt("dve", S.modv[:, l, i * 8:(i + 1) * 8, :],
                     pb[:, 0:16].rearrange("p (f w) -> p f w", w=2),
                     bm[:, l, i * 8:(i + 1) * 8].unsqueeze(2).to_broadcast([128, 8, 2]), ALU.add)
            for wn, (gi, si) in enumerate(((l, 1), (2 + l, 4))):
                dst = S.gs[:, l * 2 + wn, :, :]
                P.ts("dve", dst, S.modv[:, l, si * 8:(si + 1) * 8, :], 1.0, None, ALU.add)
                P.tt("dve", dst, dst, S.ng[:, gi, :].unsqueeze(2).to_broadcast([128, 8, 2]), ALU.mult)


def modulate(S, l, wn, blocks, st, router=None):
    P = S.P
    sq = [P.sb("sq", [128, KC, 512], BF16, st) for _ in range(2)]
    rstd = [P.sb("rstd", [128, 512], F32, st) for _ in range(2)]
    tmp = [P.sb("mtmp", [128, 512], F32, st) for _ in range(3)]
    h32 = [P.sb("mh32", [128, 512], F32, st) for _ in range(2)] if router is not None else None
    si = 0 if wn == 0 else 3
    n = 0
    for bi in blocks:
        t0, tn = TB[bi]
        which = 1 if bi == 0 else 0
        s = sq[bi % 2]
        pb = S.pb[6 + bi % 2]
        for kc in range(KC):
            P.act(s[:, kc, :tn], S.xT[:, kc, t0:t0 + tn], AF.Square)
        for kc in range(KC):
            P.mm(pb[:, :tn], S.onesb, s[:, kc, :tn], start=(kc == 0), stop=(kc == KC - 1))
        r = rstd[bi % 2]
        P.act(r[:, :tn], pb[:, :tn], AF.Sqrt, bias=S.eps_t[:, :], scale=1.0 / D)
        P.recip(r[:, :tn], r[:, :tn])
        for kc in range(KC):
            t = tmp[n % 3]
            n += 1
            P.stt("dve", t[:, :tn], S.xT[:, kc, t0:t0 + tn], S.gs[:, l * 2 + wn, kc, which:which + 1],
                  r[:, :tn], ALU.mult, ALU.mult)
            P.act(S.hT[:, kc, t0:t0 + tn], t[:, :tn], AF.Identity,
                  bias=S.modv[:, l, si * 8 + kc, which:which + 1])
            if router is not None:
                rw, logit = router
                hh = h32[kc % 2]
                P.ts("dve", hh[:, :tn], t[:, :tn], S.modv[:, l, si * 8 + kc, which:which + 1], None, ALU.add)
                pl = S.pb[5]
                for j in range(tn // 128):
                    P.mm(pl[:, j * 8:(j + 1) * 8], hh[:, j * 128:(j + 1) * 128], rw[:, kc, :],
                         start=(kc == 0 and j == 0), stop=(kc == KC - 1), skip_group_check=True)
        if router is not None:
            rw, logit = router
            tile0 = (t0 - NCX) // 128
            P.copy("dve", logit[:, tile0:tile0 + tn // 128, :],
                   S.pb[5][:, 0:(tn // 128) * 8].rearrange("p (j e) -> p j e", e=8))


def phase_final(S):
    P = S.P
    with P.phase() as st:
        sq = [P.sb("fsq", [128, KC, 512], BF16, st) for _ in range(2)]
        rstd = [P.sb("frstd", [128, 512], F32, st) for _ in range(2)]
        yT = [P.sb("fyT", [128, KC, 512], F32, st) for _ in range(2)]
        ot = [P.sb("fot", [128, D], F32, st) for _ in range(3)]
        g32 = S.ng[:, 4, :]
        n = 0
        for bi in range(1, 5):
            t0, tn = TB[bi]
            s = sq[bi % 2]
            pb = S.pb[6 + bi % 2]
            for kc in range(KC):
                P.act(s[:, kc, :], S.xT[:, kc, t0:t0 + tn], AF.Square)
            for kc in range(KC):
                P.mm(pb[:, :], S.onesb, s[:, kc, :], start=(kc == 0), stop=(kc == KC - 1))
            r = rstd[bi % 2]
            P.act(r[:, :], pb[:, :], AF.Sqrt, bias=S.eps_t[:, :], scale=1.0 / D)
            P.recip(r[:, :], r[:, :])
            y = yT[bi % 2]
            for kc in range(KC):
                P.stt("dve", y[:, kc, :], S.xT[:, kc, t0:t0 + tn], g32[:, kc:kc + 1],
                      r[:, :], ALU.mult, ALU.mult)
            for q in range(4):
                o = ot[n % 3]
                n += 1
                for half in range(2):
                    pt = S.pb[(n * 2 + half) % 4]
                    for j in range(4):
                        kc = half * 4 + j
                        P.tr(pt[:, j * 128:(j + 1) * 128], y[:, kc, q * 128:(q + 1) * 128], S.identf)
                    if half == 0:
                        P.copy("dve", o[:, 0:512], pt[:, :])
                    else:
                        P.copy("act", o[:, 512:1024], pt[:, :])
                r0 = t0 - NCX + q * 128
                P.dma("sp", S.out[r0:r0 + 128, :], o[:, :])

import math


def phase_mixer(S, l):
    P = S.P
    ctx_out = l < 1
    with P.phase() as st:
        modulate(S, l, 0, range(5), st)
    if S.cfg.get("dump_h") == l:
        P.dump("hT", S.hT[:, :, :])
    br = S.cfg.get("branches", "abc")
    if "c" in br:
        branch_c(S, l, ctx_out)
    if "b" in br:
        branch_b(S, l, ctx_out)
    if "a" in br:
        branch_a(S, l, ctx_out)
    if S.cfg.get("dump_xmix") == l:
        P.dump("xT", S.xT[:, :, :])


def merge_branch(S, l, yT, wo_dram, gcol, ctx_out, st):
    P = S.P
    wg = P.sb("wg", [128, KC, 1024], BF16, st)
    wo = P.sb("wo", [128, 4, 1024], BF16, st)
    wout = P.sb("wout", [128, KC, 1024], BF16, st)
    P.dma("pool", wg[:, :, :], S.w_in[l, :, gcol:gcol + 1024].rearrange("(kc p) f -> p kc f", p=128))
    P.dma("pool", wo[:, :, :], wo_dram[l].rearrange("(c p) f -> p c f", p=128))
    P.dma("pool", wout[:, :, :], S.w_out[l].rearrange("(kc p) f -> p kc f", p=128))
    gmb = [P.sb("gm", [128, KC, 512], BF16, st) for _ in range(2)]
    sgb = [P.sb("sg", [128, 512], F32, st) for _ in range(2)]
    for bi in (range(5) if ctx_out else range(1, 5)):
        t0, tn = TB[bi]
        which = 1 if bi == 0 else 0
        gm = gmb[bi % 2]
        for fc in range(8):
            pg = S.pb[0 + fc % 2]
            py = S.pb[2 + fc % 2]
            for kc in range(KC):
                P.mm(pg[:, :tn], wg[:, kc, fc * 128:(fc + 1) * 128], S.hT[:, kc, t0:t0 + tn],
                     start=(kc == 0), stop=(kc == KC - 1))
            for c in range(4):
                P.mm(py[:, :tn], wo[:, c, fc * 128:(fc + 1) * 128], yT[:, c, t0:t0 + tn],
                     start=(c == 0), stop=(c == 3))
            sg = sgb[fc % 2]
            P.act(sg[:, :tn], pg[:, :tn], AF.Sigmoid)
            P.tt("dve", gm[:, fc, :tn], py[:, :tn], sg[:, :tn], ALU.mult)
        for fo in range(8):
            po = S.pb[4 + fo % 2]
            for fc in range(8):
                P.mm(po[:, :tn], wout[:, fc, fo * 128:(fo + 1) * 128], gm[:, fc, :tn],
                     start=(fc == 0), stop=(fc == 7))
            P.stt("dve", S.xT[:, fo, t0:t0 + tn], po[:, :tn], S.modv[:, l, 16 + fo, which:which + 1],
                  S.xT[:, fo, t0:t0 + tn], ALU.mult, ALU.add)


def branch_c(S, l, ctx_out):
    P = S.P
    lam_init = 0.8 - 0.6 * math.exp(-0.3 * l)
    with P.phase() as sto:
        ydT = P.sb("ydT", [128, 4, NT], BF16, sto)
        if not ctx_out:
            P.memset("pool", ydT[:, :, 0:NCX], 0.0)
        with P.phase() as st:
            ropet = P.sb("ropet", [128, 2, NL], BF16, st)
            P.dma("pool", ropet[:, :, :], S.rope.rearrange("c p t -> p c t"))
            rotT = P.sb("rotT", [128, 128], BF16, st)
            P.copy("dve", rotT[:, :], S.cf[:, 128:256])
            gnbc = P.sb("gnbc", [128, 128], F32, st)
            P.dma("sp", gnbc[:, :], S.gn[l, 2].partition_broadcast(128))
            P.ts("dve", gnbc[:, :], gnbc[:, :], 1.0 - lam_init, None, ALU.mult)
            lam = P.sb("lam", [128, 4, 64], F32, st)
            P.dma("sp", lam[:, :, :], S.lam_c[l].partition_broadcast(128))
            lp = P.sb("lp", [128, 2, 64], F32, st)
            P.tt("dve", lp[:, 0, :], lam[:, 0, :], lam[:, 1, :], ALU.mult)
            P.tt("dve", lp[:, 1, :], lam[:, 2, :], lam[:, 3, :], ALU.mult)
            ls = P.sb("ls", [128, 2], F32, st)
            P.reduce("dve", ls[:, :], lp[:, :, :], ALU.add, AX.X)
            le = P.sb("le", [128, 2], F32, st)
            P.act(le[:, :], ls[:, :], AF.Exp)
            neglam = P.sb("neglam", [128, 1], F32, st)
            P.tt("dve", neglam[:, :], le[:, 1:2], le[:, 0:1], ALU.subtract)
            P.ts("dve", neglam[:, :], neglam[:, :], -lam_init, None, ALU.add)

            wC = P.sb("wC", [128, KC, 1536], BF16, st)
            P.dma("pool", wC[:, :, :], S.w_in[l, :, C_Q:C_Q + 1536].rearrange("(kc p) f -> p kc f", p=128))
            gncol = P.sb("gncol", [128, 1], F32, st)
            P.dma("sp", gncol[:, :], S.gn[l, 2, :].rearrange("(p o) -> p o", o=1))
            P.ts("dve", gncol[:, :], gncol[:, :], 1.0 - lam_init, None, ALU.mult)
            qz = [P.sb("qz", [128, NT], BF16, st) for _ in range(2)]
            P.memset("pool", qz[0][:, :], 0.0)
            P.memset("pool", qz[1][:, :], 0.0)
            kTh = P.sb("kTh", [128, NT], BF16, st)
            vtok = P.sb("vtokc", [128, NTL, 128], BF16, st)
            xqb = [P.sb("xq", [128, 512], BF16, st) for _ in range(2)]
            t1b = [P.sb("t1", [128, 512], F32, st) for _ in range(1)]
            t2b = [P.sb("t2", [128, 512], F32, st) for _ in range(1)]
            NR = 5
            pring = [P.sb("pT", [128, 512], BF16, st) for _ in range(NR)]
            r0b = [P.sb("cr0", [128, 512], F32, st) for _ in range(1)]
            r1b = [P.sb("cr1", [128, 512], F32, st) for _ in range(1)]
            t0b = [P.sb("ct0", [128, 512], F32, st) for _ in range(1)]
            odb = [P.sb("cod", [128, 512], F32, st) for _ in range(1)]
            sqb = [P.sb("csq", [128, 512], BF16, st) for _ in range(1)]
            cnt = 0
            ring = 0
            ep = 0
            for h in range(4):
                for dstT, col in ((None, h * 128), (kTh, 512 + h * 128)):
                    for bi in range(5):
                        t0, tn = TB[bi]
                        pb = S.pb[6]
                        for kc in range(KC):
                            P.mm(pb[:, :tn], wC[:, kc, col:col + 128], S.hT[:, kc, t0:t0 + tn],
                                 start=(kc == 0), stop=(kc == KC - 1))
                        if bi == 0:
                            if dstT is None:
                                P.act(qz[0][0:64, 0:tn], pb[0:64, :tn], AF.Copy)
                                P.act(qz[1][64:128, 0:tn], pb[64:128, :tn], AF.Copy)
                            else:
                                P.act(dstT[:, 0:tn], pb[:, :tn], AF.Copy)
                        else:
                            xq = xqb[cnt % 2]; t1 = t1b[0]; t2 = t2b[0]
                            cnt += 1
                            P.act(xq[:, :], pb[:, :], AF.Copy)
                            pr = S.pb[7]
                            P.mm(pr[:, :], rotT[:, :], xq[:, :])
                            lt0 = t0 - NCX
                            P.tt("dve", t1[:, :], pr[:, :], ropet[:, 1, lt0:lt0 + 512], ALU.mult)
                            P.tt("pool", t2[:, :], xq[:, :], ropet[:, 0, lt0:lt0 + 512], ALU.mult)
                            if dstT is None:
                                P.tt("dve", qz[0][0:64, t0:t0 + 512], t1[0:64, :], t2[0:64, :], ALU.add)
                                P.tt("dve", qz[1][64:128, t0:t0 + 512], t1[64:128, :], t2[64:128, :], ALU.add)
                            else:
                                P.tt("dve", dstT[:, t0:t0 + 512], t1[:, :], t2[:, :], ALU.add)
                vc = 1024 + h * 128
                for g0 in range(0, NTL, 4):
                    ng_ = min(4, NTL - g0)
                    pb = S.pb[6]
                    for j in range(ng_):
                        tt = g0 + j
                        for kc in range(KC):
                            P.mm(pb[:, j * 128:(j + 1) * 128], S.hT[:, kc, tt * 128:(tt + 1) * 128],
                                 wC[:, kc, vc:vc + 128], start=(kc == 0), stop=(kc == KC - 1))
                    P.act(vtok[:, g0:g0 + ng_, :],
                          pb[:, 0:ng_ * 128].rearrange("p (j t) -> p j t", j=ng_), AF.Copy)
                for qb in ([0] if ctx_out else []) + [1, 2, 3, 4]:
                    q0, qn = TB[qb]
                    seq = list(range(2)) if qb == 0 else list(range(NTL))
                    for m in range(2):
                        oT = S.pb[2 + m]
                        sT = S.pb[4 + m]

                        def qk(kt):
                            nonlocal ring
                            ps = S.pb[kt % 2]
                            P.mm(ps[:, :qn], kTh[:, kt * 128:(kt + 1) * 128], qz[m][:, q0:q0 + qn])
                            pt = pring[ring % NR]
                            ring += 1
                            P.act(pt[:, :qn], ps[:, :qn], AF.Exp, scale=0.125)
                            return pt
                        LA = 3
                        pts = {}
                        for i, kt in enumerate(seq):
                            if i == 0:
                                for j in range(min(LA, len(seq))):
                                    pts[seq[j]] = qk(seq[j])
                            if i + LA < len(seq):
                                pts[seq[i + LA]] = qk(seq[i + LA])
                            pt = pts.pop(kt)
                            P.mm(oT[:, :qn], vtok[:, kt, :], pt[:, :qn], start=(i == 0), stop=(i == len(seq) - 1))
                            if not S.cfg.get("c_nosum") or i == 0:
                                P.mm(sT[:, :qn], S.onesb, pt[:, :qn], start=(i == 0), stop=(i == len(seq) - 1) or bool(S.cfg.get("c_nosum")))
                    r0 = r0b[0]; r1 = r1b[0]; t0_ = t0b[0]; od = odb[0]; sq = sqb[0]
                    ep += 1
                    P.recip(r0[:, :qn], S.pb[4][:, :qn])
                    P.recip(r1[:, :qn], S.pb[5][:, :qn])
                    P.ts("dve", r1[:, :qn], r1[:, :qn], neglam[:, :], None, ALU.mult)
                    P.tt("dve", t0_[:, :qn], S.pb[2][:, :qn], r0[:, :qn], ALU.mult)
                    P.tt("dve", r1[:, :qn], S.pb[3][:, :qn], r1[:, :qn], ALU.mult)
                    P.tt("pool", od[:, :qn], t0_[:, :qn], r1[:, :qn], ALU.add)
                    P.act(sq[:, :qn], od[:, :qn], AF.Square)
                    pn = S.pb[7]
                    P.mm(pn[:, :qn], S.onesb, sq[:, :qn])
                    P.act(r0[:, :qn], pn[:, :qn], AF.Sqrt, bias=S.eps_t[:, :], scale=1.0 / 128)
                    P.recip(r0[:, :qn], r0[:, :qn])
                    P.stt("dve", ydT[:, h, q0:q0 + qn], od[:, :qn], gncol[:, :], r0[:, :qn], ALU.mult, ALU.mult)
        if S.cfg.get("dump_yd") == l:
            P.dump("ydT", ydT[:, :, :])
        if S.cfg.get("merge", True):
            with P.phase() as st:
                merge_branch(S, l, ydT, S.w_o_c, G_D, ctx_out, st)


def branch_b(S, l, ctx_out):
    P = S.P
    U = S.cf[:, 256:384]
    Lm = S.cf[:, 384:512]
    SU = S.cf[:, 512:640]
    SL = S.cf[:, 640:768]
    with P.phase() as sto:
        ybT = P.sb("ybT", [128, 4, NT], BF16, sto)
        if not ctx_out:
            P.memset("pool", ybT[:, :, 0:NCX], 0.0)
        with P.phase() as stc:
            gnbc = P.sb("gnbcb", [128, 128], F32, stc)
            P.dma("sp", gnbc[:, :], S.gn[l, 1].partition_broadcast(128))
            wg2 = P.sb("wg2", [17, 2, 256], BF16, stc)
            if S.cfg.get("t_wg2", 1):
                P.dma("pool", wg2[:, :, :], S.wg2[l])
            one_t = P.sb("one_t", [128, 1], F32, stc)
            P.memset("dve", one_t[:, :], 1.0)
            mkb = P.sb("mkb", [128, 512], BF16, stc)
            P.copy("dve", mkb[:, :], S.cf[:, 256:768])
            Ub = mkb[:, 0:128]; Lb = mkb[:, 128:256]; SUb = mkb[:, 256:384]; SLb = mkb[:, 384:512]
            for hp in range(2):
                with P.phase() as st:
                    qT = P.sb("bqT", [128, NT], BF16, st)
                    kT = P.sb("bkT", [128, NT], BF16, st)
                    ktok = P.sb("bktok", [128, NTL, 128], BF16, st)
                    vtok = P.sb("bvtok", [128, NTL, 256], BF16, st)
                    srtok = P.sb("bsrtok", [128, NTL, 256], BF16, st)
                    glrT = P.sb("bglrT", [17, 2, NT], BF16, st)
                    Sst = P.sb("bSst", [128, 2, NTL, 128], BF16, st)
                    Sf = [P.sb("bSf", [128, 128], F32, st) for _ in range(2)]
                    if S.cfg.get("t_ms", 1):
                        P.memset("pool", glrT[:, :, :], 1.0)
                    P.memset("dve", Sf[0][:, :], 0.0)
                    P.memset("dve", Sf[1][:, :], 0.0)
                    with P.phase() as stw:
                        wB = P.sb("wBp", [128, KC, 800], BF16, stw)
                        for (dst0, n_, src0) in ((0, 128, B_Q + hp * 128), (128, 128, B_K + hp * 128),
                                                 (256, 256, B_V + hp * 256), (512, 256, B_R + hp * 256),
                                                 (768, 32, B_GL)):
                            P.dma("pool", wB[:, :, dst0:dst0 + n_],
                                  S.w_in[l, :, src0:src0 + n_].rearrange("(kc p) f -> p kc f", p=128))
                        for bi in (range(5) if S.cfg.get("b_proj", 9) >= 1 else []):
                            t0, tn = TB[bi]
                            for dstT, col in ((qT, 0), (kT, 128)):
                                pb = S.pb[4 + (col // 128)]
                                for kc in range(KC):
                                    P.mm(pb[:, :tn], wB[:, kc, col:col + 128], S.hT[:, kc, t0:t0 + tn],
                                         start=(kc == 0), stop=(kc == KC - 1))
                                P.act(dstT[:, t0:t0 + tn], pb[:, :tn], AF.Copy)
                            for d in (range(2) if S.cfg.get("t_glr", 1) else []):
                                pb = S.pb[6 + d]
                                for kc in range(KC):
                                    P.mm(pb[0:16, :tn], wB[:, kc, 768 + d * 16:768 + (d + 1) * 16],
                                         S.hT[:, kc, t0:t0 + tn], start=(kc == 0), stop=(kc == KC - 1))
                                P.copy("dve", glrT[0:16, d, t0:t0 + tn], pb[0:16, :tn])
                        for n in (range(NTL) if S.cfg.get("b_proj", 9) >= 2 else []):
                            pa = S.pb[0 + n % 2]
                            pr = S.pb[2 + n % 2]
                            for kc in range(KC):
                                P.mm(pa[:, 0:384], S.hT[:, kc, n * 128:(n + 1) * 128], wB[:, kc, 128:512],
                                     start=(kc == 0), stop=(kc == KC - 1))
                            for kc in range(KC):
                                P.mm(pr[:, 0:256], S.hT[:, kc, n * 128:(n + 1) * 128], wB[:, kc, 512:768],
                                     start=(kc == 0), stop=(kc == KC - 1))
                            P.copy("dve", ktok[:, n, :], pa[:, 0:128])
                            P.act(vtok[:, n, :], pa[:, 128:384], AF.Copy)
                            P.act(srtok[:, n, :], pr[:, 0:256], AF.Silu)

                    e1b = [P.sb("be1", [128, 128], F32, st) for _ in range(2)]
                    spb = [P.sb("bsp", [128, 128], F32, st) for _ in range(2)]
                    eremb = [P.sb("berem", [128, 128], F32, st) for _ in range(2)]
                    kgb = [P.sb("bkg", [128, 128], BF16, st) for _ in range(2)]
                    sphl = [P.sb("bsphl", [128, 2, 128], BF16, st) for _ in range(2)]
                    glb = [P.sb("bgl", [128, 1], F32, st) for _ in range(4)]
                    cnt = 0

                    def gate_sp(d, n):
                        nonlocal cnt
                        px = S.pb[0 + cnt % 2]
                        e1 = e1b[cnt % 2]; sp = spb[cnt % 2]
                        cnt += 1
                        P.mm(px[:, 0:128], glrT[0:17, d, n * 128:(n + 1) * 128],
                             wg2[0:17, d, hp * 128:(hp + 1) * 128])
                        P.act(e1[:, :], px[:, 0:128], AF.Exp, scale=-1.0)
                        P.act(sp[:, :], e1[:, :], AF.Ln, bias=one_t[:, :])
                        hl = sphl[(cnt - 1) % 2]
                        P.copy("dve", hl[:, 0, :], sp[:, :])
                        P.tt("dve", hl[:, 1, :], sp[:, :], hl[:, 0, :], ALU.subtract)
                        return hl

                    if S.cfg.get("b_stop", 9) < 1:
                        continue
                    order = [list(range(NTL)), [1, 0] + list(range(NTL - 1, 1, -1))]
                    for s in range(NTL):
                        for d in range(2):
                            n = order[d][s]
                            sp = gate_sp(d, n)
                            k_ = cnt
                            prm = S.pb[2 + d]
                            for z in range(2):
                                P.mm(prm[:, 0:128], SLb if d == 0 else SUb, sp[:, z, :], start=(z == 0), stop=(z == 1))
                            for z in range(2):
                                P.mm(prm[:, 128:129], sp[:, z, :], S.onesb[:, 0:1], start=(z == 0), stop=(z == 1))
                            erem = eremb[d]; kg = kgb[d]; gl = glb[(s * 2 + d) % 4]
                            P.act(erem[:, :], prm[:, 0:128], AF.Exp, scale=-1.0 / 16)
                            P.act(gl[:, :], prm[:, 128:129], AF.Exp, scale=-1.0 / 16)
                            P.tt("dve", kg[:, :], ktok[:, n, :], erem[:, :], ALU.mult)
                            P.copy("act", Sst[:, d, n, :], Sf[d][:, :])
                            if s == NTL - 1:
                                continue
                            pS = S.pb[4 + d]
                            P.mm(pS[:, 0:256], kg[:, :], vtok[:, n, :])
                            for hh in range(2):
                                sl = slice(hh * 64, (hh + 1) * 64)
                                P.stt("dve", Sf[d][sl, :], Sf[d][sl, :], gl[sl, :], pS[sl, hh * 128:(hh + 1) * 128],
                                      ALU.mult, ALU.add)

                    if S.cfg.get("b_stop", 9) < 2:
                        continue
                    egb = [P.sb("beg", [128, 128], F32, st) for _ in range(2)]
                    eib = [P.sb("bei", [128, 128], F32, st) for _ in range(2)]
                    qtb = [P.sb("bqt", [128, 128], BF16, st) for _ in range(4)]
                    ktb = [P.sb("bkt", [128, 128], BF16, st) for _ in range(4)]
                    atb = [P.sb("bat", [128, 2, 128], BF16, st) for _ in range(4)]
                    smb = [P.sb("bsm", [128, 2], F32, st) for _ in range(4)]
                    junk = P.sb("bjunk", [128, 128], BF16, st)
                    y1b = [P.sb("by1", [128, 128], F32, st) for _ in range(2)]
                    ytb = [P.sb("byt", [128, 128], BF16, st) for _ in range(2)]
                    ptb = S.pb[7][:, 0:256].bitcast(BF16)
                    k2 = 0
                    ep = 0
                    for n in (range(NTL) if ctx_out else range(2, NTL)):
                        dd = []
                        for d in range(2):
                            sp = gate_sp(d, n)
                            pg = S.pb[2]
                            for z in range(2):
                                P.mm(pg[:, 0:128], sp[:, z, :], Ub if d == 0 else Lb, start=(z == 0), stop=(z == 1))
                            eg = egb[d]; ei = eib[d]
                            qt = qtb[k2 % 4]; kt = ktb[k2 % 4]; at = atb[k2 % 4]
                            k2 += 1
                            P.act(eg[:, :], pg[:, 0:128], AF.Exp, scale=-1.0 / 16)
                            P.act(ei[:, :], pg[:, 0:128], AF.Exp, scale=1.0 / 16)
                            P.stt("dve", qt[:, :], qT[:, n * 128:(n + 1) * 128], 0.125, eg[:, :], ALU.mult, ALU.mult)
                            P.tt("pool", kt[:, :], kT[:, n * 128:(n + 1) * 128], ei[:, :], ALU.mult)
                            if S.cfg.get("p2", 9) < 1:
                                continue
                            for hh in range(2):
                                sl = slice(hh * 64, (hh + 1) * 64)
                                P.mm(S.pb[3 + hh][:, 0:128], kt[sl, :], qt[sl, :])
                            mask = (U if d == 0 else Lm)
                            for hh in range(2):
                                P.tt("dve", at[:, hh, :], S.pb[3 + hh][:, 0:128], mask, ALU.mult)
                            dd.append((qt, at))
                        if S.cfg.get("p2", 9) < 2:
                            continue
                        for hh in range(2):
                            sl = slice(hh * 64, (hh + 1) * 64)
                            oo = S.pb[5 + hh][:, 0:128]
                            for d in range(2):
                                qt, at = dd[d]
                                P.mm(oo, qt[sl, :], Sst[sl, d, n, :], start=(d == 0), stop=False)
                                P.mm(oo, at[:, hh, :], vtok[:, n, hh * 128:(hh + 1) * 128], start=False, stop=(d == 1))
                        if S.cfg.get("p2", 9) < 3:
                            continue
                        for hh in range(2):
                            oo = S.pb[5 + hh][:, 0:128]
                            s_ = smb[ep % 4]; y1 = y1b[ep % 2]; yt = ytb[ep % 2]
                            ep += 1
                            P.memset("pool", s_[:, 0:1], 0.0)
                            P.act(junk[:, :], oo, AF.Square, accum_out=s_[:, 0:1])
                            P.act(s_[:, 1:2], s_[:, 0:1], AF.Sqrt, bias=S.eps_t[:, :], scale=1.0 / 128)
                            P.recip(s_[:, 1:2], s_[:, 1:2])
                            P.stt("dve", y1[:, :], oo, s_[:, 1:2], gnbc[:, :], ALU.mult, ALU.mult)
                            P.tt("pool", yt[:, :], y1[:, :], srtok[:, n, hh * 128:(hh + 1) * 128], ALU.mult)
                            P.tr(ptb[:, hh * 128:(hh + 1) * 128], yt[:, :], S.identb)
                        P.copy("act", ybT[:, hp * 2:hp * 2 + 2, n * 128:(n + 1) * 128],
                               ptb[:, 0:256].rearrange("p (h t) -> p h t", h=2))
        if S.cfg.get("dump_yb") == l:
            P.dump("ybT", ybT[:, :, :])
        if S.cfg.get("merge", True):
            with P.phase() as st:
                merge_branch(S, l, ybT, S.w_o_b, G_B, ctx_out, st)


def branch_a(S, l, ctx_out):
    P = S.P
    U = S.cf[:, 256:384]
    Lm = S.cf[:, 384:512]
    SU = S.cf[:, 512:640]
    SL = S.cf[:, 640:768]
    with P.phase() as sto:
        yaT = P.sb("yaT", [128, 4, NT], BF16, sto)
        if not ctx_out:
            P.memset("pool", yaT[:, :, 0:NCX], 0.0)
        with P.phase() as stc:
            gnbc = P.sb("gnbca", [128, 128], F32, stc)
            P.dma("sp", gnbc[:, :], S.gn[l, 0].partition_broadcast(128))
            one_t = P.sb("one_ta", [128, 1], F32, stc)
            P.memset("dve", one_t[:, :], 1.0)
            mkb = P.sb("mkba", [128, 4, 128], BF16, stc)
            P.copy("dve", mkb[:, :, :], S.cf[:, 256:768].rearrange("p (m c) -> p m c", m=4))
            ones_col = P.sb("onescol", [128, 1], BF16, stc)
            P.memset("dve", ones_col[:, :], 1.0)
            cum1 = P.sb("cum1", [128, 2, 129], BF16, stc)
            P.memset("dve", cum1[:, :, :], 1.0)
            P.copy("dve", cum1[:, 0, 0:128], U)
            P.copy("dve", cum1[:, 1, 0:128], Lm)
            cumb = [mkb[:, 0, :], mkb[:, 1, :]]
            gmf = [SL, SU]
            strict = [SL, SU]
            maskT = [U, Lm]
            cw = P.sb("convw", [128, 12, 3], F32, stc)
            P.dma("sp", cw[:, :, :], S.convw[l])
            adt = P.sb("adt", [128, 2, 8], F32, stc)
            P.dma("sp", adt[:, :, :], S.adt[l].partition_broadcast(128))
            nA = P.sb("nA", [128, 8], F32, stc)
            P.act(nA[:, :], adt[:, 0, :], AF.Exp)
            P.ts("dve", nA[:, :], nA[:, :], -1.0, None, ALU.mult)
            betat = P.sb("betat", [128, NTL, 8], F32, stc)
            nbetat = P.sb("nbetat", [128, NTL, 8], F32, stc)
            gt = P.sb("gt", [128, NTL, 8], F32, stc)
            with P.phase() as stw:
                wbg = P.sb("wbg", [128, KC, 16], BF16, stw)
                P.dma("pool", wbg[:, :, :], S.w_in[l, :, A_B:A_B + 16].rearrange("(kc p) f -> p kc f", p=128))
                tmpe = P.sb("tmpe", [128, NTL, 8], F32, stw)
                for n in range(NTL):
                    pb = S.pb[n % 2]
                    for kc in range(KC):
                        P.mm(pb[:, 0:16], S.hT[:, kc, n * 128:(n + 1) * 128], wbg[:, kc, :],
                             start=(kc == 0), stop=(kc == KC - 1))
                    P.act(betat[:, n, :], pb[:, 0:8], AF.Sigmoid)
                    P.tt("dve", gt[:, n, :], pb[:, 8:16], adt[:, 1, :], ALU.add)
                P.act(tmpe[:, :, :], gt[:, :, :], AF.Exp)
                P.act(gt[:, :, :], tmpe[:, :, :], AF.Ln, bias=one_t[:, :])
                P.tt("dve", gt[:, :, :], gt[:, :, :], nA[:, :].unsqueeze(1).to_broadcast([128, NTL, 8]), ALU.mult)
                P.ts("dve", nbetat[:, :, :], betat[:, :, :], -1.0, None, ALU.mult)

            for h in range(4):
                with P.phase() as st:
                    qT = P.sb("aqT", [128, NT], BF16, st)
                    kT = P.sb("akT", [128, NT], BF16, st)
                    ktok = P.sb("aktok", [128, NTL, 128], BF16, st)
                    vtok = P.sb("avtok", [128, NTL, 128], BF16, st)
                    sztok = P.sb("asztok", [128, NTL, 128], BF16, st)
                    oacc = P.sb("aoacc", [128, NTL, 128], F32, st)
                    P.memset("pool", oacc[:, :, :], 0.0)
                    ptb = S.pb[7][:, 0:256].bitcast(BF16)
                    with P.phase() as stw:
                        wA = P.sb("wAh", [128, KC, 512], BF16, stw)
                        for i, c0 in enumerate((A_Q, A_K, A_V, A_Z)):
                            P.dma("pool", wA[:, :, i * 128:(i + 1) * 128],
                                  S.w_in[l, :, c0 + h * 128:c0 + (h + 1) * 128].rearrange("(kc p) f -> p kc f", p=128))
                        pre = [P.sb("apre", [128, NT], BF16, stw) for _ in range(2)]
                        cv = [P.sb("acv", [128, NT], BF16, stw) for _ in range(2)]
                        sqb = P.sb("asq", [128, 512], BF16, stw)
                        rs = P.sb("ars", [128, 512], F32, stw)
                        vT = P.sb("avT", [128, NT], BF16, stw)
                        for i in range(3):
                            pr = pre[i % 2]; c = cv[i % 2]
                            for bi in range(5):
                                t0, tn = TB[bi]
                                pb = S.pb[bi % 2]
                                for kc in range(KC):
                                    P.mm(pb[:, :tn], wA[:, kc, i * 128:(i + 1) * 128], S.hT[:, kc, t0:t0 + tn],
                                         start=(kc == 0), stop=(kc == KC - 1))
                                P.act(pr[:, t0:t0 + tn], pb[:, :tn], AF.Copy)
                            ch = i * 4 + h
                            P.ts("dve", c[:, :], pr[:, :], cw[:, ch, 1:2], None, ALU.mult)
                            for (a, b) in ((0, NCX), (NCX, NT)):
                                P.stt("dve", c[:, a + 1:b], pr[:, a:b - 1], cw[:, ch, 0:1], c[:, a + 1:b], ALU.mult, ALU.add)
                                P.stt("dve", c[:, a:b - 1], pr[:, a + 1:b], cw[:, ch, 2:3], c[:, a:b - 1], ALU.mult, ALU.add)
                            if i == 2:
                                P.act(vT[:, :], c[:, :], AF.Silu)
                            else:
                                dst = qT if i == 0 else kT
                                P.act(c[:, :], c[:, :], AF.Silu)
                                for bi in range(5):
                                    t0, tn = TB[bi]
                                    pb = S.pb[2 + bi % 2]
                                    P.act(sqb[:, :tn], c[:, t0:t0 + tn], AF.Square)
                                    P.mm(pb[:, :tn], S.onesb, sqb[:, :tn])
                                    P.act(rs[:, :tn], pb[:, :tn], AF.Sqrt, bias=S.eps_t[:, :])
                                    P.recip(rs[:, :tn], rs[:, :tn])
                                    if i == 0:
                                        P.stt("dve", dst[:, t0:t0 + tn], c[:, t0:t0 + tn], 128.0 ** -0.5, rs[:, :tn],
                                              ALU.mult, ALU.mult)
                                    else:
                                        P.tt("dve", dst[:, t0:t0 + tn], c[:, t0:t0 + tn], rs[:, :tn], ALU.mult)
                        for n in range(NTL):
                            P.tr(ptb[:, 0:128], kT[:, n * 128:(n + 1) * 128], S.identb)
                            P.tr(ptb[:, 128:256], vT[:, n * 128:(n + 1) * 128], S.identb)
                            P.copy("dve", ktok[:, n, :], ptb[:, 0:128])
                            P.copy("dve", vtok[:, n, :], ptb[:, 128:256])
                            pz = S.pb[n % 2]
                            for kc in range(KC):
                                P.mm(pz[:, 0:128], S.hT[:, kc, n * 128:(n + 1) * 128], wA[:, kc, 384:512],
                                     start=(kc == 0), stop=(kc == KC - 1))
                            P.act(sztok[:, n, :], pz[:, 0:128], AF.Silu)

                    G = S.cfg.get("a_G", 4)
                    NRG = 4
                    def ring(nm, shape, dt):
                        return [[P.sb(nm, shape, dt, st) for _ in range(NRG)] for _ in range(2)]
                    u_r = ring("au", [128, 128], F32)
                    wT_r = ring("awT", [128, 128], BF16)
                    kg_r = ring("akg", [128, 128], BF16)
                    AT_r = ring("aAT", [128, 128], BF16)
                    sc_r = ring("asc", [128, 4], F32)

                    class WS:
                        pass
                    wss = []
                    for gi in range(G):
                        w_ = WS()
                        w_.gf = P.sb("agmf", [128, 129], F32, st)
                        w_.ghl = P.sb("agmhl", [128, 2, 129], BF16, st)
                        w_.e1 = P.sb("aE1", [128, 129], F32, st)
                        w_.e2 = P.sb("aE2", [128, 129], F32, st)
                        w_.XX = [P.sb("aXX", [128, 2, 128], F32, st) for _ in range(2)]
                        w_.PT = [P.sb("aPT", [128, 128], F32, st) for _ in range(2)]
                        w_.vb = P.sb("avb", [128, 128], BF16, st)
                        w_.kbg = P.sb("akbg", [128, 128], BF16, st)
                        w_.TT = P.sb("aTTb", [128, 128], BF16, st)
                        w_.pc = S.pb[2 + gi]
                        w_.pP = S.pb[6 + gi % 2]
                        w_.ev = "act"
                        wss.append(w_)
                    vnew = [P.sb("avnew", [128, 128], BF16, st) for _ in range(2)]
                    o1s = [P.sb("ao1s", [128, 128], F32, st) for _ in range(2)]
                    ot = [P.sb("aot", [128, 128], F32, st) for _ in range(2)]
                    Sf = [P.sb("aSf", [128, 128], F32, st) for _ in range(2)]
                    Sb = [P.sb("aSb", [128, 128], BF16, st) for _ in range(2)]
                    for d in range(2):
                        P.memset("dve", Sf[d][:, :], 0.0)
                        P.memset("dve", Sb[d][:, :], 0.0)
                    order = [list(range(NTL)), [1, 0] + list(range(NTL - 1, 1, -1))]

                    def precompute(d, n, slot, w_):
                        col = d * 4 + h
                        g = gt[:, n, col:col + 1]
                        beta = betat[:, n, col:col + 1]
                        nbeta = nbetat[:, n, col:col + 1]
                        tl = slice(n * 128, (n + 1) * 128)
                        sc = sc_r[d][slot]
                        pc = w_.pc
                        gf = w_.gf; ghl = w_.ghl; e1 = w_.e1; e2 = w_.e2
                        P.ts("dve", gf[:, 0:128], gmf[d], g, None, ALU.mult)
                        P.copy("dve", gf[:, 128:129], g)
                        P.copy("dve", ghl[:, 0, :], gf[:, :])
                        P.tt("dve", ghl[:, 1, :], gf[:, :], ghl[:, 0, :], ALU.subtract)
                        for z in range(2):
                            P.mm(pc[:, 0:129], cumb[d], ghl[:, z, :], start=(z == 0), stop=(z == 1))
                        for z in range(2):
                            P.mm(pc[:, 256:385], ghl[:, z, 0:128], cum1[:, d, :], start=(z == 0), stop=(z == 1))
                        P.act(e1[:, :], pc[:, 0:129], AF.Exp)
                        P.act(e2[:, :], pc[:, 256:385], AF.Exp)
                        yield
                        P.tt("pool", e1[:, 0:128], e1[:, 0:128], strict[d], ALU.mult)
                        P.tt("pool", e2[:, 0:128], e2[:, 0:128], maskT[d], ALU.mult)
                        P.tt("dve", sc[:, 2:3], e1[:, 128:129], e2[:, 128:129], ALU.mult)
                        P.tt("dve", sc[:, 3:4], e1[:, 128:129], beta, ALU.mult)
                        P.copy("dve", sc[:, 0:1], e1[:, 128:129])
                        P.mm(pc[:, 0:128], kT[:, tl], kT[:, tl])
                        P.mm(pc[:, 128:256], kT[:, tl], qT[:, tl])
                        XX = w_.XX; PT = w_.PT
                        P.stt("dve", XX[0][:, 0, :], pc[:, 0:128], nbeta, e1[:, 0:128], ALU.mult, ALU.mult)
                        P.tt("dve", AT_r[d][slot][:, :], pc[:, 128:256], e2[:, 0:128], ALU.mult)
                        P.ts("dve", kg_r[d][slot][:, :], ktok[:, n, :], e2[:, 128:129], None, ALU.mult)
                        P.ts("dve", w_.vb[:, :], vtok[:, n, :], beta, None, ALU.mult)
                        P.ts("dve", w_.kbg[:, :], ktok[:, n, :], sc[:, 3:4], None, ALU.mult)
                        yield
                        P.tr(pc[:, 384:512], XX[0][:, 0, :], S.identf)
                        P.copy(w_.ev, XX[0][:, 1, :], pc[:, 384:512])
                        P.tt("dve", PT[0][:, :], pc[:, 384:512], S.identf, ALU.add)
                        yield
                        cur = 0
                        for lev in range(1, 7):
                            nxt = 1 - cur
                            if lev > 1:
                                P.mm(w_.pP[:, 0:128], XX[cur][:, 0, :], PT[lev % 2][:, :])
                                P.tt("dve", PT[1 - (lev % 2)][:, :], w_.pP[:, 0:128], PT[lev % 2][:, :], ALU.add)
                            P.mm(pc[:, 0:128], XX[cur][:, 1, :], XX[cur][:, 0, :])
                            if lev < 6:
                                P.mm(pc[:, 128:256], XX[cur][:, 0, :], XX[cur][:, 1, :])
                                P.copy(w_.ev, XX[nxt][:, :, :], pc[:, 0:256].rearrange("p (a b) -> p a b", a=2))
                            else:
                                P.copy(w_.ev, XX[nxt][:, 0, :], pc[:, 0:128])
                            cur = nxt
                            yield
                        P.mm(w_.pP[:, 0:128], XX[cur][:, 0, :], PT[1][:, :])
                        P.tt("dve", PT[0][:, :], w_.pP[:, 0:128], PT[1][:, :], ALU.add)
                        P.copy("act", w_.TT[:, :], PT[0][:, :])
                        yield
                        P.mm(pc[:, 0:128], w_.TT[:, :], w_.vb[:, :])
                        P.mm(pc[:, 128:256], w_.kbg[:, :], w_.TT[:, :])
                        P.copy("act", u_r[d][slot][:, :], pc[:, 0:128])
                        P.copy("act", wT_r[d][slot][:, :], pc[:, 128:256])

                    def recur(d, n, slot, last):
                        tl = slice(n * 128, (n + 1) * 128)
                        pr = S.pb[d]
                        sc = sc_r[d][slot]
                        P.mm(pr[:, 0:128], wT_r[d][slot][:, :], Sb[d][:, :])
                        P.mm(pr[:, 128:256], qT[:, tl], Sb[d][:, :])
                        P.tt("dve", vnew[d][:, :], u_r[d][slot][:, :], pr[:, 0:128], ALU.subtract)
                        want_out = ctx_out or n >= 2
                        yield
                        if want_out:
                            P.mm(pr[:, 256:384], AT_r[d][slot][:, :], vnew[d][:, :])
                        if not last:
                            P.mm(pr[:, 384:512], kg_r[d][slot][:, :], vnew[d][:, :])
                        if want_out:
                            P.act(o1s[d][:, :], pr[:, 128:256], AF.Copy, scale=sc[:, 0:1])
                        if not last:
                            P.stt("dve", Sf[d][:, :], Sf[d][:, :], sc[:, 2:3], pr[:, 384:512], ALU.mult, ALU.add)
                            P.copy("act", Sb[d][:, :], Sf[d][:, :])
                        if want_out:
                            P.tt("dve", ot[d][:, :], pr[:, 256:384], o1s[d][:, :], ALU.add)
                            P.tt("pool", oacc[:, n, :], oacc[:, n, :], ot[d][:, :], ALU.add)
                        yield

                    units = [(d, s) for s in range(NTL) for d in range(2)]
                    pre_done = set()
                    active = []
                    next_unit = 0
                    rec_step = [0, 0]
                    rec_gen = [None, None]
                    free_ws = list(range(G))
                    while True:
                        while free_ws and next_unit < len(units):
                            d_, s_ = units[next_unit]
                            if s_ - rec_step[d_] >= NRG - 1:
                                break
                            wi = free_ws.pop(0)
                            active.append(("pre", (d_, s_, wi), precompute(d_, order[d_][s_], s_ % NRG, wss[wi])))
                            next_unit += 1
                        for d_ in range(2):
                            if rec_gen[d_] is None and rec_step[d_] < NTL and (d_, rec_step[d_]) in pre_done:
                                s_ = rec_step[d_]
                                rec_gen[d_] = recur(d_, order[d_][s_], s_ % NRG, s_ == NTL - 1)
                        if not active and rec_gen[0] is None and rec_gen[1] is None:
                            if next_unit >= len(units) and rec_step[0] >= NTL and rec_step[1] >= NTL:
                                break
                        S.a_hist = getattr(S, "a_hist", [])
                        S.a_hist.append((len(active), rec_gen[0] is not None, rec_gen[1] is not None))
                        for item in list(active):
                            kind, key, gen = item
                            try:
                                next(gen)
                            except StopIteration:
                                active.remove(item)
                                pre_done.add((key[0], key[1]))
                                free_ws.append(key[2])
                        for d_ in range(2):
                            if rec_gen[d_] is not None:
                                try:
                                    next(rec_gen[d_])
                                except StopIteration:
                                    rec_gen[d_] = None
                                    rec_step[d_] += 1

                    smb = [P.sb("asm", [128, 2], F32, st) for _ in range(4)]
                    junk = P.sb("ajunk", [128, 128], BF16, st)
                    y1b = [P.sb("ay1", [128, 128], F32, st) for _ in range(2)]
                    ytb = [P.sb("ayt", [128, 128], BF16, st) for _ in range(2)]
                    ep = 0
                    for n in (range(NTL) if ctx_out else range(2, NTL)):
                        s_ = smb[ep % 4]; y1 = y1b[ep % 2]; yt = ytb[ep % 2]
                        ep += 1
                        P.memset("pool", s_[:, 0:1], 0.0)
                        P.act(junk[:, :], oacc[:, n, :], AF.Square, accum_out=s_[:, 0:1])
                        P.act(s_[:, 1:2], s_[:, 0:1], AF.Sqrt, bias=S.eps_t[:, :], scale=1.0 / 128)
                        P.recip(s_[:, 1:2], s_[:, 1:2])
                        P.stt("dve", y1[:, :], oacc[:, n, :], s_[:, 1:2], gnbc[:, :], ALU.mult, ALU.mult)
                        P.tt("pool", yt[:, :], y1[:, :], sztok[:, n, :], ALU.mult)
                        P.tr(ptb[:, 0:128], yt[:, :], S.identb)
                        P.copy("act", yaT[:, h, n * 128:(n + 1) * 128], ptb[:, 0:128])
        if S.cfg.get("dump_ya") == l:
            P.dump("yaT", yaT[:, :, :])
        if S.cfg.get("merge", True):
            with P.phase() as st:
                merge_branch(S, l, yaT, S.w_o_a, G_A, ctx_out, st)


def phase_ffn(S, l):
    P = S.P
    moe = (l % 2 == 1)
    ctx_out = l < 1
    blocks = list(range(5)) if ctx_out else list(range(1, 5))
    i_ = l // 2
    with P.phase() as sto:
        gate = None
        if moe:
            logit = P.sb("logit", [128, 16, 8], F32, sto)
            gate = P.sb("gate", [128, 16, 8], F32, sto)
        with P.phase() as st:
            router = None
            if moe:
                rw = P.sb("rw", [128, KC, 8], F32, st)
                P.dma("sp", rw[:, :, :], S.router_w[i_].rearrange("(kc p) e -> p kc e", p=128))
                router = (rw, logit)
            modulate(S, l, 1, blocks, st, router=router)
        if moe:
            with P.phase() as st:
                m1 = P.sb("m1", [128, 16], F32, st)
                m2 = P.sb("m2", [128, 16], F32, st)
                eq1 = P.sb("eq1", [128, 16, 8], F32, st)
                eq2 = P.sb("eq2", [128, 16, 8], F32, st)
                l2 = P.sb("l2", [128, 16, 8], F32, st)
                ww = P.sb("ww", [128, 3, 16], F32, st)
                bc = lambda a: a.unsqueeze(2).to_broadcast([128, 16, 8])
                P.reduce("dve", m1[:, :], logit[:, :, :], ALU.max, AX.X)
                P.tt("dve", eq1[:, :, :], logit[:, :, :], bc(m1[:, :]), ALU.is_equal)
                P.stt("dve", l2[:, :, :], eq1[:, :, :], -1e30, logit[:, :, :], ALU.mult, ALU.add)
                P.reduce("dve", m2[:, :], l2[:, :, :], ALU.max, AX.X)
                P.tt("dve", eq2[:, :, :], l2[:, :, :], bc(m2[:, :]), ALU.is_equal)
                P.tt("dve", ww[:, 0, :], m2[:, :], m1[:, :], ALU.subtract)
                P.act(ww[:, 0, :], ww[:, 0, :], AF.Exp)
                P.ts("dve", ww[:, 1, :], ww[:, 0, :], 1.0, None, ALU.add)
                P.recip(ww[:, 1, :], ww[:, 1, :])
                P.tt("dve", ww[:, 2, :], ww[:, 0, :], ww[:, 1, :], ALU.mult)
                P.tt("dve", eq1[:, :, :], eq1[:, :, :], bc(ww[:, 1, :]), ALU.mult)
                P.tt("dve", eq2[:, :, :], eq2[:, :, :], bc(ww[:, 2, :]), ALU.mult)
                P.tt("dve", gate[:, :, :], eq1[:, :, :], eq2[:, :, :], ALU.add)
            if S.cfg.get("dump_gate"):
                P.dump("gate", gate[:, :, :])
        with P.phase() as st:
            GR = 6
            groups = [(0, 6), (6, 6), (12, 6), (18, 4)]
            gbuf = P.sb("gbuf", [128, GR, NL if moe else NT], BF16, st)
            w13 = [P.sb("w13", [128, 2, KC, 384], BF16, st) for _ in range(2)]
            w2b = [P.sb("w2b", [128, GR, 1024], BF16, st) for _ in range(2)]
            sil = [P.sb("sil", [128, 512], F32, st) for _ in range(2)]
            tmb = [P.sb("tmb", [128, 512], F32 if moe else BF16, st) for _ in range(2)]
            gbce = P.sb("gbce", [128, NL], BF16, st) if moe else None
            gdg = [P.sb("gdg", [128, 128], F32, st) for _ in range(2)] if moe else None
            nw13 = 0; nw2 = 0; nt = 0; ng_ = 0
            experts = range(8) if moe else range(1)
            if moe:
                blks = [(bi, TB[bi][0], TB[bi][0] - NCX, TB[bi][1]) for bi in blocks]
            else:
                blks = [(bi, TB[bi][0], TB[bi][0], TB[bi][1]) for bi in blocks]
            for e in experts:
                W1 = S.moe_w1[i_, e] if moe else S.ffn_w1[i_]
                W3 = S.moe_w3[i_, e] if moe else S.ffn_w3[i_]
                W2 = S.moe_w2[i_, e] if moe else S.ffn_w2[i_]
                if moe:
                    for b4 in range(4):
                        pg = S.pb[7]
                        for j in range(4):
                            tile = b4 * 4 + j
                            gd = gdg[tile % 2]
                            P.ts("dve", gd[:, :], S.identf, gate[:, tile, e:e + 1], None, ALU.mult)
                            P.mm(pg[:, j * 128:(j + 1) * 128], S.onesf, gd[:, :])
                        P.copy("act", gbce[:, b4 * 512:(b4 + 1) * 512], pg[:, :])
                for (f0, nf) in groups:
                    w2t = w2b[nw2 % 2]; nw2 += 1
                    P.dma("pool", w2t[:, 0:nf, :],
                          W2[f0 * 128:(f0 + nf) * 128, :].rearrange("(c p) f -> p c f", p=128))
                    for p0 in range(0, nf, 3):
                        npf = min(3, nf - p0)
                        wt = w13[nw13 % 2]; nw13 += 1
                        c0 = (f0 + p0) * 128
                        P.dma("pool", wt[:, 0, :, 0:npf * 128],
                              W1[:, c0:c0 + npf * 128].rearrange("(kc p) f -> p kc f", p=128))
                        P.dma("pool", wt[:, 1, :, 0:npf * 128],
                              W3[:, c0:c0 + npf * 128].rearrange("(kc p) f -> p kc f", p=128))
                        for (bi, x0, g0, tn) in blks:
                            for fi in range(npf):
                                pa = S.pb[0 + nt % 2]; pbb = S.pb[2 + nt % 2]
                                sl_ = sil[nt % 2]; tm = tmb[nt % 2]
                                nt += 1
                                for kc in range(KC):
                                    P.mm(pa[:, :tn], wt[:, 0, kc, fi * 128:(fi + 1) * 128], S.hT[:, kc, x0:x0 + tn],
                                         start=(kc == 0), stop=(kc == KC - 1))
                                for kc in range(KC):
                                    P.mm(pbb[:, :tn], wt[:, 1, kc, fi * 128:(fi + 1) * 128], S.hT[:, kc, x0:x0 + tn],
                                         start=(kc == 0), stop=(kc == KC - 1))
                                P.act(sl_[:, :tn], pa[:, :tn], AF.Silu)
                                gdst = gbuf[:, p0 + fi, g0:g0 + tn]
                                if moe:
                                    P.tt("dve", tm[:, :tn], pbb[:, :tn], sl_[:, :tn], ALU.mult)
                                    P.tt("pool", gdst, tm[:, :tn], gbce[:, g0:g0 + tn], ALU.mult)
                                else:
                                    P.tt("dve", gdst, pbb[:, :tn], sl_[:, :tn], ALU.mult)
                    for fo in range(8):
                        for (bi, x0, g0, tn) in blks:
                            which = 1 if bi == 0 else 0
                            po = S.pb[4 + ng_ % 3]; ng_ += 1
                            for fc in range(nf):
                                P.mm(po[:, :tn], w2t[:, fc, fo * 128:(fo + 1) * 128], gbuf[:, fc, g0:g0 + tn],
                                     start=(fc == 0), stop=(fc == nf - 1))
                            P.stt("dve", S.xT[:, fo, x0:x0 + tn], po[:, :tn], S.modv[:, l, 40 + fo, which:which + 1],
                                  S.xT[:, fo, x0:x0 + tn], ALU.mult, ALU.add)
    if S.cfg.get("dump_xffn") == l:
        P.dump("xT", S.xT[:, :, :])

def extra_shared(shared, inputs, f):
    shared["rope"] = _rope_tables()
    shared["gn"] = f(np.stack([inputs["gn_a"], inputs["gn_b"], inputs["gn_c"]], axis=1))
    shared["lam_c"] = f(inputs["lam_c"])
    cw = np.asarray(inputs["conv_a"], np.float32)
    shared["convw"] = f(cw.reshape(2, 3, 12, 128).transpose(0, 3, 2, 1))
    shared["adt"] = f(np.stack([np.asarray(inputs["a_log"], np.float32).reshape(2, 8),
                                np.asarray(inputs["dt_bias"], np.float32).reshape(2, 8)], axis=1))
    wg2 = np.concatenate([np.asarray(inputs["w_gate2"], np.float32),
                          np.asarray(inputs["b_gate"], np.float32)[:, :, None, :]], axis=2)
    shared["wg2"] = f(wg2.transpose(0, 2, 1, 3))
    for k in ("w_o_a", "w_o_b", "w_o_c", "w_out", "ffn_w1", "ffn_w3", "ffn_w2", "router_w", "moe_w1", "moe_w3", "moe_w2"):
        shared[k] = f(inputs[k])

def _consts():
    c = np.zeros((128, 1024), np.float32)
    c[:, 0:128] = np.eye(128, dtype=np.float32)
    for p in range(128):
        if (p % 32) < 16:
            c[p + 16, 128 + p] = -1.0
        else:
            c[p - 16, 128 + p] = 1.0
    k = np.arange(128)[:, None]
    i = np.arange(128)[None, :]
    c[:, 256:384] = (k <= i)
    c[:, 384:512] = (k >= i)
    c[:, 512:640] = (k < i)
    c[:, 640:768] = (k > i)
    return c


def _rope_tables():
    t = np.arange(2048)
    row = (t // 64).astype(np.float32)
    col = (t % 64).astype(np.float32)
    inv_freq = (np.float32(10000.0) ** (-np.arange(16, dtype=np.float32) / np.float32(16))).astype(np.float32)
    tab = np.zeros((2, 128, 2048), np.float32)
    for p in range(128):
        d = p % 64
        pos = row if d < 32 else col
        ang = (pos * inv_freq[d % 16]).astype(np.float32)
        tab[0, p] = np.cos(ang)
        tab[1, p] = np.sin(ang)
    return tab


def _fm(v):
    v = np.asarray(v)
    lead = v.shape[:-1]
    n = v.shape[-1] // 128
    w = v.reshape(lead + (n, 128))
    return np.ascontiguousarray(np.moveaxis(w, -1, 0))


_CACHE = {}


def _get_prog(cfg_key, cfg):
    if cfg_key not in _CACHE:
        _CACHE[cfg_key] = build(cfg)
    return _CACHE[cfg_key]


def make_in_maps(inputs, cfg):
    f = lambda a: np.ascontiguousarray(np.asarray(a, dtype=np.float32))
    x = f(inputs["x"]); c = f(inputs["c"]); ctx = f(inputs["ctx"]); c_ctx = f(inputs["c_ctx"])
    shared = {}
    shared["w_mod"] = f(inputs["w_mod"])
    shared["b_mod"] = np.ascontiguousarray(f(inputs["b_mod"]).reshape(2, 48, 128).transpose(0, 2, 1))
    ng = np.stack([_fm(inputs["norm1_g"][0]), _fm(inputs["norm1_g"][1]), _fm(inputs["norm2_g"][0]),
                   _fm(inputs["norm2_g"][1]), _fm(inputs["final_g"])], axis=1)
    shared["norm_g"] = f(ng)
    shared["w_in"] = f(inputs["w_in"])
    shared["consts"] = _consts()
    extra_shared(shared, inputs, f)
    maps = []
    for b in range(8):
        m = dict(shared)
        m["x"] = x[b]
        m["ctx"] = ctx[b]
        m["cvec"] = f(np.stack([_fm(c[b]), _fm(c_ctx)], axis=-1))
        maps.append(m)
    return maps


def run(inputs, cfg, trace=False):
    P = _get_prog(repr(sorted(cfg.items())), cfg)
    maps = make_in_maps(inputs, cfg)
    names = set()
    for ins in P.nc.main_func.allocations if False else []:
        pass
    n = cfg.get("ncores", 8)
    res = run_bass_kernel_spmd(P.nc, maps[:n], core_ids=list(range(n)), trace=trace)
    return P, res


def kernel(**inputs):
    cfg = dict(layers=2)
    P, res = run(inputs, cfg)
    out = np.stack([np.asarray(res.results[b]["out"], dtype=np.float32) for b in range(8)], axis=0)
    return out
```

```python
import contextlib
import numpy as np
import concourse.bass as bass
import concourse.mybir as mybir
from concourse.bass_utils import run_bass_kernel_spmd

F32 = mybir.dt.float32
BF16 = mybir.dt.bfloat16
AF = mybir.ActivationFunctionType
ALU = mybir.AluOpType
AX = mybir.AxisListType


class _Op:
    __slots__ = ("idx", "eng", "fn", "deps", "dma", "sem", "semval", "needs_inc", "prev_dma")


class _Unit:
    __slots__ = ("w", "r", "rd")

    def __init__(self):
        self.w = None
        self.r = {}
        self.rd = []


class V:
    __slots__ = ("ap", "key")

    def __init__(self, ap, key):
        self.ap = ap
        self.key = key


def _apk(x):
    if isinstance(x, V):
        return x.ap, x.key
    return x, None


class Prog:
    NDMA = 8

    def __init__(self):
        self.nc = bass.Bass("TRN2", target_bir_lowering=False)
        self.ops = []
        self.names = {}
        self.base_deps = []
        self.last = {}
        self.dma_since_barrier = []
        self.stack = contextlib.ExitStack()
        self.n_dma = {"sp": 0, "act": 0, "pool": 0}
        self.dma_last = {}
        self.dbg = []
        self._uid = 0
        self.psum_names = set()

    def uid(self, base):
        self._uid += 1
        return f"{base}_{self._uid}"

    def sb(self, name, shape, dtype, stack=None):
        st = stack if stack is not None else self.stack
        return st.enter_context(self.nc.sbuf_tensor(self.uid(name), list(shape), dtype))

    def ps(self, name, shape, dtype=F32, stack=None):
        st = stack if stack is not None else self.stack
        t = st.enter_context(self.nc.psum_tensor(self.uid(name), list(shape), dtype))
        self.psum_names.add(t[:].name)
        return t

    def dram(self, name, shape, dtype, kind="Internal"):
        return self.nc.dram_tensor(name, list(shape), dtype, kind=kind)

    @contextlib.contextmanager
    def phase(self):
        st = contextlib.ExitStack()
        try:
            yield st
        finally:
            self.barrier()
            st.close()

    def _conf(self, name, sub):
        d = self.names.setdefault(name, {})
        if sub is None:
            if None not in d:
                d[None] = _Unit()
            return d[None], list(d.values())
        if sub not in d:
            d[sub] = _Unit()
        res = [d[sub]]
        if None in d:
            res.append(d[None])
        return d[sub], res

    def add(self, eng, fn, reads, writes, dma=False):
        op = _Op()
        op.idx = len(self.ops)
        op.eng = eng
        op.fn = fn
        op.dma = dma
        op.sem = None
        op.semval = 0
        op.needs_inc = False
        op.prev_dma = None
        deps = {}
        for b in self.base_deps:
            deps[b.idx] = b
        for x in reads:
            if x is None or isinstance(x, (int, float)):
                continue
            ap, key = _apk(x)
            prim, conf = self._conf(ap.name, key)
            for u in conf:
                if u.w is not None:
                    deps[u.w.idx] = u.w
                if ap.name in self.psum_names:
                    for re_, r in u.r.items():
                        if re_ != eng:
                            deps[r.idx] = r
            if dma:
                prim.rd.append(op)
            else:
                prim.r[eng] = op
        for x in writes:
            ap, key = _apk(x)
            prim, conf = self._conf(ap.name, key)
            for u in conf:
                if u.w is not None:
                    deps[u.w.idx] = u.w
                for r in u.r.values():
                    deps[r.idx] = r
                for r in u.rd:
                    deps[r.idx] = r
            prim.w = op
            prim.r = {}
            prim.rd = []
        deps.pop(op.idx, None)
        if dma:
            slot = self.n_dma[eng] % self.NDMA
            self.n_dma[eng] += 1
            prev = self.dma_last.get((eng, slot))
            op.prev_dma = prev
            op.sem = (eng, slot)
            op.semval = (prev.semval if prev is not None else 0) + 16
            self.dma_last[(eng, slot)] = op
            self.dma_since_barrier.append(op)
        op.deps = list(deps.values())
        self.ops.append(op)
        self.last[eng] = op
        return op

    def barrier(self):
        b = list(self.last.values()) + list(self.dma_since_barrier)
        self.base_deps = b
        self.dma_since_barrier = []

    def mm(self, out, lhsT, rhs, start=True, stop=True, **kw):
        o, _ = _apk(out); l, _ = _apk(lhsT); r, _ = _apk(rhs)
        nc = self.nc
        return self.add("pe", lambda: nc.tensor.matmul(o, l, r, start=start, stop=stop, **kw),
                        [lhsT, rhs], [out])

    def tr(self, out, in_, ident):
        o, _ = _apk(out); i, _ = _apk(in_); d, _ = _apk(ident)
        nc = self.nc
        return self.add("pe", lambda: nc.tensor.transpose(o, i, d), [in_, ident], [out])

    def act(self, out, in_, func, bias=None, scale=None, accum_out=None):
        o, _ = _apk(out); i, _ = _apk(in_)
        kw = {}
        rd = [in_]
        wr = [out]
        if bias is not None:
            kw["bias"] = _apk(bias)[0] if not isinstance(bias, (int, float)) else bias
            rd.append(bias)
        if scale is not None:
            kw["scale"] = _apk(scale)[0] if not isinstance(scale, (int, float)) else scale
            rd.append(scale)
        if accum_out is not None:
            kw["accum_out"] = _apk(accum_out)[0]
            wr.append(accum_out)
        nc = self.nc
        return self.add("act", lambda: nc.scalar.activation(out=o, in_=i, func=func, **kw), rd, wr)

    def _ve(self, eng):
        return self.nc.vector if eng == "dve" else self.nc.gpsimd

    def tt(self, eng, out, in0, in1, op):
        o, _ = _apk(out); a, _ = _apk(in0); b, _ = _apk(in1)
        e = self._ve(eng)
        return self.add(eng, lambda: e.tensor_tensor(out=o, in0=a, in1=b, op=op), [in0, in1], [out])

    def ts(self, eng, out, in0, s1, s2=None, op0=ALU.mult, op1=None, accum_out=None):
        o, _ = _apk(out); a, _ = _apk(in0)
        e = self._ve(eng)
        s1v = s1 if isinstance(s1, (int, float)) else _apk(s1)[0]
        s2v = s2 if (s2 is None or isinstance(s2, (int, float))) else _apk(s2)[0]
        kw = {}
        wr = [out]
        if op1 is not None:
            kw["op1"] = op1
        if accum_out is not None:
            kw["accum_out"] = _apk(accum_out)[0]
            wr.append(accum_out)
        return self.add(eng, lambda: e.tensor_scalar(out=o, in0=a, scalar1=s1v, scalar2=s2v, op0=op0, **kw),
                        [in0, s1, s2], wr)

    def stt(self, eng, out, in0, scalar, in1, op0, op1):
        o, _ = _apk(out); a, _ = _apk(in0); b, _ = _apk(in1)
        e = self._ve(eng)
        sv = scalar if isinstance(scalar, (int, float)) else _apk(scalar)[0]
        return self.add(eng, lambda: e.scalar_tensor_tensor(out=o, in0=a, scalar=sv, in1=b, op0=op0, op1=op1),
                        [in0, scalar, in1], [out])

    def copy(self, eng, out, in_):
        o, _ = _apk(out); i, _ = _apk(in_)
        nc = self.nc
        if eng == "act":
            return self.add("act", lambda: nc.scalar.copy(out=o, in_=i), [in_], [out])
        e = self._ve(eng)
        return self.add(eng, lambda: e.tensor_copy(out=o, in_=i), [in_], [out])

    def memset(self, eng, out, val):
        o, _ = _apk(out)
        e = self._ve(eng)
        return self.add(eng, lambda: e.memset(o, val), [], [out])

    def reduce(self, eng, out, in_, op, axis=AX.X):
        o, _ = _apk(out); i, _ = _apk(in_)
        e = self._ve(eng)
        return self.add(eng, lambda: e.tensor_reduce(out=o, in_=i, axis=axis, op=op), [in_], [out])

    def recip(self, out, in_):
        o, _ = _apk(out); i, _ = _apk(in_)
        nc = self.nc
        return self.add("dve", lambda: nc.vector.reciprocal(out=o, in_=i), [in_], [out])

    def dma(self, q, out, in_):
        o, _ = _apk(out); i, _ = _apk(in_)
        e = {"sp": self.nc.sync, "act": self.nc.scalar, "pool": self.nc.gpsimd}[q]
        return self.add(q, lambda: e.dma_start(out=o, in_=i), [in_], [out], dma=True)

    def dump(self, name, ap, dtype=None):
        a, _ = _apk(ap)
        t = self.nc.dram_tensor("dbg_" + name, list(a.shape), dtype or a.dtype, kind="ExternalOutput")
        self.dbg.append("dbg_" + name)
        self.dma("sp", t.ap(), ap)

    def emit(self):
        nc = self.nc
        engobj = {"pe": nc.tensor, "act": nc.scalar, "dve": nc.vector, "pool": nc.gpsimd, "sp": nc.sync}
        st = self.stack
        esem = {e: st.enter_context(nc.semaphore("s_" + e)) for e in ("pe", "act", "dve", "pool")}
        dsem = {}
        for q in ("sp", "act", "pool"):
            for s in range(self.NDMA):
                if (q, s) in self.dma_last:
                    dsem[(q, s)] = st.enter_context(nc.semaphore(f"d_{q}{s}"))
        for op in self.ops:
            for p in op.deps:
                if not p.dma and not (p.eng == "pe" and op.eng == "pe"):
                    p.needs_inc = True
        finals = list(self.last.values())
        for p in finals:
            if not p.dma:
                p.needs_inc = True
        cnt = {e: 0 for e in esem}
        for op in self.ops:
            if not op.dma and op.needs_inc:
                cnt[op.eng] += 1
                op.semval = cnt[op.eng]
        waited = {e: {} for e in engobj}
        nwait = 0
        for op in self.ops:
            e = engobj[op.eng]
            need = {}
            for p in op.deps:
                if p.dma:
                    k = ("d", p.sem)
                    v = p.semval
                else:
                    if p.eng == "pe" and op.eng == "pe":
                        continue
                    k = ("e", p.eng)
                    v = p.semval
                if need.get(k, 0) < v:
                    need[k] = v
            if op.dma and op.prev_dma is not None:
                k = ("d", op.sem)
                if need.get(k, 0) < op.prev_dma.semval:
                    need[k] = op.prev_dma.semval
            w = waited[op.eng]
            for k, v in need.items():
                if w.get(k, 0) >= v:
                    continue
                w[k] = v
                sem = dsem[k[1]] if k[0] == "d" else esem[k[1]]
                e.wait_ge(sem, v)
                nwait += 1
            ins = op.fn()
            if op.dma:
                ins.then_inc(dsem[op.sem], 16)
            elif op.needs_inc:
                ins.then_inc(esem[op.eng], 1)
        sp = nc.sync
        for en, sem in esem.items():
            if cnt[en] > 0:
                sp.wait_ge(sem, cnt[en])
        for k, p in self.dma_last.items():
            sp.wait_ge(dsem[k], p.semval)
        self.stats = dict(n_ops=len(self.ops), n_wait=nwait, incs=dict(cnt))
        return nc

D = 1024
KC = 8
NL = 2048
NCX = 256
NT = 2304
NTL = 18
TB = [(0, 256), (256, 512), (768, 512), (1280, 512), (1792, 512)]
DFF = 2816
FC = 22
EPS = 1e-6
A_Q, A_K, A_V, A_Z, A_B, A_G = 0, 512, 1024, 1536, 2048, 2056
B_Q, B_K, B_V, B_R, B_GL = 2064, 2320, 2576, 3088, 3600
C_Q, C_K, C_V = 3632, 4144, 4656
G_A, G_B, G_D = 5168, 6192, 7216


class Ctx:
    pass


def build(cfg):
    P = Prog()
    nc = P.nc
    S = Ctx()
    S.P = P
    S.cfg = cfg
    L = cfg.get("layers", 2)

    def din(name, shape, dt=F32):
        return nc.dram_tensor(name, list(shape), dt, kind="ExternalInput").ap()

    S.x = din("x", [NL, D])
    S.ctx = din("ctx", [NCX, D])
    S.cvec = din("cvec", [128, KC, 2])
    S.w_mod = din("w_mod", [2, D, 6 * D])
    S.b_mod = din("b_mod", [2, 128, 48])
    S.norm_g = din("norm_g", [128, 5, KC])
    S.w_in = din("w_in", [2, D, 8240])
    S.consts = din("consts", [128, 1024])
    S.rope = din("rope", [2, 128, NL])
    S.gn = din("gn", [2, 3, 128])
    S.lam_c = din("lam_c", [2, 4, 64])
    S.wg2 = din("wg2", [2, 17, 2, 256])
    S.convw = din("convw", [2, 128, 12, 3])
    S.adt = din("adt", [2, 2, 8])
    S.w_o_a = din("w_o_a", [2, 512, D])
    S.w_o_b = din("w_o_b", [2, 512, D])
    S.w_o_c = din("w_o_c", [2, 512, D])
    S.w_out = din("w_out", [2, D, D])
    S.ffn_w1 = din("ffn_w1", [1, D, DFF])
    S.ffn_w3 = din("ffn_w3", [1, D, DFF])
    S.ffn_w2 = din("ffn_w2", [1, DFF, D])
    S.router_w = din("router_w", [1, D, 8])
    S.moe_w1 = din("moe_w1", [1, 8, D, DFF])
    S.moe_w3 = din("moe_w3", [1, 8, D, DFF])
    S.moe_w2 = din("moe_w2", [1, 8, DFF, D])
    S.out = nc.dram_tensor("out", [NL, D], F32, kind="ExternalOutput").ap()

    st = P.stack
    S.xT = P.sb("xT", [128, KC, NT], F32)
    S.hT = P.sb("hT", [128, KC, NT], BF16)
    S.cf = P.sb("cf", [128, 1024], F32)
    S.identf = S.cf[:, 0:128]
    S.identb_t = P.sb("identb", [128, 128], BF16)
    S.identb = S.identb_t[:, :]
    S.onesb_t = P.sb("onesb", [128, 128], BF16)
    S.onesb = S.onesb_t[:, :]
    S.onesf_t = P.sb("onesf", [128, 128], F32)
    S.onesf = S.onesf_t[:, :]
    S.modv = P.sb("modv", [128, 2, 48, 2], F32)
    S.ng = P.sb("ng", [128, 5, KC], F32)
    S.gs = P.sb("gs", [128, 4, KC, 2], F32)
    S.pb = [P.ps(f"pb{i}", [128, 512], F32) for i in range(8)]

    P.dma("sp", S.cf[:, :], S.consts)
    P.dma("sp", S.ng[:, :, :], S.norm_g)
    P.copy("dve", S.identb, S.identf)
    P.memset("dve", S.onesb, 1.0)
    P.memset("dve", S.onesf, 1.0)
    S.eps_t = P.sb("eps_t", [128, 1], F32)
    P.memset("dve", S.eps_t[:, :], EPS)
    S.zero_t = P.sb("zero_t", [128, 1], F32)
    P.memset("dve", S.zero_t[:, :], 0.0)

    phase_load(S)
    phase_mod(S, L)
    for l in range(L):
        if cfg.get("mixer", True):
            phase_mixer(S, l)
        if cfg.get("ffn", True):
            phase_ffn(S, l)
    phase_final(S)
    P.emit()
    return P


def phase_load(S):
    P = S.P
    with P.phase() as st:
        tin = [P.sb("ld_in", [128, D], F32, st) for _ in range(3)]
        for tt in range(NTL):
            src = S.ctx[tt * 128:(tt + 1) * 128, :] if tt < 2 else S.x[(tt - 2) * 128:(tt - 1) * 128, :]
            ti = tin[tt % 3]
            P.dma("sp", ti[:, :], src)
            for half in range(2):
                pb = S.pb[(tt * 2 + half) % 4]
                for j in range(4):
                    kc = half * 4 + j
                    P.tr(pb[:, j * 128:(j + 1) * 128], ti[:, kc * 128:(kc + 1) * 128], S.identf)
                dst = S.xT[:, half * 4:(half + 1) * 4, tt * 128:(tt + 1) * 128]
                srcp = pb[:, :].rearrange("p (j t) -> p j t", j=4)
                if half == 0:
                    P.copy("dve", dst, srcp)
                else:
                    P.copy("act", dst, srcp)


def phase_mod(S, L):
    P = S.P
    with P.phase() as st:
        cv = P.sb("cv", [128, KC, 2], F32, st)
        cvb = P.sb("cvb", [128, KC, 2], BF16, st)
        bm = P.sb("bm", [128, 2, 48], F32, st)
        wm = [P.sb("wm", [128, KC, 1024], BF16, st) for _ in range(2)]
        P.dma("sp", cv[:, :, :], S.cvec)
        P.dma("sp", bm[:, :, :], S.b_mod.rearrange("l p f -> p l f"))
        P.act(cvb[:, :, :], cv[:, :, :], AF.Silu)
        n = 0
        for l in range(L):
            for i in range(6):
                w = wm[n % 2]
                n += 1
                P.dma("pool", w[:, :, :],
                      S.w_mod[l, :, i * 1024:(i + 1) * 1024].rearrange("(kc p) f -> p kc f", p=128))
                pb = S.pb[4 + (n % 2)]
                for fc in range(8):
                    for kc in range(KC):
                        P.mm(pb[:, fc * 2:fc * 2 + 2], w[:, kc, fc * 128:(fc + 1) * 128], cvb[:, kc, :],
                             start=(kc == 0), stop=(kc == KC - 1))
                P.tt("dve", S.modv[:, l, i * 8:(i + 1) * 8, :],
                     pb[:, 0:16].rearrange("p (f w) -> p f w", w=2),
                     bm[:, l, i * 8:(i + 1) * 8].unsqueeze(2).to_broadcast([128, 8, 2]), ALU.add)
            for wn, (gi, si) in enumerate(((l, 1), (2 + l, 4))):
                dst = S.gs[:, l * 2 + wn, :, :]
                P.ts("dve", dst, S.modv[:, l, si * 8:(si + 1) * 8, :], 1.0, None, ALU.add)
                P.tt("dve", dst, dst, S.ng[:, gi, :].unsqueeze(2).to_broadcast([128, 8, 2]), ALU.mult)


def modulate(S, l, wn, blocks, st, router=None):
    P = S.P
    sq = [P.sb("sq", [128, KC, 512], BF16, st) for _ in range(2)]
    rstd = [P.sb("rstd", [128, 512], F32, st) for _ in range(2)]
    tmp = [P.sb("mtmp", [128, 512], F32, st) for _ in range(3)]
    h32 = [P.sb("mh32", [128, 512], F32, st) for _ in range(2)] if router is not None else None
    si = 0 if wn == 0 else 3
    n = 0
    for bi in blocks:
        t0, tn = TB[bi]
        which = 1 if bi == 0 else 0
        s = sq[bi % 2]
        pb = S.pb[6 + bi % 2]
        for kc in range(KC):
            P.act(s[:, kc, :tn], S.xT[:, kc, t0:t0 + tn], AF.Square)
        for kc in range(KC):
            P.mm(pb[:, :tn], S.onesb, s[:, kc, :tn], start=(kc == 0), stop=(kc == KC - 1))
        r = rstd[bi % 2]
        P.act(r[:, :tn], pb[:, :tn], AF.Sqrt, bias=S.eps_t[:, :], scale=1.0 / D)
        P.recip(r[:, :tn], r[:, :tn])
        for kc in range(KC):
            t = tmp[n % 3]
            n += 1
            P.stt("dve", t[:, :tn], S.xT[:, kc, t0:t0 + tn], S.gs[:, l * 2 + wn, kc, which:which + 1],
                  r[:, :tn], ALU.mult, ALU.mult)
            P.act(S.hT[:, kc, t0:t0 + tn], t[:, :tn], AF.Identity,
                  bias=S.modv[:, l, si * 8 + kc, which:which + 1])
            if router is not None:
                rw, logit = router
                hh = h32[kc % 2]
                P.ts("dve", hh[:, :tn], t[:, :tn], S.modv[:, l, si * 8 + kc, which:which + 1], None, ALU.add)
                pl = S.pb[5]
                for j in range(tn // 128):
                    P.mm(pl[:, j * 8:(j + 1) * 8], hh[:, j * 128:(j + 1) * 128], rw[:, kc, :],
                         start=(kc == 0 and j == 0), stop=(kc == KC - 1), skip_group_check=True)
        if router is not None:
            rw, logit = router
            tile0 = (t0 - NCX) // 128
            P.copy("dve", logit[:, tile0:tile0 + tn // 128, :],
                   S.pb[5][:, 0:(tn // 128) * 8].rearrange("p (j e) -> p j e", e=8))


def phase_final(S):
    P = S.P
    with P.phase() as st:
        sq = [P.sb("fsq", [128, KC, 512], BF16, st) for _ in range(2)]
        rstd = [P.sb("frstd", [128, 512], F32, st) for _ in range(2)]
        yT = [P.sb("fyT", [128, KC, 512], F32, st) for _ in range(2)]
        ot = [P.sb("fot", [128, D], F32, st) for _ in range(3)]
        g32 = S.ng[:, 4, :]
        n = 0
        for bi in range(1, 5):
            t0, tn = TB[bi]
            s = sq[bi % 2]
            pb = S.pb[6 + bi % 2]
            for kc in range(KC):
                P.act(s[:, kc, :], S.xT[:, kc, t0:t0 + tn], AF.Square)
            for kc in range(KC):
                P.mm(pb[:, :], S.onesb, s[:, kc, :], start=(kc == 0), stop=(kc == KC - 1))
            r = rstd[bi % 2]
            P.act(r[:, :], pb[:, :], AF.Sqrt, bias=S.eps_t[:, :], scale=1.0 / D)
            P.recip(r[:, :], r[:, :])
            y = yT[bi % 2]
            for kc in range(KC):
                P.stt("dve", y[:, kc, :], S.xT[:, kc, t0:t0 + tn], g32[:, kc:kc + 1],
                      r[:, :], ALU.mult, ALU.mult)
            for q in range(4):
                o = ot[n % 3]
                n += 1
                for half in range(2):
                    pt = S.pb[(n * 2 + half) % 4]
                    for j in range(4):
                        kc = half * 4 + j
                        P.tr(pt[:, j * 128:(j + 1) * 128], y[:, kc, q * 128:(q + 1) * 128], S.identf)
                    if half == 0:
                        P.copy("dve", o[:, 0:512], pt[:, :])
                    else:
                        P.copy("act", o[:, 512:1024], pt[:, :])
                r0 = t0 - NCX + q * 128
                P.dma("sp", S.out[r0:r0 + 128, :], o[:, :])

import math


def phase_mixer(S, l):
    P = S.P
    ctx_out = l < 1
    with P.phase() as st:
        modulate(S, l, 0, range(5), st)
    if S.cfg.get("dump_h") == l:
        P.dump("hT", S.hT[:, :, :])
    br = S.cfg.get("branches", "abc")
    if "c" in br:
        branch_c(S, l, ctx_out)
    if "b" in br:
        branch_b(S, l, ctx_out)
    if "a" in br:
        branch_a(S, l, ctx_out)
    if S.cfg.get("dump_xmix") == l:
        P.dump("xT", S.xT[:, :, :])


def merge_branch(S, l, yT, wo_dram, gcol, ctx_out, st):
    P = S.P
    wg = P.sb("wg", [128, KC, 1024], BF16, st)
    wo = P.sb("wo", [128, 4, 1024], BF16, st)
    wout = P.sb("wout", [128, KC, 1024], BF16, st)
    P.dma("pool", wg[:, :, :], S.w_in[l, :, gcol:gcol + 1024].rearrange("(kc p) f -> p kc f", p=128))
    P.dma("pool", wo[:, :, :], wo_dram[l].rearrange("(c p) f -> p c f", p=128))
    P.dma("pool", wout[:, :, :], S.w_out[l].rearrange("(kc p) f -> p kc f", p=128))
    gmb = [P.sb("gm", [128, KC, 512], BF16, st) for _ in range(2)]
    sgb = [P.sb("sg", [128, 512], F32, st) for _ in range(2)]
    for bi in (range(5) if ctx_out else range(1, 5)):
        t0, tn = TB[bi]
        which = 1 if bi == 0 else 0
        gm = gmb[bi % 2]
        for fc in range(8):
            pg = S.pb[0 + fc % 2]
            py = S.pb[2 + fc % 2]
            for kc in range(KC):
                P.mm(pg[:, :tn], wg[:, kc, fc * 128:(fc + 1) * 128], S.hT[:, kc, t0:t0 + tn],
                     start=(kc == 0), stop=(kc == KC - 1))
            for c in range(4):
                P.mm(py[:, :tn], wo[:, c, fc * 128:(fc + 1) * 128], yT[:, c, t0:t0 + tn],
                     start=(c == 0), stop=(c == 3))
            sg = sgb[fc % 2]
            P.act(sg[:, :tn], pg[:, :tn], AF.Sigmoid)
            P.tt("dve", gm[:, fc, :tn], py[:, :tn], sg[:, :tn], ALU.mult)
        for fo in range(8):
            po = S.pb[4 + fo % 2]
            for fc in range(8):
                P.mm(po[:, :tn], wout[:, fc, fo * 128:(fo + 1) * 128], gm[:, fc, :tn],
                     start=(fc == 0), stop=(fc == 7))
            P.stt("dve", S.xT[:, fo, t0:t0 + tn], po[:, :tn], S.modv[:, l, 16 + fo, which:which + 1],
                  S.xT[:, fo, t0:t0 + tn], ALU.mult, ALU.add)


def branch_c(S, l, ctx_out):
    P = S.P
    lam_init = 0.8 - 0.6 * math.exp(-0.3 * l)
    with P.phase() as sto:
        ydT = P.sb("ydT", [128, 4, NT], BF16, sto)
        if not ctx_out:
            P.memset("pool", ydT[:, :, 0:NCX], 0.0)
        with P.phase() as st:
            ropet = P.sb("ropet", [128, 2, NL], BF16, st)
            P.dma("pool", ropet[:, :, :], S.rope.rearrange("c p t -> p c t"))
            rotT = P.sb("rotT", [128, 128], BF16, st)
            P.copy("dve", rotT[:, :], S.cf[:, 128:256])
            gnbc = P.sb("gnbc", [128, 128], F32, st)
            P.dma("sp", gnbc[:, :], S.gn[l, 2].partition_broadcast(128))
            P.ts("dve", gnbc[:, :], gnbc[:, :], 1.0 - lam_init, None, ALU.mult)
            lam = P.sb("lam", [128, 4, 64], F32, st)
            P.dma("sp", lam[:, :, :], S.lam_c[l].partition_broadcast(128))
            lp = P.sb("lp", [128, 2, 64], F32, st)
            P.tt("dve", lp[:, 0, :], lam[:, 0, :], lam[:, 1, :], ALU.mult)
            P.tt("dve", lp[:, 1, :], lam[:, 2, :], lam[:, 3, :], ALU.mult)
            ls = P.sb("ls", [128, 2], F32, st)
            P.reduce("dve", ls[:, :], lp[:, :, :], ALU.add, AX.X)
            le = P.sb("le", [128, 2], F32, st)
            P.act(le[:, :], ls[:, :], AF.Exp)
            neglam = P.sb("neglam", [128, 1], F32, st)
            P.tt("dve", neglam[:, :], le[:, 1:2], le[:, 0:1], ALU.subtract)
            P.ts("dve", neglam[:, :], neglam[:, :], -lam_init, None, ALU.add)

            wC = P.sb("wC", [128, KC, 1536], BF16, st)
            P.dma("pool", wC[:, :, :], S.w_in[l, :, C_Q:C_Q + 1536].rearrange("(kc p) f -> p kc f", p=128))
            gncol = P.sb("gncol", [128, 1], F32, st)
            P.dma("sp", gncol[:, :], S.gn[l, 2, :].rearrange("(p o) -> p o", o=1))
            P.ts("dve", gncol[:, :], gncol[:, :], 1.0 - lam_init, None, ALU.mult)
            qz = [P.sb("qz", [128, NT], BF16, st) for _ in range(2)]
            P.memset("pool", qz[0][:, :], 0.0)
            P.memset("pool", qz[1][:, :], 0.0)
            kTh = P.sb("kTh", [128, NT], BF16, st)
            vtok = P.sb("vtokc", [128, NTL, 128], BF16, st)
            xqb = [P.sb("xq", [128, 512], BF16, st) for _ in range(2)]
            t1b = [P.sb("t1", [128, 512], F32, st) for _ in range(1)]
            t2b = [P.sb("t2", [128, 512], F32, st) for _ in range(1)]
            NR = 5
            pring = [P.sb("pT", [128, 512], BF16, st) for _ in range(NR)]
            r0b = [P.sb("cr0", [128, 512], F32, st) for _ in range(1)]
            r1b = [P.sb("cr1", [128, 512], F32, st) for _ in range(1)]
            t0b = [P.sb("ct0", [128, 512], F32, st) for _ in range(1)]
            odb = [P.sb("cod", [128, 512], F32, st) for _ in range(1)]
            sqb = [P.sb("csq", [128, 512], BF16, st) for _ in range(1)]
            cnt = 0
            ring = 0
            ep = 0
            for h in range(4):
                for dstT, col in ((None, h * 128), (kTh, 512 + h * 128)):
                    for bi in range(5):
                        t0, tn = TB[bi]
                        pb = S.pb[6]
                        for kc in range(KC):
                            P.mm(pb[:, :tn], wC[:, kc, col:col + 128], S.hT[:, kc, t0:t0 + tn],
                                 start=(kc == 0), stop=(kc == KC - 1))
                        if bi == 0:
                            if dstT is None:
                                P.act(qz[0][0:64, 0:tn], pb[0:64, :tn], AF.Copy)
                                P.act(qz[1][64:128, 0:tn], pb[64:128, :tn], AF.Copy)
                            else:
                                P.act(dstT[:, 0:tn], pb[:, :tn], AF.Copy)
                        else:
                            xq = xqb[cnt % 2]; t1 = t1b[0]; t2 = t2b[0]
                            cnt += 1
                            P.act(xq[:, :], pb[:, :], AF.Copy)
                            pr = S.pb[7]
                            P.mm(pr[:, :], rotT[:, :], xq[:, :])
                            lt0 = t0 - NCX
                            P.tt("dve", t1[:, :], pr[:, :], ropet[:, 1, lt0:lt0 + 512], ALU.mult)
                            P.tt("pool", t2[:, :], xq[:, :], ropet[:, 0, lt0:lt0 + 512], ALU.mult)
                            if dstT is None:
                                P.tt("dve", qz[0][0:64, t0:t0 + 512], t1[0:64, :], t2[0:64, :], ALU.add)
                                P.tt("dve", qz[1][64:128, t0:t0 + 512], t1[64:128, :], t2[64:128, :], ALU.add)
                            else:
                                P.tt("dve", dstT[:, t0:t0 + 512], t1[:, :], t2[:, :], ALU.add)
                vc = 1024 + h * 128
                for g0 in range(0, NTL, 4):
                    ng_ = min(4, NTL - g0)
                    pb = S.pb[6]
                    for j in range(ng_):
                        tt = g0 + j
                        for kc in range(KC):
                            P.mm(pb[:, j * 128:(j + 1) * 128], S.hT[:, kc, tt * 128:(tt + 1) * 128],
                                 wC[:, kc, vc:vc + 128], start=(kc == 0), stop=(kc == KC - 1))
                    P.act(vtok[:, g0:g0 + ng_, :],
                          pb[:, 0:ng_ * 128].rearrange("p (j t) -> p j t", j=ng_), AF.Copy)
                for qb in ([0] if ctx_out else []) + [1, 2, 3, 4]:
                    q0, qn = TB[qb]
                    seq = list(range(2)) if qb == 0 else list(range(NTL))
                    for m in range(2):
                        oT = S.pb[2 + m]
                        sT = S.pb[4 + m]

                        def qk(kt):
                            nonlocal ring
                            ps = S.pb[kt % 2]
                            P.mm(ps[:, :qn], kTh[:, kt * 128:(kt + 1) * 128], qz[m][:, q0:q0 + qn])
                            pt = pring[ring % NR]
                            ring += 1
                            P.act(pt[:, :qn], ps[:, :qn], AF.Exp, scale=0.125)
                            return pt
                        LA = 3
                        pts = {}
                        for i, kt in enumerate(seq):
                            if i == 0:
                                for j in range(min(LA, len(seq))):
                                    pts[seq[j]] = qk(seq[j])
                            if i + LA < len(seq):
                                pts[seq[i + LA]] = qk(seq[i + LA])
                            pt = pts.pop(kt)
                            P.mm(oT[:, :qn], vtok[:, kt, :], pt[:, :qn], start=(i == 0), stop=(i == len(seq) - 1))
                            if not S.cfg.get("c_nosum") or i == 0:
                                P.mm(sT[:, :qn], S.onesb, pt[:, :qn], start=(i == 0), stop=(i == len(seq) - 1) or bool(S.cfg.get("c_nosum")))
                    r0 = r0b[0]; r1 = r1b[0]; t0_ = t0b[0]; od = odb[0]; sq = sqb[0]
                    ep += 1
                    P.recip(r0[:, :qn], S.pb[4][:, :qn])
                    P.recip(r1[:, :qn], S.pb[5][:, :qn])
                    P.ts("dve", r1[:, :qn], r1[:, :qn], neglam[:, :], None, ALU.mult)
                    P.tt("dve", t0_[:, :qn], S.pb[2][:, :qn], r0[:, :qn], ALU.mult)
                    P.tt("dve", r1[:, :qn], S.pb[3][:, :qn], r1[:, :qn], ALU.mult)
                    P.tt("pool", od[:, :qn], t0_[:, :qn], r1[:, :qn], ALU.add)
                    P.act(sq[:, :qn], od[:, :qn], AF.Square)
                    pn = S.pb[7]
                    P.mm(pn[:, :qn], S.onesb, sq[:, :qn])
                    P.act(r0[:, :qn], pn[:, :qn], AF.Sqrt, bias=S.eps_t[:, :], scale=1.0 / 128)
                    P.recip(r0[:, :qn], r0[:, :qn])
                    P.stt("dve", ydT[:, h, q0:q0 + qn], od[:, :qn], gncol[:, :], r0[:, :qn], ALU.mult, ALU.mult)
        if S.cfg.get("dump_yd") == l:
            P.dump("ydT", ydT[:, :, :])
        if S.cfg.get("merge", True):
            with P.phase() as st:
                merge_branch(S, l, ydT, S.w_o_c, G_D, ctx_out, st)


def branch_b(S, l, ctx_out):
    P = S.P
    U = S.cf[:, 256:384]
    Lm = S.cf[:, 384:512]
    SU = S.cf[:, 512:640]
    SL = S.cf[:, 640:768]
    with P.phase() as sto:
        ybT = P.sb("ybT", [128, 4, NT], BF16, sto)
        if not ctx_out:
            P.memset("pool", ybT[:, :, 0:NCX], 0.0)
        with P.phase() as stc:
            gnbc = P.sb("gnbcb", [128, 128], F32, stc)
            P.dma("sp", gnbc[:, :], S.gn[l, 1].partition_broadcast(128))
            wg2 = P.sb("wg2", [17, 2, 256], BF16, stc)
            if S.cfg.get("t_wg2", 1):
                P.dma("pool", wg2[:, :, :], S.wg2[l])
            one_t = P.sb("one_t", [128, 1], F32, stc)
            P.memset("dve", one_t[:, :], 1.0)
            mkb = P.sb("mkb", [128, 512], BF16, stc)
            P.copy("dve", mkb[:, :], S.cf[:, 256:768])
            Ub = mkb[:, 0:128]; Lb = mkb[:, 128:256]; SUb = mkb[:, 256:384]; SLb = mkb[:, 384:512]
            for hp in range(2):
                with P.phase() as st:
                    qT = P.sb("bqT", [128, NT], BF16, st)
                    kT = P.sb("bkT", [128, NT], BF16, st)
                    ktok = P.sb("bktok", [128, NTL, 128], BF16, st)
                    vtok = P.sb("bvtok", [128, NTL, 256], BF16, st)
                    srtok = P.sb("bsrtok", [128, NTL, 256], BF16, st)
                    glrT = P.sb("bglrT", [17, 2, NT], BF16, st)
                    Sst = P.sb("bSst", [128, 2, NTL, 128], BF16, st)
                    Sf = [P.sb("bSf", [128, 128], F32, st) for _ in range(2)]
                    if S.cfg.get("t_ms", 1):
                        P.memset("pool", glrT[:, :, :], 1.0)
                    P.memset("dve", Sf[0][:, :], 0.0)
                    P.memset("dve", Sf[1][:, :], 0.0)
                    with P.phase() as stw:
                        wB = P.sb("wBp", [128, KC, 800], BF16, stw)
                        for (dst0, n_, src0) in ((0, 128, B_Q + hp * 128), (128, 128, B_K + hp * 128),
                                                 (256, 256, B_V + hp * 256), (512, 256, B_R + hp * 256),
                                                 (768, 32, B_GL)):
                            P.dma("pool", wB[:, :, dst0:dst0 + n_],
                                  S.w_in[l, :, src0:src0 + n_].rearrange("(kc p) f -> p kc f", p=128))
                        for bi in (range(5) if S.cfg.get("b_proj", 9) >= 1 else []):
                            t0, tn = TB[bi]
                            for dstT, col in ((qT, 0), (kT, 128)):
                                pb = S.pb[4 + (col // 128)]
                                for kc in range(KC):
                                    P.mm(pb[:, :tn], wB[:, kc, col:col + 128], S.hT[:, kc, t0:t0 + tn],
                                         start=(kc == 0), stop=(kc == KC - 1))
                                P.act(dstT[:, t0:t0 + tn], pb[:, :tn], AF.Copy)
                            for d in (range(2) if S.cfg.get("t_glr", 1) else []):
                                pb = S.pb[6 + d]
                                for kc in range(KC):
                                    P.mm(pb[0:16, :tn], wB[:, kc, 768 + d * 16:768 + (d + 1) * 16],
                                         S.hT[:, kc, t0:t0 + tn], start=(kc == 0), stop=(kc == KC - 1))
                                P.copy("dve", glrT[0:16, d, t0:t0 + tn], pb[0:16, :tn])
                        for n in (range(NTL) if S.cfg.get("b_proj", 9) >= 2 else []):
                            pa = S.pb[0 + n % 2]
                            pr = S.pb[2 + n % 2]
                            for kc in range(KC):
                                P.mm(pa[:, 0:384], S.hT[:, kc, n * 128:(n + 1) * 128], wB[:, kc, 128:512],
                                     start=(kc == 0), stop=(kc == KC - 1))
                            for kc in range(KC):
                                P.mm(pr[:, 0:256], S.hT[:, kc, n * 128:(n + 1) * 128], wB[:, kc, 512:768],
                                     start=(kc == 0), stop=(kc == KC - 1))
                            P.copy("dve", ktok[:, n, :], pa[:, 0:128])
                            P.act(vtok[:, n, :], pa[:, 128:384], AF.Copy)
                            P.act(srtok[:, n, :], pr[:, 0:256], AF.Silu)

                    e1b = [P.sb("be1", [128, 128], F32, st) for _ in range(2)]
                    spb = [P.sb("bsp", [128, 128], F32, st) for _ in range(2)]
                    eremb = [P.sb("berem", [128, 128], F32, st) for _ in range(2)]
                    kgb = [P.sb("bkg", [128, 128], BF16, st) for _ in range(2)]
                    sphl = [P.sb("bsphl", [128, 2, 128], BF16, st) for _ in range(2)]
                    glb = [P.sb("bgl", [128, 1], F32, st) for _ in range(4)]
                    cnt = 0

                    def gate_sp(d, n):
                        nonlocal cnt
                        px = S.pb[0 + cnt % 2]
                        e1 = e1b[cnt % 2]; sp = spb[cnt % 2]
                        cnt += 1
                        P.mm(px[:, 0:128], glrT[0:17, d, n * 128:(n + 1) * 128],
                             wg2[0:17, d, hp * 128:(hp + 1) * 128])
                        P.act(e1[:, :], px[:, 0:128], AF.Exp, scale=-1.0)
                        P.act(sp[:, :], e1[:, :], AF.Ln, bias=one_t[:, :])
                        hl = sphl[(cnt - 1) % 2]
                        P.copy("dve", hl[:, 0, :], sp[:, :])
                        P.tt("dve", hl[:, 1, :], sp[:, :], hl[:, 0, :], ALU.subtract)
                        return hl

                    if S.cfg.get("b_stop", 9) < 1:
                        continue
                    order = [list(range(NTL)), [1, 0] + list(range(NTL - 1, 1, -1))]
                    for s in range(NTL):
                        for d in range(2):
                            n = order[d][s]
                            sp = gate_sp(d, n)
                            k_ = cnt
                            prm = S.pb[2 + d]
                            for z in range(2):
                                P.mm(prm[:, 0:128], SLb if d == 0 else SUb, sp[:, z, :], start=(z == 0), stop=(z == 1))
                            for z in range(2):
                                P.mm(prm[:, 128:129], sp[:, z, :], S.onesb[:, 0:1], start=(z == 0), stop=(z == 1))
                            erem = eremb[d]; kg = kgb[d]; gl = glb[(s * 2 + d) % 4]
                            P.act(erem[:, :], prm[:, 0:128], AF.Exp, scale=-1.0 / 16)
                            P.act(gl[:, :], prm[:, 128:129], AF.Exp, scale=-1.0 / 16)
                            P.tt("dve", kg[:, :], ktok[:, n, :], erem[:, :], ALU.mult)
                            P.copy("act", Sst[:, d, n, :], Sf[d][:, :])
                            if s == NTL - 1:
                                continue
                            pS = S.pb[4 + d]
                            P.mm(pS[:, 0:256], kg[:, :], vtok[:, n, :])
                            for hh in range(2):
                                sl = slice(hh * 64, (hh + 1) * 64)
                                P.stt("dve", Sf[d][sl, :], Sf[d][sl, :], gl[sl, :], pS[sl, hh * 128:(hh + 1) * 128],
                                      ALU.mult, ALU.add)

                    if S.cfg.get("b_stop", 9) < 2:
                        continue
                    egb = [P.sb("beg", [128, 128], F32, st) for _ in range(2)]
                    eib = [P.sb("bei", [128, 128], F32, st) for _ in range(2)]
                    qtb = [P.sb("bqt", [128, 128], BF16, st) for _ in range(4)]
                    ktb = [P.sb("bkt", [128, 128], BF16, st) for _ in range(4)]
                    atb = [P.sb("bat", [128, 2, 128], BF16, st) for _ in range(4)]
                    smb = [P.sb("bsm", [128, 2], F32, st) for _ in range(4)]
                    junk = P.sb("bjunk", [128, 128], BF16, st)
                    y1b = [P.sb("by1", [128, 128], F32, st) for _ in range(2)]
                    ytb = [P.sb("byt", [128, 128], BF16, st) for _ in range(2)]
                    ptb = S.pb[7][:, 0:256].bitcast(BF16)
                    k2 = 0
                    ep = 0
                    for n in (range(NTL) if ctx_out else range(2, NTL)):
                        dd = []
                        for d in range(2):
                            sp = gate_sp(d, n)
                            pg = S.pb[2]
                            for z in range(2):
                                P.mm(pg[:, 0:128], sp[:, z, :], Ub if d == 0 else Lb, start=(z == 0), stop=(z == 1))
                            eg = egb[d]; ei = eib[d]
                            qt = qtb[k2 % 4]; kt = ktb[k2 % 4]; at = atb[k2 % 4]
                            k2 += 1
                            P.act(eg[:, :], pg[:, 0:128], AF.Exp, scale=-1.0 / 16)
                            P.act(ei[:, :], pg[:, 0:128], AF.Exp, scale=1.0 / 16)
                            P.stt("dve", qt[:, :], qT[:, n * 128:(n + 1) * 128], 0.125, eg[:, :], ALU.mult, ALU.mult)
                            P.tt("pool", kt[:, :], kT[:, n * 128:(n + 1) * 128], ei[:, :], ALU.mult)
                            if S.cfg.get("p2", 9) < 1:
                                continue
                            for hh in range(2):
                                sl = slice(hh * 64, (hh + 1) * 64)
                                P.mm(S.pb[3 + hh][:, 0:128], kt[sl, :], qt[sl, :])
                            mask = (U if d == 0 else Lm)
                            for hh in range(2):
                                P.tt("dve", at[:, hh, :], S.pb[3 + hh][:, 0:128], mask, ALU.mult)
                            dd.append((qt, at))
                        if S.cfg.get("p2", 9) < 2:
                            continue
                        for hh in range(2):
                            sl = slice(hh * 64, (hh + 1) * 64)
                            oo = S.pb[5 + hh][:, 0:128]
                            for d in range(2):
                                qt, at = dd[d]
                                P.mm(oo, qt[sl, :], Sst[sl, d, n, :], start=(d == 0), stop=False)
                                P.mm(oo, at[:, hh, :], vtok[:, n, hh * 128:(hh + 1) * 128], start=False, stop=(d == 1))
                        if S.cfg.get("p2", 9) < 3:
                            continue
                        for hh in range(2):
                            oo = S.pb[5 + hh][:, 0:128]
                            s_ = smb[ep % 4]; y1 = y1b[ep % 2]; yt = ytb[ep % 2]
                            ep += 1
                            P.memset("pool", s_[:, 0:1], 0.0)
                            P.act(junk[:, :], oo, AF.Square, accum_out=s_[:, 0:1])
                            P.act(s_[:, 1:2], s_[:, 0:1], AF.Sqrt, bias=S.eps_t[:, :], scale=1.0 / 128)
                            P.recip(s_[:, 1:2], s_[:, 1:2])
                            P.stt("dve", y1[:, :], oo, s_[:, 1:2], gnbc[:, :], ALU.mult, ALU.mult)
                            P.tt("pool", yt[:, :], y1[:, :], srtok[:, n, hh * 128:(hh + 1) * 128], ALU.mult)
                            P.tr(ptb[:, hh * 128:(hh + 1) * 128], yt[:, :], S.identb)
                        P.copy("act", ybT[:, hp * 2:hp * 2 + 2, n * 128:(n + 1) * 128],
                               ptb[:, 0:256].rearrange("p (h t) -> p h t", h=2))
        if S.cfg.get("dump_yb") == l:
            P.dump("ybT", ybT[:, :, :])
        if S.cfg.get("merge", True):
            with P.phase() as st:
                merge_branch(S, l, ybT, S.w_o_b, G_B, ctx_out, st)


def branch_a(S, l, ctx_out):
    P = S.P
    U = S.cf[:, 256:384]
    Lm = S.cf[:, 384:512]
    SU = S.cf[:, 512:640]
    SL = S.cf[:, 640:768]
    with P.phase() as sto:
        yaT = P.sb("yaT", [128, 4, NT], BF16, sto)
        if not ctx_out:
            P.memset("pool", yaT[:, :, 0:NCX], 0.0)
        with P.phase() as stc:
            gnbc = P.sb("gnbca", [128, 128], F32, stc)
            P.dma("sp", gnbc[:, :], S.gn[l, 0].partition_broadcast(128))
            one_t = P.sb("one_ta", [128, 1], F32, stc)
            P.memset("dve", one_t[:, :], 1.0)
            mkb = P.sb("mkba", [128, 4, 128], BF16, stc)
            P.copy("dve", mkb[:, :, :], S.cf[:, 256:768].rearrange("p (m c) -> p m c", m=4))
            ones_col = P.sb("onescol", [128, 1], BF16, stc)
            P.memset("dve", ones_col[:, :], 1.0)
            cum1 = P.sb("cum1", [128, 2, 129], BF16, stc)
            P.memset("dve", cum1[:, :, :], 1.0)
            P.copy("dve", cum1[:, 0, 0:128], U)
            P.copy("dve", cum1[:, 1, 0:128], Lm)
            cumb = [mkb[:, 0, :], mkb[:, 1, :]]
            gmf = [SL, SU]
            strict = [SL, SU]
            maskT = [U, Lm]
            cw = P.sb("convw", [128, 12, 3], F32, stc)
            P.dma("sp", cw[:, :, :], S.convw[l])
            adt = P.sb("adt", [128, 2, 8], F32, stc)
            P.dma("sp", adt[:, :, :], S.adt[l].partition_broadcast(128))
            nA = P.sb("nA", [128, 8], F32, stc)
            P.act(nA[:, :], adt[:, 0, :], AF.Exp)
            P.ts("dve", nA[:, :], nA[:, :], -1.0, None, ALU.mult)
            betat = P.sb("betat", [128, NTL, 8], F32, stc)
            nbetat = P.sb("nbetat", [128, NTL, 8], F32, stc)
            gt = P.sb("gt", [128, NTL, 8], F32, stc)
            with P.phase() as stw:
                wbg = P.sb("wbg", [128, KC, 16], BF16, stw)
                P.dma("pool", wbg[:, :, :], S.w_in[l, :, A_B:A_B + 16].rearrange("(kc p) f -> p kc f", p=128))
                tmpe = P.sb("tmpe", [128, NTL, 8], F32, stw)
                for n in range(NTL):
                    pb = S.pb[n % 2]
                    for kc in range(KC):
                        P.mm(pb[:, 0:16], S.hT[:, kc, n * 128:(n + 1) * 128], wbg[:, kc, :],
                             start=(kc == 0), stop=(kc == KC - 1))
                    P.act(betat[:, n, :], pb[:, 0:8], AF.Sigmoid)
                    P.tt("dve", gt[:, n, :], pb[:, 8:16], adt[:, 1, :], ALU.add)
                P.act(tmpe[:, :, :], gt[:, :, :], AF.Exp)
                P.act(gt[:, :, :], tmpe[:, :, :], AF.Ln, bias=one_t[:, :])
                P.tt("dve", gt[:, :, :], gt[:, :, :], nA[:, :].unsqueeze(1).to_broadcast([128, NTL, 8]), ALU.mult)
                P.ts("dve", nbetat[:, :, :], betat[:, :, :], -1.0, None, ALU.mult)

            for h in range(4):
                with P.phase() as st:
                    qT = P.sb("aqT", [128, NT], BF16, st)
                    kT = P.sb("akT", [128, NT], BF16, st)
                    ktok = P.sb("aktok", [128, NTL, 128], BF16, st)
                    vtok = P.sb("avtok", [128, NTL, 128], BF16, st)
                    sztok = P.sb("asztok", [128, NTL, 128], BF16, st)
                    oacc = P.sb("aoacc", [128, NTL, 128], F32, st)
                    P.memset("pool", oacc[:, :, :], 0.0)
                    ptb = S.pb[7][:, 0:256].bitcast(BF16)
                    with P.phase() as stw:
                        wA = P.sb("wAh", [128, KC, 512], BF16, stw)
                        for i, c0 in enumerate((A_Q, A_K, A_V, A_Z)):
                            P.dma("pool", wA[:, :, i * 128:(i + 1) * 128],
                                  S.w_in[l, :, c0 + h * 128:c0 + (h + 1) * 128].rearrange("(kc p) f -> p kc f", p=128))
                        pre = [P.sb("apre", [128, NT], BF16, stw) for _ in range(2)]
                        cv = [P.sb("acv", [128, NT], BF16, stw) for _ in range(2)]
                        sqb = P.sb("asq", [128, 512], BF16, stw)
                        rs = P.sb("ars", [128, 512], F32, stw)
                        vT = P.sb("avT", [128, NT], BF16, stw)
                        for i in range(3):
                            pr = pre[i % 2]; c = cv[i % 2]
                            for bi in range(5):
                                t0, tn = TB[bi]
                                pb = S.pb[bi % 2]
                                for kc in range(KC):
                                    P.mm(pb[:, :tn], wA[:, kc, i * 128:(i + 1) * 128], S.hT[:, kc, t0:t0 + tn],
                                         start=(kc == 0), stop=(kc == KC - 1))
                                P.act(pr[:, t0:t0 + tn], pb[:, :tn], AF.Copy)
                            ch = i * 4 + h
                            P.ts("dve", c[:, :], pr[:, :], cw[:, ch, 1:2], None, ALU.mult)
                            for (a, b) in ((0, NCX), (NCX, NT)):
                                P.stt("dve", c[:, a + 1:b], pr[:, a:b - 1], cw[:, ch, 0:1], c[:, a + 1:b], ALU.mult, ALU.add)
                                P.stt("dve", c[:, a:b - 1], pr[:, a + 1:b], cw[:, ch, 2:3], c[:, a:b - 1], ALU.mult, ALU.add)
                            if i == 2:
                                P.act(vT[:, :], c[:, :], AF.Silu)
                            else:
                                dst = qT if i == 0 else kT
                                P.act(c[:, :], c[:, :], AF.Silu)
                                for bi in range(5):
                                    t0, tn = TB[bi]
                                    pb = S.pb[2 + bi % 2]
                                    P.act(sqb[:, :tn], c[:, t0:t0 + tn], AF.Square)
                                    P.mm(pb[:, :tn], S.onesb, sqb[:, :tn])
                                    P.act(rs[:, :tn], pb[:, :tn], AF.Sqrt, bias=S.eps_t[:, :])
                                    P.recip(rs[:, :tn], rs[:, :tn])
                                    if i == 0:
                                        P.stt("dve", dst[:, t0:t0 + tn], c[:, t0:t0 + tn], 128.0 ** -0.5, rs[:, :tn],
                                              ALU.mult, ALU.mult)
                                    else:
                                        P.tt("dve", dst[:, t0:t0 + tn], c[:, t0:t0 + tn], rs[:, :tn], ALU.mult)
                        for n in range(NTL):
                            P.tr(ptb[:, 0:128], kT[:, n * 128:(n + 1) * 128], S.identb)
                            P.tr(ptb[:, 128:256], vT[:, n * 128:(n + 1) * 128], S.identb)
                            P.copy("dve", ktok[:, n, :], ptb[:, 0:128])
                            P.copy("dve", vtok[:, n, :], ptb[:, 128:256])
                            pz = S.pb[n % 2]
                            for kc in range(KC):
                                P.mm(pz[:, 0:128], S.hT[:, kc, n * 128:(n + 1) * 128], wA[:, kc, 384:512],
                                     start=(kc == 0), stop=(kc == KC - 1))
                            P.act(sztok[:, n, :], pz[:, 0:128], AF.Silu)

                    G = S.cfg.get("a_G", 4)
                    NRG = 4
                    def ring(nm, shape, dt):
                        return [[P.sb(nm, shape, dt, st) for _ in range(NRG)] for _ in range(2)]
                    u_r = ring("au", [128, 128], F32)
                    wT_r = ring("awT", [128, 128], BF16)
                    kg_r = ring("akg", [128, 128], BF16)
                    AT_r = ring("aAT", [128, 128], BF16)
                    sc_r = ring("asc", [128, 4], F32)

                    class WS:
                        pass
                    wss = []
                    for gi in range(G):
                        w_ = WS()
                        w_.gf = P.sb("agmf", [128, 129], F32, st)
                        w_.ghl = P.sb("agmhl", [128, 2, 129], BF16, st)
                        w_.e1 = P.sb("aE1", [128, 129], F32, st)
                        w_.e2 = P.sb("aE2", [128, 129], F32, st)
                        w_.XX = [P.sb("aXX", [128, 2, 128], F32, st) for _ in range(2)]
                        w_.PT = [P.sb("aPT", [128, 128], F32, st) for _ in range(2)]
                        w_.vb = P.sb("avb", [128, 128], BF16, st)
                        w_.kbg = P.sb("akbg", [128, 128], BF16, st)
                        w_.TT = P.sb("aTTb", [128, 128], BF16, st)
                        w_.pc = S.pb[2 + gi]
                        w_.pP = S.pb[6 + gi % 2]
                        w_.ev = "act"
                        wss.append(w_)
                    vnew = [P.sb("avnew", [128, 128], BF16, st) for _ in range(2)]
                    o1s = [P.sb("ao1s", [128, 128], F32, st) for _ in range(2)]
                    ot = [P.sb("aot", [128, 128], F32, st) for _ in range(2)]
                    Sf = [P.sb("aSf", [128, 128], F32, st) for _ in range(2)]
                    Sb = [P.sb("aSb", [128, 128], BF16, st) for _ in range(2)]
                    for d in range(2):
                        P.memset("dve", Sf[d][:, :], 0.0)
                        P.memset("dve", Sb[d][:, :], 0.0)
                    order = [list(range(NTL)), [1, 0] + list(range(NTL - 1, 1, -1))]

                    def precompute(d, n, slot, w_):
                        col = d * 4 + h
                        g = gt[:, n, col:col + 1]
                        beta = betat[:, n, col:col + 1]
                        nbeta = nbetat[:, n, col:col + 1]
                        tl = slice(n * 128, (n + 1) * 128)
                        sc = sc_r[d][slot]
                        pc = w_.pc
                        gf = w_.gf; ghl = w_.ghl; e1 = w_.e1; e2 = w_.e2
                        P.ts("dve", gf[:, 0:128], gmf[d], g, None, ALU.mult)
                        P.copy("dve", gf[:, 128:129], g)
                        P.copy("dve", ghl[:, 0, :], gf[:, :])
                        P.tt("dve", ghl[:, 1, :], gf[:, :], ghl[:, 0, :], ALU.subtract)
                        for z in range(2):
                            P.mm(pc[:, 0:129], cumb[d], ghl[:, z, :], start=(z == 0), stop=(z == 1))
                        for z in range(2):
                            P.mm(pc[:, 256:385], ghl[:, z, 0:128], cum1[:, d, :], start=(z == 0), stop=(z == 1))
                        P.act(e1[:, :], pc[:, 0:129], AF.Exp)
                        P.act(e2[:, :], pc[:, 256:385], AF.Exp)
                        yield
                        P.tt("pool", e1[:, 0:128], e1[:, 0:128], strict[d], ALU.mult)
                        P.tt("pool", e2[:, 0:128], e2[:, 0:128], maskT[d], ALU.mult)
                        P.tt("dve", sc[:, 2:3], e1[:, 128:129], e2[:, 128:129], ALU.mult)
                        P.tt("dve", sc[:, 3:4], e1[:, 128:129], beta, ALU.mult)
                        P.copy("dve", sc[:, 0:1], e1[:, 128:129])
                        P.mm(pc[:, 0:128], kT[:, tl], kT[:, tl])
                        P.mm(pc[:, 128:256], kT[:, tl], qT[:, tl])
                        XX = w_.XX; PT = w_.PT
                        P.stt("dve", XX[0][:, 0, :], pc[:, 0:128], nbeta, e1[:, 0:128], ALU.mult, ALU.mult)
                        P.tt("dve", AT_r[d][slot][:, :], pc[:, 128:256], e2[:, 0:128], ALU.mult)
                        P.ts("dve", kg_r[d][slot][:, :], ktok[:, n, :], e2[:, 128:129], None, ALU.mult)
                        P.ts("dve", w_.vb[:, :], vtok[:, n, :], beta, None, ALU.mult)
                        P.ts("dve", w_.kbg[:, :], ktok[:, n, :], sc[:, 3:4], None, ALU.mult)
                        yield
                        P.tr(pc[:, 384:512], XX[0][:, 0, :], S.identf)
                        P.copy(w_.ev, XX[0][:, 1, :], pc[:, 384:512])
                        P.tt("dve", PT[0][:, :], pc[:, 384:512], S.identf, ALU.add)
                        yield
                        cur = 0
                        for lev in range(1, 7):
                            nxt = 1 - cur
                            if lev > 1:
                                P.mm(w_.pP[:, 0:128], XX[cur][:, 0, :], PT[lev % 2][:, :])
                                P.tt("dve", PT[1 - (lev % 2)][:, :], w_.pP[:, 0:128], PT[lev % 2][:, :], ALU.add)
                            P.mm(pc[:, 0:128], XX[cur][:, 1, :], XX[cur][:, 0, :])
                            if lev < 6:
                                P.mm(pc[:, 128:256], XX[cur][:, 0, :], XX[cur][:, 1, :])
                                P.copy(w_.ev, XX[nxt][:, :, :], pc[:, 0:256].rearrange("p (a b) -> p a b", a=2))
                            else:
                                P.copy(w_.ev, XX[nxt][:, 0, :], pc[:, 0:128])
                            cur = nxt
                            yield
                        P.mm(w_.pP[:, 0:128], XX[cur][:, 0, :], PT[1][:, :])
                        P.tt("dve", PT[0][:, :], w_.pP[:, 0:128], PT[1][:, :], ALU.add)
                        P.copy("act", w_.TT[:, :], PT[0][:, :])
                        yield
                        P.mm(pc[:, 0:128], w_.TT[:, :], w_.vb[:, :])
                        P.mm(pc[:, 128:256], w_.kbg[:, :], w_.TT[:, :])
                        P.copy("act", u_r[d][slot][:, :], pc[:, 0:128])
                        P.copy("act", wT_r[d][slot][:, :], pc[:, 128:256])

                    def recur(d, n, slot, last):
                        tl = slice(n * 128, (n + 1) * 128)
                        pr = S.pb[d]
                        sc = sc_r[d][slot]
                        P.mm(pr[:, 0:128], wT_r[d][slot][:, :], Sb[d][:, :])
                        P.mm(pr[:, 128:256], qT[:, tl], Sb[d][:, :])
                        P.tt("dve", vnew[d][:, :], u_r[d][slot][:, :], pr[:, 0:128], ALU.subtract)
                        want_out = ctx_out or n >= 2
                        yield
                        if want_out:
                            P.mm(pr[:, 256:384], AT_r[d][slot][:, :], vnew[d][:, :])
                        if not last:
                            P.mm(pr[:, 384:512], kg_r[d][slot][:, :], vnew[d][:, :])
                        if want_out:
                            P.act(o1s[d][:, :], pr[:, 128:256], AF.Copy, scale=sc[:, 0:1])
                        if not last:
                            P.stt("dve", Sf[d][:, :], Sf[d][:, :], sc[:, 2:3], pr[:, 384:512], ALU.mult, ALU.add)
                            P.copy("act", Sb[d][:, :], Sf[d][:, :])
                        if want_out:
                            P.tt("dve", ot[d][:, :], pr[:, 256:384], o1s[d][:, :], ALU.add)
                            P.tt("pool", oacc[:, n, :], oacc[:, n, :], ot[d][:, :], ALU.add)
                        yield

                    units = [(d, s) for s in range(NTL) for d in range(2)]
                    pre_done = set()
                    active = []
                    next_unit = 0
                    rec_step = [0, 0]
                    rec_gen = [None, None]
                    free_ws = list(range(G))
                    while True:
                        while free_ws and next_unit < len(units):
                            d_, s_ = units[next_unit]
                            if s_ - rec_step[d_] >= NRG - 1:
                                break
                            wi = free_ws.pop(0)
                            active.append(("pre", (d_, s_, wi), precompute(d_, order[d_][s_], s_ % NRG, wss[wi])))
                            next_unit += 1
                        for d_ in range(2):
                            if rec_gen[d_] is None and rec_step[d_] < NTL and (d_, rec_step[d_]) in pre_done:
                                s_ = rec_step[d_]
                                rec_gen[d_] = recur(d_, order[d_][s_], s_ % NRG, s_ == NTL - 1)
                        if not active and rec_gen[0] is None and rec_gen[1] is None:
                            if next_unit >= len(units) and rec_step[0] >= NTL and rec_step[1] >= NTL:
                                break
                        S.a_hist = getattr(S, "a_hist", [])
                        S.a_hist.append((len(active), rec_gen[0] is not None, rec_gen[1] is not None))
                        for item in list(active):
                            kind, key, gen = item
                            try:
                                next(gen)
                            except StopIteration:
                                active.remove(item)
                                pre_done.add((key[0], key[1]))
                                free_ws.append(key[2])
                        for d_ in range(2):
                            if rec_gen[d_] is not None:
                                try:
                                    next(rec_gen[d_])
                                except StopIteration:
                                    rec_gen[d_] = None
                                    rec_step[d_] += 1

                    smb = [P.sb("asm", [128, 2], F32, st) for _ in range(4)]
                    junk = P.sb("ajunk", [128, 128], BF16, st)
                    y1b = [P.sb("ay1", [128, 128], F32, st) for _ in range(2)]
                    ytb = [P.sb("ayt", [128, 128], BF16, st) for _ in range(2)]
                    ep = 0
                    for n in (range(NTL) if ctx_out else range(2, NTL)):
                        s_ = smb[ep % 4]; y1 = y1b[ep % 2]; yt = ytb[ep % 2]
                        ep += 1
                        P.memset("pool", s_[:, 0:1], 0.0)
                        P.act(junk[:, :], oacc[:, n, :], AF.Square, accum_out=s_[:, 0:1])
                        P.act(s_[:, 1:2], s_[:, 0:1], AF.Sqrt, bias=S.eps_t[:, :], scale=1.0 / 128)
                        P.recip(s_[:, 1:2], s_[:, 1:2])
                        P.stt("dve", y1[:, :], oacc[:, n, :], s_[:, 1:2], gnbc[:, :], ALU.mult, ALU.mult)
                        P.tt("pool", yt[:, :], y1[:, :], sztok[:, n, :], ALU.mult)
                        P.tr(ptb[:, 0:128], yt[:, :], S.identb)
                        P.copy("act", yaT[:, h, n * 128:(n + 1) * 128], ptb[:, 0:128])
        if S.cfg.get("dump_ya") == l:
            P.dump("yaT", yaT[:, :, :])
        if S.cfg.get("merge", True):
            with P.phase() as st:
                merge_branch(S, l, yaT, S.w_o_a, G_A, ctx_out, st)


def phase_ffn(S, l):
    P = S.P
    moe = (l % 2 == 1)
    ctx_out = l < 1
    blocks = list(range(5)) if ctx_out else list(range(1, 5))
    i_ = l // 2
    with P.phase() as sto:
        gate = None
        if moe:
            logit = P.sb("logit", [128, 16, 8], F32, sto)
            gate = P.sb("gate", [128, 16, 8], F32, sto)
        with P.phase() as st:
            router = None
            if moe:
                rw = P.sb("rw", [128, KC, 8], F32, st)
                P.dma("sp", rw[:, :, :], S.router_w[i_].rearrange("(kc p) e -> p kc e", p=128))
                router = (rw, logit)
            modulate(S, l, 1, blocks, st, router=router)
        if moe:
            with P.phase() as st:
                m1 = P.sb("m1", [128, 16], F32, st)
                m2 = P.sb("m2", [128, 16], F32, st)
                eq1 = P.sb("eq1", [128, 16, 8], F32, st)
                eq2 = P.sb("eq2", [128, 16, 8], F32, st)
                l2 = P.sb("l2", [128, 16, 8], F32, st)
                ww = P.sb("ww", [128, 3, 16], F32, st)
                bc = lambda a: a.unsqueeze(2).to_broadcast([128, 16, 8])
                P.reduce("dve", m1[:, :], logit[:, :, :], ALU.max, AX.X)
                P.tt("dve", eq1[:, :, :], logit[:, :, :], bc(m1[:, :]), ALU.is_equal)
                P.stt("dve", l2[:, :, :], eq1[:, :, :], -1e30, logit[:, :, :], ALU.mult, ALU.add)
                P.reduce("dve", m2[:, :], l2[:, :, :], ALU.max, AX.X)
                P.tt("dve", eq2[:, :, :], l2[:, :, :], bc(m2[:, :]), ALU.is_equal)
                P.tt("dve", ww[:, 0, :], m2[:, :], m1[:, :], ALU.subtract)
                P.act(ww[:, 0, :], ww[:, 0, :], AF.Exp)
                P.ts("dve", ww[:, 1, :], ww[:, 0, :], 1.0, None, ALU.add)
                P.recip(ww[:, 1, :], ww[:, 1, :])
                P.tt("dve", ww[:, 2, :], ww[:, 0, :], ww[:, 1, :], ALU.mult)
                P.tt("dve", eq1[:, :, :], eq1[:, :, :], bc(ww[:, 1, :]), ALU.mult)
                P.tt("dve", eq2[:, :, :], eq2[:, :, :], bc(ww[:, 2, :]), ALU.mult)
                P.tt("dve", gate[:, :, :], eq1[:, :, :], eq2[:, :, :], ALU.add)
            if S.cfg.get("dump_gate"):
                P.dump("gate", gate[:, :, :])
        with P.phase() as st:
            GR = 6
            groups = [(0, 6), (6, 6), (12, 6), (18, 4)]
            gbuf = P.sb("gbuf", [128, GR, NL if moe else NT], BF16, st)
            w13 = [P.sb("w13", [128, 2, KC, 384], BF16, st) for _ in range(2)]
            w2b = [P.sb("w2b", [128, GR, 1024], BF16, st) for _ in range(2)]
            sil = [P.sb("sil", [128, 512], F32, st) for _ in range(2)]
            tmb = [P.sb("tmb", [128, 512], F32 if moe else BF16, st) for _ in range(2)]
            gbce = P.sb("gbce", [128, NL], BF16, st) if moe else None
            gdg = [P.sb("gdg", [128, 128], F32, st) for _ in range(2)] if moe else None
            nw13 = 0; nw2 = 0; nt = 0; ng_ = 0
            experts = range(8) if moe else range(1)
            if moe:
                blks = [(bi, TB[bi][0], TB[bi][0] - NCX, TB[bi][1]) for bi in blocks]
            else:
                blks = [(bi, TB[bi][0], TB[bi][0], TB[bi][1]) for bi in blocks]
            def Wsel(e):
                if moe:
                    return S.moe_w1[i_, e], S.moe_w3[i_, e], S.moe_w2[i_, e]
                return S.ffn_w1[i_], S.ffn_w3[i_], S.ffn_w2[i_]
            pieces = []
            for e in experts:
                for gi_, (f0, nf) in enumerate(groups):
                    plist = list(range(0, nf, 3))
                    for pi_, p0 in enumerate(plist):
                        pieces.append(dict(e=e, f0=f0, nf=nf, p0=p0, npf=min(3, nf - p0),
                                           first_in_group=(pi_ == 0), last_in_group=(pi_ == len(plist) - 1),
                                           first_in_expert=(gi_ == 0 and pi_ == 0)))
            def issue_w13(k):
                pc_ = pieces[k]
                W1, W3, W2 = Wsel(pc_["e"])
                wt = w13[k % 2]
                c0 = (pc_["f0"] + pc_["p0"]) * 128
                npf = pc_["npf"]
                P.dma("pool", wt[:, 0, :, 0:npf * 128],
                      W1[:, c0:c0 + npf * 128].rearrange("(kc p) f -> p kc f", p=128))
                P.dma("pool", wt[:, 1, :, 0:npf * 128],
                      W3[:, c0:c0 + npf * 128].rearrange("(kc p) f -> p kc f", p=128))
            def issue_w2(k):
                pc_ = pieces[k]
                W1, W3, W2 = Wsel(pc_["e"])
                w2t = w2b[pc_["gidx"] % 2]
                f0, nf = pc_["f0"], pc_["nf"]
                P.dma("pool", w2t[:, 0:nf, :],
                      W2[f0 * 128:(f0 + nf) * 128, :].rearrange("(c p) f -> p c f", p=128))
            gidx = -1
            for pc_ in pieces:
                if pc_["first_in_group"]:
                    gidx += 1
                pc_["gidx"] = gidx
            issue_w13(0)
            issue_w2(0)
            for k, pc_ in enumerate(pieces):
                e = pc_["e"]; f0 = pc_["f0"]; nf = pc_["nf"]; p0 = pc_["p0"]; npf = pc_["npf"]
                wt = w13[k % 2]
                w2t = w2b[pc_["gidx"] % 2]
                if k + 1 < len(pieces):
                    issue_w13(k + 1)
                    if pieces[k + 1]["first_in_group"]:
                        issue_w2(k + 1)
                if moe and pc_["first_in_expert"]:
                    for b4 in range(4):
                        pg = S.pb[7]
                        for j in range(4):
                            tile = b4 * 4 + j
                            gd = gdg[tile % 2]
                            P.ts("dve", gd[:, :], S.identf, gate[:, tile, e:e + 1], None, ALU.mult)
                            P.mm(pg[:, j * 128:(j + 1) * 128], S.onesf, gd[:, :])
                        P.copy("act", gbce[:, b4 * 512:(b4 + 1) * 512], pg[:, :])
                for (bi, x0, g0, tn) in blks:
                    for fi in range(npf):
                        pa = S.pb[0 + nt % 2]; pbb = S.pb[2 + nt % 2]
                        sl_ = sil[nt % 2]; tm = tmb[nt % 2]
                        nt += 1
                        for kc in range(KC):
                            P.mm(pa[:, :tn], wt[:, 0, kc, fi * 128:(fi + 1) * 128], S.hT[:, kc, x0:x0 + tn],
                                 start=(kc == 0), stop=(kc == KC - 1))
                        for kc in range(KC):
                            P.mm(pbb[:, :tn], wt[:, 1, kc, fi * 128:(fi + 1) * 128], S.hT[:, kc, x0:x0 + tn],
                                 start=(kc == 0), stop=(kc == KC - 1))
                        P.act(sl_[:, :tn], pa[:, :tn], AF.Silu)
                        gdst = gbuf[:, p0 + fi, g0:g0 + tn]
                        if moe:
                            P.tt("dve", tm[:, :tn], pbb[:, :tn], sl_[:, :tn], ALU.mult)
                            P.tt("pool", gdst, tm[:, :tn], gbce[:, g0:g0 + tn], ALU.mult)
                        else:
                            P.tt("dve", gdst, pbb[:, :tn], sl_[:, :tn], ALU.mult)
                if pc_["last_in_group"]:
                    for fo in range(8):
                        for (bi, x0, g0, tn) in blks:
                            which = 1 if bi == 0 else 0
                            po = S.pb[4 + ng_ % 3]; ng_ += 1
                            for fc in range(nf):
                                P.mm(po[:, :tn], w2t[:, fc, fo * 128:(fo + 1) * 128], gbuf[:, fc, g0:g0 + tn],
                                     start=(fc == 0), stop=(fc == nf - 1))
                            P.stt("dve", S.xT[:, fo, x0:x0 + tn], po[:, :tn], S.modv[:, l, 40 + fo, which:which + 1],
                                  S.xT[:, fo, x0:x0 + tn], ALU.mult, ALU.add)
    if S.cfg.get("dump_xffn") == l:
        P.dump("xT", S.xT[:, :, :])

def extra_shared(shared, inputs, f):
    shared["rope"] = _rope_tables()
    shared["gn"] = f(np.stack([inputs["gn_a"], inputs["gn_b"], inputs["gn_c"]], axis=1))
    shared["lam_c"] = f(inputs["lam_c"])
    cw = np.asarray(inputs["conv_a"], np.float32)
    shared["convw"] = f(cw.reshape(2, 3, 12, 128).transpose(0, 3, 2, 1))
    shared["adt"] = f(np.stack([np.asarray(inputs["a_log"], np.float32).reshape(2, 8),
                                np.asarray(inputs["dt_bias"], np.float32).reshape(2, 8)], axis=1))
    wg2 = np.concatenate([np.asarray(inputs["w_gate2"], np.float32),
                          np.asarray(inputs["b_gate"], np.float32)[:, :, None, :]], axis=2)
    shared["wg2"] = f(wg2.transpose(0, 2, 1, 3))
    for k in ("w_o_a", "w_o_b", "w_o_c", "w_out", "ffn_w1", "ffn_w3", "ffn_w2", "router_w", "moe_w1", "moe_w3", "moe_w2"):
        shared[k] = f(inputs[k])

def _consts():
    c = np.zeros((128, 1024), np.float32)
    c[:, 0:128] = np.eye(128, dtype=np.float32)
    for p in range(128):
        if (p % 32) < 16:
            c[p + 16, 128 + p] = -1.0
        else:
            c[p - 16, 128 + p] = 1.0
    k = np.arange(128)[:, None]
    i = np.arange(128)[None, :]
    c[:, 256:384] = (k <= i)
    c[:, 384:512] = (k >= i)
    c[:, 512:640] = (k < i)
    c[:, 640:768] = (k > i)
    return c


def _rope_tables():
    t = np.arange(2048)
    row = (t // 64).astype(np.float32)
    col = (t % 64).astype(np.float32)
    inv_freq = (np.float32(10000.0) ** (-np.arange(16, dtype=np.float32) / np.float32(16))).astype(np.float32)
    tab = np.zeros((2, 128, 2048), np.float32)
    for p in range(128):
        d = p % 64
        pos = row if d < 32 else col
        ang = (pos * inv_freq[d % 16]).astype(np.float32)
        tab[0, p] = np.cos(ang)
        tab[1, p] = np.sin(ang)
    return tab


def _fm(v):
    v = np.asarray(v)
    lead = v.shape[:-1]
    n = v.shape[-1] // 128
    w = v.reshape(lead + (n, 128))
    return np.ascontiguousarray(np.moveaxis(w, -1, 0))


_CACHE = {}


def _get_prog(cfg_key, cfg):
    if cfg_key not in _CACHE:
        _CACHE[cfg_key] = build(cfg)
    return _CACHE[cfg_key]


def make_in_maps(inputs, cfg):
    f = lambda a: np.ascontiguousarray(np.asarray(a, dtype=np.float32))
    x = f(inputs["x"]); c = f(inputs["c"]); ctx = f(inputs["ctx"]); c_ctx = f(inputs["c_ctx"])
    shared = {}
    shared["w_mod"] = f(inputs["w_mod"])
    shared["b_mod"] = np.ascontiguousarray(f(inputs["b_mod"]).reshape(2, 48, 128).transpose(0, 2, 1))
    ng = np.stack([_fm(inputs["norm1_g"][0]), _fm(inputs["norm1_g"][1]), _fm(inputs["norm2_g"][0]),
                   _fm(inputs["norm2_g"][1]), _fm(inputs["final_g"])], axis=1)
    shared["norm_g"] = f(ng)
    shared["w_in"] = f(inputs["w_in"])
    shared["consts"] = _consts()
    extra_shared(shared, inputs, f)
    maps = []
    for b in range(8):
        m = dict(shared)
        m["x"] = x[b]
        m["ctx"] = ctx[b]
        m["cvec"] = f(np.stack([_fm(c[b]), _fm(c_ctx)], axis=-1))
        maps.append(m)
    return maps


def run(inputs, cfg, trace=False):
    P = _get_prog(repr(sorted(cfg.items())), cfg)
    maps = make_in_maps(inputs, cfg)
    names = set()
    for ins in P.nc.main_func.allocations if False else []:
        pass
    n = cfg.get("ncores", 8)
    res = run_bass_kernel_spmd(P.nc, maps[:n], core_ids=list(range(n)), trace=trace)
    return P, res


def kernel(**inputs):
    cfg = dict(layers=2)
    P, res = run(inputs, cfg)
    out = np.stack([np.asarray(res.results[b]["out"], dtype=np.float32) for b in range(8)], axis=0)
    return out
```

```python
import contextlib
import numpy as np
import concourse.bass as bass
import concourse.mybir as mybir
from concourse.bass_utils import run_bass_kernel_spmd

F32 = mybir.dt.float32
BF16 = mybir.dt.bfloat16
AF = mybir.ActivationFunctionType
ALU = mybir.AluOpType
AX = mybir.AxisListType


class _Op:
    __slots__ = ("idx", "eng", "fn", "deps", "dma", "sem", "semval", "needs_inc", "prev_dma")


class _Unit:
    __slots__ = ("w", "r", "rd")

    def __init__(self):
        self.w = None
        self.r = {}
        self.rd = []


class V:
    __slots__ = ("ap", "key")

    def __init__(self, ap, key):
        self.ap = ap
        self.key = key


def _apk(x):
    if isinstance(x, V):
        return x.ap, x.key
    return x, None


class Prog:
    NDMA = 8

    def __init__(self):
        self.nc = bass.Bass("TRN2", target_bir_lowering=False)
        self.ops = []
        self.names = {}
        self.base_deps = []
        self.last = {}
        self.dma_since_barrier = []
        self.stack = contextlib.ExitStack()
        self.n_dma = {"sp": 0, "act": 0, "pool": 0}
        self.dma_last = {}
        self.dbg = []
        self._uid = 0
        self.psum_names = set()

    def uid(self, base):
        self._uid += 1
        return f"{base}_{self._uid}"

    def sb(self, name, shape, dtype, stack=None):
        st = stack if stack is not None else self.stack
        return st.enter_context(self.nc.sbuf_tensor(self.uid(name), list(shape), dtype))

    def ps(self, name, shape, dtype=F32, stack=None):
        st = stack if stack is not None else self.stack
        t = st.enter_context(self.nc.psum_tensor(self.uid(name), list(shape), dtype))
        self.psum_names.add(t[:].name)
        return t

    def dram(self, name, shape, dtype, kind="Internal"):
        return self.nc.dram_tensor(name, list(shape), dtype, kind=kind)

    @contextlib.contextmanager
    def phase(self):
        st = contextlib.ExitStack()
        try:
            yield st
        finally:
            self.barrier()
            st.close()

    def _conf(self, name, sub):
        d = self.names.setdefault(name, {})
        if sub is None:
            if None not in d:
                d[None] = _Unit()
            return d[None], list(d.values())
        if sub not in d:
            d[sub] = _Unit()
        res = [d[sub]]
        if None in d:
            res.append(d[None])
        return d[sub], res

    def add(self, eng, fn, reads, writes, dma=False):
        op = _Op()
        op.idx = len(self.ops)
        op.eng = eng
        op.fn = fn
        op.dma = dma
        op.sem = None
        op.semval = 0
        op.needs_inc = False
        op.prev_dma = None
        deps = {}
        for b in self.base_deps:
            deps[b.idx] = b
        for x in reads:
            if x is None or isinstance(x, (int, float)):
                continue
            ap, key = _apk(x)
            prim, conf = self._conf(ap.name, key)
            for u in conf:
                if u.w is not None:
                    deps[u.w.idx] = u.w
                if ap.name in self.psum_names:
                    for re_, r in u.r.items():
                        if re_ != eng:
                            deps[r.idx] = r
            if dma:
                prim.rd.append(op)
            else:
                prim.r[eng] = op
        for x in writes:
            ap, key = _apk(x)
            prim, conf = self._conf(ap.name, key)
            for u in conf:
                if u.w is not None:
                    deps[u.w.idx] = u.w
                for r in u.r.values():
                    deps[r.idx] = r
                for r in u.rd:
                    deps[r.idx] = r
            prim.w = op
            prim.r = {}
            prim.rd = []
        deps.pop(op.idx, None)
        if dma:
            slot = self.n_dma[eng] % self.NDMA
            self.n_dma[eng] += 1
            prev = self.dma_last.get((eng, slot))
            op.prev_dma = prev
            op.sem = (eng, slot)
            op.semval = (prev.semval if prev is not None else 0) + 16
            self.dma_last[(eng, slot)] = op
            self.dma_since_barrier.append(op)
        op.deps = list(deps.values())
        self.ops.append(op)
        self.last[eng] = op
        return op

    def barrier(self):
        b = list(self.last.values()) + list(self.dma_since_barrier)
        self.base_deps = b
        self.dma_since_barrier = []

    def mm(self, out, lhsT, rhs, start=True, stop=True, **kw):
        o, _ = _apk(out); l, _ = _apk(lhsT); r, _ = _apk(rhs)
        nc = self.nc
        return self.add("pe", lambda: nc.tensor.matmul(o, l, r, start=start, stop=stop, **kw),
                        [lhsT, rhs], [out])

    def tr(self, out, in_, ident):
        o, _ = _apk(out); i, _ = _apk(in_); d, _ = _apk(ident)
        nc = self.nc
        return self.add("pe", lambda: nc.tensor.transpose(o, i, d), [in_, ident], [out])

    def act(self, out, in_, func, bias=None, scale=None, accum_out=None):
        o, _ = _apk(out); i, _ = _apk(in_)
        kw = {}
        rd = [in_]
        wr = [out]
        if bias is not None:
            kw["bias"] = _apk(bias)[0] if not isinstance(bias, (int, float)) else bias
            rd.append(bias)
        if scale is not None:
            kw["scale"] = _apk(scale)[0] if not isinstance(scale, (int, float)) else scale
            rd.append(scale)
        if accum_out is not None:
            kw["accum_out"] = _apk(accum_out)[0]
            wr.append(accum_out)
        nc = self.nc
        return self.add("act", lambda: nc.scalar.activation(out=o, in_=i, func=func, **kw), rd, wr)

    def _ve(self, eng):
        return self.nc.vector if eng == "dve" else self.nc.gpsimd

    def tt(self, eng, out, in0, in1, op):
        o, _ = _apk(out); a, _ = _apk(in0); b, _ = _apk(in1)
        e = self._ve(eng)
        return self.add(eng, lambda: e.tensor_tensor(out=o, in0=a, in1=b, op=op), [in0, in1], [out])

    def ts(self, eng, out, in0, s1, s2=None, op0=ALU.mult, op1=None, accum_out=None):
        o, _ = _apk(out); a, _ = _apk(in0)
        e = self._ve(eng)
        s1v = s1 if isinstance(s1, (int, float)) else _apk(s1)[0]
        s2v = s2 if (s2 is None or isinstance(s2, (int, float))) else _apk(s2)[0]
        kw = {}
        wr = [out]
        if op1 is not None:
            kw["op1"] = op1
        if accum_out is not None:
            kw["accum_out"] = _apk(accum_out)[0]
            wr.append(accum_out)
        return self.add(eng, lambda: e.tensor_scalar(out=o, in0=a, scalar1=s1v, scalar2=s2v, op0=op0, **kw),
                        [in0, s1, s2], wr)

    def stt(self, eng, out, in0, scalar, in1, op0, op1):
        o, _ = _apk(out); a, _ = _apk(in0); b, _ = _apk(in1)
        e = self._ve(eng)
        sv = scalar if isinstance(scalar, (int, float)) else _apk(scalar)[0]
        return self.add(eng, lambda: e.scalar_tensor_tensor(out=o, in0=a, scalar=sv, in1=b, op0=op0, op1=op1),
                        [in0, scalar, in1], [out])

    def copy(self, eng, out, in_):
        o, _ = _apk(out); i, _ = _apk(in_)
        nc = self.nc
        if eng == "act":
            return self.add("act", lambda: nc.scalar.copy(out=o, in_=i), [in_], [out])
        e = self._ve(eng)
        return self.add(eng, lambda: e.tensor_copy(out=o, in_=i), [in_], [out])

    def memset(self, eng, out, val):
        o, _ = _apk(out)
        e = self._ve(eng)
        return self.add(eng, lambda: e.memset(o, val), [], [out])

    def reduce(self, eng, out, in_, op, axis=AX.X):
        o, _ = _apk(out); i, _ = _apk(in_)
        e = self._ve(eng)
        return self.add(eng, lambda: e.tensor_reduce(out=o, in_=i, axis=axis, op=op), [in_], [out])

    def recip(self, out, in_):
        o, _ = _apk(out); i, _ = _apk(in_)
        nc = self.nc
        return self.add("dve", lambda: nc.vector.reciprocal(out=o, in_=i), [in_], [out])

    def dma(self, q, out, in_):
        o, _ = _apk(out); i, _ = _apk(in_)
        e = {"sp": self.nc.sync, "act": self.nc.scalar, "pool": self.nc.gpsimd}[q]
        return self.add(q, lambda: e.dma_start(out=o, in_=i), [in_], [out], dma=True)

    def dump(self, name, ap, dtype=None):
        a, _ = _apk(ap)
        t = self.nc.dram_tensor("dbg_" + name, list(a.shape), dtype or a.dtype, kind="ExternalOutput")
        self.dbg.append("dbg_" + name)
        self.dma("sp", t.ap(), ap)

    def emit(self):
        nc = self.nc
        engobj = {"pe": nc.tensor, "act": nc.scalar, "dve": nc.vector, "pool": nc.gpsimd, "sp": nc.sync}
        st = self.stack
        esem = {e: st.enter_context(nc.semaphore("s_" + e)) for e in ("pe", "act", "dve", "pool")}
        dsem = {}
        for q in ("sp", "act", "pool"):
            for s in range(self.NDMA):
                if (q, s) in self.dma_last:
                    dsem[(q, s)] = st.enter_context(nc.semaphore(f"d_{q}{s}"))
        for op in self.ops:
            for p in op.deps:
                if not p.dma and not (p.eng == "pe" and op.eng == "pe"):
                    p.needs_inc = True
        finals = list(self.last.values())
        for p in finals:
            if not p.dma:
                p.needs_inc = True
        cnt = {e: 0 for e in esem}
        for op in self.ops:
            if not op.dma and op.needs_inc:
                cnt[op.eng] += 1
                op.semval = cnt[op.eng]
        waited = {e: {} for e in engobj}
        nwait = 0
        for op in self.ops:
            e = engobj[op.eng]
            need = {}
            for p in op.deps:
                if p.dma:
                    k = ("d", p.sem)
                    v = p.semval
                else:
                    if p.eng == "pe" and op.eng == "pe":
                        continue
                    k = ("e", p.eng)
                    v = p.semval
                if need.get(k, 0) < v:
                    need[k] = v
            if op.dma and op.prev_dma is not None:
                k = ("d", op.sem)
                if need.get(k, 0) < op.prev_dma.semval:
                    need[k] = op.prev_dma.semval
            w = waited[op.eng]
            for k, v in need.items():
                if w.get(k, 0) >= v:
                    continue
                w[k] = v
                sem = dsem[k[1]] if k[0] == "d" else esem[k[1]]
                e.wait_ge(sem, v)
                nwait += 1
            ins = op.fn()
            if op.dma:
                ins.then_inc(dsem[op.sem], 16)
            elif op.needs_inc:
                ins.then_inc(esem[op.eng], 1)
        sp = nc.sync
        for en, sem in esem.items():
            if cnt[en] > 0:
                sp.wait_ge(sem, cnt[en])
        for k, p in self.dma_last.items():
            sp.wait_ge(dsem[k], p.semval)
        self.stats = dict(n_ops=len(self.ops), n_wait=nwait, incs=dict(cnt))
        return nc

D = 1024
KC = 8
NL = 2048
NCX = 256
NT = 2304
NTL = 18
TB = [(0, 256), (256, 512), (768, 512), (1280, 512), (1792, 512)]
DFF = 2816
FC = 22
EPS = 1e-6
A_Q, A_K, A_V, A_Z, A_B, A_G = 0, 512, 1024, 1536, 2048, 2056
B_Q, B_K, B_V, B_R, B_GL = 2064, 2320, 2576, 3088, 3600
C_Q, C_K, C_V = 3632, 4144, 4656
G_A, G_B, G_D = 5168, 6192, 7216


class Ctx:
    pass


def build(cfg):
    P = Prog()
    nc = P.nc
    S = Ctx()
    S.P = P
    S.cfg = cfg
    L = cfg.get("layers", 2)

    def din(name, shape, dt=F32):
        return nc.dram_tensor(name, list(shape), dt, kind="ExternalInput").ap()

    S.x = din("x", [NL, D])
    S.ctx = din("ctx", [NCX, D])
    S.cvec = din("cvec", [128, KC, 2])
    S.w_mod = din("w_mod", [2, D, 6 * D])
    S.b_mod = din("b_mod", [2, 128, 48])
    S.norm_g = din("norm_g", [128, 5, KC])
    S.w_in = din("w_in", [2, D, 8240])
    S.consts = din("consts", [128, 1024])
    S.rope = din("rope", [2, 128, NL])
    S.gn = din("gn", [2, 3, 128])
    S.lam_c = din("lam_c", [2, 4, 64])
    S.wg2 = din("wg2", [2, 17, 2, 256])
    S.convw = din("convw", [2, 128, 12, 3])
    S.adt = din("adt", [2, 2, 8])
    S.w_o_a = din("w_o_a", [2, 512, D])
    S.w_o_b = din("w_o_b", [2, 512, D])
    S.w_o_c = din("w_o_c", [2, 512, D])
    S.w_out = din("w_out", [2, D, D])
    S.ffn_w1 = din("ffn_w1", [1, D, DFF])
    S.ffn_w3 = din("ffn_w3", [1, D, DFF])
    S.ffn_w2 = din("ffn_w2", [1, DFF, D])
    S.router_w = din("router_w", [1, D, 8])
    S.moe_w1 = din("moe_w1", [1, 8, D, DFF])
    S.moe_w3 = din("moe_w3", [1, 8, D, DFF])
    S.moe_w2 = din("moe_w2", [1, 8, DFF, D])
    S.out = nc.dram_tensor("out", [NL, D], F32, kind="ExternalOutput").ap()

    st = P.stack
    S.xT = P.sb("xT", [128, KC, NT], F32)
    S.hT = P.sb("hT", [128, KC, NT], BF16)
    S.cf = P.sb("cf", [128, 1024], F32)
    S.identf = S.cf[:, 0:128]
    S.identb_t = P.sb("identb", [128, 128], BF16)
    S.identb = S.identb_t[:, :]
    S.onesb_t = P.sb("onesb", [128, 128], BF16)
    S.onesb = S.onesb_t[:, :]
    S.onesf_t = P.sb("onesf", [128, 128], F32)
    S.onesf = S.onesf_t[:, :]
    S.modv = P.sb("modv", [128, 2, 48, 2], F32)
    S.ng = P.sb("ng", [128, 5, KC], F32)
    S.gs = P.sb("gs", [128, 4, KC, 2], F32)
    S.pb = [P.ps(f"pb{i}", [128, 512], F32) for i in range(8)]

    P.dma("sp", S.cf[:, :], S.consts)
    P.dma("sp", S.ng[:, :, :], S.norm_g)
    P.copy("dve", S.identb, S.identf)
    P.memset("dve", S.onesb, 1.0)
    P.memset("dve", S.onesf, 1.0)
    S.eps_t = P.sb("eps_t", [128, 1], F32)
    P.memset("dve", S.eps_t[:, :], EPS)
    S.zero_t = P.sb("zero_t", [128, 1], F32)
    P.memset("dve", S.zero_t[:, :], 0.0)

    phase_load(S)
    phase_mod(S, L)
    for l in range(L):
        if cfg.get("mixer", True):
            phase_mixer(S, l)
        if cfg.get("ffn", True):
            phase_ffn(S, l)
    phase_final(S)
    P.emit()
    return P


def phase_load(S):
    P = S.P
    with P.phase() as st:
        tin = [P.sb("ld_in", [128, D], F32, st) for _ in range(3)]
        for tt in range(NTL):
            src = S.ctx[tt * 128:(tt + 1) * 128, :] if tt < 2 else S.x[(tt - 2) * 128:(tt - 1) * 128, :]
            ti = tin[tt % 3]
            P.dma("sp", ti[:, :], src)
            for half in range(2):
                pb = S.pb[(tt * 2 + half) % 4]
                for j in range(4):
                    kc = half * 4 + j
                    P.tr(pb[:, j * 128:(j + 1) * 128], ti[:, kc * 128:(kc + 1) * 128], S.identf)
                dst = S.xT[:, half * 4:(half + 1) * 4, tt * 128:(tt + 1) * 128]
                srcp = pb[:, :].rearrange("p (j t) -> p j t", j=4)
                if half == 0:
                    P.copy("dve", dst, srcp)
                else:
                    P.copy("act", dst, srcp)


def phase_mod(S, L):
    P = S.P
    with P.phase() as st:
        cv = P.sb("cv", [128, KC, 2], F32, st)
        cvb = P.sb("cvb", [128, KC, 2], BF16, st)
        bm = P.sb("bm", [128, 2, 48], F32, st)
        wm = [P.sb("wm", [128, KC, 1024], BF16, st) for _ in range(2)]
        P.dma("sp", cv[:, :, :], S.cvec)
        P.dma("sp", bm[:, :, :], S.b_mod.rearrange("l p f -> p l f"))
        P.act(cvb[:, :, :], cv[:, :, :], AF.Silu)
        n = 0
        for l in range(L):
            for i in range(6):
                w = wm[n % 2]
                n += 1
                P.dma("pool", w[:, :, :],
                      S.w_mod[l, :, i * 1024:(i + 1) * 1024].rearrange("(kc p) f -> p kc f", p=128))
                pb = S.pb[4 + (n % 2)]
                for fc in range(8):
                    for kc in range(KC):
                        P.mm(pb[:, fc * 2:fc * 2 + 2], w[:, kc, fc * 128:(fc + 1) * 128], cvb[:, kc, :],
                             start=(kc == 0), stop=(kc == KC - 1))
                P.tt("dve", S.modv[:, l, i * 8:(i + 1) * 8, :],
                     pb[:, 0:16].rearrange("p (f w) -> p f w", w=2),
                     bm[:, l, i * 8:(i + 1) * 8].unsqueeze(2).to_broadcast([128, 8, 2]), ALU.add)
            for wn, (gi, si) in enumerate(((l, 1), (2 + l, 4))):
                dst = S.gs[:, l * 2 + wn, :, :]
                P.ts("dve", dst, S.modv[:, l, si * 8:(si + 1) * 8, :], 1.0, None, ALU.add)
                P.tt("dve", dst, dst, S.ng[:, gi, :].unsqueeze(2).to_broadcast([128, 8, 2]), ALU.mult)


def modulate(S, l, wn, blocks, st, router=None):
    P = S.P
    sq = [P.sb("sq", [128, KC, 512], BF16, st) for _ in range(2)]
    rstd = [P.sb("rstd", [128, 512], F32, st) for _ in range(2)]
    tmp = [P.sb("mtmp", [128, 512], F32, st) for _ in range(3)]
    h32 = [P.sb("mh32", [128, 512], F32, st) for _ in range(2)] if router is not None else None
    si = 0 if wn == 0 else 3
    n = 0
    for bi in blocks:
        t0, tn = TB[bi]
        which = 1 if bi == 0 else 0
        s = sq[bi % 2]
        pb = S.pb[6 + bi % 2]
        for kc in range(KC):
            P.act(s[:, kc, :tn], S.xT[:, kc, t0:t0 + tn], AF.Square)
        for kc in range(KC):
            P.mm(pb[:, :tn], S.onesb, s[:, kc, :tn], start=(kc == 0), stop=(kc == KC - 1))
        r = rstd[bi % 2]
        P.act(r[:, :tn], pb[:, :tn], AF.Sqrt, bias=S.eps_t[:, :], scale=1.0 / D)
        P.recip(r[:, :tn], r[:, :tn])
        for kc in range(KC):
            t = tmp[n % 3]
            n += 1
            P.stt("dve", t[:, :tn], S.xT[:, kc, t0:t0 + tn], S.gs[:, l * 2 + wn, kc, which:which + 1],
                  r[:, :tn], ALU.mult, ALU.mult)
            P.act(S.hT[:, kc, t0:t0 + tn], t[:, :tn], AF.Identity,
                  bias=S.modv[:, l, si * 8 + kc, which:which + 1])
            if router is not None:
                rw, logit = router
                hh = h32[kc % 2]
                P.ts("dve", hh[:, :tn], t[:, :tn], S.modv[:, l, si * 8 + kc, which:which + 1], None, ALU.add)
                pl = S.pb[5]
                for j in range(tn // 128):
                    P.mm(pl[:, j * 8:(j + 1) * 8], hh[:, j * 128:(j + 1) * 128], rw[:, kc, :],
                         start=(kc == 0 and j == 0), stop=(kc == KC - 1), skip_group_check=True)
        if router is not None:
            rw, logit = router
            tile0 = (t0 - NCX) // 128
            P.copy("dve", logit[:, tile0:tile0 + tn // 128, :],
                   S.pb[5][:, 0:(tn // 128) * 8].rearrange("p (j e) -> p j e", e=8))


def phase_final(S):
    P = S.P
    with P.phase() as st:
        sq = [P.sb("fsq", [128, KC, 512], BF16, st) for _ in range(2)]
        rstd = [P.sb("frstd", [128, 512], F32, st) for _ in range(2)]
        yT = [P.sb("fyT", [128, KC, 512], F32, st) for _ in range(2)]
        ot = [P.sb("fot", [128, D], F32, st) for _ in range(3)]
        g32 = S.ng[:, 4, :]
        n = 0
        for bi in range(1, 5):
            t0, tn = TB[bi]
            s = sq[bi % 2]
            pb = S.pb[6 + bi % 2]
            for kc in range(KC):
                P.act(s[:, kc, :], S.xT[:, kc, t0:t0 + tn], AF.Square)
            for kc in range(KC):
                P.mm(pb[:, :], S.onesb, s[:, kc, :], start=(kc == 0), stop=(kc == KC - 1))
            r = rstd[bi % 2]
            P.act(r[:, :], pb[:, :], AF.Sqrt, bias=S.eps_t[:, :], scale=1.0 / D)
            P.recip(r[:, :], r[:, :])
            y = yT[bi % 2]
            for kc in range(KC):
                P.stt("dve", y[:, kc, :], S.xT[:, kc, t0:t0 + tn], g32[:, kc:kc + 1],
                      r[:, :], ALU.mult, ALU.mult)
            for q in range(4):
                o = ot[n % 3]
                n += 1
                for half in range(2):
                    pt = S.pb[(n * 2 + half) % 4]
                    for j in range(4):
                        kc = half * 4 + j
                        P.tr(pt[:, j * 128:(j + 1) * 128], y[:, kc, q * 128:(q + 1) * 128], S.identf)
                    if half == 0:
                        P.copy("dve", o[:, 0:512], pt[:, :])
                    else:
                        P.copy("act", o[:, 512:1024], pt[:, :])
                r0 = t0 - NCX + q * 128
                P.dma("sp", S.out[r0:r0 + 128, :], o[:, :])

import math


def phase_mixer(S, l):
    P = S.P
    ctx_out = l < 1
    with P.phase() as st:
        modulate(S, l, 0, range(5), st)
    if S.cfg.get("dump_h") == l:
        P.dump("hT", S.hT[:, :, :])
    br = S.cfg.get("branches", "abc")
    if "c" in br:
        branch_c(S, l, ctx_out)
    if "b" in br:
        branch_b(S, l, ctx_out)
    if "a" in br:
        branch_a(S, l, ctx_out)
    if S.cfg.get("dump_xmix") == l:
        P.dump("xT", S.xT[:, :, :])


def merge_branch(S, l, yT, wo_dram, gcol, ctx_out, st):
    P = S.P
    wg = P.sb("wg", [128, KC, 1024], BF16, st)
    wo = P.sb("wo", [128, 4, 1024], BF16, st)
    wout = P.sb("wout", [128, KC, 1024], BF16, st)
    P.dma("pool", wg[:, :, :], S.w_in[l, :, gcol:gcol + 1024].rearrange("(kc p) f -> p kc f", p=128))
    P.dma("pool", wo[:, :, :], wo_dram[l].rearrange("(c p) f -> p c f", p=128))
    P.dma("pool", wout[:, :, :], S.w_out[l].rearrange("(kc p) f -> p kc f", p=128))
    gmb = [P.sb("gm", [128, KC, 512], BF16, st) for _ in range(2)]
    sgb = [P.sb("sg", [128, 512], F32, st) for _ in range(2)]
    for bi in (range(5) if ctx_out else range(1, 5)):
        t0, tn = TB[bi]
        which = 1 if bi == 0 else 0
        gm = gmb[bi % 2]
        for fc in range(8):
            pg = S.pb[0 + fc % 2]
            py = S.pb[2 + fc % 2]
            for kc in range(KC):
                P.mm(pg[:, :tn], wg[:, kc, fc * 128:(fc + 1) * 128], S.hT[:, kc, t0:t0 + tn],
                     start=(kc == 0), stop=(kc == KC - 1))
            for c in range(4):
                P.mm(py[:, :tn], wo[:, c, fc * 128:(fc + 1) * 128], yT[:, c, t0:t0 + tn],
                     start=(c == 0), stop=(c == 3))
            sg = sgb[fc % 2]
            P.act(sg[:, :tn], pg[:, :tn], AF.Sigmoid)
            P.tt("dve", gm[:, fc, :tn], py[:, :tn], sg[:, :tn], ALU.mult)
        for fo in range(8):
            po = S.pb[4 + fo % 2]
            for fc in range(8):
                P.mm(po[:, :tn], wout[:, fc, fo * 128:(fo + 1) * 128], gm[:, fc, :tn],
                     start=(fc == 0), stop=(fc == 7))
            P.stt("dve", S.xT[:, fo, t0:t0 + tn], po[:, :tn], S.modv[:, l, 16 + fo, which:which + 1],
                  S.xT[:, fo, t0:t0 + tn], ALU.mult, ALU.add)


def branch_c(S, l, ctx_out):
    P = S.P
    lam_init = 0.8 - 0.6 * math.exp(-0.3 * l)
    with P.phase() as sto:
        ydT = P.sb("ydT", [128, 4, NT], BF16, sto)
        if not ctx_out:
            P.memset("pool", ydT[:, :, 0:NCX], 0.0)
        with P.phase() as st:
            ropet = P.sb("ropet", [128, 2, NL], BF16, st)
            P.dma("pool", ropet[:, :, :], S.rope.rearrange("c p t -> p c t"))
            rotT = P.sb("rotT", [128, 128], BF16, st)
            P.copy("dve", rotT[:, :], S.cf[:, 128:256])
            gnbc = P.sb("gnbc", [128, 128], F32, st)
            P.dma("sp", gnbc[:, :], S.gn[l, 2].partition_broadcast(128))
            P.ts("dve", gnbc[:, :], gnbc[:, :], 1.0 - lam_init, None, ALU.mult)
            lam = P.sb("lam", [128, 4, 64], F32, st)
            P.dma("sp", lam[:, :, :], S.lam_c[l].partition_broadcast(128))
            lp = P.sb("lp", [128, 2, 64], F32, st)
            P.tt("dve", lp[:, 0, :], lam[:, 0, :], lam[:, 1, :], ALU.mult)
            P.tt("dve", lp[:, 1, :], lam[:, 2, :], lam[:, 3, :], ALU.mult)
            ls = P.sb("ls", [128, 2], F32, st)
            P.reduce("dve", ls[:, :], lp[:, :, :], ALU.add, AX.X)
            le = P.sb("le", [128, 2], F32, st)
            P.act(le[:, :], ls[:, :], AF.Exp)
            neglam = P.sb("neglam", [128, 1], F32, st)
            P.tt("dve", neglam[:, :], le[:, 1:2], le[:, 0:1], ALU.subtract)
            P.ts("dve", neglam[:, :], neglam[:, :], -lam_init, None, ALU.add)

            wC = P.sb("wC", [128, KC, 1536], BF16, st)
            P.dma("pool", wC[:, :, :], S.w_in[l, :, C_Q:C_Q + 1536].rearrange("(kc p) f -> p kc f", p=128))
            gncol = P.sb("gncol", [128, 1], F32, st)
            P.dma("sp", gncol[:, :], S.gn[l, 2, :].rearrange("(p o) -> p o", o=1))
            P.ts("dve", gncol[:, :], gncol[:, :], 1.0 - lam_init, None, ALU.mult)
            qz = [P.sb("qz", [128, NT], BF16, st) for _ in range(2)]
            P.memset("pool", qz[0][:, :], 0.0)
            P.memset("pool", qz[1][:, :], 0.0)
            kTh = P.sb("kTh", [128, NT], BF16, st)
            vtok = P.sb("vtokc", [128, NTL, 128], BF16, st)
            xqb = [P.sb("xq", [128, 512], BF16, st) for _ in range(2)]
            t1b = [P.sb("t1", [128, 512], F32, st) for _ in range(1)]
            t2b = [P.sb("t2", [128, 512], F32, st) for _ in range(1)]
            NR = 5
            pring = [P.sb("pT", [128, 512], BF16, st) for _ in range(NR)]
            r0b = [P.sb("cr0", [128, 512], F32, st) for _ in range(1)]
            r1b = [P.sb("cr1", [128, 512], F32, st) for _ in range(1)]
            t0b = [P.sb("ct0", [128, 512], F32, st) for _ in range(1)]
            odb = [P.sb("cod", [128, 512], F32, st) for _ in range(1)]
            sqb = [P.sb("csq", [128, 512], BF16, st) for _ in range(1)]
            cnt = 0
            ring = 0
            ep = 0
            pending_ep = []

            def flush_ep():
                while pending_ep:
                    h_, q0_, qn_ = pending_ep.pop(0)
                    r0_ = r0b[0]; od_ = odb[0]; sq_ = sqb[0]
                    pn = S.pb[7]
                    P.mm(pn[:, :qn_], S.onesb, sq_[:, :qn_])
                    P.act(r0_[:, :qn_], pn[:, :qn_], AF.Sqrt, bias=S.eps_t[:, :], scale=1.0 / 128)
                    P.recip(r0_[:, :qn_], r0_[:, :qn_])
                    P.stt("dve", ydT[:, h_, q0_:q0_ + qn_], od_[:, :qn_], gncol[:, :], r0_[:, :qn_], ALU.mult, ALU.mult)

            for h in range(4):
                for dstT, col in ((None, h * 128), (kTh, 512 + h * 128)):
                    for bi in range(5):
                        t0, tn = TB[bi]
                        pb = S.pb[6]
                        for kc in range(KC):
                            P.mm(pb[:, :tn], wC[:, kc, col:col + 128], S.hT[:, kc, t0:t0 + tn],
                                 start=(kc == 0), stop=(kc == KC - 1))
                        if bi == 0:
                            if dstT is None:
                                P.act(qz[0][0:64, 0:tn], pb[0:64, :tn], AF.Copy)
                                P.act(qz[1][64:128, 0:tn], pb[64:128, :tn], AF.Copy)
                            else:
                                P.act(dstT[:, 0:tn], pb[:, :tn], AF.Copy)
                        else:
                            xq = xqb[cnt % 2]; t1 = t1b[0]; t2 = t2b[0]
                            cnt += 1
                            P.act(xq[:, :], pb[:, :], AF.Copy)
                            pr = S.pb[7]
                            P.mm(pr[:, :], rotT[:, :], xq[:, :])
                            lt0 = t0 - NCX
                            P.tt("dve", t1[:, :], pr[:, :], ropet[:, 1, lt0:lt0 + 512], ALU.mult)
                            P.tt("pool", t2[:, :], xq[:, :], ropet[:, 0, lt0:lt0 + 512], ALU.mult)
                            if dstT is None:
                                P.tt("dve", qz[0][0:64, t0:t0 + 512], t1[0:64, :], t2[0:64, :], ALU.add)
                                P.tt("dve", qz[1][64:128, t0:t0 + 512], t1[64:128, :], t2[64:128, :], ALU.add)
                            else:
                                P.tt("dve", dstT[:, t0:t0 + 512], t1[:, :], t2[:, :], ALU.add)
                vc = 1024 + h * 128
                for g0 in range(0, NTL, 4):
                    ng_ = min(4, NTL - g0)
                    pb = S.pb[6]
                    for j in range(ng_):
                        tt = g0 + j
                        for kc in range(KC):
                            P.mm(pb[:, j * 128:(j + 1) * 128], S.hT[:, kc, tt * 128:(tt + 1) * 128],
                                 wC[:, kc, vc:vc + 128], start=(kc == 0), stop=(kc == KC - 1))
                    P.act(vtok[:, g0:g0 + ng_, :],
                          pb[:, 0:ng_ * 128].rearrange("p (j t) -> p j t", j=ng_), AF.Copy)
                for qb in ([0] if ctx_out else []) + [1, 2, 3, 4]:
                    q0, qn = TB[qb]
                    seq = list(range(2)) if qb == 0 else list(range(NTL))
                    for m in range(2):
                        oT = S.pb[2 + m]
                        sT = S.pb[4 + m]

                        def qk(kt):
                            nonlocal ring
                            ps = S.pb[kt % 2]
                            P.mm(ps[:, :qn], kTh[:, kt * 128:(kt + 1) * 128], qz[m][:, q0:q0 + qn])
                            pt = pring[ring % NR]
                            ring += 1
                            P.act(pt[:, :qn], ps[:, :qn], AF.Exp, scale=0.125)
                            return pt
                        LA = 3
                        pts = {}
                        for i, kt in enumerate(seq):
                            if i == 0:
                                for j in range(min(LA, len(seq))):
                                    pts[seq[j]] = qk(seq[j])
                            if i + LA < len(seq):
                                pts[seq[i + LA]] = qk(seq[i + LA])
                            pt = pts.pop(kt)
                            if m == 0 and i == min(6, len(seq) - 1):
                                flush_ep()
                            P.mm(oT[:, :qn], vtok[:, kt, :], pt[:, :qn], start=(i == 0), stop=(i == len(seq) - 1))
                            if not S.cfg.get("c_nosum") or i == 0:
                                P.mm(sT[:, :qn], S.onesb, pt[:, :qn], start=(i == 0), stop=(i == len(seq) - 1) or bool(S.cfg.get("c_nosum")))
                    r0 = r0b[0]; r1 = r1b[0]; t0_ = t0b[0]; od = odb[0]; sq = sqb[0]
                    ep += 1
                    P.recip(r0[:, :qn], S.pb[4][:, :qn])
                    P.recip(r1[:, :qn], S.pb[5][:, :qn])
                    P.ts("dve", r1[:, :qn], r1[:, :qn], neglam[:, :], None, ALU.mult)
                    P.tt("dve", t0_[:, :qn], S.pb[2][:, :qn], r0[:, :qn], ALU.mult)
                    P.tt("dve", r1[:, :qn], S.pb[3][:, :qn], r1[:, :qn], ALU.mult)
                    P.tt("pool", od[:, :qn], t0_[:, :qn], r1[:, :qn], ALU.add)
                    P.tt("pool", sq[:, :qn], od[:, :qn], od[:, :qn], ALU.mult)
                    pending_ep.append((h, q0, qn))
            flush_ep()
        if S.cfg.get("dump_yd") == l:
            P.dump("ydT", ydT[:, :, :])
        if S.cfg.get("merge", True):
            with P.phase() as st:
                merge_branch(S, l, ydT, S.w_o_c, G_D, ctx_out, st)


def branch_b(S, l, ctx_out):
    P = S.P
    U = S.cf[:, 256:384]
    Lm = S.cf[:, 384:512]
    SU = S.cf[:, 512:640]
    SL = S.cf[:, 640:768]
    with P.phase() as sto:
        ybT = P.sb("ybT", [128, 4, NT], BF16, sto)
        if not ctx_out:
            P.memset("pool", ybT[:, :, 0:NCX], 0.0)
        with P.phase() as stc:
            gnbc = P.sb("gnbcb", [128, 128], F32, stc)
            P.dma("sp", gnbc[:, :], S.gn[l, 1].partition_broadcast(128))
            wg2 = P.sb("wg2", [17, 2, 256], BF16, stc)
            if S.cfg.get("t_wg2", 1):
                P.dma("pool", wg2[:, :, :], S.wg2[l])
            one_t = P.sb("one_t", [128, 1], F32, stc)
            P.memset("dve", one_t[:, :], 1.0)
            mkb = P.sb("mkb", [128, 512], BF16, stc)
            P.copy("dve", mkb[:, :], S.cf[:, 256:768])
            Ub = mkb[:, 0:128]; Lb = mkb[:, 128:256]; SUb = mkb[:, 256:384]; SLb = mkb[:, 384:512]
            for hp in range(2):
                with P.phase() as st:
                    qT = P.sb("bqT", [128, NT], BF16, st)
                    kT = P.sb("bkT", [128, NT], BF16, st)
                    ktok = P.sb("bktok", [128, NTL, 128], BF16, st)
                    vtok = P.sb("bvtok", [128, NTL, 256], BF16, st)
                    srtok = P.sb("bsrtok", [128, NTL, 256], BF16, st)
                    glrT = P.sb("bglrT", [17, 2, NT], BF16, st)
                    Sst = P.sb("bSst", [128, 2, NTL, 128], BF16, st)
                    Sf = [P.sb("bSf", [128, 128], F32, st) for _ in range(2)]
                    if S.cfg.get("t_ms", 1):
                        P.memset("pool", glrT[:, :, :], 1.0)
                    P.memset("dve", Sf[0][:, :], 0.0)
                    P.memset("dve", Sf[1][:, :], 0.0)
                    with P.phase() as stw:
                        wB = P.sb("wBp", [128, KC, 800], BF16, stw)
                        for (dst0, n_, src0) in ((0, 128, B_Q + hp * 128), (128, 128, B_K + hp * 128),
                                                 (256, 256, B_V + hp * 256), (512, 256, B_R + hp * 256),
                                                 (768, 32, B_GL)):
                            P.dma("pool", wB[:, :, dst0:dst0 + n_],
                                  S.w_in[l, :, src0:src0 + n_].rearrange("(kc p) f -> p kc f", p=128))
                        for bi in (range(5) if S.cfg.get("b_proj", 9) >= 1 else []):
                            t0, tn = TB[bi]
                            for dstT, col in ((qT, 0), (kT, 128)):
                                pb = S.pb[4 + (col // 128)]
                                for kc in range(KC):
                                    P.mm(pb[:, :tn], wB[:, kc, col:col + 128], S.hT[:, kc, t0:t0 + tn],
                                         start=(kc == 0), stop=(kc == KC - 1))
                                P.act(dstT[:, t0:t0 + tn], pb[:, :tn], AF.Copy)
                            for d in (range(2) if S.cfg.get("t_glr", 1) else []):
                                pb = S.pb[6 + d]
                                for kc in range(KC):
                                    P.mm(pb[0:16, :tn], wB[:, kc, 768 + d * 16:768 + (d + 1) * 16],
                                         S.hT[:, kc, t0:t0 + tn], start=(kc == 0), stop=(kc == KC - 1))
                                P.copy("dve", glrT[0:16, d, t0:t0 + tn], pb[0:16, :tn])
                        for n in (range(NTL) if S.cfg.get("b_proj", 9) >= 2 else []):
                            pa = S.pb[0 + n % 2]
                            pr = S.pb[2 + n % 2]
                            for kc in range(KC):
                                P.mm(pa[:, 0:384], S.hT[:, kc, n * 128:(n + 1) * 128], wB[:, kc, 128:512],
                                     start=(kc == 0), stop=(kc == KC - 1))
                            for kc in range(KC):
                                P.mm(pr[:, 0:256], S.hT[:, kc, n * 128:(n + 1) * 128], wB[:, kc, 512:768],
                                     start=(kc == 0), stop=(kc == KC - 1))
                            P.copy("dve", ktok[:, n, :], pa[:, 0:128])
                            P.act(vtok[:, n, :], pa[:, 128:384], AF.Copy)
                            P.act(srtok[:, n, :], pr[:, 0:256], AF.Silu)

                    e1b = [P.sb("be1", [128, 128], F32, st) for _ in range(2)]
                    spb = [P.sb("bsp", [128, 128], F32, st) for _ in range(2)]
                    eremb = [P.sb("berem", [128, 128], F32, st) for _ in range(2)]
                    kgb = [P.sb("bkg", [128, 128], BF16, st) for _ in range(2)]
                    sphl = [P.sb("bsphl", [128, 2, 128], BF16, st) for _ in range(2)]
                    glb = [P.sb("bgl", [128, 1], F32, st) for _ in range(4)]
                    cnt = 0

                    def gate_sp(d, n):
                        nonlocal cnt
                        px = S.pb[0 + cnt % 2]
                        e1 = e1b[cnt % 2]; sp = spb[cnt % 2]
                        cnt += 1
                        P.mm(px[:, 0:128], glrT[0:17, d, n * 128:(n + 1) * 128],
                             wg2[0:17, d, hp * 128:(hp + 1) * 128])
                        P.act(e1[:, :], px[:, 0:128], AF.Exp, scale=-1.0)
                        P.act(sp[:, :], e1[:, :], AF.Ln, bias=one_t[:, :])
                        hl = sphl[(cnt - 1) % 2]
                        P.copy("dve", hl[:, 0, :], sp[:, :])
                        P.tt("dve", hl[:, 1, :], sp[:, :], hl[:, 0, :], ALU.subtract)
                        return hl

                    if S.cfg.get("b_stop", 9) < 1:
                        continue
                    order = [list(range(NTL)), [1, 0] + list(range(NTL - 1, 1, -1))]
                    for s in range(NTL):
                        for d in range(2):
                            n = order[d][s]
                            sp = gate_sp(d, n)
                            k_ = cnt
                            prm = S.pb[2 + d]
                            for z in range(2):
                                P.mm(prm[:, 0:128], SLb if d == 0 else SUb, sp[:, z, :], start=(z == 0), stop=(z == 1))
                            for z in range(2):
                                P.mm(prm[:, 128:129], sp[:, z, :], S.onesb[:, 0:1], start=(z == 0), stop=(z == 1))
                            erem = eremb[d]; kg = kgb[d]; gl = glb[(s * 2 + d) % 4]
                            P.act(erem[:, :], prm[:, 0:128], AF.Exp, scale=-1.0 / 16)
                            P.act(gl[:, :], prm[:, 128:129], AF.Exp, scale=-1.0 / 16)
                            P.tt("dve", kg[:, :], ktok[:, n, :], erem[:, :], ALU.mult)
                            P.copy("act", Sst[:, d, n, :], Sf[d][:, :])
                            if s == NTL - 1:
                                continue
                            pS = S.pb[4 + d]
                            P.mm(pS[:, 0:256], kg[:, :], vtok[:, n, :])
                            for hh in range(2):
                                sl = slice(hh * 64, (hh + 1) * 64)
                                P.stt("dve", Sf[d][sl, :], Sf[d][sl, :], gl[sl, :], pS[sl, hh * 128:(hh + 1) * 128],
                                      ALU.mult, ALU.add)

                    if S.cfg.get("b_stop", 9) < 2:
                        continue
                    egb = [P.sb("beg", [128, 128], F32, st) for _ in range(2)]
                    eib = [P.sb("bei", [128, 128], F32, st) for _ in range(2)]
                    qtb = [P.sb("bqt", [128, 128], BF16, st) for _ in range(4)]
                    ktb = [P.sb("bkt", [128, 128], BF16, st) for _ in range(4)]
                    atb = [P.sb("bat", [128, 2, 128], BF16, st) for _ in range(4)]
                    smb = [P.sb("bsm", [128, 2], F32, st) for _ in range(4)]
                    junk = P.sb("bjunk", [128, 128], BF16, st)
                    y1b = [P.sb("by1", [128, 128], F32, st) for _ in range(2)]
                    ytb = [P.sb("byt", [128, 128], BF16, st) for _ in range(2)]
                    ptb = S.pb[7][:, 0:256].bitcast(BF16)
                    k2 = 0
                    ep = 0
                    for n in (range(NTL) if ctx_out else range(2, NTL)):
                        dd = []
                        for d in range(2):
                            sp = gate_sp(d, n)
                            pg = S.pb[2]
                            for z in range(2):
                                P.mm(pg[:, 0:128], sp[:, z, :], Ub if d == 0 else Lb, start=(z == 0), stop=(z == 1))
                            eg = egb[d]; ei = eib[d]
                            qt = qtb[k2 % 4]; kt = ktb[k2 % 4]; at = atb[k2 % 4]
                            k2 += 1
                            P.act(eg[:, :], pg[:, 0:128], AF.Exp, scale=-1.0 / 16)
                            P.act(ei[:, :], pg[:, 0:128], AF.Exp, scale=1.0 / 16)
                            P.stt("dve", qt[:, :], qT[:, n * 128:(n + 1) * 128], 0.125, eg[:, :], ALU.mult, ALU.mult)
                            P.tt("pool", kt[:, :], kT[:, n * 128:(n + 1) * 128], ei[:, :], ALU.mult)
                            if S.cfg.get("p2", 9) < 1:
                                continue
                            for hh in range(2):
                                sl = slice(hh * 64, (hh + 1) * 64)
                                P.mm(S.pb[3 + hh][:, 0:128], kt[sl, :], qt[sl, :])
                            mask = (U if d == 0 else Lm)
                            for hh in range(2):
                                P.tt("dve", at[:, hh, :], S.pb[3 + hh][:, 0:128], mask, ALU.mult)
                            dd.append((qt, at))
                        if S.cfg.get("p2", 9) < 2:
                            continue
                        for hh in range(2):
                            sl = slice(hh * 64, (hh + 1) * 64)
                            oo = S.pb[5 + hh][:, 0:128]
                            for d in range(2):
                                qt, at = dd[d]
                                P.mm(oo, qt[sl, :], Sst[sl, d, n, :], start=(d == 0), stop=False)
                                P.mm(oo, at[:, hh, :], vtok[:, n, hh * 128:(hh + 1) * 128], start=False, stop=(d == 1))
                        if S.cfg.get("p2", 9) < 3:
                            continue
                        for hh in range(2):
                            oo = S.pb[5 + hh][:, 0:128]
                            s_ = smb[ep % 4]; y1 = y1b[ep % 2]; yt = ytb[ep % 2]
                            ep += 1
                            P.memset("pool", s_[:, 0:1], 0.0)
                            P.act(junk[:, :], oo, AF.Square, accum_out=s_[:, 0:1])
                            P.act(s_[:, 1:2], s_[:, 0:1], AF.Sqrt, bias=S.eps_t[:, :], scale=1.0 / 128)
                            P.recip(s_[:, 1:2], s_[:, 1:2])
                            P.stt("dve", y1[:, :], oo, s_[:, 1:2], gnbc[:, :], ALU.mult, ALU.mult)
                            P.tt("pool", yt[:, :], y1[:, :], srtok[:, n, hh * 128:(hh + 1) * 128], ALU.mult)
                            P.tr(ptb[:, hh * 128:(hh + 1) * 128], yt[:, :], S.identb)
                        P.copy("act", ybT[:, hp * 2:hp * 2 + 2, n * 128:(n + 1) * 128],
                               ptb[:, 0:256].rearrange("p (h t) -> p h t", h=2))
        if S.cfg.get("dump_yb") == l:
            P.dump("ybT", ybT[:, :, :])
        if S.cfg.get("merge", True):
            with P.phase() as st:
                merge_branch(S, l, ybT, S.w_o_b, G_B, ctx_out, st)


def branch_a(S, l, ctx_out):
    P = S.P
    U = S.cf[:, 256:384]
    Lm = S.cf[:, 384:512]
    SU = S.cf[:, 512:640]
    SL = S.cf[:, 640:768]
    with P.phase() as sto:
        yaT = P.sb("yaT", [128, 4, NT], BF16, sto)
        if not ctx_out:
            P.memset("pool", yaT[:, :, 0:NCX], 0.0)
        with P.phase() as stc:
            gnbc = P.sb("gnbca", [128, 128], F32, stc)
            P.dma("sp", gnbc[:, :], S.gn[l, 0].partition_broadcast(128))
            one_t = P.sb("one_ta", [128, 1], F32, stc)
            P.memset("dve", one_t[:, :], 1.0)
            mkb = P.sb("mkba", [128, 4, 128], BF16, stc)
            P.copy("dve", mkb[:, :, :], S.cf[:, 256:768].rearrange("p (m c) -> p m c", m=4))
            ones_col = P.sb("onescol", [128, 1], BF16, stc)
            P.memset("dve", ones_col[:, :], 1.0)
            cum1 = P.sb("cum1", [128, 2, 129], BF16, stc)
            P.memset("dve", cum1[:, :, :], 1.0)
            P.copy("dve", cum1[:, 0, 0:128], U)
            P.copy("dve", cum1[:, 1, 0:128], Lm)
            cumb = [mkb[:, 0, :], mkb[:, 1, :]]
            gmf = [SL, SU]
            strict = [SL, SU]
            maskT = [U, Lm]
            cw = P.sb("convw", [128, 12, 3], F32, stc)
            P.dma("sp", cw[:, :, :], S.convw[l])
            adt = P.sb("adt", [128, 2, 8], F32, stc)
            P.dma("sp", adt[:, :, :], S.adt[l].partition_broadcast(128))
            nA = P.sb("nA", [128, 8], F32, stc)
            P.act(nA[:, :], adt[:, 0, :], AF.Exp)
            P.ts("dve", nA[:, :], nA[:, :], -1.0, None, ALU.mult)
            betat = P.sb("betat", [128, NTL, 8], F32, stc)
            nbetat = P.sb("nbetat", [128, NTL, 8], F32, stc)
            gt = P.sb("gt", [128, NTL, 8], F32, stc)
            with P.phase() as stw:
                wbg = P.sb("wbg", [128, KC, 16], BF16, stw)
                P.dma("pool", wbg[:, :, :], S.w_in[l, :, A_B:A_B + 16].rearrange("(kc p) f -> p kc f", p=128))
                tmpe = P.sb("tmpe", [128, NTL, 8], F32, stw)
                for n in range(NTL):
                    pb = S.pb[n % 2]
                    for kc in range(KC):
                        P.mm(pb[:, 0:16], S.hT[:, kc, n * 128:(n + 1) * 128], wbg[:, kc, :],
                             start=(kc == 0), stop=(kc == KC - 1))
                    P.act(betat[:, n, :], pb[:, 0:8], AF.Sigmoid)
                    P.tt("dve", gt[:, n, :], pb[:, 8:16], adt[:, 1, :], ALU.add)
                P.act(tmpe[:, :, :], gt[:, :, :], AF.Exp)
                P.act(gt[:, :, :], tmpe[:, :, :], AF.Ln, bias=one_t[:, :])
                P.tt("dve", gt[:, :, :], gt[:, :, :], nA[:, :].unsqueeze(1).to_broadcast([128, NTL, 8]), ALU.mult)
                P.ts("dve", nbetat[:, :, :], betat[:, :, :], -1.0, None, ALU.mult)

            for h in range(4):
                with P.phase() as st:
                    qT = P.sb("aqT", [128, NT], BF16, st)
                    kT = P.sb("akT", [128, NT], BF16, st)
                    ktok = P.sb("aktok", [128, NTL, 128], BF16, st)
                    vtok = P.sb("avtok", [128, NTL, 128], BF16, st)
                    sztok = P.sb("asztok", [128, NTL, 128], BF16, st)
                    oacc = P.sb("aoacc", [128, NTL, 128], F32, st)
                    P.memset("pool", oacc[:, :, :], 0.0)
                    ptb = S.pb[7][:, 0:256].bitcast(BF16)
                    with P.phase() as stw:
                        wA = P.sb("wAh", [128, KC, 512], BF16, stw)
                        for i, c0 in enumerate((A_Q, A_K, A_V, A_Z)):
                            P.dma("pool", wA[:, :, i * 128:(i + 1) * 128],
                                  S.w_in[l, :, c0 + h * 128:c0 + (h + 1) * 128].rearrange("(kc p) f -> p kc f", p=128))
                        pre = [P.sb("apre", [128, NT], BF16, stw) for _ in range(2)]
                        cv = [P.sb("acv", [128, NT], BF16, stw) for _ in range(2)]
                        sqb = P.sb("asq", [128, 512], BF16, stw)
                        rs = P.sb("ars", [128, 512], F32, stw)
                        vT = P.sb("avT", [128, NT], BF16, stw)
                        for i in range(3):
                            pr = pre[i % 2]; c = cv[i % 2]
                            for bi in range(5):
                                t0, tn = TB[bi]
                                pb = S.pb[bi % 2]
                                for kc in range(KC):
                                    P.mm(pb[:, :tn], wA[:, kc, i * 128:(i + 1) * 128], S.hT[:, kc, t0:t0 + tn],
                                         start=(kc == 0), stop=(kc == KC - 1))
                                P.act(pr[:, t0:t0 + tn], pb[:, :tn], AF.Copy)
                            ch = i * 4 + h
                            P.ts("dve", c[:, :], pr[:, :], cw[:, ch, 1:2], None, ALU.mult)
                            for (a, b) in ((0, NCX), (NCX, NT)):
                                P.stt("dve", c[:, a + 1:b], pr[:, a:b - 1], cw[:, ch, 0:1], c[:, a + 1:b], ALU.mult, ALU.add)
                                P.stt("dve", c[:, a:b - 1], pr[:, a + 1:b], cw[:, ch, 2:3], c[:, a:b - 1], ALU.mult, ALU.add)
                            if i == 2:
                                P.act(vT[:, :], c[:, :], AF.Silu)
                            else:
                                dst = qT if i == 0 else kT
                                P.act(c[:, :], c[:, :], AF.Silu)
                                for bi in range(5):
                                    t0, tn = TB[bi]
                                    pb = S.pb[2 + bi % 2]
                                    P.act(sqb[:, :tn], c[:, t0:t0 + tn], AF.Square)
                                    P.mm(pb[:, :tn], S.onesb, sqb[:, :tn])
                                    P.act(rs[:, :tn], pb[:, :tn], AF.Sqrt, bias=S.eps_t[:, :])
                                    P.recip(rs[:, :tn], rs[:, :tn])
                                    if i == 0:
                                        P.stt("dve", dst[:, t0:t0 + tn], c[:, t0:t0 + tn], 128.0 ** -0.5, rs[:, :tn],
                                              ALU.mult, ALU.mult)
                                    else:
                                        P.tt("dve", dst[:, t0:t0 + tn], c[:, t0:t0 + tn], rs[:, :tn], ALU.mult)
                        for n in range(NTL):
                            P.tr(ptb[:, 0:128], kT[:, n * 128:(n + 1) * 128], S.identb)
                            P.tr(ptb[:, 128:256], vT[:, n * 128:(n + 1) * 128], S.identb)
                            P.copy("dve", ktok[:, n, :], ptb[:, 0:128])
                            P.copy("dve", vtok[:, n, :], ptb[:, 128:256])
                            pz = S.pb[n % 2]
                            for kc in range(KC):
                                P.mm(pz[:, 0:128], S.hT[:, kc, n * 128:(n + 1) * 128], wA[:, kc, 384:512],
                                     start=(kc == 0), stop=(kc == KC - 1))
                            P.act(sztok[:, n, :], pz[:, 0:128], AF.Silu)

                    G = S.cfg.get("a_G", 4)
                    NRG = 4
                    def ring(nm, shape, dt):
                        return [[P.sb(nm, shape, dt, st) for _ in range(NRG)] for _ in range(2)]
                    u_r = ring("au", [128, 128], F32)
                    wT_r = ring("awT", [128, 128], BF16)
                    kg_r = ring("akg", [128, 128], BF16)
                    AT_r = ring("aAT", [128, 128], BF16)
                    sc_r = ring("asc", [128, 4], F32)

                    class WS:
                        pass
                    wss = []
                    for gi in range(G):
                        w_ = WS()
                        w_.gf = P.sb("agmf", [128, 129], F32, st)
                        w_.ghl = P.sb("agmhl", [128, 2, 129], BF16, st)
                        w_.e1 = P.sb("aE1", [128, 129], F32, st)
                        w_.e2 = P.sb("aE2", [128, 129], F32, st)
                        w_.XX = [P.sb("aXX", [128, 2, 128], F32, st) for _ in range(2)]
                        w_.PT = [P.sb("aPT", [128, 128], F32, st) for _ in range(2)]
                        w_.vb = P.sb("avb", [128, 128], BF16, st)
                        w_.kbg = P.sb("akbg", [128, 128], BF16, st)
                        w_.TT = P.sb("aTTb", [128, 128], BF16, st)
                        w_.pc = S.pb[2 + gi]
                        w_.pP = S.pb[6 + gi % 2]
                        w_.ev = "act"
                        wss.append(w_)
                    vnew = [P.sb("avnew", [128, 128], BF16, st) for _ in range(2)]
                    o1s = [P.sb("ao1s", [128, 128], F32, st) for _ in range(2)]
                    ot = [P.sb("aot", [128, 128], F32, st) for _ in range(2)]
                    Sf = [P.sb("aSf", [128, 128], F32, st) for _ in range(2)]
                    Sb = [P.sb("aSb", [128, 128], BF16, st) for _ in range(2)]
                    for d in range(2):
                        P.memset("dve", Sf[d][:, :], 0.0)
                        P.memset("dve", Sb[d][:, :], 0.0)
                    order = [list(range(NTL)), [1, 0] + list(range(NTL - 1, 1, -1))]

                    def precompute(d, n, slot, w_):
                        col = d * 4 + h
                        g = gt[:, n, col:col + 1]
                        beta = betat[:, n, col:col + 1]
                        nbeta = nbetat[:, n, col:col + 1]
                        tl = slice(n * 128, (n + 1) * 128)
                        sc = sc_r[d][slot]
                        pc = w_.pc
                        gf = w_.gf; ghl = w_.ghl; e1 = w_.e1; e2 = w_.e2
                        P.ts("dve", gf[:, 0:128], gmf[d], g, None, ALU.mult)
                        P.copy("dve", gf[:, 128:129], g)
                        P.copy("dve", ghl[:, 0, :], gf[:, :])
                        P.tt("dve", ghl[:, 1, :], gf[:, :], ghl[:, 0, :], ALU.subtract)
                        for z in range(2):
                            P.mm(pc[:, 0:129], cumb[d], ghl[:, z, :], start=(z == 0), stop=(z == 1))
                        for z in range(2):
                            P.mm(pc[:, 256:385], ghl[:, z, 0:128], cum1[:, d, :], start=(z == 0), stop=(z == 1))
                        P.act(e1[:, :], pc[:, 0:129], AF.Exp)
                        P.act(e2[:, :], pc[:, 256:385], AF.Exp)
                        yield
                        P.tt("pool", e1[:, 0:128], e1[:, 0:128], strict[d], ALU.mult)
                        P.tt("pool", e2[:, 0:128], e2[:, 0:128], maskT[d], ALU.mult)
                        P.tt("dve", sc[:, 2:3], e1[:, 128:129], e2[:, 128:129], ALU.mult)
                        P.tt("dve", sc[:, 3:4], e1[:, 128:129], beta, ALU.mult)
                        P.copy("dve", sc[:, 0:1], e1[:, 128:129])
                        P.mm(pc[:, 0:128], kT[:, tl], kT[:, tl])
                        P.mm(pc[:, 128:256], kT[:, tl], qT[:, tl])
                        XX = w_.XX; PT = w_.PT
                        P.stt("dve", XX[0][:, 0, :], pc[:, 0:128], nbeta, e1[:, 0:128], ALU.mult, ALU.mult)
                        P.tt("dve", AT_r[d][slot][:, :], pc[:, 128:256], e2[:, 0:128], ALU.mult)
                        P.ts("dve", kg_r[d][slot][:, :], ktok[:, n, :], e2[:, 128:129], None, ALU.mult)
                        P.ts("dve", w_.vb[:, :], vtok[:, n, :], beta, None, ALU.mult)
                        P.ts("dve", w_.kbg[:, :], ktok[:, n, :], sc[:, 3:4], None, ALU.mult)
                        yield
                        P.tr(pc[:, 384:512], XX[0][:, 0, :], S.identf)
                        P.copy(w_.ev, XX[0][:, 1, :], pc[:, 384:512])
                        P.tt("dve", PT[0][:, :], pc[:, 384:512], S.identf, ALU.add)
                        yield
                        cur = 0
                        for lev in range(1, 7):
                            nxt = 1 - cur
                            if lev > 1:
                                P.mm(w_.pP[:, 0:128], XX[cur][:, 0, :], PT[lev % 2][:, :])
                                P.tt("dve", PT[1 - (lev % 2)][:, :], w_.pP[:, 0:128], PT[lev % 2][:, :], ALU.add)
                            P.mm(pc[:, 0:128], XX[cur][:, 1, :], XX[cur][:, 0, :])
                            if lev < 6:
                                P.mm(pc[:, 128:256], XX[cur][:, 0, :], XX[cur][:, 1, :])
                                P.copy(w_.ev, XX[nxt][:, :, :], pc[:, 0:256].rearrange("p (a b) -> p a b", a=2))
                            else:
                                P.copy(w_.ev, XX[nxt][:, 0, :], pc[:, 0:128])
                            cur = nxt
                            yield
                        P.mm(w_.pP[:, 0:128], XX[cur][:, 0, :], PT[1][:, :])
                        P.tt("dve", PT[0][:, :], w_.pP[:, 0:128], PT[1][:, :], ALU.add)
                        P.copy("act", w_.TT[:, :], PT[0][:, :])
                        yield
                        P.mm(pc[:, 0:128], w_.TT[:, :], w_.vb[:, :])
                        P.mm(pc[:, 128:256], w_.kbg[:, :], w_.TT[:, :])
                        P.copy("act", u_r[d][slot][:, :], pc[:, 0:128])
                        P.copy("act", wT_r[d][slot][:, :], pc[:, 128:256])

                    def recur(d, n, slot, last):
                        tl = slice(n * 128, (n + 1) * 128)
                        pr = S.pb[d]
                        sc = sc_r[d][slot]
                        P.mm(pr[:, 0:128], wT_r[d][slot][:, :], Sb[d][:, :])
                        P.mm(pr[:, 128:256], qT[:, tl], Sb[d][:, :])
                        P.tt("dve", vnew[d][:, :], u_r[d][slot][:, :], pr[:, 0:128], ALU.subtract)
                        want_out = ctx_out or n >= 2
                        yield
                        if want_out:
                            P.mm(pr[:, 256:384], AT_r[d][slot][:, :], vnew[d][:, :])
                        if not last:
                            P.mm(pr[:, 384:512], kg_r[d][slot][:, :], vnew[d][:, :])
                        if want_out:
                            P.act(o1s[d][:, :], pr[:, 128:256], AF.Copy, scale=sc[:, 0:1])
                        if not last:
                            P.stt("dve", Sf[d][:, :], Sf[d][:, :], sc[:, 2:3], pr[:, 384:512], ALU.mult, ALU.add)
                            P.copy("act", Sb[d][:, :], Sf[d][:, :])
                        if want_out:
                            P.tt("dve", ot[d][:, :], pr[:, 256:384], o1s[d][:, :], ALU.add)
                            P.tt("pool", oacc[:, n, :], oacc[:, n, :], ot[d][:, :], ALU.add)
                        yield

                    units = [(d, s) for s in range(NTL) for d in range(2)]
                    pre_done = set()
                    active = []
                    next_unit = 0
                    rec_step = [0, 0]
                    rec_gen = [None, None]
                    free_ws = list(range(G))
                    while True:
                        while free_ws and next_unit < len(units):
                            d_, s_ = units[next_unit]
                            if s_ - rec_step[d_] >= NRG - 1:
                                break
                            wi = free_ws.pop(0)
                            active.append(("pre", (d_, s_, wi), precompute(d_, order[d_][s_], s_ % NRG, wss[wi])))
                            next_unit += 1
                        for d_ in range(2):
                            if rec_gen[d_] is None and rec_step[d_] < NTL and (d_, rec_step[d_]) in pre_done:
                                s_ = rec_step[d_]
                                rec_gen[d_] = recur(d_, order[d_][s_], s_ % NRG, s_ == NTL - 1)
                        if not active and rec_gen[0] is None and rec_gen[1] is None:
                            if next_unit >= len(units) and rec_step[0] >= NTL and rec_step[1] >= NTL:
                                break
                        S.a_hist = getattr(S, "a_hist", [])
                        S.a_hist.append((len(active), rec_gen[0] is not None, rec_gen[1] is not None))
                        for item in list(active):
                            kind, key, gen = item
                            try:
                                next(gen)
                            except StopIteration:
                                active.remove(item)
                                pre_done.add((key[0], key[1]))
                                free_ws.append(key[2])
                        for d_ in range(2):
                            if rec_gen[d_] is not None:
                                try:
                                    next(rec_gen[d_])
                                except StopIteration:
                                    rec_gen[d_] = None
                                    rec_step[d_] += 1

                    smb = [P.sb("asm", [128, 2], F32, st) for _ in range(4)]
                    junk = P.sb("ajunk", [128, 128], BF16, st)
                    y1b = [P.sb("ay1", [128, 128], F32, st) for _ in range(2)]
                    ytb = [P.sb("ayt", [128, 128], BF16, st) for _ in range(2)]
                    ep = 0
                    for n in (range(NTL) if ctx_out else range(2, NTL)):
                        s_ = smb[ep % 4]; y1 = y1b[ep % 2]; yt = ytb[ep % 2]
                        ep += 1
                        P.memset("pool", s_[:, 0:1], 0.0)
                        P.act(junk[:, :], oacc[:, n, :], AF.Square, accum_out=s_[:, 0:1])
                        P.act(s_[:, 1:2], s_[:, 0:1], AF.Sqrt, bias=S.eps_t[:, :], scale=1.0 / 128)
                        P.recip(s_[:, 1:2], s_[:, 1:2])
                        P.stt("dve", y1[:, :], oacc[:, n, :], s_[:, 1:2], gnbc[:, :], ALU.mult, ALU.mult)
                        P.tt("pool", yt[:, :], y1[:, :], sztok[:, n, :], ALU.mult)
                        P.tr(ptb[:, 0:128], yt[:, :], S.identb)
                        P.copy("act", yaT[:, h, n * 128:(n + 1) * 128], ptb[:, 0:128])
        if S.cfg.get("dump_ya") == l:
            P.dump("yaT", yaT[:, :, :])
        if S.cfg.get("merge", True):
            with P.phase() as st:
                merge_branch(S, l, yaT, S.w_o_a, G_A, ctx_out, st)


def phase_ffn(S, l):
    P = S.P
    moe = (l % 2 == 1)
    ctx_out = l < 1
    blocks = list(range(5)) if ctx_out else list(range(1, 5))
    i_ = l // 2
    with P.phase() as sto:
        gate = None
        if moe:
            logit = P.sb("logit", [128, 16, 8], F32, sto)
            gate = P.sb("gate", [128, 16, 8], F32, sto)
        with P.phase() as st:
            router = None
            if moe:
                rw = P.sb("rw", [128, KC, 8], F32, st)
                P.dma("sp", rw[:, :, :], S.router_w[i_].rearrange("(kc p) e -> p kc e", p=128))
                router = (rw, logit)
            modulate(S, l, 1, blocks, st, router=router)
        if moe:
            with P.phase() as st:
                m1 = P.sb("m1", [128, 16], F32, st)
                m2 = P.sb("m2", [128, 16], F32, st)
                eq1 = P.sb("eq1", [128, 16, 8], F32, st)
                eq2 = P.sb("eq2", [128, 16, 8], F32, st)
                l2 = P.sb("l2", [128, 16, 8], F32, st)
                ww = P.sb("ww", [128, 3, 16], F32, st)
                bc = lambda a: a.unsqueeze(2).to_broadcast([128, 16, 8])
                P.reduce("dve", m1[:, :], logit[:, :, :], ALU.max, AX.X)
                P.tt("dve", eq1[:, :, :], logit[:, :, :], bc(m1[:, :]), ALU.is_equal)
                P.stt("dve", l2[:, :, :], eq1[:, :, :], -1e30, logit[:, :, :], ALU.mult, ALU.add)
                P.reduce("dve", m2[:, :], l2[:, :, :], ALU.max, AX.X)
                P.tt("dve", eq2[:, :, :], l2[:, :, :], bc(m2[:, :]), ALU.is_equal)
                P.tt("dve", ww[:, 0, :], m2[:, :], m1[:, :], ALU.subtract)
                P.act(ww[:, 0, :], ww[:, 0, :], AF.Exp)
                P.ts("dve", ww[:, 1, :], ww[:, 0, :], 1.0, None, ALU.add)
                P.recip(ww[:, 1, :], ww[:, 1, :])
                P.tt("dve", ww[:, 2, :], ww[:, 0, :], ww[:, 1, :], ALU.mult)
                P.tt("dve", eq1[:, :, :], eq1[:, :, :], bc(ww[:, 1, :]), ALU.mult)
                P.tt("dve", eq2[:, :, :], eq2[:, :, :], bc(ww[:, 2, :]), ALU.mult)
                P.tt("dve", gate[:, :, :], eq1[:, :, :], eq2[:, :, :], ALU.add)
            if S.cfg.get("dump_gate"):
                P.dump("gate", gate[:, :, :])
        with P.phase() as st:
            GR = 6
            groups = [(0, 6), (6, 6), (12, 6), (18, 4)]
            gbuf = P.sb("gbuf", [128, GR, NL if moe else NT], BF16, st)
            w13 = [P.sb("w13", [128, 2, KC, 384], BF16, st) for _ in range(2)]
            w2b = [P.sb("w2b", [128, GR, 1024], BF16, st) for _ in range(2)]
            sil = [P.sb("sil", [128, 512], F32, st) for _ in range(2)]
            tmb = [P.sb("tmb", [128, 512], F32 if moe else BF16, st) for _ in range(2)]
            gbce = P.sb("gbce", [128, NL], BF16, st) if moe else None
            gdg = [P.sb("gdg", [128, 128], F32, st) for _ in range(2)] if moe else None
            nw13 = 0; nw2 = 0; nt = 0; ng_ = 0
            experts = range(8) if moe else range(1)
            if moe:
                blks = [(bi, TB[bi][0], TB[bi][0] - NCX, TB[bi][1]) for bi in blocks]
            else:
                blks = [(bi, TB[bi][0], TB[bi][0], TB[bi][1]) for bi in blocks]
            def Wsel(e):
                if moe:
                    return S.moe_w1[i_, e], S.moe_w3[i_, e], S.moe_w2[i_, e]
                return S.ffn_w1[i_], S.ffn_w3[i_], S.ffn_w2[i_]
            pieces = []
            for e in experts:
                for gi_, (f0, nf) in enumerate(groups):
                    plist = list(range(0, nf, 3))
                    for pi_, p0 in enumerate(plist):
                        pieces.append(dict(e=e, f0=f0, nf=nf, p0=p0, npf=min(3, nf - p0),
                                           first_in_group=(pi_ == 0), last_in_group=(pi_ == len(plist) - 1),
                                           first_in_expert=(gi_ == 0 and pi_ == 0)))
            def issue_w13(k):
                pc_ = pieces[k]
                W1, W3, W2 = Wsel(pc_["e"])
                wt = w13[k % 2]
                c0 = (pc_["f0"] + pc_["p0"]) * 128
                npf = pc_["npf"]
                P.dma("pool", wt[:, 0, :, 0:npf * 128],
                      W1[:, c0:c0 + npf * 128].rearrange("(kc p) f -> p kc f", p=128))
                P.dma("pool", wt[:, 1, :, 0:npf * 128],
                      W3[:, c0:c0 + npf * 128].rearrange("(kc p) f -> p kc f", p=128))
            def issue_w2(k):
                pc_ = pieces[k]
                W1, W3, W2 = Wsel(pc_["e"])
                w2t = w2b[pc_["gidx"] % 2]
                f0, nf = pc_["f0"], pc_["nf"]
                P.dma("pool", w2t[:, 0:nf, :],
                      W2[f0 * 128:(f0 + nf) * 128, :].rearrange("(c p) f -> p c f", p=128))
            gidx = -1
            for pc_ in pieces:
                if pc_["first_in_group"]:
                    gidx += 1
                pc_["gidx"] = gidx
            issue_w13(0)
            issue_w2(0)
            for k, pc_ in enumerate(pieces):
                e = pc_["e"]; f0 = pc_["f0"]; nf = pc_["nf"]; p0 = pc_["p0"]; npf = pc_["npf"]
                wt = w13[k % 2]
                w2t = w2b[pc_["gidx"] % 2]
                if k + 1 < len(pieces):
                    issue_w13(k + 1)
                    if pieces[k + 1]["first_in_group"]:
                        issue_w2(k + 1)
                if moe and pc_["first_in_expert"]:
                    for b4 in range(4):
                        pg = S.pb[7]
                        for j in range(4):
                            tile = b4 * 4 + j
                            gd = gdg[tile % 2]
                            P.ts("dve", gd[:, :], S.identf, gate[:, tile, e:e + 1], None, ALU.mult)
                            P.mm(pg[:, j * 128:(j + 1) * 128], S.onesf, gd[:, :])
                        P.copy("act", gbce[:, b4 * 512:(b4 + 1) * 512], pg[:, :])
                for (bi, x0, g0, tn) in blks:
                    for fi in range(npf):
                        pa = S.pb[0 + nt % 2]; pbb = S.pb[2 + nt % 2]
                        sl_ = sil[nt % 2]; tm = tmb[nt % 2]
                        nt += 1
                        for kc in range(KC):
                            P.mm(pa[:, :tn], wt[:, 0, kc, fi * 128:(fi + 1) * 128], S.hT[:, kc, x0:x0 + tn],
                                 start=(kc == 0), stop=(kc == KC - 1))
                        for kc in range(KC):
                            P.mm(pbb[:, :tn], wt[:, 1, kc, fi * 128:(fi + 1) * 128], S.hT[:, kc, x0:x0 + tn],
                                 start=(kc == 0), stop=(kc == KC - 1))
                        P.act(sl_[:, :tn], pa[:, :tn], AF.Silu)
                        gdst = gbuf[:, p0 + fi, g0:g0 + tn]
                        if moe:
                            P.tt("dve", tm[:, :tn], pbb[:, :tn], sl_[:, :tn], ALU.mult)
                            P.tt("pool", gdst, tm[:, :tn], gbce[:, g0:g0 + tn], ALU.mult)
                        else:
                            P.tt("dve", gdst, pbb[:, :tn], sl_[:, :tn], ALU.mult)
                if pc_["last_in_group"]:
                    for fo in range(8):
                        for (bi, x0, g0, tn) in blks:
                            which = 1 if bi == 0 else 0
                            po = S.pb[4 + ng_ % 3]; ng_ += 1
                            for fc in range(nf):
                                P.mm(po[:, :tn], w2t[:, fc, fo * 128:(fo + 1) * 128], gbuf[:, fc, g0:g0 + tn],
                                     start=(fc == 0), stop=(fc == nf - 1))
                            P.stt("dve", S.xT[:, fo, x0:x0 + tn], po[:, :tn], S.modv[:, l, 40 + fo, which:which + 1],
                                  S.xT[:, fo, x0:x0 + tn], ALU.mult, ALU.add)
    if S.cfg.get("dump_xffn") == l:
        P.dump("xT", S.xT[:, :, :])

def extra_shared(shared, inputs, f):
    shared["rope"] = _rope_tables()
    shared["gn"] = f(np.stack([inputs["gn_a"], inputs["gn_b"], inputs["gn_c"]], axis=1))
    shared["lam_c"] = f(inputs["lam_c"])
    cw = np.asarray(inputs["conv_a"], np.float32)
    shared["convw"] = f(cw.reshape(2, 3, 12, 128).transpose(0, 3, 2, 1))
    shared["adt"] = f(np.stack([np.asarray(inputs["a_log"], np.float32).reshape(2, 8),
                                np.asarray(inputs["dt_bias"], np.float32).reshape(2, 8)], axis=1))
    wg2 = np.concatenate([np.asarray(inputs["w_gate2"], np.float32),
                          np.asarray(inputs["b_gate"], np.float32)[:, :, None, :]], axis=2)
    shared["wg2"] = f(wg2.transpose(0, 2, 1, 3))
    for k in ("w_o_a", "w_o_b", "w_o_c", "w_out", "ffn_w1", "ffn_w3", "ffn_w2", "router_w", "moe_w1", "moe_w3", "moe_w2"):
        shared[k] = f(inputs[k])

def _consts():
    c = np.zeros((128, 1024), np.float32)
    c[:, 0:128] = np.eye(128, dtype=np.float32)
    for p in range(128):
        if (p % 32) < 16:
            c[p + 16, 128 + p] = -1.0
        else:
            c[p - 16, 128 + p] = 1.0
    k = np.arange(128)[:, None]
    i = np.arange(128)[None, :]
    c[:, 256:384] = (k <= i)
    c[:, 384:512] = (k >= i)
    c[:, 512:640] = (k < i)
    c[:, 640:768] = (k > i)
    return c


def _rope_tables():
    t = np.arange(2048)
    row = (t // 64).astype(np.float32)
    col = (t % 64).astype(np.float32)
    inv_freq = (np.float32(10000.0) ** (-np.arange(16, dtype=np.float32) / np.float32(16))).astype(np.float32)
    tab = np.zeros((2, 128, 2048), np.float32)
    for p in range(128):
        d = p % 64
        pos = row if d < 32 else col
        ang = (pos * inv_freq[d % 16]).astype(np.float32)
        tab[0, p] = np.cos(ang)
        tab[1, p] = np.sin(ang)
    return tab


def _fm(v):
    v = np.asarray(v)
    lead = v.shape[:-1]
    n = v.shape[-1] // 128
    w = v.reshape(lead + (n, 128))
    return np.ascontiguousarray(np.moveaxis(w, -1, 0))


_CACHE = {}


def _get_prog(cfg_key, cfg):
    if cfg_key not in _CACHE:
        _CACHE[cfg_key] = build(cfg)
    return _CACHE[cfg_key]


def make_in_maps(inputs, cfg):
    f = lambda a: np.ascontiguousarray(np.asarray(a, dtype=np.float32))
    x = f(inputs["x"]); c = f(inputs["c"]); ctx = f(inputs["ctx"]); c_ctx = f(inputs["c_ctx"])
    shared = {}
    shared["w_mod"] = f(inputs["w_mod"])
    shared["b_mod"] = np.ascontiguousarray(f(inputs["b_mod"]).reshape(2, 48, 128).transpose(0, 2, 1))
    ng = np.stack([_fm(inputs["norm1_g"][0]), _fm(inputs["norm1_g"][1]), _fm(inputs["norm2_g"][0]),
                   _fm(inputs["norm2_g"][1]), _fm(inputs["final_g"])], axis=1)
    shared["norm_g"] = f(ng)
    shared["w_in"] = f(inputs["w_in"])
    shared["consts"] = _consts()
    extra_shared(shared, inputs, f)
    maps = []
    for b in range(8):
        m = dict(shared)
        m["x"] = x[b]
        m["ctx"] = ctx[b]
        m["cvec"] = f(np.stack([_fm(c[b]), _fm(c_ctx)], axis=-1))
        maps.append(m)
    return maps


def run(inputs, cfg, trace=False):
    P = _get_prog(repr(sorted(cfg.items())), cfg)
    maps = make_in_maps(inputs, cfg)
    names = set()
    for ins in P.nc.main_func.allocations if False else []:
        pass
    n = cfg.get("ncores", 8)
    res = run_bass_kernel_spmd(P.nc, maps[:n], core_ids=list(range(n)), trace=trace)
    return P, res


def kernel(**inputs):
    cfg = dict(layers=2)
    P, res = run(inputs, cfg)
    out = np.stack([np.asarray(res.results[b]["out"], dtype=np.float32) for b in range(8)], axis=0)
    return out
```
